# Optimizing a Trainium2 kernel written in Bass

```python
import math
import jax
import jax.numpy as jnp
from jax import lax
import numpy as np

D_MODEL = 1024
BATCH = 8
SEQ = 4096
DEPTH = 4

GRID_W = 64
CTX_LEN = 256
EPS = 1e-6
NEG_INF = -1e30

MIX_W = 256
N_BRANCH = 4
HEAD_DIM = 64
ATTN_SCALE = HEAD_DIM ** -0.5

SSD_HEADS = MIX_W // HEAD_DIM
SSD_GROUPS = 2
SSD_STATE = 128
SSD_CONV = 5
SSD_CHUNK = 128
SSD_XBC = MIX_W + 2 * SSD_GROUPS * SSD_STATE

S5_GROUP = 16
S5_GROUPS = MIX_W // S5_GROUP
S5_STATE = 64

GA_HEADS = MIX_W // HEAD_DIM
GA_KV = 2
Q_BLOCK = 128
ROPE_BASE = 10000.0
ROPE_FREQS = HEAD_DIM // 4

SW_HEADS = MIX_W // HEAD_DIM
SW_KV = 2
WINDOW = 128

FFN_DIM = 2816
N_EXPERTS = 8
TOP_K = 2
EXPERT_DIM = 3584
MOE_BLOCK = 256
N_DENSE = (DEPTH + 1) // 2
N_MOE = DEPTH // 2

IN_NAMES = ('a_z', 'a_xbc', 'a_dt', 'b_u', 'c_q', 'c_k', 'c_v', 'd_q', 'd_k', 'd_v', 'gates')
IN_SIZES = (MIX_W, SSD_XBC, 2 * SSD_HEADS, MIX_W,
            GA_HEADS * HEAD_DIM, GA_KV * HEAD_DIM, GA_KV * HEAD_DIM,
            SW_HEADS * HEAD_DIM, SW_KV * HEAD_DIM, SW_KV * HEAD_DIM,
            N_BRANCH * D_MODEL)
IN_COLS = sum(IN_SIZES)

kernel_name = 'hybrid_ssd_s5_gqa_swa_moe_dit'


def _rms(x, g):
    xf = x.astype(jnp.float32)
    y = xf * lax.rsqrt(jnp.mean(xf * xf, axis=-1, keepdims=True) + EPS)
    return (y * g.astype(jnp.float32)).astype(x.dtype)


def _flip(t):
    return jnp.flip(t, axis=1)


def _split_cols(p):
    points, acc = [], 0
    for s in IN_SIZES[:-1]:
        acc += s
        points.append(acc)
    return jnp.split(p, points, axis=-1)


def _axial_rope_tables(rows):
    pos_r = jnp.repeat(jnp.arange(rows, dtype=jnp.float32), GRID_W)
    pos_c = jnp.tile(jnp.arange(GRID_W, dtype=jnp.float32), rows)
    inv = ROPE_BASE ** (-jnp.arange(ROPE_FREQS, dtype=jnp.float32) / ROPE_FREQS)
    ang = jnp.concatenate([pos_r[:, None] * inv, pos_c[:, None] * inv], axis=-1)
    return jnp.cos(ang), jnp.sin(ang)


def _apply_rope(x, cos, sin):
    f = ROPE_FREQS
    xf = x.astype(jnp.float32)
    x1 = jnp.concatenate([xf[..., 0:f], xf[..., 2 * f:3 * f]], axis=-1)
    x2 = jnp.concatenate([xf[..., f:2 * f], xf[..., 3 * f:4 * f]], axis=-1)
    cs, sn = cos[None, :, None, :], sin[None, :, None, :]
    r1 = x1 * cs - x2 * sn
    r2 = x2 * cs + x1 * sn
    out = jnp.concatenate([r1[..., :f], r2[..., :f], r1[..., f:], r2[..., f:]], axis=-1)
    return out.astype(x.dtype)


def _qkv(q, k, v, n_heads, n_kv, q_gain, k_gain, rope):
    b, n, _ = q.shape
    q = q.reshape(b, n, n_heads, HEAD_DIM)
    k = k.reshape(b, n, n_kv, HEAD_DIM)
    v = v.reshape(b, n, n_kv, HEAD_DIM)
    if q_gain is not None:
        q, k = _rms(q, q_gain), _rms(k, k_gain)
    if rope is not None:
        q, k = _apply_rope(q, *rope), _apply_rope(k, *rope)
    return q.reshape(b, n, n_kv, n_heads // n_kv, HEAD_DIM), k, v


def _attend(q, k, v, sink=None):
    s = jnp.einsum('bqkgd,bskd->bkgqs', q, k, preferred_element_type=jnp.float32) * ATTN_SCALE
    if sink is not None:
        sk = jnp.broadcast_to(sink.astype(jnp.float32)[None, :, :, None, None], s.shape[:-1] + (1,))
        p = jax.nn.softmax(jnp.concatenate([s, sk], axis=-1), axis=-1)[..., :-1]
    else:
        p = jax.nn.softmax(s, axis=-1)
    return jnp.einsum('bkgqs,bskd->bqkgd', p.astype(v.dtype), v)


def _global_attention(q_l, k_l, v_l, q_c, k_c, v_c, ctx_out):
    b, n, kv, g, dh = q_l.shape
    k_all = jnp.concatenate([k_c, k_l], axis=1)
    v_all = jnp.concatenate([v_c, v_l], axis=1)
    qb = q_l.reshape(b, n // Q_BLOCK, Q_BLOCK, kv, g, dh).transpose(1, 0, 2, 3, 4, 5)
    ob = lax.map(lambda qq: _attend(qq, k_all, v_all), qb)
    o_l = ob.transpose(1, 0, 2, 3, 4, 5).reshape(b, n, kv * g * dh)
    o_c = _attend(q_c, k_c, v_c).reshape(b, k_c.shape[1], kv * g * dh) if ctx_out else None
    return o_l, o_c


def _window_attention(q_l, k_l, v_l, q_c, k_c, v_c, sink, ctx_out):
    b, n, kv, g, dh = q_l.shape
    nb = n // WINDOW
    sink_kg = sink.reshape(kv, g)
    qb = q_l.reshape(b, nb, WINDOW, kv, g, dh)

    def band(t):
        z = jnp.zeros((b, WINDOW) + t.shape[2:], t.dtype)
        tp = jnp.concatenate([z, t, z], axis=1).reshape((b, nb + 2, WINDOW) + t.shape[2:])
        return jnp.concatenate([tp[:, :-2], tp[:, 1:-1], tp[:, 2:]], axis=2)

    kw, vw = band(k_l), band(v_l)
    s_w = jnp.einsum('bnqkgd,bnskd->bnkgqs', qb, kw, preferred_element_type=jnp.float32) * ATTN_SCALE
    blk = jnp.arange(nb)[:, None]
    qpos = blk * WINDOW + jnp.arange(WINDOW)[None, :]
    kpos = (blk - 1) * WINDOW + jnp.arange(3 * WINDOW)[None, :]
    mask = (jnp.abs(qpos[:, :, None] - kpos[:, None, :]) <= WINDOW) & ((kpos >= 0) & (kpos < n))[:, None, :]
    s_w = jnp.where(mask[None, :, None, None], s_w, NEG_INF)
    s_c = jnp.einsum('bnqkgd,bskd->bnkgqs', qb, k_c, preferred_element_type=jnp.float32) * ATTN_SCALE
    sk = jnp.broadcast_to(sink_kg.astype(jnp.float32)[None, None, :, :, None, None], s_c.shape[:-1] + (1,))
    p = jax.nn.softmax(jnp.concatenate([s_c, s_w, sk], axis=-1), axis=-1).astype(v_l.dtype)
    nc = k_c.shape[1]
    o = (jnp.einsum('bnkgqs,bskd->bnqkgd', p[..., :nc], v_c)
         + jnp.einsum('bnkgqs,bnskd->bnqkgd', p[..., nc:nc + 3 * WINDOW], vw))
    o_l = o.reshape(b, n, kv * g * dh)
    o_c = _attend(q_c, k_c, v_c, sink_kg).reshape(b, nc, kv * g * dh) if ctx_out else None
    return o_l, o_c


def _dwconv(x, w, bias):
    ch = x.shape[-1]
    y = lax.conv_general_dilated(x, w[:, None, :].astype(x.dtype), window_strides=(1,),
                                 padding=[(SSD_CONV // 2, SSD_CONV // 2)],
                                 dimension_numbers=('NWC', 'WIO', 'NWC'), feature_group_count=ch)
    return y + bias


def _segsum_decay(a_cs):
    q = a_cs.shape[-1]
    diff = a_cs[..., :, None] - a_cs[..., None, :]
    tri = jnp.tril(jnp.ones((q, q), dtype=bool))
    return jnp.where(tri, jnp.exp(jnp.where(tri, diff, 0.0)), 0.0)


def _ssd_scan(xs, dt, a, bm, cm, init):
    b, n, h, p = xs.shape
    nst = bm.shape[-1]
    nc, q = n // SSD_CHUNK, SSD_CHUNK
    la = (dt * a).reshape(b, nc, q, h).transpose(0, 3, 1, 2)
    a_cs = jnp.cumsum(la, axis=-1)
    xdt = (xs.astype(jnp.float32) * dt[..., None]).reshape(b, nc, q, h, p)
    bc = bm.astype(jnp.float32).reshape(b, nc, q, h, nst)
    cc = cm.astype(jnp.float32).reshape(b, nc, q, h, nst)
    scores = jnp.einsum('bclhn,bcshn->bhcls', cc, bc) * _segsum_decay(a_cs)
    y_diag = jnp.einsum('bhcls,bcshp->bclhp', scores, xdt)
    decay_to_end = jnp.exp(a_cs[..., -1:] - a_cs)
    chunk_states = jnp.einsum('bclhn,bhcl,bclhp->bchpn', bc, decay_to_end, xdt)
    chunk_decay = jnp.exp(a_cs[..., -1])

    def carry(state, inp):
        dec, st = inp
        return state * dec[..., None, None] + st, state

    final, entering = lax.scan(carry, init.astype(jnp.float32),
                               (jnp.moveaxis(chunk_decay, 2, 0), jnp.moveaxis(chunk_states, 1, 0)))
    entering = jnp.moveaxis(entering, 0, 1)
    y_off = jnp.einsum('bclhn,bchpn,bhcl->bclhp', cc, entering, jnp.exp(a_cs))
    return (y_diag + y_off).reshape(b, n, h, p), final


def _ssd_mixer(z_l, xbc_l, dt_l, z_c, xbc_c, dt_c, conv_w, conv_b, a_log, dt_bias, d_skip, norm_g, ctx_out):
    a = -jnp.exp(a_log.astype(jnp.float32))
    rep = SSD_HEADS // SSD_GROUPS

    def prep(xbc, dt):
        bsz, n, _ = xbc.shape
        xbc = jax.nn.silu(_dwconv(xbc, conv_w, conv_b))
        xs, bm, cm = jnp.split(xbc, [MIX_W, MIX_W + SSD_GROUPS * SSD_STATE], axis=-1)
        xs = xs.reshape(bsz, n, SSD_HEADS, HEAD_DIM)
        bm = jnp.repeat(bm.reshape(bsz, n, SSD_GROUPS, SSD_STATE), rep, axis=2)
        cm = jnp.repeat(cm.reshape(bsz, n, SSD_GROUPS, SSD_STATE), rep, axis=2)
        dt = jax.nn.softplus(dt.astype(jnp.float32).reshape(bsz, n, 2, SSD_HEADS) + dt_bias.astype(jnp.float32))
        return xs, bm, cm, dt

    xs_c, b_c, c_c, dt_c = prep(xbc_c, dt_c)
    xs_l, b_l, c_l, dt_l = prep(xbc_l, dt_l)
    init = jnp.zeros((xs_l.shape[0], SSD_HEADS, HEAD_DIM, SSD_STATE), jnp.float32)
    yc_f, st_f = _ssd_scan(xs_c, dt_c[:, :, 0], a[0], b_c, c_c, init)
    yl_f, _ = _ssd_scan(xs_l, dt_l[:, :, 0], a[0], b_l, c_l, st_f)
    yc_b, st_b = _ssd_scan(_flip(xs_c), _flip(dt_c[:, :, 1]), a[1], _flip(b_c), _flip(c_c), init)
    yl_b, _ = _ssd_scan(_flip(xs_l), _flip(dt_l[:, :, 1]), a[1], _flip(b_l), _flip(c_l), st_b)

    def finish(y_f, y_b, xs, z):
        bsz, n = xs.shape[:2]
        y = y_f + _flip(y_b) + d_skip.astype(jnp.float32)[:, None] * xs.astype(jnp.float32)
        y = y.reshape(bsz, n, MIX_W) * jax.nn.silu(z.astype(jnp.float32))
        return _rms(y, norm_g).astype(z.dtype)

    o_l = finish(yl_f, yl_b, xs_l, z_l)
    o_c = finish(yc_f, yc_b, xs_c, z_c) if ctx_out else None
    return o_l, o_c


def _s5_discretize(lam_re, lam_im, log_step, b_re, b_im):
    step = jnp.exp(log_step.astype(jnp.float32))[:, None]
    lr = jnp.minimum(lam_re.astype(jnp.float32), -1e-4)
    li = lam_im.astype(jnp.float32)
    mag = jnp.exp(lr * step)
    ang = li * step
    ab_re, ab_im = mag * jnp.cos(ang), mag * jnp.sin(ang)
    den = lr * lr + li * li
    f_re = ((ab_re - 1.0) * lr + ab_im * li) / den
    f_im = (ab_im * lr - (ab_re - 1.0) * li) / den
    br, bi = b_re.astype(jnp.float32), b_im.astype(jnp.float32)
    bb_re = f_re[..., None] * br - f_im[..., None] * bi
    bb_im = f_re[..., None] * bi + f_im[..., None] * br
    return ab_re, ab_im, bb_re, bb_im


def _s5_scan(u, disc, c_re, c_im, init):
    ab_re, ab_im, bb_re, bb_im = disc
    s0_re, s0_im = init
    bu_re = jnp.einsum('blgp,gnp->blgn', u, bb_re)
    bu_im = jnp.einsum('blgp,gnp->blgn', u, bb_im)
    bu_re = bu_re.at[:, 0].add(ab_re * s0_re - ab_im * s0_im)
    bu_im = bu_im.at[:, 0].add(ab_re * s0_im + ab_im * s0_re)
    a_re = jnp.broadcast_to(ab_re, bu_re.shape)
    a_im = jnp.broadcast_to(ab_im, bu_im.shape)

    def combine(e1, e2):
        a1r, a1i, b1r, b1i = e1
        a2r, a2i, b2r, b2i = e2
        return (a1r * a2r - a1i * a2i, a1r * a2i + a1i * a2r,
                a2r * b1r - a2i * b1i + b2r, a2r * b1i + a2i * b1r + b2i)

    _, _, s_re, s_im = lax.associative_scan(combine, (a_re, a_im, bu_re, bu_im), axis=1)
    y = (jnp.einsum('blgn,gpn->blgp', s_re, c_re.astype(jnp.float32))
         - jnp.einsum('blgn,gpn->blgp', s_im, c_im.astype(jnp.float32)))
    return y, (s_re[:, -1], s_im[:, -1])


def _s5_mixer(u_l, u_c, lam_re, lam_im, log_step, b_re, b_im, c_re, c_im, d_skip, glu_w, ctx_out):
    def grouped(u):
        return u.astype(jnp.float32).reshape(u.shape[0], u.shape[1], S5_GROUPS, S5_GROUP)

    ul, uc = grouped(u_l), grouped(u_c)
    zero = jnp.zeros((u_l.shape[0], S5_GROUPS, S5_STATE), jnp.float32)
    disc_f = _s5_discretize(lam_re[0], lam_im[0], log_step[0], b_re[0], b_im[0])
    disc_b = _s5_discretize(lam_re[1], lam_im[1], log_step[1], b_re[1], b_im[1])
    yc_f, st_f = _s5_scan(uc, disc_f, c_re[0], c_im[0], (zero, zero))
    yl_f, _ = _s5_scan(ul, disc_f, c_re[0], c_im[0], st_f)
    yc_b, st_b = _s5_scan(_flip(uc), disc_b, c_re[1], c_im[1], (zero, zero))
    yl_b, _ = _s5_scan(_flip(ul), disc_b, c_re[1], c_im[1], st_b)
    d = d_skip.astype(jnp.float32).reshape(S5_GROUPS, S5_GROUP)

    def finish(y_f, y_b, u, like):
        y = y_f + _flip(y_b) + d * u
        v = jax.nn.gelu(y.reshape(u.shape[0], u.shape[1], MIX_W)).astype(like.dtype)
        val, gate = jnp.split(v @ glu_w, 2, axis=-1)
        return val * jax.nn.sigmoid(gate)

    o_l = finish(yl_f, yl_b, ul, u_l)
    o_c = finish(yc_f, yc_b, uc, u_c) if ctx_out else None
    return o_l, o_c


def _merge(branches, gates, w_branch, w_out):
    gs = jnp.split(gates, N_BRANCH, axis=-1)
    acc = jax.nn.sigmoid(gs[0]) * (branches[0] @ w_branch[0])
    for i in range(1, N_BRANCH):
        acc = acc + jax.nn.sigmoid(gs[i]) * (branches[i] @ w_branch[i])
    return acc @ w_out


def _swiglu(x, w_in, w_out):
    g, u = jnp.split(x @ w_in, 2, axis=-1)
    return (jax.nn.silu(g) * u) @ w_out


def _moe_swiglu(xf, router, w_in, w_out):
    t, d = xf.shape
    logits = (xf @ router).astype(jnp.float32)
    top_v, top_i = lax.top_k(logits, TOP_K)
    gate = jax.nn.softmax(top_v, axis=-1)
    flat_e = top_i.reshape(-1).astype(jnp.int32)
    n_assign = t * TOP_K
    order = jnp.argsort(flat_e)
    sorted_e = flat_e[order]
    counts = jnp.bincount(flat_e, length=N_EXPERTS).astype(jnp.int32)
    starts = jnp.cumsum(counts) - counts
    padded = (counts + MOE_BLOCK - 1) // MOE_BLOCK * MOE_BLOCK
    ends = jnp.cumsum(padded)
    pstarts = ends - padded
    dest_sorted = (pstarts[sorted_e] + jnp.arange(n_assign, dtype=jnp.int32) - starts[sorted_e]).astype(jnp.int32)
    n_blocks = -(-n_assign // MOE_BLOCK) + N_EXPERTS
    n_rows = n_blocks * MOE_BLOCK
    row_tok = jnp.full((n_rows,), t, jnp.int32).at[dest_sorted].set((order // TOP_K).astype(jnp.int32))
    block_e = jnp.minimum(jnp.searchsorted(ends, jnp.arange(n_blocks, dtype=jnp.int32) * MOE_BLOCK, side='right'),
                          N_EXPERTS - 1)
    x_rows = jnp.concatenate([xf, jnp.zeros((1, d), xf.dtype)], axis=0)[row_tok].reshape(n_blocks, MOE_BLOCK, d)

    def expert_block(args):
        xb, e = args
        return _swiglu(xb, w_in[e], w_out[e])

    y_rows = lax.map(expert_block, (x_rows, block_e)).reshape(n_rows, d)
    dest = jnp.zeros((n_assign,), jnp.int32).at[order].set(dest_sorted)
    return jnp.sum(y_rows[dest].reshape(t, TOP_K, d) * gate[..., None].astype(y_rows.dtype), axis=1)


def setup_inputs(seed: int = 0) -> dict:
    key = jax.random.key(seed)
    ks = jax.random.split(key, 40)
    D = D_MODEL

    def nrm(i, shape, scale):
        return scale * jax.random.normal(ks[i], shape, jnp.float32)

    def gain(i, shape, s=0.02):
        return 1.0 + s * jax.random.normal(ks[i], shape, jnp.float32)

    dt0 = jnp.exp(jax.random.uniform(ks[12], (DEPTH, 2, SSD_HEADS), jnp.float32, math.log(1e-3), math.log(1e-1)))
    s5_shape = (DEPTH, 2, S5_GROUPS, S5_STATE)
    return {
        'x': nrm(0, (BATCH, SEQ, D), 1.0),
        'c': nrm(1, (BATCH, D), 1.0),
        'ctx': nrm(2, (BATCH, CTX_LEN, D), 1.0),
        'c_ctx': nrm(3, (D,), 1.0),
        'norm1_g': gain(4, (DEPTH, D)),
        'norm2_g': gain(5, (DEPTH, D)),
        'ada_w': nrm(6, (DEPTH, D, 6 * D), 0.5 * D ** -0.5),
        'ada_b': nrm(7, (DEPTH, 6 * D), 0.02),
        'w_in': nrm(8, (DEPTH, D, IN_COLS), D ** -0.5),
        'ssd_conv_w': nrm(9, (DEPTH, SSD_CONV, SSD_XBC), SSD_CONV ** -0.5),
        'ssd_conv_b': nrm(10, (DEPTH, SSD_XBC), 0.02),
        'ssd_a_log': jnp.log(jax.random.uniform(ks[11], (DEPTH, 2, SSD_HEADS), jnp.float32, 1.0, 16.0)),
        'ssd_dt_bias': dt0 + jnp.log(-jnp.expm1(-dt0)),
        'ssd_d': gain(13, (DEPTH, SSD_HEADS), 0.1),
        'ssd_norm_g': gain(14, (DEPTH, MIX_W)),
        's5_lam_re': -0.5 + nrm(15, s5_shape, 0.01),
        's5_lam_im': jnp.pi * jnp.arange(S5_STATE, dtype=jnp.float32) + nrm(16, s5_shape, 0.01),
        's5_log_step': jax.random.uniform(ks[17], (DEPTH, 2, S5_GROUPS), jnp.float32, math.log(1e-3), math.log(1e-1)),
        's5_b_re': nrm(18, (DEPTH, 2, S5_GROUPS, S5_STATE, S5_GROUP), (2 * S5_GROUP) ** -0.5),
        's5_b_im': nrm(19, (DEPTH, 2, S5_GROUPS, S5_STATE, S5_GROUP), (2 * S5_GROUP) ** -0.5),
        's5_c_re': nrm(20, (DEPTH, 2, S5_GROUPS, S5_GROUP, S5_STATE), (2 * S5_STATE) ** -0.5),
        's5_c_im': nrm(21, (DEPTH, 2, S5_GROUPS, S5_GROUP, S5_STATE), (2 * S5_STATE) ** -0.5),
        's5_d': nrm(22, (DEPTH, MIX_W), 1.0),
        's5_glu_w': nrm(23, (DEPTH, MIX_W, 2 * MIX_W), MIX_W ** -0.5),
        'qk_norm_g': gain(24, (DEPTH, 2, HEAD_DIM)),
        'swa_sink': nrm(25, (DEPTH, SW_HEADS), 0.5),
        'w_branch': nrm(26, (DEPTH, N_BRANCH, MIX_W, D), MIX_W ** -0.5),
        'w_out': nrm(27, (DEPTH, D, D), D ** -0.5),
        'ffn_w_in': nrm(28, (N_DENSE, D, 2 * FFN_DIM), D ** -0.5),
        'ffn_w_out': nrm(29, (N_DENSE, FFN_DIM, D), FFN_DIM ** -0.5),
        'moe_router': nrm(30, (N_MOE, D, N_EXPERTS), D ** -0.5),
        'moe_w_in': nrm(31, (N_MOE, N_EXPERTS, D, 2 * EXPERT_DIM), D ** -0.5),
        'moe_w_out': nrm(32, (N_MOE, N_EXPERTS, EXPERT_DIM, D), EXPERT_DIM ** -0.5),
        'final_norm_g': gain(33, (D,)),
    }


def reference(x, c, ctx, c_ctx, norm1_g, norm2_g, ada_w, ada_b, w_in, ssd_conv_w, ssd_conv_b, ssd_a_log,
              ssd_dt_bias, ssd_d, ssd_norm_g, s5_lam_re, s5_lam_im, s5_log_step, s5_b_re, s5_b_im, s5_c_re,
              s5_c_im, s5_d, s5_glu_w, qk_norm_g, swa_sink, w_branch, w_out, ffn_w_in, ffn_w_out, moe_router,
              moe_w_in, moe_w_out, final_norm_g):
    bsz, n_lat, d_model = x.shape
    rows = n_lat // GRID_W
    rope = _axial_rope_tables(rows)
    c_act = jax.nn.silu(c)
    cc_act = jax.nn.silu(c_ctx)
    xc = ctx
    for l in range(DEPTH):
        ctx_out = l < DEPTH - 1
        mod = jnp.split((c_act @ ada_w[l] + ada_b[l])[:, None, :], 6, axis=-1)
        mod_c = jnp.split(cc_act @ ada_w[l] + ada_b[l], 6, axis=-1)
        h = _rms(x, norm1_g[l]) * (1 + mod[1]) + mod[0]
        hc = _rms(xc, norm1_g[l]) * (1 + mod_c[1]) + mod_c[0]
        pl = dict(zip(IN_NAMES, _split_cols(h @ w_in[l])))
        pc = dict(zip(IN_NAMES, _split_cols(hc @ w_in[l])))

        ya_l, ya_c = _ssd_mixer(pl['a_z'], pl['a_xbc'], pl['a_dt'], pc['a_z'], pc['a_xbc'], pc['a_dt'],
                                ssd_conv_w[l], ssd_conv_b[l], ssd_a_log[l], ssd_dt_bias[l], ssd_d[l],
                                ssd_norm_g[l], ctx_out)
        yb_l, yb_c = _s5_mixer(pl['b_u'], pc['b_u'], s5_lam_re[l], s5_lam_im[l], s5_log_step[l], s5_b_re[l],
                               s5_b_im[l], s5_c_re[l], s5_c_im[l], s5_d[l], s5_glu_w[l], ctx_out)
        qg, kg = qk_norm_g[l, 0], qk_norm_g[l, 1]
        q1, k1, v1 = _qkv(pl['c_q'], pl['c_k'], pl['c_v'], GA_HEADS, GA_KV, qg, kg, rope)
        q1c, k1c, v1c = _qkv(pc['c_q'], pc['c_k'], pc['c_v'], GA_HEADS, GA_KV, qg, kg, None)
        yc_l, yc_c = _global_attention(q1, k1, v1, q1c, k1c, v1c, ctx_out)
        q2, k2, v2 = _qkv(pl['d_q'], pl['d_k'], pl['d_v'], SW_HEADS, SW_KV, None, None, rope)
        q2c, k2c, v2c = _qkv(pc['d_q'], pc['d_k'], pc['d_v'], SW_HEADS, SW_KV, None, None, None)
        yd_l, yd_c = _window_attention(q2, k2, v2, q2c, k2c, v2c, swa_sink[l], ctx_out)

        x = x + mod[2] * _merge([ya_l, yb_l, yc_l, yd_l], pl['gates'], w_branch[l], w_out[l])
        h2 = _rms(x, norm2_g[l]) * (1 + mod[4]) + mod[3]
        if ctx_out:
            xc = xc + mod_c[2] * _merge([ya_c, yb_c, yc_c, yd_c], pc['gates'], w_branch[l], w_out[l])
            h2c = _rms(xc, norm2_g[l]) * (1 + mod_c[4]) + mod_c[3]

        if l % 2 == 0:
            x = x + mod[5] * _swiglu(h2, ffn_w_in[l // 2], ffn_w_out[l // 2])
            if ctx_out:
                xc = xc + mod_c[5] * _swiglu(h2c, ffn_w_in[l // 2], ffn_w_out[l // 2])
        else:
            n_l = bsz * n_lat
            if ctx_out:
                toks = jnp.concatenate([h2.reshape(n_l, d_model), h2c.reshape(-1, d_model)], axis=0)
            else:
                toks = h2.reshape(n_l, d_model)
            f = _moe_swiglu(toks, moe_router[l // 2], moe_w_in[l // 2], moe_w_out[l // 2])
            x = x + mod[5] * f[:n_l].reshape(x.shape)
            if ctx_out:
                xc = xc + mod_c[5] * f[n_l:].reshape(xc.shape)
    return _rms(x, final_norm_g)
```

```python
import numpy as np
from contextlib import ExitStack
import concourse.bass as bass
import concourse.mybir as mybir
from concourse.bass_utils import run_bass_kernel_spmd

F32 = mybir.dt.float32
BF16 = mybir.dt.bfloat16
I32 = mybir.dt.int32
ALU = mybir.AluOpType
AF = mybir.ActivationFunctionType
AX = mybir.AxisListType

D = 1024
KC = 8
NCTX = 256
NLAT = 4096
S = NCTX + NLAT
NT = S // 128
DEPTH = 4
FT = [(0, 256)] + [(256 + 512 * i, 512) for i in range(8)]
EPS = 1e-6
PZ, PDT, PU, PCQ, PCK, PCV, PDQ, PDK, PDV = 0, 256, 264, 520, 776, 904, 1032, 1288, 1416
NPTM = 1544
FFN_DIM = 2816
EXPERT_DIM = 3584
NEG = -30000.0
SAME_ENG_SYNC = True
import os
XBC_LEVEL = int(os.environ.get('XBC_LEVEL', '2'))
ATT_LEVEL = int(os.environ.get('ATT_LEVEL', '2'))
SSD_LEVEL = int(os.environ.get('SSD_LEVEL', '9'))
S5_LEVEL = int(os.environ.get('S5_LEVEL', '9'))


class Prog:
    def __init__(self, nc, es):
        self.nc = nc
        self.es = es
        self.E = {"pe": nc.tensor, "act": nc.scalar, "dve": nc.vector, "pool": nc.gpsimd, "sp": nc.sync}
        self.csem = {}
        self.ccnt = {}
        for e in ("pe", "act", "dve", "pool"):
            self.csem[e] = es.enter_context(nc.semaphore("c_" + e))
            self.ccnt[e] = 0
        self.dsem = {}
        self.dcnt = {}
        self.dnext = {}
        for q in ("sp", "pool", "act"):
            self.dsem[q] = [es.enter_context(nc.semaphore("d_%s%d" % (q, i))) for i in range(8)]
            self.dcnt[q] = [0] * 8
            self.dnext[q] = 0
        self.sems = {}
        for e in self.csem:
            self.sems[("c", e)] = self.csem[e]
        for q in self.dsem:
            for i, s in enumerate(self.dsem[q]):
                self.sems[("d", q, i)] = s
        self.waited = {e: {} for e in self.E}
        self.state = {}
        self.nins = 0

    def _deps(self, r, w):
        deps = {}

        def add(tok):
            if tok is None:
                return
            k, v = tok
            if deps.get(k, 0) < v:
                deps[k] = v

        for key in r:
            st = self.state.get(key)
            if st:
                add(st[0])
        for key in w:
            st = self.state.get(key)
            if st:
                add(st[0])
                for t in st[1]:
                    add(t)
        return deps

    def _wait(self, eng, deps):
        wd = self.waited[eng]
        for k, v in deps.items():
            if wd.get(k, 0) >= v:
                continue
            if k == ("c", eng) and (eng == "pe" or not SAME_ENG_SYNC):
                continue
            self.E[eng].wait_ge(self.sems[k], v)
            wd[k] = v
            self.nins += 1

    def _commit(self, tok, r, w):
        for key in r:
            st = self.state.setdefault(key, [None, []])
            st[1].append(tok)
            if len(st[1]) > 24:
                mx = {}
                for k, v in st[1]:
                    if mx.get(k, 0) < v:
                        mx[k] = v
                st[1] = list(mx.items())
        for key in w:
            self.state[key] = [tok, []]

    def op(self, eng, fn, r=(), w=()):
        self._wait(eng, self._deps(r, w))
        ins = fn(self.E[eng])
        self.ccnt[eng] += 1
        ins.then_inc(self.csem[eng], 1)
        tok = (("c", eng), self.ccnt[eng])
        self._commit(tok, r, w)
        self.nins += 1
        return tok

    def dma(self, q, out, in_, r=(), w=()):
        i = self.dnext[q]
        self.dnext[q] = (i + 1) % 8
        deps = self._deps(r, w)
        k = ("d", q, i)
        if self.dcnt[q][i] > 0:
            deps[k] = max(deps.get(k, 0), self.dcnt[q][i])
        self._wait(q, deps)
        self.dcnt[q][i] += 16
        self.E[q].dma_start(out=out, in_=in_).then_inc(self.dsem[q][i], 16)
        tok = (k, self.dcnt[q][i])
        self._commit(tok, r, w)
        self.nins += 1
        return tok

    def barrier(self):
        deps = {}
        for e in self.csem:
            if self.ccnt[e]:
                deps[("c", e)] = self.ccnt[e]
        for q in self.dsem:
            for i in range(8):
                if self.dcnt[q][i]:
                    deps[("d", q, i)] = self.dcnt[q][i]
        for e in self.E:
            d = {k: v for k, v in deps.items() if k != ("c", e)}
            self._wait(e, d)
        self.state = {}

    def mm(self, out, lhsT, rhs, start=True, stop=True, r=(), w=()):
        return self.op("pe", lambda e: e.matmul(out, lhsT, rhs, start=start, stop=stop), r, w)

    def tr(self, out, in_, ident, r=(), w=()):
        return self.op("pe", lambda e: e.transpose(out, in_, ident), r, w)

    def act(self, out, in_, func, bias=0.0, scale=1.0, r=(), w=(), eng="act"):
        return self.op(eng, lambda e: e.activation(out=out, in_=in_, func=func, bias=bias, scale=scale), r, w)

    def tt(self, out, in0, in1, op, r=(), w=(), eng="dve"):
        return self.op(eng, lambda e: e.tensor_tensor(out=out, in0=in0, in1=in1, op=op), r, w)

    def ts(self, out, in0, s1, s2=None, op0=ALU.mult, op1=None, r=(), w=(), eng="dve"):
        if op1 is None:
            return self.op(eng, lambda e: e.tensor_scalar(out=out, in0=in0, scalar1=s1, scalar2=None, op0=op0), r, w)
        return self.op(eng, lambda e: e.tensor_scalar(out=out, in0=in0, scalar1=s1, scalar2=s2, op0=op0, op1=op1), r, w)

    def stt(self, out, in0, scalar, in1, op0=ALU.mult, op1=ALU.add, r=(), w=(), eng="dve"):
        return self.op(eng, lambda e: e.scalar_tensor_tensor(out=out, in0=in0, scalar=scalar, in1=in1, op0=op0, op1=op1), r, w)

    def cp(self, out, in_, r=(), w=(), eng="dve"):
        if eng == "act":
            return self.op("act", lambda e: e.copy(out=out, in_=in_), r, w)
        return self.op(eng, lambda e: e.tensor_copy(out=out, in_=in_), r, w)

    def memset(self, ap, val, w=(), eng="dve"):
        return self.op(eng, lambda e: e.memset(ap, val), (), w)

    def sb(self, es, name, shape, dt):
        self.uid = getattr(self, "uid", 0) + 1
        return es.enter_context(self.nc.sbuf_tensor("s%d_%s" % (self.uid, name), list(shape), dt))

    def ps(self, es, name, shape, dt=F32):
        self.uid = getattr(self, "uid", 0) + 1
        return es.enter_context(self.nc.psum_tensor("p%d_%s" % (self.uid, name), list(shape), dt))


def make_consts():
    i = np.arange(128)
    ident = np.eye(128, dtype=np.float32)
    J = ident[::-1].copy()
    U = (i[:, None] <= i[None, :]).astype(np.float32)
    L = (i[:, None] >= i[None, :]).astype(np.float32)
    ones = np.ones((128, 128), np.float32)
    nmf = np.where(i[:, None] <= i[None, :], 0.0, NEG).astype(np.float32)
    nmb = np.where(i[:, None] >= i[None, :], 0.0, NEG).astype(np.float32)
    ramp = np.tile(np.arange(1, 129, dtype=np.float32)[None, :], (128, 1))
    sel = np.zeros((128, 8 * 128), np.float32)
    for e in range(8):
        sel[e, e * 128:(e + 1) * 128] = 1.0
    return np.concatenate([ident, J, U, L, ones, nmf, nmb, ramp, sel], axis=1)


C_ID, C_J, C_U, C_L, C_ONE, C_NMF, C_NMB, C_RAMP, C_SEL = [k * 128 for k in range(9)]
NCONST = 16 * 128


def make_rope():
    pos = np.arange(NLAT)
    pr = (pos // 64).astype(np.float32)
    pc = (pos % 64).astype(np.float32)
    inv = (np.float32(10000.0) ** (-np.arange(16, dtype=np.float32) / np.float32(16))).astype(np.float32)
    ang = np.concatenate([pr[:, None] * inv, pc[:, None] * inv], axis=-1).astype(np.float32)
    t = np.zeros((S, 64), np.float32)
    t[:NCTX, :32] = 1.0
    t[NCTX:, :32] = np.cos(ang)
    t[NCTX:, 32:] = np.sin(ang)
    return t


class Ctx:
    pass


def build(dbg=None, layers=DEPTH, stop=None, skip=()):
    dbg = dbg or {}
    nc = bass.Bass("TRN2", target_bir_lowering=False)
    g = Ctx()
    g.nc = nc

    def din(name, shape, dt=F32):
        return nc.dram_tensor(name, list(shape), dt, kind="ExternalInput").ap()

    def dscr(name, shape, dt=F32):
        kind = "ExternalOutput" if name in dbg else "Internal"
        return nc.dram_tensor(name, list(shape), dt, kind=kind).ap()

    I = {}
    I["x"] = din("x", [NLAT, D])
    I["ctx"] = din("ctx", [NCTX, D])
    I["cc"] = din("cc", [16, 128])
    I["consts"] = din("consts", [128, NCONST])
    I["rope"] = din("rope", [S, 64])
    shapes = dict(
        norm1_g=[DEPTH, D], norm2_g=[DEPTH, D], ada_w=[DEPTH, D, 6 * D], ada_b=[DEPTH, 6 * D],
        w_in=[DEPTH, D, 6408], ssd_conv_w=[DEPTH, 5, 768], ssd_conv_b=[DEPTH, 768], ssd_a_log=[DEPTH, 2, 4],
        ssd_dt_bias=[DEPTH, 2, 4], ssd_d=[DEPTH, 4], ssd_norm_g=[DEPTH, 256], s5_lam_re=[DEPTH, 2, 16, 64],
        s5_lam_im=[DEPTH, 2, 16, 64], s5_log_step=[DEPTH, 2, 16], s5_b_re=[DEPTH, 2, 16, 64, 16],
        s5_b_im=[DEPTH, 2, 16, 64, 16], s5_c_re=[DEPTH, 2, 16, 16, 64], s5_c_im=[DEPTH, 2, 16, 16, 64],
        s5_d=[DEPTH, 256], s5_glu_w=[DEPTH, 256, 512], qk_norm_g=[DEPTH, 2, 64], swa_sink=[DEPTH, 4],
        w_branch=[DEPTH, 4, 256, D], w_out=[DEPTH, D, D], ffn_w_in=[2, D, 2 * FFN_DIM], ffn_w_out=[2, FFN_DIM, D],
        moe_router=[2, D, 8], moe_w_in=[2, 8, D, 2 * EXPERT_DIM], moe_w_out=[2, 8, EXPERT_DIM, D],
        final_norm_g=[D],
    )
    for k, shp in shapes.items():
        if k not in skip:
            I[k] = din(k, shp)
    out = nc.dram_tensor("out", [NLAT, D], F32, kind="ExternalOutput").ap()

    g.I = I
    g.xres = dscr("xres", [KC, 128, S])
    g.hT = dscr("hT", [KC, 128, S], BF16)
    g.ptm = dscr("ptm", [S, NPTM])
    g.xbcT = dscr("xbcT", [6, 128, S], BF16)
    g.xbctm = dscr("xbctm", [S, 512], BF16)
    g.yT = dscr("yT", [8, 128, S], BF16)
    g.actT = dscr("actT", [28, 128, 512], BF16)

    with ExitStack() as es:
        p = Prog(nc, es)
        g.p = p
        blk = es.enter_context(nc.Block())
        g.cst = p.sb(es, "cst", [128, NCONST], F32)
        g.cstb = p.sb(es, "cstb", [128, NCONST], BF16)
        g.par = p.sb(es, "par", [128, 512], F32)
        g.mod = p.sb(es, "modv", [128, 48, 2], F32)
        g.AB = p.sb(es, "AB", [128, 4, 8, 2], F32)
        g.cact = p.sb(es, "cact", [128, 8, 2], F32)

        def body(_e):
            setup(g)
            for l in range(layers if stop != "setup" else 0):
                layer(g, l, stop)
                if stop is not None and stop[0] == l:
                    break
            final(g, out, stop is None)
            p.barrier()

        blk.gpsimd(body)
    return nc, p


def load_rows_T(g, es_ps, dst, src2d, n, key):
    p = g.p
    with ExitStack() as es:
        tmp = p.sb(es, "lrt_tmp", [128, 128], F32)
        pst = p.ps(es, "lrt_ps", [128, 128], F32)
        p.dma("sp", tmp[0:n, :], src2d, w=["lrt_tmp"])
        p.tr(pst[:, 0:n], tmp[0:n, :], g.cst[0:n, C_ID:C_ID + n], r=["lrt_tmp", "cst"], w=["lrt_ps"])
        p.cp(dst, pst[:, 0:n], r=["lrt_ps"], w=[key])
        p.barrier()


def setup(g):
    p, nc, I = g.p, g.nc, g.I
    p.dma("sp", g.cst[:, :], I["consts"][:, :], w=["cst"])
    p.cp(g.cstb[:, :], g.cst[:, :], r=["cst"], w=["cstb"])
    with ExitStack() as es:
        t = p.sb(es, "su_t", [128, 16], F32)
        load_rows_T(g, None, t[:, :], I["cc"][:, :], 16, "su_t")
        for i in range(2):
            p.act(g.cact[:, :, i], t[:, i * 8:(i + 1) * 8], AF.Silu, r=["su_t"], w=["cact"])
        p.barrier()
    with ExitStack() as es:
        xin = [p.sb(es, "su_x%d" % i, [128, D], F32) for i in range(2)]
        xo = [p.sb(es, "su_o%d" % i, [128, KC, 128], F32) for i in range(2)]
        pst = [p.ps(es, "su_ps%d" % i, [128, KC, 128], F32) for i in range(2)]
        for t in range(NT):
            b = t % 2
            src = I["ctx"][t * 128:(t + 1) * 128, :] if t < 2 else I["x"][(t - 2) * 128:(t - 1) * 128, :]
            p.dma("sp", xin[b][:, :], src, w=[("xin", b)])
            for c in range(KC):
                p.tr(pst[b][:, c, :], xin[b][:, c * 128:(c + 1) * 128], g.cst[:, C_ID:C_ID + 128],
                     r=[("xin", b), "cst"], w=[("xps", b)])
            p.cp(xo[b][:, :, :], pst[b][:, :, :], r=[("xps", b)], w=[("xo", b)], eng="act" if b else "dve")
            p.dma("sp", g.xres[:, :, t * 128:(t + 1) * 128].rearrange("c p s -> p c s"), xo[b][:, :, :],
                  r=[("xo", b)], w=["xres"])
        p.barrier()


def adaln(g, l):
    p, I = g.p, g.I
    with ExitStack() as es:
        wt = [p.sb(es, "ada_w%d" % i, [128, KC, 768], F32) for i in range(2)]
        adab = p.sb(es, "ada_b", [128, 48], F32)
        gn = p.sb(es, "ada_g", [128, 16], F32)
        mps = p.ps(es, "ada_ps", [128, 48, 2], F32)
        load_rows_T(g, None, adab[:, :], I["ada_b"][l].rearrange("(j q) -> j q", q=128), 48, "adab")
        load_rows_T(g, None, gn[:, 0:8], I["norm1_g"][l].rearrange("(j q) -> j q", q=128), 8, "gn")
        load_rows_T(g, None, gn[:, 8:16], I["norm2_g"][l].rearrange("(j q) -> j q", q=128), 8, "gn")
        wv = I["ada_w"][l].rearrange("(kc q) n -> q kc n", q=128)
        for ob in range(8):
            b = ob % 2
            for kc in range(KC):
                p.dma("sp" if kc % 2 == 0 else "act", wt[b][:, kc, :], wv[:, kc, ob * 768:(ob + 1) * 768], w=[("adaw", b, kc)])
            for jj in range(6):
                j = ob * 6 + jj
                for kc in range(KC):
                    p.mm(mps[:, j, :], wt[b][:, kc, jj * 128:(jj + 1) * 128], g.cact[:, kc, :],
                         start=(kc == 0), stop=(kc == KC - 1), r=[("adaw", b, kc), "cact"], w=["mps"])
        for i in range(2):
            p.tt(g.mod[:, :, i], mps[:, :, i], adab[:, :], ALU.add, r=["mps", "adab"], w=["mod"])
        for i in range(2):
            p.stt(g.AB[:, 0, :, i], g.mod[:, 8:16, i], 1.0, gn[:, 0:8], op0=ALU.add, op1=ALU.mult, r=["mod", "gn"], w=["AB"])
            p.cp(g.AB[:, 1, :, i], g.mod[:, 0:8, i], r=["mod"], w=["AB"])
            p.stt(g.AB[:, 2, :, i], g.mod[:, 32:40, i], 1.0, gn[:, 8:16], op0=ALU.add, op1=ALU.mult, r=["mod", "gn"], w=["AB"])
            p.cp(g.AB[:, 3, :, i], g.mod[:, 24:32, i], r=["mod"], w=["AB"])
        p.barrier()


def norm_tiles(g, which, sink, want32=False):
    p = g.p
    with ExitStack() as es:
        xt = [p.sb(es, "nm_x%d" % i, [128, KC, 512], F32) for i in range(2)]
        sq = p.sb(es, "nm_sq", [128, KC, 512], F32)
        rs = p.sb(es, "nm_rs", [128, 512], F32)
        ht = [p.sb(es, "nm_h%d" % i, [128, KC, 512], BF16) for i in range(2)]
        ssq = p.ps(es, "nm_ps", [128, 512], F32)
        for ti, (s0, n) in enumerate(FT):
            b = ti % 2
            ic = 1 if s0 == 0 else 0
            p.dma("sp", xt[b][:, :, 0:n], g.xres[:, :, s0:s0 + n].rearrange("c p s -> p c s"), r=["xres"], w=[("nmx", b)])
            for c in range(KC):
                p.act(sq[:, c, 0:n], xt[b][:, c, 0:n], AF.Square, r=[("nmx", b)], w=[("nmsq", c)])
                p.mm(ssq[:, 0:n], g.cst[:, C_ONE:C_ONE + 128], sq[:, c, 0:n], start=(c == 0), stop=(c == KC - 1),
                     r=[("nmsq", c), "cst"], w=["nmps"])
            p.act(rs[:, 0:n], ssq[:, 0:n], AF.Ln, bias=EPS, scale=1.0 / D, r=["nmps"], w=["nmrs"])
            p.act(rs[:, 0:n], rs[:, 0:n], AF.Exp, scale=-0.5, r=["nmrs"], w=["nmrs"])
            for c in range(KC):
                p.tt(sq[:, c, 0:n], xt[b][:, c, 0:n], rs[:, 0:n], ALU.mult, r=[("nmx", b), "nmrs"], w=[("nmsq", c)],
                     eng="dve" if c % 2 == 0 else "pool")
                if want32:
                    p.ts(sq[:, c, 0:n], sq[:, c, 0:n], g.AB[:, 2 * which, c, ic:ic + 1], g.AB[:, 2 * which + 1, c, ic:ic + 1],
                         op0=ALU.mult, op1=ALU.add, r=[("nmsq", c), "AB"], w=[("nmsq", c)], eng="dve" if c % 2 == 0 else "pool")
                    p.cp(ht[b][:, c, 0:n], sq[:, c, 0:n], r=[("nmsq", c)], w=[("nmh", b)], eng="dve" if c % 2 == 0 else "pool")
                else:
                    p.ts(ht[b][:, c, 0:n], sq[:, c, 0:n], g.AB[:, 2 * which, c, ic:ic + 1], g.AB[:, 2 * which + 1, c, ic:ic + 1],
                         op0=ALU.mult, op1=ALU.add, r=[("nmsq", c), "AB"], w=[("nmh", b)], eng="dve" if c % 2 == 0 else "pool")
            if want32:
                sink(ti, s0, n, ht[b], ("nmh", b), sq)
            else:
                sink(ti, s0, n, ht[b], ("nmh", b), None)
        p.barrier()


def phase_norm1(g, l):
    p = g.p

    def sink(ti, s0, n, ht, key, h32):
        p.dma("sp", g.hT[:, :, s0:s0 + n].rearrange("c p s -> p c s"), ht[:, :, 0:n], r=[key], w=["hT_d"])

    norm_tiles(g, 0, sink)


def phase_ptm(g, l):
    p, I = g.p, g.I
    wv = I["w_in"][l].rearrange("(kc q) n -> q kc n", q=128)
    with ExitStack() as es:
        w = p.sb(es, "ptm_w", [128, KC, NPTM], BF16)
        ht = [p.sb(es, "ptm_h%d" % i, [128, KC, 512], BF16) for i in range(2)]
        ob = [p.sb(es, "ptm_o%d" % i, [128, NPTM], F32) for i in range(2)]
        ps = [p.ps(es, "ptm_ps%d" % i, [128, 4, 512], F32) for i in range(2)]
        for kc in range(KC):
            p.dma("pool", w[:, kc, 0:256], wv[:, kc, 0:256], w=[("w", kc)])
            p.dma("pool", w[:, kc, 256:NPTM], wv[:, kc, 1024:2312], w=[("w", kc)])
        cols = [(0, 512), (512, 512), (1024, 512), (1536, 8)]
        cnt = 0
        for ti, (s0, n) in enumerate(FT):
            hb = ti % 2
            p.dma("sp", ht[hb][:, :, 0:n], g.hT[:, :, s0:s0 + n].rearrange("c p s -> p c s"), r=["hT_d"], w=[("h", hb)])
            for sub in range(n // 128):
                b = cnt % 2
                cnt += 1
                for kc in range(KC):
                    for bi, (c0, cn) in enumerate(cols):
                        p.mm(ps[b][:, bi, 0:cn], ht[hb][:, kc, sub * 128:(sub + 1) * 128], w[:, kc, c0:c0 + cn],
                             start=(kc == 0), stop=(kc == KC - 1), r=[("h", hb), ("w", kc)], w=[("ps", b, bi)])
                for bi, (c0, cn) in enumerate(cols):
                    p.cp(ob[b][:, c0:c0 + cn], ps[b][:, bi, 0:cn], r=[("ps", b, bi)], w=[("o", b)],
                         eng="act" if bi % 2 else "dve")
                t0 = s0 + sub * 128
                p.dma("sp", g.ptm[t0:t0 + 128, :], ob[b][:, :], r=[("o", b)], w=["ptm_d"])
        p.barrier()


def phase_xbc(g, l):
    p, I = g.p, g.I
    wv = I["w_in"][l].rearrange("(kc q) n -> q kc n", q=128)
    with ExitStack() as es:
        w = p.sb(es, "xb_w", [128, KC, 768], BF16)
        hT = p.sb(es, "xb_h", [128, KC, S], BF16)
        cw = p.sb(es, "xb_cw", [128, 36], F32)
        rawc = p.sb(es, "xb_rc", [128, NCTX + 4], F32)
        rawl = p.sb(es, "xb_rl", [128, NLAT + 4], F32)
        acc = p.sb(es, "xb_acc", [128, S], F32)
        xs = [p.sb(es, "xb_s%d" % i, [128, S], BF16) for i in range(2)]
        tmo = [p.sb(es, "xb_t%d" % i, [128, 4, 128], BF16) for i in range(2)]
        ps = [p.ps(es, "xb_ps%d" % i, [128, 512], F32) for i in range(2)]
        pst = [p.ps(es, "xb_pt%d" % i, [128, 4, 128], F32) for i in range(2)]
        load_rows_T(g, None, cw[:, 0:30], I["ssd_conv_w"][l].rearrange("k (c q) -> (k c) q", q=128), 30, "cw")
        load_rows_T(g, None, cw[:, 30:36], I["ssd_conv_b"][l].rearrange("(c q) -> c q", q=128), 6, "cw")
        for kc in range(KC):
            p.dma("pool", w[:, kc, :], wv[:, kc, 256:1024], w=[("w", kc)])
        for ti, (s0, n) in enumerate(FT):
            p.dma("sp", hT[:, :, s0:s0 + n], g.hT[:, :, s0:s0 + n].rearrange("c p s -> p c s"), r=["hT_d"], w=[("h", ti)])
        p.memset(rawc[:, :], 0.0, w=["rawc"])
        p.memset(rawl[:, :], 0.0, w=["rawl"], eng="pool")
        cnt = 0
        gcnt = 0
        for j in range(6):
            xb = xs[j % 2]
            for ti, (s0, n) in enumerate(FT):
                b = cnt % 2
                cnt += 1
                for kc in range(KC):
                    p.mm(ps[b][:, 0:n], w[:, kc, j * 128:(j + 1) * 128], hT[:, kc, s0:s0 + n], start=(kc == 0),
                         stop=(kc == KC - 1), r=[("w", kc), ("h", ti)], w=[("ps", b)])
                if s0 == 0:
                    p.cp(rawc[:, 2:2 + n], ps[b][:, 0:n], r=[("ps", b)], w=["rawc"], eng="act")
                else:
                    p.cp(rawl[:, 2 + s0 - NCTX:2 + s0 - NCTX + n], ps[b][:, 0:n], r=[("ps", b)], w=["rawl"], eng="act")
            if XBC_LEVEL < 1:
                continue
            pieces = [(rawc, "rawc", 0, 0, NCTX)] + [(rawl, "rawl", q * 1024, NCTX + q * 1024, 1024) for q in range(4)]
            for pi, (rw, rk, o, so, n) in enumerate(pieces):
                eng = "dve"
                a = acc[:, so:so + n]
                p.ts(a, rw[:, o:o + n], cw[:, j:j + 1], r=[rk, "cw"], w=[("acc", pi)], eng=eng)
                for k in range(1, 5):
                    p.stt(a, rw[:, o + k:o + k + n], cw[:, k * 6 + j:k * 6 + j + 1], a, r=[rk, "cw", ("acc", pi)],
                          w=[("acc", pi)], eng=eng)
                p.act(xb[:, so:so + n], a, AF.Silu, bias=cw[:, 30 + j:31 + j], r=[("acc", pi), "cw"], w=[("xs", j % 2, pi)])
            p.dma("sp", g.xbcT[j, :, :], xb[:, :], r=[("xs", j % 2, pi) for pi in range(5)], w=["xbcT_d"])
            if j < 4 and XBC_LEVEL >= 2:
                for t0 in range(0, NT, 4):
                    nt = min(4, NT - t0)
                    b = gcnt % 2
                    gcnt += 1
                    for tt_ in range(nt):
                        t = t0 + tt_
                        p.mm(pst[b][:, tt_, :], xb[:, t * 128:(t + 1) * 128], g.cstb[:, C_ID:C_ID + 128],
                             r=[("xs", j % 2, pi) for pi in range(5)] + ["cstb"], w=[("pt", b)])
                    p.cp(tmo[b][:, 0:nt, :], pst[b][:, 0:nt, :], r=[("pt", b)], w=[("tmo", b)], eng="dve" if b else "act")
                    p.dma("sp", g.xbctm[t0 * 128:(t0 + nt) * 128, j * 128:(j + 1) * 128].rearrange("(t q) c -> q t c", q=128),
                          tmo[b][:, 0:nt, :], r=[("tmo", b)], w=["xbctm_d"])
        p.barrier()


def phase_attn(g, l):
    p, I = g.p, g.I
    with ExitStack() as es:
        qT = p.sb(es, "at_qT", [128, 4, S], BF16)
        kT = p.sb(es, "at_kT", [128, 4, S], BF16)
        vv = p.sb(es, "at_v", [128, NT, 4, 128], BF16)
        yo = p.sb(es, "at_y", [128, 4, S], BF16)
        gbc = p.sb(es, "at_g", [128, 6, 64], F32)
        es_ = p.sb(es, "at_es", [128, 4], F32)
        with ExitStack() as es2:
            pin = [p.sb(es2, "at_in%d" % i, [128, 1024], F32) for i in range(2)]
            rp = [p.sb(es2, "at_rp%d" % i, [128, 64], F32) for i in range(2)]
            sq = p.sb(es2, "at_sq", [128, 384], F32)
            ms = p.sb(es2, "at_ms", [128, 6], F32)
            t1 = p.sb(es2, "at_t1", [128, 12, 2, 16], F32)
            t2 = p.sb(es2, "at_t2", [128, 12, 2, 16], F32)
            ro = p.sb(es2, "at_ro", [128, 12, 64], F32)
            tb = p.sb(es2, "at_tb", [128, 8, 128], BF16)
            pst = [p.ps(es2, "at_pt%d" % i, [128, 8, 128], F32) for i in range(2)]
            for i in range(4):
                p.dma("sp", gbc[:, i, :], I["qk_norm_g"][l, 0:1, :].partition_broadcast(128), w=["gbc"])
            for i in range(2):
                p.dma("sp", gbc[:, 4 + i, :], I["qk_norm_g"][l, 1:2, :].partition_broadcast(128), w=["gbc"])
            p.dma("sp", es_[:, :], I["swa_sink"][l:l + 1, :].partition_broadcast(128), w=["es"])
            p.act(es_[:, :], es_[:, :], AF.Exp, r=["es"], w=["es"])
            p.memset(vv[:, :, :, 64:128], 1.0, w=["vv1"])
            for t in range(NT):
                b = t % 2
                x = pin[b]
                p.dma("sp", x[:, :], g.ptm[t * 128:(t + 1) * 128, PCQ:PCQ + 1024], r=["ptm_d"], w=[("in", b)])
                p.dma("sp", rp[b][:, :], I["rope"][t * 128:(t + 1) * 128, :], w=[("rp", b)])
                p.act(sq[:, :], x[:, 0:384], AF.Square, r=[("in", b)], w=["sq"])
                p.op("dve", lambda e: e.tensor_reduce(out=ms[:, :], in_=sq[:, :].rearrange("p (h f) -> p h f", f=64), axis=AX.X, op=ALU.add),
                     r=["sq"], w=["ms"])
                p.act(ms[:, :], ms[:, :], AF.Ln, bias=EPS, scale=1.0 / 64, r=["ms"], w=["ms"])
                p.act(ms[:, :], ms[:, :], AF.Exp, scale=-0.5, r=["ms"], w=["ms"])
                xg = x[:, 0:384].rearrange("p (h f) -> p h f", f=64)
                p.tt(xg, xg, ms[:, :].unsqueeze(2).broadcast_to([128, 6, 64]), ALU.mult, r=[("in", b), "ms"], w=[("in", b)])
                p.tt(xg, xg, gbc[:, :, :], ALU.mult, r=[("in", b), "gbc"], w=[("in", b)])
                cosb = rp[b][:, 0:32].rearrange("p (a f) -> p a f", a=2)
                sinb = rp[b][:, 32:64].rearrange("p (a f) -> p a f", a=2)
                for (c0, h0, nh) in ((0, 0, 6), (512, 6, 6)):
                    xv = x[:, c0:c0 + nh * 64].rearrange("p (h a t f) -> p h a t f", a=2, t=2, f=16)
                    ov = ro[:, h0:h0 + nh, :].rearrange("p h (a t f) -> p h a t f", a=2, t=2, f=16)
                    for h in range(nh):
                        x1, x2 = xv[:, h, :, 0, :], xv[:, h, :, 1, :]
                        p.tt(t1[:, h0 + h, :, :], x1, cosb, ALU.mult, r=[("in", b), ("rp", b)], w=["t1"])
                        p.tt(t2[:, h0 + h, :, :], x2, sinb, ALU.mult, r=[("in", b), ("rp", b)], w=["t2"], eng="pool")
                        p.tt(ov[:, h, :, 0, :], t1[:, h0 + h, :, :], t2[:, h0 + h, :, :], ALU.subtract, r=["t1", "t2"], w=["ro"])
                        p.tt(t1[:, h0 + h, :, :], x2, cosb, ALU.mult, r=[("in", b), ("rp", b), "ro"], w=["t1"])
                        p.tt(t2[:, h0 + h, :, :], x1, sinb, ALU.mult, r=[("in", b), ("rp", b), "ro"], w=["t2"], eng="pool")
                        p.tt(ov[:, h, :, 1, :], t1[:, h0 + h, :, :], t2[:, h0 + h, :, :], ALU.add, r=["t1", "t2"], w=["ro"])
                rof = ro[:, :, :].rearrange("p h f -> p (h f)")
                p.cp(tb[:, 0:2, :], rof[:, 0:256].rearrange("p (c f) -> p c f", f=128), r=["ro"], w=["tb"])
                p.cp(tb[:, 4:6, :], rof[:, 384:640].rearrange("p (c f) -> p c f", f=128), r=["ro"], w=["tb"])
                for kv in range(2):
                    for d in range(2):
                        p.cp(tb[:, 2 + kv, d * 64:(d + 1) * 64], ro[:, 4 + kv, :], r=["ro"], w=["tb"], eng="pool")
                        p.cp(tb[:, 6 + kv, d * 64:(d + 1) * 64], ro[:, 10 + kv, :], r=["ro"], w=["tb"], eng="pool")
                p.cp(vv[:, t, 0:2, 0:64], x[:, 384:512].rearrange("p (k f) -> p k f", f=64), r=[("in", b)], w=[("vv", t)], eng="act")
                p.cp(vv[:, t, 2:4, 0:64], x[:, 896:1024].rearrange("p (k f) -> p k f", f=64), r=[("in", b)], w=[("vv", t)], eng="act")
                for c in range(8):
                    p.mm(pst[b][:, c, :], tb[:, c, :], g.cstb[:, C_ID:C_ID + 128], r=["tb", "cstb"], w=[("pt", b)])
                sl = slice(t * 128, (t + 1) * 128)
                ce = "act" if b else "dve"
                p.cp(qT[:, 0:2, sl], pst[b][:, 0:2, :], r=[("pt", b)], w=[("qk", t)], eng=ce)
                p.cp(kT[:, 0:2, sl], pst[b][:, 2:4, :], r=[("pt", b)], w=[("qk", t)], eng=ce)
                p.cp(qT[:, 2:4, sl], pst[b][:, 4:6, :], r=[("pt", b)], w=[("qk", t)], eng=ce)
                p.cp(kT[:, 2:4, sl], pst[b][:, 6:8, :], r=[("pt", b)], w=[("qk", t)], eng=ce)
            p.barrier()
        with ExitStack() as es2:
            pss = [p.ps(es2, "ga_s%d" % i, [128, 512], F32) for i in range(3)]
            pso = [p.ps(es2, "ga_o%d" % i, [128, 512], F32) for i in range(2)]
            pt = [p.sb(es2, "ga_p%d" % i, [128, 512], BF16) for i in range(3)]
            rd = p.sb(es2, "ga_rd", [128, 512], F32)
            cs = 0
            co = 0
            for ti, (s0, n) in enumerate(FT):
                kts = range(2) if s0 == 0 else range(NT)
                for h in range(4):
                    c, hp, kv = h // 2, (h % 2) * 64, h // 2
                    ob = co % 2
                    co += 1
                    for kt in kts:
                        sb_ = cs % 3
                        cs += 1
                        p.mm(pss[sb_][:, 0:n], kT[hp:hp + 64, c, kt * 128:(kt + 1) * 128], qT[hp:hp + 64, c, s0:s0 + n],
                             w=[("gs", sb_)])
                        p.act(pt[sb_][:, 0:n], pss[sb_][:, 0:n], AF.Exp, scale=0.125, r=[("gs", sb_)], w=[("gp", sb_)])
                        p.mm(pso[ob][:, 0:n], vv[:, kt, kv, :], pt[sb_][:, 0:n], start=(kt == kts[0]), stop=(kt == kts[-1]),
                             r=[("gp", sb_)], w=[("go", ob)])
                    p.op("dve", lambda e: e.reciprocal(out=rd[0:64, 0:n], in_=pso[ob][64:128, 0:n]), r=[("go", ob)], w=["rd"])
                    p.tt(yo[hp:hp + 64, c, s0:s0 + n], pso[ob][0:64, 0:n], rd[0:64, 0:n], ALU.mult, r=[("go", ob), "rd"], w=[("yo", c)])
            p.barrier()
        with ExitStack() as es2:
            pss = [p.ps(es2, "wa_s%d" % i, [128, 4, 128], F32) for i in range(3)]
            pso = [p.ps(es2, "wa_o%d" % i, [128, 4, 128], F32) for i in range(2)]
            pt = [p.sb(es2, "wa_p%d" % i, [128, 4, 128], BF16) for i in range(3)]
            dt_ = p.sb(es2, "wa_d", [128, 4, 128], F32)
            rd = p.sb(es2, "wa_rd", [128, 4, 128], F32)
            cs = 0
            co = 0
            for qb in range(NT):
                kl = [(0, None), (1, None)]
                if qb >= 2:
                    if qb - 1 >= 2:
                        kl.append((qb - 1, C_L))
                    kl.append((qb, None))
                    if qb + 1 < NT:
                        kl.append((qb + 1, C_U))
                qs = slice(qb * 128, (qb + 1) * 128)
                for h in range(4):
                    c, hp = 2 + h // 2, (h % 2) * 64
                    ob = co % 2
                    co += 1
                    for ki, (kt, msk) in enumerate(kl):
                        sb_ = cs % 3
                        cs += 1
                        p.mm(pss[sb_][:, 0, :], kT[hp:hp + 64, c, kt * 128:(kt + 1) * 128], qT[hp:hp + 64, c, qs], w=[("ws", sb_)])
                        p.act(pt[sb_][:, 0, :], pss[sb_][:, 0, :], AF.Exp, scale=0.125, r=[("ws", sb_)], w=[("wp", sb_)])
                        if msk is not None:
                            p.tt(pt[sb_][:, 0, :], pt[sb_][:, 0, :], g.cstb[:, msk:msk + 128], ALU.mult, r=[("wp", sb_), "cstb"],
                                 w=[("wp", sb_)])
                        p.mm(pso[ob][:, 0, :], vv[:, kt, 2 + h // 2, :], pt[sb_][:, 0, :], start=(ki == 0), stop=(ki == len(kl) - 1),
                             r=[("wp", sb_)], w=[("wo", ob)])
                    p.ts(dt_[64:128, 0, :], pso[ob][64:128, 0, :], es_[64:128, h:h + 1], op0=ALU.add, r=[("wo", ob), "es"], w=["wd"])
                    p.op("dve", lambda e: e.reciprocal(out=rd[0:64, 0, :], in_=dt_[64:128, 0, :]), r=["wd"], w=["wrd"])
                    p.tt(yo[hp:hp + 64, c, qs], pso[ob][0:64, 0, :], rd[0:64, 0, :], ALU.mult, r=[("wo", ob), "wrd"], w=[("yo", c)])
            p.barrier()
        for c in range(4):
            p.dma("sp", g.yT[4 + c, :, :], yo[:, c, :], w=["yT_d"])
        p.barrier()


def phase_ssd(g, l):
    p, I = g.p, g.I
    NC8 = NT * 8
    with ExitStack() as es:
        xsB = p.sb(es, "sd_xsB", [128, NT, 512], BF16)
        zdt = p.sb(es, "sd_zdt", [128, NT, 264], F32)
        ysum = p.sb(es, "sd_y", [128, NT, 256], F32)
        prm = p.sb(es, "sd_prm", [128, 20], F32)
        ng = p.sb(es, "sd_ng", [128, 256], F32)
        for t0 in range(0, NT, 4):
            nt = min(4, NT - t0)
            ts_ = slice(t0, t0 + nt)
            p.dma("sp", xsB[:, ts_, :], g.xbctm[t0 * 128:(t0 + nt) * 128, :].rearrange("(t q) c -> q t c", q=128),
                  r=["xbctm_d"], w=["xsB"])
            p.dma("act", zdt[:, ts_, :], g.ptm[t0 * 128:(t0 + nt) * 128, 0:264].rearrange("(t q) c -> q t c", q=128),
                  r=["ptm_d"], w=["zdt"])
        p.dma("sp", prm[:, 0:8], I["ssd_dt_bias"][l:l + 1].rearrange("o d h -> o (d h)").partition_broadcast(128), w=["prm"])
        p.dma("sp", prm[:, 8:16], I["ssd_a_log"][l:l + 1].rearrange("o d h -> o (d h)").partition_broadcast(128), w=["prm"])
        p.dma("sp", prm[:, 16:20], I["ssd_d"][l:l + 1, :].partition_broadcast(128), w=["prm"])
        p.dma("sp", ng[:, :], I["ssd_norm_g"][l:l + 1, :].partition_broadcast(128), w=["ng"])
        p.act(prm[:, 8:16], prm[:, 8:16], AF.Exp, r=["prm"], w=["prm"])
        p.ts(prm[:, 8:16], prm[:, 8:16], -1.0, r=["prm"], w=["prm"])
        with ExitStack() as es1:
            BCT = p.sb(es1, "sd_bct", [128, 4, S], BF16)
            dt = p.sb(es1, "sd_dt", [128, NT, 8], F32)
            la = p.sb(es1, "sd_la", [128, NT, 8], F32)
            cum = p.sb(es1, "sd_cum", [128, NT, 8], F32)
            tot = p.sb(es1, "sd_tot", [128, NT, 8], F32)
            eoff = p.sb(es1, "sd_eoff", [128, NT, 8], F32)
            dtdte = p.sb(es1, "sd_dte", [128, NT, 8], F32)
            etot = p.sb(es1, "sd_etot", [128, NT, 8], F32)
            ncum = p.sb(es1, "sd_ncum", [128, NT, 8], F32)
            nm4 = p.sb(es1, "sd_nm4", [128, 2, 4, 128], F32)
            laU = [p.sb(es1, "sd_laU%d" % i, [128, 4, 128], F32) for i in range(2)]
            dec = p.sb(es1, "sd_dec", [128, 4, 128], F32)
            WT = p.sb(es1, "sd_WT", [128, 4, 128], BF16)
            xdt = p.sb(es1, "sd_xdt", [128, 4, 64], BF16)
            xdte = p.sb(es1, "sd_xdte", [128, 4, 64], BF16)
            ydsb = p.sb(es1, "sd_yd", [128, 256], F32)
            Sst = p.sb(es1, "sd_S", [128, 4, 64], F32)
            Sb = p.sb(es1, "sd_Sb", [128, 4, 64], BF16)
            pc = p.ps(es1, "sd_pc", [128, NC8], F32)
            dps = [p.ps(es1, "sd_dps%d" % i, [128, 4, 128], F32) for i in range(2)]
            gps = p.ps(es1, "sd_gps", [128, 2, 128], F32)
            ydp = p.ps(es1, "sd_ydp", [128, 4, 64], F32)
            yop = p.ps(es1, "sd_yop", [128, 4, 64], F32)
            stp = p.ps(es1, "sd_stp", [128, 4, 64], F32)
            for c in range(4):
                p.dma("sp", BCT[:, c, :], g.xbcT[2 + c, :, :], r=["xbcT_d"], w=["BCT"])
            for d in range(2):
                for h in range(4):
                    nm = C_NMF if d == 0 else C_NMB
                    p.cp(nm4[:, d, h, :], g.cst[:, nm:nm + 128], r=["cst"], w=["nm4"], eng="pool")
            bc8 = lambda ap: ap.unsqueeze(1).broadcast_to([128, NT, 8])
            p.tt(dt[:, :, :], zdt[:, :, 256:264], bc8(prm[:, 0:8]), ALU.add, r=["zdt", "prm"], w=["dt"])
            p.act(dt[:, :, :], dt[:, :, :], AF.Exp, r=["dt"], w=["dt"])
            p.act(dt[:, :, :], dt[:, :, :], AF.Ln, bias=1.0, r=["dt"], w=["dt"])
            p.tt(la[:, :, :], dt[:, :, :], bc8(prm[:, 8:16]), ALU.mult, r=["dt", "prm"], w=["la"])
            laf = la[:, :, :].rearrange("p t c -> p (t c)")
            pc3 = pc[:, :].rearrange("p (t c) -> p t c", c=8)
            p.mm(pc[:, :], g.cst[:, C_U:C_U + 128], laf, r=["la", "cst"], w=["pc"])
            p.cp(cum[:, :, 0:4], pc3[:, :, 0:4], r=["pc"], w=["cum"])
            p.mm(pc[:, :], g.cst[:, C_L:C_L + 128], laf, r=["la", "cst"], w=["pc"])
            p.cp(cum[:, :, 4:8], pc3[:, :, 4:8], r=["pc"], w=["cum"])
            p.mm(pc[:, :], g.cst[:, C_ONE:C_ONE + 128], laf, r=["la", "cst"], w=["pc"])
            p.cp(tot[:, :, :], pc3, r=["pc"], w=["tot"])
            p.act(eoff[:, :, :], cum[:, :, :], AF.Exp, r=["cum"], w=["eoff"])
            p.act(etot[:, :, :], tot[:, :, :], AF.Exp, r=["tot"], w=["etot"])
            p.tt(dtdte[:, :, :], tot[:, :, :], cum[:, :, :], ALU.subtract, r=["tot", "cum"], w=["dtdte"])
            p.act(dtdte[:, :, :], dtdte[:, :, :], AF.Exp, r=["dtdte"], w=["dtdte"])
            p.tt(dtdte[:, :, :], dtdte[:, :, :], dt[:, :, :], ALU.mult, r=["dtdte", "dt"], w=["dtdte"])
            p.ts(ncum[:, :, :], cum[:, :, :], -1.0, r=["cum"], w=["ncum"])
            cnt = 0
            for d in range(min(2, SSD_LEVEL)):
                order = list(range(NT)) if d == 0 else [1, 0] + list(range(NT - 1, 1, -1))
                tri = C_U if d == 0 else C_L
                p.memset(Sst[:, :, :], 0.0, w=["S"])
                p.memset(Sb[:, :, :], 0.0, w=["Sb"])
                for t in order:
                    b = cnt % 2
                    cnt += 1
                    sl = slice(t * 128, (t + 1) * 128)
                    for h in range(4):
                        p.ts(laU[b][:, h, :], g.cst[:, tri:tri + 128], la[:, t, d * 4 + h:d * 4 + h + 1], r=["la", "cst"],
                             w=[("laU", b)], eng="pool" if h % 2 else "dve")
                    dflat = dps[b][:, :, :].rearrange("p h l -> p (h l)")
                    p.mm(dflat, g.cst[:, C_ONE:C_ONE + 128], laU[b][:, :, :].rearrange("p h l -> p (h l)"), start=True, stop=False,
                         r=[("laU", b), "cst"], w=[("dps", b)])
                    p.mm(dflat, g.cst[:, C_ID:C_ID + 128], nm4[:, d, :, :].rearrange("p h l -> p (h l)"), start=False, stop=True,
                         r=["nm4", "cst"], w=[("dps", b)])
                    for h in range(4):
                        p.act(dec[:, h, :], dps[b][:, h, :], AF.Exp, bias=ncum[:, t, d * 4 + h:d * 4 + h + 1],
                              r=[("dps", b), "ncum"], w=[("dec", h)])
                    for gq in range(2):
                        p.mm(gps[:, gq, :], BCT[:, gq, sl], BCT[:, 2 + gq, sl], r=["BCT"], w=[("gps", gq)])
                    for h in range(4):
                        p.tt(WT[:, h, :], dec[:, h, :], gps[:, h // 2, :], ALU.mult, r=[("dec", h), ("gps", h // 2)], w=[("WT", h)])
                    xs4 = xsB[:, t, 0:256].rearrange("p (h f) -> p h f", f=64)
                    p.tt(xdt[:, :, :], xs4, dt[:, t, d * 4:d * 4 + 4].unsqueeze(2).broadcast_to([128, 4, 64]), ALU.mult,
                         r=["xsB", "dt"], w=["xdt"], eng="pool")
                    p.tt(xdte[:, :, :], xs4, dtdte[:, t, d * 4:d * 4 + 4].unsqueeze(2).broadcast_to([128, 4, 64]), ALU.mult,
                         r=["xsB", "dtdte"], w=["xdte"], eng="pool")
                    for h in range(4):
                        p.mm(ydp[:, h, :], WT[:, h, :], xdt[:, h, :], r=[("WT", h), "xdt"], w=["ydp"])
                    for h in range(4):
                        p.mm(yop[:, h, :], BCT[:, 2 + h // 2, sl], Sb[:, h, :], r=["BCT", "Sb"], w=["yop"])
                    p.cp(ydsb[:, :], ydp[:, :, :].rearrange("p h f -> p (h f)"), r=["ydp"], w=["ydsb"], eng="act")
                    if d == 1:
                        p.tt(ydsb[:, :], ydsb[:, :], ysum[:, t, :], ALU.add, r=["ydsb", ("ysum", t)], w=["ydsb"])
                    for h in range(4):
                        p.stt(ysum[:, t, h * 64:(h + 1) * 64], yop[:, h, :], eoff[:, t, d * 4 + h:d * 4 + h + 1], ydsb[:, h * 64:(h + 1) * 64],
                              r=["yop", "eoff", "ydsb"], w=[("ysum", t)])
                    for h in range(4):
                        p.mm(stp[:, h, :], xsB[:, t, 256 + (h // 2) * 128:256 + (h // 2 + 1) * 128], xdte[:, h, :], r=["xsB", "xdte"], w=["stp"])
                    for h in range(4):
                        p.stt(Sst[:, h, :], Sst[:, h, :], etot[:, t, d * 4 + h:d * 4 + h + 1], stp[:, h, :], r=["S", "etot", "stp"], w=["S"])
                    p.cp(Sb[:, :, :], Sst[:, :, :], r=["S"], w=["Sb"], eng="act")
            p.barrier()
        with ExitStack() as es1:
          if SSD_LEVEL >= 3:
              sq = p.sb(es1, "sd_sq", [128, 256], F32)
              ms = p.sb(es1, "sd_ms", [128, NT], F32)
              yaT = p.sb(es1, "sd_yaT", [128, 2, S], BF16)
              yab = p.sb(es1, "sd_yab", [128, NT, 256], BF16)
              pst = [p.ps(es1, "sd_pt%d" % i, [128, 4, 128], F32) for i in range(2)]
              for t in range(NT):
                  e1 = "dve"
                  for h in range(4):
                      hs = slice(h * 64, (h + 1) * 64)
                      p.stt(ysum[:, t, hs], xsB[:, t, hs], prm[:, 16 + h:17 + h], ysum[:, t, hs], r=["xsB", "prm", ("ys", t)], w=[("ys", t)])
                  p.act(zdt[:, t, 0:256], zdt[:, t, 0:256], AF.Silu, r=[("z", t)], w=[("z", t)])
                  p.tt(ysum[:, t, :], ysum[:, t, :], zdt[:, t, 0:256], ALU.mult, r=[("z", t), ("ys", t)], w=[("ys", t)])
                  p.act(sq[:, :], ysum[:, t, :], AF.Square, r=[("ys", t)], w=["sq"])
                  p.op("dve", lambda e: e.tensor_reduce(out=ms[:, t:t + 1], in_=sq[:, :], axis=AX.X, op=ALU.add), r=["sq"], w=["ms"])
              p.act(ms[:, :], ms[:, :], AF.Ln, bias=EPS, scale=1.0 / 256, r=["ms"], w=["ms"])
              p.act(ms[:, :], ms[:, :], AF.Exp, scale=-0.5, r=["ms"], w=["ms"])
              for t in range(NT):
                  p.stt(yab[:, t, :], ysum[:, t, :], ms[:, t:t + 1], ng[:, :], op0=ALU.mult, op1=ALU.mult, r=[("ys", t), "ms", "ng"], w=["yab"])
              k = 0
              for c in (range(2) if SSD_LEVEL >= 4 else []):
                  for t0 in range(0, NT, 4):
                      nt = min(4, NT - t0)
                      b = k % 2
                      k += 1
                      for i in range(nt):
                          p.mm(pst[b][:, i, :], yab[:, t0 + i, c * 128:(c + 1) * 128], g.cstb[:, C_ID:C_ID + 128],
                               r=["yab", "cstb"], w=[("pt", b)])
                      p.cp(yaT[:, c, t0 * 128:(t0 + nt) * 128].rearrange("p (t s) -> p t s", t=nt), pst[b][:, 0:nt, :],
                           r=[("pt", b)], w=[("yaT", c)], eng="act" if b else "dve")
              for c in range(2):
                  p.dma("sp", g.yT[c, :, :], yaT[:, c, :], r=[("yaT", c)], w=["yT_d"])
              p.barrier()


def phase_s5(g, l):
    p, I = g.p, g.I
    TWO_PI = 2.0 * np.pi
    rt = lambda c: (1 - c) if c < 2 else (35 - c)
    with ExitStack() as es:
        u_tm = p.sb(es, "s5_utm", [128, NT, 256], F32)
        useq = p.sb(es, "s5_useq", [128, 2, S], F32)
        ysum = p.sb(es, "s5_ysum", [128, 2, S], F32)
        dcol = p.sb(es, "s5_d", [128, 2], F32)
        zero8 = p.sb(es, "s5_z8", [128, 8], F32)
        load_rows_T(g, None, dcol[:, :], I["s5_d"][l].rearrange("(c q) -> c q", q=128), 2, "dcol")
        p.memset(zero8[:, :], 0.0, w=["zero8"])
        for t0 in range(0, NT, 4):
            nt = min(4, NT - t0)
            p.dma("sp", u_tm[:, t0:t0 + nt, :], g.ptm[t0 * 128:(t0 + nt) * 128, PU:PU + 256].rearrange("(t q) c -> q t c", q=128),
                  r=["ptm_d"], w=["u_tm"])
        with ExitStack() as es1:
            pst = [p.ps(es1, "s5_pu%d" % i, [128, 4, 128], F32) for i in range(2)]
            bre = p.ps(es1, "s5_bre", [128, 4, 128], F32)
            bim = p.ps(es1, "s5_bim", [128, 4, 128], F32)
            yp = p.ps(es1, "s5_yp", [128, 128], F32)
            ytm = p.ps(es1, "s5_ytm", [128, 128], F32)
            prm = p.sb(es1, "s5_prm", [128, 16, 8], F32)
            T16 = p.sb(es1, "s5_T16", [128, 2, 16], F32)
            st16 = p.sb(es1, "s5_st16", [128, 16], F32)
            rr = p.sb(es1, "s5_r", [128, 8, 128], F32)
            ki = p.sb(es1, "s5_ki", [128, 8, 128], I32)
            kf = p.sb(es1, "s5_kf", [128, 8, 128], F32)
            cosT = p.sb(es1, "s5_cos", [128, 8, 128], F32)
            sinT = p.sb(es1, "s5_sin", [128, 8, 128], F32)
            Mre = p.sb(es1, "s5_Mre", [128, 8, 128], F32)
            Mim = p.sb(es1, "s5_Mim", [128, 8, 128], F32)
            Bre = p.sb(es1, "s5_Bre", [128, 8, 128], F32)
            Bim = p.sb(es1, "s5_Bim", [128, 8, 128], F32)
            Cre = p.sb(es1, "s5_Cre", [128, 8, 128], F32)
            Cim = p.sb(es1, "s5_Cim", [128, 8, 128], F32)
            tA = p.sb(es1, "s5_tA", [128, 4, 128], F32)
            tB = p.sb(es1, "s5_tB", [128, 4, 128], F32)
            vre = p.sb(es1, "s5_vre", [128, 4, 128], F32)
            vim = p.sb(es1, "s5_vim", [128, 4, 128], F32)
            wre = p.sb(es1, "s5_wre", [128, 4, 128], F32)
            wim = p.sb(es1, "s5_wim", [128, 4, 128], F32)
            xre = [p.sb(es1, "s5_xre%d" % i, [128, 8, 128], F32) for i in range(2)]
            xim = [p.sb(es1, "s5_xim%d" % i, [128, 8, 128], F32) for i in range(2)]
            ysb = p.sb(es1, "s5_ysb", [128, 128], F32)
            ident = g.cst[:, C_ID:C_ID + 128]
            LR, LI, ST, RHO, THP, CO, SI, ABR, ABI, DEN, FRE, FIM, TMP, TMP2 = range(14)
            for d in range(2):
                k = 0
                for c in (range(2) if S5_LEVEL >= 0 else []):
                    for t0 in range(0, NT, 4):
                        nt = min(4, NT - t0)
                        b = k % 2
                        k += 1
                        for i in range(nt):
                            src = t0 + i if d == 0 else rt(t0 + i)
                            rhs = ident if d == 0 else g.cst[:, C_J:C_J + 128]
                            p.mm(pst[b][:, i, :], u_tm[:, src, c * 128:(c + 1) * 128], rhs, r=["u_tm", "cst"], w=[("pu", b)])
                        dst = useq[:, c, t0 * 128:(t0 + nt) * 128].rearrange("p (t s) -> p t s", t=nt)
                        p.cp(dst, pst[b][:, 0:nt, :], r=[("pu", b)], w=["useq"], eng="act")
                        if d == 0:
                            yd_ = ysum[:, c, t0 * 128:(t0 + nt) * 128].rearrange("p (t s) -> p t s", t=nt)
                            p.ts(yd_, dst, dcol[:, c:c + 1], r=["useq", "dcol"], w=["ysum"])
                if S5_LEVEL < 1:
                    continue
                for which, nm in ((0, "s5_lam_re"), (1, "s5_lam_im")):
                    p.dma("sp", tA[0:16, 0, 0:64], I[nm][l, d, :, :], w=["tA"])
                    p.tr(yp[0:64, 0:16], tA[0:16, 0, 0:64], g.cst[0:16, C_ID:C_ID + 16], r=["tA", "cst"], w=["yp"])
                    p.cp(T16[0:64, which, :], yp[0:64, 0:16], r=["yp"], w=["T16"])
                    tv = T16[0:64, which, :].rearrange("p (gb gl) -> p gl gb", gl=2)
                    p.cp(prm[0:64, which, :], tv[:, 0, :], r=["T16"], w=["prm"])
                    p.cp(prm[64:128, which, :], tv[:, 1, :], r=["T16"], w=["prm"])
                p.dma("sp", st16[:, :], I["s5_log_step"][l, d:d + 1, :].partition_broadcast(128), w=["st16"])
                p.act(st16[:, :], st16[:, :], AF.Exp, r=["st16"], w=["st16"])
                sv = st16[:, :].rearrange("p (gb gl) -> p gl gb", gl=2)
                p.cp(prm[0:64, ST, :], sv[0:64, 0, :], r=["st16"], w=["prm"])
                p.cp(prm[64:128, ST, :], sv[64:128, 1, :], r=["st16"], w=["prm"])
                P = lambda i: prm[:, i, :]
                p.ts(P(LR), P(LR), -1e-4, op0=ALU.min, r=["prm"], w=["prm"])
                p.tt(P(TMP), P(LR), P(ST), ALU.mult, r=["prm"], w=["prm"])
                p.act(P(RHO), P(TMP), AF.Exp, r=["prm"], w=["prm"])
                p.tt(P(THP), P(LI), P(ST), ALU.mult, r=["prm"], w=["prm"])
                p.ts(P(THP), P(THP), 1.0 / TWO_PI, r=["prm"], w=["prm"])
                if S5_LEVEL < 2:
                    continue
                for gb in range(8):
                    p.ts(rr[:, gb, :], g.cst[:, C_RAMP:C_RAMP + 128], prm[:, THP, gb:gb + 1], r=["prm", "cst"], w=["rr"],
                         eng="pool" if gb % 2 else "dve")
                for (tab, shift) in ((sinT, 0.0), (cosT, 0.25)):
                    for hf in range(2):
                        hs = slice(hf * 4, hf * 4 + 4)
                        if shift:
                            p.ts(kf[:, hs, :], rr[:, hs, :], shift, op0=ALU.add, r=["rr"], w=["kf"])
                            src = kf
                        else:
                            src = rr
                        p.cp(ki[:, hs, :], src[:, hs, :], r=["rr", "kf"], w=["ki"])
                        p.cp(tab[:, hs, :], ki[:, hs, :], r=["ki"], w=["tab"])
                        p.tt(tab[:, hs, :], src[:, hs, :], tab[:, hs, :], ALU.subtract, r=["tab", "rr", "kf"], w=["tab"])
                        p.act(tab[:, hs, :], tab[:, hs, :], AF.Sin, scale=TWO_PI, r=["tab"], w=["tab"])
                p.cp(P(CO), cosT[:, :, 0], r=["tab"], w=["prm"])
                p.cp(P(SI), sinT[:, :, 0], r=["tab"], w=["prm"])
                p.tt(P(ABR), P(RHO), P(CO), ALU.mult, r=["prm"], w=["prm"])
                p.tt(P(ABI), P(RHO), P(SI), ALU.mult, r=["prm"], w=["prm"])
                p.tt(P(DEN), P(LR), P(LR), ALU.mult, r=["prm"], w=["prm"])
                p.tt(P(TMP), P(LI), P(LI), ALU.mult, r=["prm"], w=["prm"])
                p.tt(P(DEN), P(DEN), P(TMP), ALU.add, r=["prm"], w=["prm"])
                p.op("dve", lambda e: e.reciprocal(out=P(DEN), in_=P(DEN)), r=["prm"], w=["prm"])
                p.ts(P(ABR), P(ABR), -1.0, op0=ALU.add, r=["prm"], w=["prm"])
                p.tt(P(TMP), P(ABR), P(LR), ALU.mult, r=["prm"], w=["prm"])
                p.tt(P(TMP2), P(ABI), P(LI), ALU.mult, r=["prm"], w=["prm"])
                p.tt(P(FRE), P(TMP), P(TMP2), ALU.add, r=["prm"], w=["prm"])
                p.tt(P(FRE), P(FRE), P(DEN), ALU.mult, r=["prm"], w=["prm"])
                p.tt(P(TMP), P(ABI), P(LR), ALU.mult, r=["prm"], w=["prm"])
                p.tt(P(TMP2), P(ABR), P(LI), ALU.mult, r=["prm"], w=["prm"])
                p.tt(P(FIM), P(TMP), P(TMP2), ALU.subtract, r=["prm"], w=["prm"])
                p.tt(P(FIM), P(FIM), P(DEN), ALU.mult, r=["prm"], w=["prm"])
                if S5_LEVEL < 3:
                    continue
                for m_ in (Mre, Mim):
                    for hf in range(2):
                        p.memset(m_[:, hf * 4:hf * 4 + 4, :], 0.0, w=["M"], eng="pool" if hf else "dve")
                for gi in range(16):
                    gb, gl, gic = gi // 2, gi % 2, gi % 8
                    p.dma("sp", Mre[gl * 64:(gl + 1) * 64, gb, gic * 16:(gic + 1) * 16], I["s5_b_re"][l, d, gi, :, :], r=[], w=["M"])
                    p.dma("act", Mim[gl * 64:(gl + 1) * 64, gb, gic * 16:(gic + 1) * 16], I["s5_b_im"][l, d, gi, :, :], r=[], w=["M"])
                for gb in range(8):
                    fr, fi = prm[:, FRE, gb:gb + 1], prm[:, FIM, gb:gb + 1]
                    p.ts(tA[:, 0, :], Mim[:, gb, :], fi, r=["M", "prm"], w=["tA"])
                    p.stt(Bre[:, gb, :], Mre[:, gb, :], fr, tA[:, 0, :], op0=ALU.mult, op1=ALU.subtract, r=["M", "prm", "tA"], w=["B0"])
                    p.ts(tB[:, 0, :], Mre[:, gb, :], fi, r=["M", "prm"], w=["tB"])
                    p.stt(Bim[:, gb, :], Mim[:, gb, :], fr, tB[:, 0, :], op0=ALU.mult, op1=ALU.add, r=["M", "prm", "tB"], w=["B0"])
                for (src, dst, key) in ((Bre, Mre, "BT"), (Bim, Mim, "BT")):
                    for hf in range(2):
                        b = hf
                        for j in range(4):
                            p.tr(pst[b][:, j, :], src[:, hf * 4 + j, :], ident, r=["B0", "cst"], w=[("pu", b)])
                        p.cp(dst[:, hf * 4:hf * 4 + 4, :], pst[b][:, :, :], r=[("pu", b)], w=[key], eng="act")
                BTre, BTim = Mre, Mim
                for m_ in (Bre, Bim):
                    for hf in range(2):
                        p.memset(m_[:, hf * 4:hf * 4 + 4, :], 0.0, w=["B0"], eng="pool" if hf else "dve")
                for gi in range(16):
                    gb, gl, gic = gi // 2, gi % 2, gi % 8
                    p.dma("sp", Bre[gic * 16:(gic + 1) * 16, gb, gl * 64:(gl + 1) * 64], I["s5_c_re"][l, d, gi, :, :], r=[], w=["B0"])
                    p.dma("act", Bim[gic * 16:(gic + 1) * 16, gb, gl * 64:(gl + 1) * 64], I["s5_c_im"][l, d, gi, :, :], r=[], w=["B0"])
                for (src, dst, neg) in ((Bre, Cre, False), (Bim, Cim, True)):
                    for hf in range(2):
                        b = hf
                        for j in range(4):
                            p.tr(pst[b][:, j, :], src[:, hf * 4 + j, :], ident, r=["B0", "cst"], w=[("pu", b)])
                        if neg:
                            p.ts(dst[:, hf * 4:hf * 4 + 4, :], pst[b][:, :, :], -1.0, r=[("pu", b)], w=["CT"])
                        else:
                            p.cp(dst[:, hf * 4:hf * 4 + 4, :], pst[b][:, :, :], r=[("pu", b)], w=["CT"], eng="act")
                if S5_LEVEL < 4:
                    continue
                for c in range(NT):
                    cur, prv = c % 2, (c + 1) % 2
                    ps_ = slice(c * 128, (c + 1) * 128)
                    for hf in range(2):
                        hs = slice(hf * 4, hf * 4 + 4)
                        for j in range(4):
                            p.mm(bre[:, j, :], BTre[:, hf * 4 + j, :], useq[:, hf, ps_], r=["BT", "useq"], w=["bre"])
                        for j in range(4):
                            p.mm(bim[:, j, :], BTim[:, hf * 4 + j, :], useq[:, hf, ps_], r=["BT", "useq"], w=["bim"])
                        p.tt(tA[:, :, :], bre[:, :, :], cosT[:, hs, :], ALU.mult, r=["bre", "tab"], w=["tA"])
                        p.tt(tB[:, :, :], bim[:, :, :], sinT[:, hs, :], ALU.mult, r=["bim", "tab"], w=["tB"])
                        p.tt(vre[:, :, :], tA[:, :, :], tB[:, :, :], ALU.add, r=["tA", "tB"], w=["vre"], eng="pool")
                        p.tt(tA[:, :, :], bim[:, :, :], cosT[:, hs, :], ALU.mult, r=["bim", "tab", "vre"], w=["tA"])
                        p.tt(tB[:, :, :], bre[:, :, :], sinT[:, hs, :], ALU.mult, r=["bre", "tab", "vre"], w=["tB"])
                        p.tt(vim[:, :, :], tA[:, :, :], tB[:, :, :], ALU.subtract, r=["tA", "tB"], w=["vim"], eng="pool")
                        for j in range(4):
                            gb = hf * 4 + j
                            rho_b = prm[:, RHO, gb:gb + 1].broadcast_to([128, 128])
                            i_re = zero8[:, 0:1] if c == 0 else xre[prv][:, gb, 127:128]
                            i_im = zero8[:, 0:1] if c == 0 else xim[prv][:, gb, 127:128]
                            p.op("dve", lambda e, j=j, i_re=i_re, rho_b=rho_b: e.tensor_tensor_scan(
                                out=wre[:, j, :], data0=rho_b, data1=vre[:, j, :], initial=i_re, op0=ALU.mult, op1=ALU.add),
                                r=["vre", "prm", ("x", prv), "zero8"], w=["wre"])
                            p.op("dve", lambda e, j=j, i_im=i_im, rho_b=rho_b: e.tensor_tensor_scan(
                                out=wim[:, j, :], data0=rho_b, data1=vim[:, j, :], initial=i_im, op0=ALU.mult, op1=ALU.add),
                                r=["vim", "prm", ("x", prv), "zero8"], w=["wim"])
                        p.tt(tA[:, :, :], wre[:, :, :], cosT[:, hs, :], ALU.mult, r=["wre", "tab"], w=["tA"], eng="pool")
                        p.tt(tB[:, :, :], wim[:, :, :], sinT[:, hs, :], ALU.mult, r=["wim", "tab"], w=["tB"], eng="pool")
                        p.tt(xre[cur][:, hs, :], tA[:, :, :], tB[:, :, :], ALU.subtract, r=["tA", "tB"], w=[("x", cur)], eng="pool")
                        p.tt(tA[:, :, :], wre[:, :, :], sinT[:, hs, :], ALU.mult, r=["wre", "tab", ("x", cur)], w=["tA"])
                        p.tt(tB[:, :, :], wim[:, :, :], cosT[:, hs, :], ALU.mult, r=["wim", "tab", ("x", cur)], w=["tB"])
                        p.tt(xim[cur][:, hs, :], tA[:, :, :], tB[:, :, :], ALU.add, r=["tA", "tB"], w=[("x", cur)])
                        if d == 0:
                            for j in range(4):
                                gb = hf * 4 + j
                                p.mm(yp[:, :], Cre[:, gb, :], xre[cur][:, gb, :], start=(j == 0), stop=False, r=["CT", ("x", cur)], w=["yp"])
                                p.mm(yp[:, :], Cim[:, gb, :], xim[cur][:, gb, :], start=False, stop=(j == 3), r=["CT", ("x", cur)], w=["yp"])
                            p.tt(ysum[:, hf, ps_], yp[:, :], ysum[:, hf, ps_], ALU.add, r=["yp", "ysum"], w=["ysum"])
                        else:
                            for j in range(4):
                                gb = hf * 4 + j
                                p.mm(ytm[:, :], xre[cur][:, gb, :], Cre[:, gb, :], start=(j == 0), stop=False, r=["CT", ("x", cur)], w=["ytm"])
                                p.mm(ytm[:, :], xim[cur][:, gb, :], Cim[:, gb, :], start=False, stop=(j == 3), r=["CT", ("x", cur)], w=["ytm"])
                            p.cp(ysb[:, :], ytm[:, :], r=["ytm"], w=["ysb"], eng="act")
                            p.mm(yp[:, :], ysb[:, :], g.cst[:, C_J:C_J + 128], r=["ysb", "cst"], w=["yp"])
                            os_ = slice(rt(c) * 128, (rt(c) + 1) * 128)
                            p.tt(ysum[:, hf, os_], yp[:, :], ysum[:, hf, os_], ALU.add, r=["yp", "ysum"], w=["ysum"])
                p.barrier()
        with ExitStack() as es1:
            wg = p.sb(es1, "s5_wg", [128, 2, 512], BF16)
            vT = p.sb(es1, "s5_vT", [128, 2, 512], BF16)
            t1 = p.sb(es1, "s5_t1", [128, 512], F32)
            t2 = p.sb(es1, "s5_t2", [128, 512], F32)
            sg = p.sb(es1, "s5_sg", [128, 512], F32)
            yb = [p.sb(es1, "s5_yb%d" % i, [128, 2, 512], BF16) for i in range(2)]
            pv = [p.ps(es1, "s5_pv%d" % i, [128, 512], F32) for i in range(2)]
            pg = [p.ps(es1, "s5_pg%d" % i, [128, 512], F32) for i in range(2)]
            for kc in range(2):
                p.dma("pool", wg[:, kc, :], I["s5_glu_w"][l, kc * 128:(kc + 1) * 128, :], w=["wg"])
            k = 0
            for ti, (s0, n) in enumerate(FT if S5_LEVEL >= 5 else []):
                ob = ti % 2
                for c in range(2):
                    x = ysum[:, c, s0:s0 + n]
                    p.tt(t1[:, 0:n], x, x, ALU.mult, r=["ysum"], w=["t1"])
                    p.ts(t1[:, 0:n], t1[:, 0:n], 0.044715, 1.0, op0=ALU.mult, op1=ALU.add, r=["t1"], w=["t1"])
                    p.tt(t1[:, 0:n], t1[:, 0:n], x, ALU.mult, r=["t1", "ysum"], w=["t1"])
                    p.act(t2[:, 0:n], t1[:, 0:n], AF.Tanh, scale=0.7978845608028654, r=["t1"], w=["t2"])
                    p.ts(t2[:, 0:n], t2[:, 0:n], 0.5, 0.5, op0=ALU.mult, op1=ALU.add, r=["t2"], w=["t2"], eng="pool")
                    p.tt(vT[:, c, 0:n], t2[:, 0:n], x, ALU.mult, r=["t2", "ysum"], w=[("vT", c)])
                for c in range(2):
                    q = k % 2
                    k += 1
                    for kc in range(2):
                        p.mm(pv[q][:, 0:n], wg[:, kc, c * 128:(c + 1) * 128], vT[:, kc, 0:n], start=(kc == 0), stop=(kc == 1),
                             r=["wg", ("vT", kc)], w=[("pv", q)])
                    for kc in range(2):
                        p.mm(pg[q][:, 0:n], wg[:, kc, 256 + c * 128:256 + (c + 1) * 128], vT[:, kc, 0:n], start=(kc == 0), stop=(kc == 1),
                             r=["wg", ("vT", kc)], w=[("pg", q)])
                    p.act(sg[:, 0:n], pg[q][:, 0:n], AF.Sigmoid, r=[("pg", q)], w=["sg"])
                    p.tt(yb[ob][:, c, 0:n], pv[q][:, 0:n], sg[:, 0:n], ALU.mult, r=[("pv", q), "sg"], w=[("yb", ob)])
                p.dma("sp", g.yT[2:4, :, s0:s0 + n].rearrange("c p s -> p c s"), yb[ob][:, :, 0:n], r=[("yb", ob)], w=["yT_d"])
            p.barrier()


def phase_merge(g, l):
    p, I = g.p, g.I
    wv = I["w_in"][l].rearrange("(kc q) n -> q kc n", q=128)
    with ExitStack() as es:
        wg = p.sb(es, "mg_wg", [128, KC, 4096], BF16)
        wb = p.sb(es, "mg_wb", [128, 8, D], BF16)
        wo = p.sb(es, "mg_wo", [128, KC, D], BF16)
        ht = [p.sb(es, "mg_h%d" % i, [128, KC, 512], BF16) for i in range(2)]
        yt = [p.sb(es, "mg_y%d" % i, [128, 8, 512], BF16) for i in range(2)]
        xt = [p.sb(es, "mg_x%d" % i, [128, KC, 512], F32) for i in range(2)]
        sg = [p.sb(es, "mg_s%d" % i, [128, 512], F32) for i in range(2)]
        acc = p.sb(es, "mg_acc", [128, 512], F32)
        tmp = p.sb(es, "mg_tmp", [128, 512], F32)
        accT = p.sb(es, "mg_aT", [128, KC, 512], BF16)
        psg = [p.ps(es, "mg_pg%d" % i, [128, 512], F32) for i in range(2)]
        psb = [p.ps(es, "mg_pb%d" % i, [128, 512], F32) for i in range(2)]
        pso = [p.ps(es, "mg_po%d" % i, [128, 512], F32) for i in range(2)]
        for kc in range(KC):
            p.dma("pool", wg[:, kc, :], wv[:, kc, 2312:6408], w=[("wg", kc)])
            p.dma("pool", wo[:, kc, :], I["w_out"][l, kc * 128:(kc + 1) * 128, :], w=[("wo", kc)])
            p.dma("pool", wb[:, kc, :], I["w_branch"][l, kc // 2, (kc % 2) * 128:(kc % 2 + 1) * 128, :], w=[("wb", kc)])
        cg = 0
        co = 0
        for ti, (s0, n) in enumerate(FT):
            b = ti % 2
            ic = 1 if s0 == 0 else 0
            p.dma("sp", ht[b][:, :, 0:n], g.hT[:, :, s0:s0 + n].rearrange("c p s -> p c s"), r=["hT_d"], w=[("h", b)])
            p.dma("sp", yt[b][:, :, 0:n], g.yT[:, :, s0:s0 + n].rearrange("c p s -> p c s"), r=["yT_d"], w=[("y", b)])
            p.dma("sp", xt[b][:, :, 0:n], g.xres[:, :, s0:s0 + n].rearrange("c p s -> p c s"), r=["xres"], w=[("x", b)])
            for oc in range(KC):
                for br in range(4):
                    q = cg % 2
                    cg += 1
                    for kc in range(KC):
                        c0 = br * D + oc * 128
                        p.mm(psg[q][:, 0:n], wg[:, kc, c0:c0 + 128], ht[b][:, kc, 0:n], start=(kc == 0), stop=(kc == KC - 1),
                             r=[("wg", kc), ("h", b)], w=[("pg", q)])
                    for k2 in range(2):
                        p.mm(psb[q][:, 0:n], wb[:, br * 2 + k2, oc * 128:(oc + 1) * 128], yt[b][:, br * 2 + k2, 0:n], start=(k2 == 0),
                             stop=(k2 == 1), r=[("wb", br * 2 + k2), ("y", b)], w=[("pb", q)])
                    p.act(sg[q][:, 0:n], psg[q][:, 0:n], AF.Sigmoid, r=[("pg", q)], w=[("sg", q)])
                    if br == 0:
                        p.tt(acc[:, 0:n], psb[q][:, 0:n], sg[q][:, 0:n], ALU.mult, r=[("pb", q), ("sg", q)], w=["acc"])
                    else:
                        p.tt(tmp[:, 0:n], psb[q][:, 0:n], sg[q][:, 0:n], ALU.mult, r=[("pb", q), ("sg", q)], w=["tmp"])
                        p.tt(acc[:, 0:n], acc[:, 0:n], tmp[:, 0:n], ALU.add, r=["tmp", "acc"], w=["acc"], eng="pool")
                p.cp(accT[:, oc, 0:n], acc[:, 0:n], r=["acc"], w=[("aT", oc)], eng="act")
            for oc in range(KC):
                q = co % 2
                co += 1
                for kc in range(KC):
                    p.mm(pso[q][:, 0:n], wo[:, kc, oc * 128:(oc + 1) * 128], accT[:, kc, 0:n], start=(kc == 0), stop=(kc == KC - 1),
                         r=[("wo", kc), ("aT", kc)], w=[("po", q)])
                p.stt(xt[b][:, oc, 0:n], pso[q][:, 0:n], g.mod[:, 16 + oc, ic:ic + 1], xt[b][:, oc, 0:n], op0=ALU.mult, op1=ALU.add,
                      r=[("po", q), "mod", ("x", b)], w=[("x", b)])
            p.dma("sp", g.xres[:, :, s0:s0 + n].rearrange("c p s -> p c s"), xt[b][:, :, 0:n], r=[("x", b)], w=["xres"])
        p.barrier()


def phase_ffn(g, l):
    p, I = g.p, g.I
    moe = (l % 2 == 1)
    m = l // 2
    if moe:
        H, GS = EXPERT_DIM, 4
        experts = [(I["moe_w_in"][m, e], I["moe_w_out"][m, e]) for e in range(8)]
    else:
        H, GS = FFN_DIM, 2
        experts = [(I["ffn_w_in"][m], I["ffn_w_out"][m])]
    HC = H // 128
    NG = HC // GS
    with ExitStack() as es:
        wT = p.sb(es, "ff_wT", [8, S], F32)
        if moe:
            with ExitStack() as es0:
                rw = p.sb(es0, "ff_rw", [128, KC, 8], F32)
                lsb = p.sb(es0, "ff_lsb", [128, 8], F32)
                m8 = p.sb(es0, "ff_m8", [128, 8], F32)
                gt = p.sb(es0, "ff_gt", [128, 4], F32)
                e1 = p.sb(es0, "ff_e1", [128, 8], F32)
                e2 = p.sb(es0, "ff_e2", [128, 8], F32)
                lg = p.ps(es0, "ff_lg", [128, 8], F32)
                wtp = p.ps(es0, "ff_wtp", [8, 128], F32)
                p.dma("sp", rw[:, :, :], I["moe_router"][m].rearrange("(kc q) e -> q kc e", q=128), w=["rw"])

                def sink(ti, s0, n, ht, key, h32):
                    p.dma("sp", g.hT[:, :, s0:s0 + n].rearrange("c p s -> p c s"), ht[:, :, 0:n], r=[key], w=["hT_d"])
                    for sub in range(n // 128):
                        ss = slice(sub * 128, (sub + 1) * 128)
                        for kc in range(KC):
                            p.mm(lg[:, :], h32[:, kc, ss], rw[:, kc, :], start=(kc == 0), stop=(kc == KC - 1),
                                 r=[("nmsq", kc), "rw"], w=["lg"])
                        p.cp(lsb[:, :], lg[:, :], r=["lg"], w=["lsb"])
                        p.op("dve", lambda e: e.max(out=m8[:, :], in_=lsb[:, :]), r=["lsb"], w=["m8"])
                        p.tt(gt[:, 0:1], m8[:, 0:1], m8[:, 1:2], ALU.subtract, r=["m8"], w=["gt"])
                        p.act(gt[:, 1:2], gt[:, 0:1], AF.Sigmoid, r=["gt"], w=["gt1"])
                        p.act(gt[:, 2:3], gt[:, 0:1], AF.Sigmoid, scale=-1.0, r=["gt"], w=["gt2"])
                        p.ts(e1[:, :], lsb[:, :], m8[:, 0:1], gt[:, 1:2], op0=ALU.is_equal, op1=ALU.mult, r=["lsb", "m8", "gt1"], w=["e1"])
                        p.ts(e2[:, :], lsb[:, :], m8[:, 1:2], gt[:, 2:3], op0=ALU.is_equal, op1=ALU.mult, r=["lsb", "m8", "gt2"], w=["e2"])
                        p.tt(e1[:, :], e1[:, :], e2[:, :], ALU.add, r=["e1", "e2"], w=["e1"])
                        p.tr(wtp[:, :], e1[:, :], g.cst[:, C_ID:C_ID + 128], r=["e1", "cst"], w=["wtp"])
                        p.cp(wT[0:8, s0 + sub * 128:s0 + (sub + 1) * 128], wtp[:, :], r=["wtp"], w=["wT"])

                norm_tiles(g, 1, sink, want32=True)
        else:
            def sink(ti, s0, n, ht, key, h32):
                p.dma("sp", g.hT[:, :, s0:s0 + n].rearrange("c p s -> p c s"), ht[:, :, 0:n], r=[key], w=["hT_d"])

            norm_tiles(g, 1, sink)
        ht = [p.sb(es, "ff_h%d" % i, [128, KC, 512], BF16) for i in range(2)]
        wg = [p.sb(es, "ff_wg%d" % i, [128, KC, GS * 128], BF16) for i in range(2)]
        wu = [p.sb(es, "ff_wu%d" % i, [128, KC, GS * 128], BF16) for i in range(2)]
        wo = p.sb(es, "ff_wo", [128, HC, D], BF16)
        actT = p.sb(es, "ff_act", [128, HC, 512], BF16)
        oacc = p.sb(es, "ff_oacc", [128, KC, 512], F32)
        xt = p.sb(es, "ff_x", [128, KC, 512], F32)
        sgt = [p.sb(es, "ff_sg%d" % i, [128, 512], F32) for i in range(2)]
        tmp = [p.sb(es, "ff_tmp%d" % i, [128, 512], F32) for i in range(2)]
        wbc = p.sb(es, "ff_wbc", [128, 512], F32)
        psg = [p.ps(es, "ff_pg%d" % i, [128, 512], F32) for i in range(2)]
        psu = [p.ps(es, "ff_pu%d" % i, [128, 512], F32) for i in range(2)]
        pso = [p.ps(es, "ff_po%d" % i, [128, 512], F32) for i in range(2)]
        psw = p.ps(es, "ff_pw", [128, 512], F32)
        gcnt = 0
        jc = 0
        oc_ = 0
        for ti, (s0, n) in enumerate(FT):
            hb = ti % 2
            ic = 1 if s0 == 0 else 0
            p.dma("sp", ht[hb][:, :, 0:n], g.hT[:, :, s0:s0 + n].rearrange("c p s -> p c s"), r=["hT_d"], w=[("h", hb)])
            p.dma("sp", xt[:, :, 0:n], g.xres[:, :, s0:s0 + n].rearrange("c p s -> p c s"), r=["xres"], w=["x"])
            for ei, (w_in, w_out) in enumerate(experts):
                wiv = w_in.rearrange("(kc q) n -> q kc n", q=128)
                wov = w_out.rearrange("(hc q) n -> q hc n", q=128)
                if moe:
                    p.mm(psw[:, 0:n], g.cst[0:8, C_SEL + ei * 128:C_SEL + (ei + 1) * 128], wT[0:8, s0:s0 + n], r=["wT", "cst"], w=["psw"])
                    p.cp(wbc[:, 0:n], psw[:, 0:n], r=["psw"], w=["wbc"], eng="act")
                for h0 in range(0, HC, 4):
                    hn = min(4, HC - h0)
                    p.dma("pool", wo[:, h0:h0 + hn, :], wov[:, h0:h0 + hn, :], w=[("wo", h0)])
                for gi in range(NG):
                    gb = gcnt % 2
                    gcnt += 1
                    c0 = gi * GS * 128
                    p.dma("pool", wg[gb][:, :, :], wiv[:, :, c0:c0 + GS * 128], w=[("wg", gb)])
                    p.dma("pool", wu[gb][:, :, :], wiv[:, :, H + c0:H + c0 + GS * 128], w=[("wu", gb)])
                    for j in range(GS):
                        q = jc % 2
                        jc += 1
                        hc = gi * GS + j
                        for kc in range(KC):
                            p.mm(psg[q][:, 0:n], wg[gb][:, kc, j * 128:(j + 1) * 128], ht[hb][:, kc, 0:n], start=(kc == 0),
                                 stop=(kc == KC - 1), r=[("wg", gb), ("h", hb)], w=[("pg", q)])
                        for kc in range(KC):
                            p.mm(psu[q][:, 0:n], wu[gb][:, kc, j * 128:(j + 1) * 128], ht[hb][:, kc, 0:n], start=(kc == 0),
                                 stop=(kc == KC - 1), r=[("wu", gb), ("h", hb)], w=[("pu", q)])
                        p.act(sgt[q][:, 0:n], psg[q][:, 0:n], AF.Silu, r=[("pg", q)], w=[("sg", q)])
                        if moe:
                            p.tt(tmp[q][:, 0:n], psu[q][:, 0:n], sgt[q][:, 0:n], ALU.mult, r=[("pu", q), ("sg", q)], w=[("tmp", q)])
                            p.tt(actT[:, hc, 0:n], tmp[q][:, 0:n], wbc[:, 0:n], ALU.mult, r=[("tmp", q), "wbc"], w=[("act", hc)], eng="pool")
                        else:
                            p.tt(actT[:, hc, 0:n], psu[q][:, 0:n], sgt[q][:, 0:n], ALU.mult, r=[("pu", q), ("sg", q)], w=[("act", hc)])
                for oc in range(KC):
                    q = oc_ % 2
                    oc_ += 1
                    for hc in range(HC):
                        p.mm(pso[q][:, 0:n], wo[:, hc, oc * 128:(oc + 1) * 128], actT[:, hc, 0:n], start=(hc == 0), stop=(hc == HC - 1),
                             r=[("wo", (hc // 4) * 4), ("act", hc)], w=[("po", q)])
                    if ei == 0:
                        p.cp(oacc[:, oc, 0:n], pso[q][:, 0:n], r=[("po", q)], w=[("oacc", oc)])
                    else:
                        p.tt(oacc[:, oc, 0:n], pso[q][:, 0:n], oacc[:, oc, 0:n], ALU.add, r=[("po", q), ("oacc", oc)], w=[("oacc", oc)])
            for oc in range(KC):
                p.stt(xt[:, oc, 0:n], oacc[:, oc, 0:n], g.mod[:, 40 + oc, ic:ic + 1], xt[:, oc, 0:n], op0=ALU.mult, op1=ALU.add,
                      r=[("oacc", oc), "mod", "x"], w=["x"])
            p.dma("sp", g.xres[:, :, s0:s0 + n].rearrange("c p s -> p c s"), xt[:, :, 0:n], r=["x"], w=["xres"])
        p.barrier()


def layer(g, l, stop):
    phases = [("ada", adaln), ("n1", phase_norm1), ("ptm", phase_ptm), ("xbc", phase_xbc), ("ssd", phase_ssd), ("s5", phase_s5), ("attn", phase_attn), ("merge", phase_merge), ("ffn", phase_ffn)]
    for name, fn in phases:
        fn(g, l)
        if stop is not None and stop == (l, name):
            return


def final(g, out, real):
    p, I = g.p, g.I
    if not real:
        with ExitStack() as es:
            z = p.sb(es, "fz", [128, D], F32)
            p.memset(z[:, :], 0.0, w=["fz"])
            p.dma("sp", out[0:128, :], z[:, :], r=["fz"], w=["out"])
            p.barrier()
        return
    with ExitStack() as es:
        gf = p.sb(es, "fn_g", [128, 8], F32)
        load_rows_T(g, None, gf[:, :], I["final_norm_g"].rearrange("(j q) -> j q", q=128), 8, "gf")
        xt = [p.sb(es, "fn_x%d" % i, [128, KC, 512], F32) for i in range(2)]
        sq = p.sb(es, "fn_sq", [128, KC, 512], F32)
        rs = p.sb(es, "fn_rs", [128, 512], F32)
        ot = [p.sb(es, "fn_o%d" % i, [128, KC, 128], F32) for i in range(2)]
        ssq = p.ps(es, "fn_ps", [128, 512], F32)
        pst = [p.ps(es, "fn_pt%d" % i, [128, KC, 128], F32) for i in range(2)]
        cnt = 0
        for ti, (s0, n) in enumerate(FT[1:]):
            b = ti % 2
            p.dma("sp", xt[b][:, :, 0:n], g.xres[:, :, s0:s0 + n].rearrange("c p s -> p c s"), r=["xres"], w=[("fx", b)])
            for c in range(KC):
                p.act(sq[:, c, 0:n], xt[b][:, c, 0:n], AF.Square, r=[("fx", b)], w=[("fsq", c)])
                p.mm(ssq[:, 0:n], g.cst[:, C_ONE:C_ONE + 128], sq[:, c, 0:n], start=(c == 0), stop=(c == KC - 1),
                     r=[("fsq", c), "cst"], w=["fps"])
            p.act(rs[:, 0:n], ssq[:, 0:n], AF.Ln, bias=EPS, scale=1.0 / D, r=["fps"], w=["frs"])
            p.act(rs[:, 0:n], rs[:, 0:n], AF.Exp, scale=-0.5, r=["frs"], w=["frs"])
            for c in range(KC):
                p.stt(sq[:, c, 0:n], xt[b][:, c, 0:n], gf[:, c:c + 1], rs[:, 0:n], op0=ALU.mult, op1=ALU.mult,
                      r=[("fx", b), "frs", "gf"], w=[("fsq", c)])
            for sub in range(n // 128):
                ob = cnt % 2
                cnt += 1
                for c in range(KC):
                    p.tr(pst[ob][:, c, :], sq[:, c, sub * 128:(sub + 1) * 128], g.cst[:, C_ID:C_ID + 128],
                         r=[("fsq", c), "cst"], w=[("fpt", ob)])
                p.cp(ot[ob][:, :, :], pst[ob][:, :, :], r=[("fpt", ob)], w=[("fo", ob)], eng="act" if ob else "dve")
                t0 = s0 - NCTX + sub * 128
                p.dma("sp", out[t0:t0 + 128, :], ot[ob][:, :, :].rearrange("p c f -> p (c f)"), r=[("fo", ob)], w=["out"])
        p.barrier()


_NC_CACHE = {}


def kernel(**inputs):
    x = np.asarray(inputs["x"], np.float32)
    nb = x.shape[0]
    if "nc" not in _NC_CACHE:
        _NC_CACHE["nc"] = build()[0]
    nc = _NC_CACHE["nc"]
    consts = make_consts()
    rope = make_rope()
    shared = {k: np.ascontiguousarray(np.asarray(v, np.float32)) for k, v in inputs.items() if k not in ("x", "c", "ctx", "c_ctx")}
    c = np.asarray(inputs["c"], np.float32)
    cctx = np.asarray(inputs["c_ctx"], np.float32)
    in_maps = []
    for b in range(nb):
        m = dict(shared)
        m["x"] = np.ascontiguousarray(x[b])
        m["ctx"] = np.ascontiguousarray(np.asarray(inputs["ctx"], np.float32)[b])
        m["cc"] = np.ascontiguousarray(np.concatenate([c[b].reshape(8, 128), cctx.reshape(8, 128)], 0))
        m["consts"] = consts
        m["rope"] = rope
        in_maps.append(m)
    res = run_bass_kernel_spmd(nc, in_maps, core_ids=list(range(nb)))
    return np.stack([r["out"] for r in res.results], axis=0).astype(np.float32)
```

```python
import numpy as np
from contextlib import ExitStack
import concourse.bass as bass
import concourse.mybir as mybir
from concourse.bass_utils import run_bass_kernel_spmd

F32 = mybir.dt.float32
BF16 = mybir.dt.bfloat16
I32 = mybir.dt.int32
ALU = mybir.AluOpType
AF = mybir.ActivationFunctionType
AX = mybir.AxisListType

D = 1024
KC = 8
NCTX = 256
NLAT = 4096
S = NCTX + NLAT
NT = S // 128
DEPTH = 4
FT = [(0, 256)] + [(256 + 512 * i, 512) for i in range(8)]
EPS = 1e-6
PZ, PDT, PU, PCQ, PCK, PCV, PDQ, PDK, PDV = 0, 256, 264, 520, 776, 904, 1032, 1288, 1416
NPTM = 1544
FFN_DIM = 2816
EXPERT_DIM = 3584
NEG = -30000.0
import os
SAME_ENG_SYNC = bool(int(os.environ.get('SAME_ENG_SYNC', '1')))
XBC_LEVEL = int(os.environ.get('XBC_LEVEL', '2'))
ATT_LEVEL = int(os.environ.get('ATT_LEVEL', '2'))
SSD_LEVEL = int(os.environ.get('SSD_LEVEL', '9'))
S5_LEVEL = int(os.environ.get('S5_LEVEL', '9'))


class Prog:
    def __init__(self, nc, es):
        self.nc = nc
        self.es = es
        self.E = {"pe": nc.tensor, "act": nc.scalar, "dve": nc.vector, "pool": nc.gpsimd, "sp": nc.sync}
        self.csem = {}
        self.ccnt = {}
        for e in ("pe", "act", "dve", "pool"):
            self.csem[e] = es.enter_context(nc.semaphore("c_" + e))
            self.ccnt[e] = 0
        self.dsem = {}
        self.dcnt = {}
        self.dnext = {}
        for q in ("sp", "pool", "act"):
            self.dsem[q] = [es.enter_context(nc.semaphore("d_%s%d" % (q, i))) for i in range(8)]
            self.dcnt[q] = [0] * 8
            self.dnext[q] = 0
        self.sems = {}
        for e in self.csem:
            self.sems[("c", e)] = self.csem[e]
        for q in self.dsem:
            for i, s in enumerate(self.dsem[q]):
                self.sems[("d", q, i)] = s
        self.waited = {e: {} for e in self.E}
        self.state = {}
        self.nins = 0

    def _deps(self, r, w):
        deps = {}

        def add(tok):
            if tok is None:
                return
            k, v = tok
            if deps.get(k, 0) < v:
                deps[k] = v

        for key in r:
            st = self.state.get(key)
            if st:
                add(st[0])
        for key in w:
            st = self.state.get(key)
            if st:
                add(st[0])
                for t in st[1]:
                    add(t)
        return deps

    def _wait(self, eng, deps):
        wd = self.waited[eng]
        for k, v in deps.items():
            if wd.get(k, 0) >= v:
                continue
            if k == ("c", eng) and (eng == "pe" or not SAME_ENG_SYNC):
                continue
            self.E[eng].wait_ge(self.sems[k], v)
            wd[k] = v
            self.nins += 1

    def _commit(self, tok, r, w):
        for key in r:
            st = self.state.setdefault(key, [None, []])
            st[1].append(tok)
            if len(st[1]) > 24:
                mx = {}
                for k, v in st[1]:
                    if mx.get(k, 0) < v:
                        mx[k] = v
                st[1] = list(mx.items())
        for key in w:
            self.state[key] = [tok, []]

    def op(self, eng, fn, r=(), w=()):
        self._wait(eng, self._deps(r, w))
        ins = fn(self.E[eng])
        self.ccnt[eng] += 1
        ins.then_inc(self.csem[eng], 1)
        tok = (("c", eng), self.ccnt[eng])
        self._commit(tok, r, w)
        self.nins += 1
        return tok

    def dma(self, q, out, in_, r=(), w=()):
        i = self.dnext[q]
        self.dnext[q] = (i + 1) % 8
        deps = self._deps(r, w)
        k = ("d", q, i)
        if self.dcnt[q][i] > 0:
            deps[k] = max(deps.get(k, 0), self.dcnt[q][i])
        self._wait(q, deps)
        self.dcnt[q][i] += 16
        self.E[q].dma_start(out=out, in_=in_).then_inc(self.dsem[q][i], 16)
        tok = (k, self.dcnt[q][i])
        self._commit(tok, r, w)
        self.nins += 1
        return tok

    def barrier(self):
        deps = {}
        for e in self.csem:
            if self.ccnt[e]:
                deps[("c", e)] = self.ccnt[e]
        for q in self.dsem:
            for i in range(8):
                if self.dcnt[q][i]:
                    deps[("d", q, i)] = self.dcnt[q][i]
        for e in self.E:
            d = {k: v for k, v in deps.items() if k != ("c", e)}
            self._wait(e, d)
        self.state = {}

    def mm(self, out, lhsT, rhs, start=True, stop=True, r=(), w=()):
        return self.op("pe", lambda e: e.matmul(out, lhsT, rhs, start=start, stop=stop), r, w)

    def tr(self, out, in_, ident, r=(), w=()):
        return self.op("pe", lambda e: e.transpose(out, in_, ident), r, w)

    def act(self, out, in_, func, bias=0.0, scale=1.0, r=(), w=(), eng="act"):
        return self.op(eng, lambda e: e.activation(out=out, in_=in_, func=func, bias=bias, scale=scale), r, w)

    def tt(self, out, in0, in1, op, r=(), w=(), eng="dve"):
        return self.op(eng, lambda e: e.tensor_tensor(out=out, in0=in0, in1=in1, op=op), r, w)

    def ts(self, out, in0, s1, s2=None, op0=ALU.mult, op1=None, r=(), w=(), eng="dve"):
        if op1 is None:
            return self.op(eng, lambda e: e.tensor_scalar(out=out, in0=in0, scalar1=s1, scalar2=None, op0=op0), r, w)
        return self.op(eng, lambda e: e.tensor_scalar(out=out, in0=in0, scalar1=s1, scalar2=s2, op0=op0, op1=op1), r, w)

    def stt(self, out, in0, scalar, in1, op0=ALU.mult, op1=ALU.add, r=(), w=(), eng="dve"):
        return self.op(eng, lambda e: e.scalar_tensor_tensor(out=out, in0=in0, scalar=scalar, in1=in1, op0=op0, op1=op1), r, w)

    def cp(self, out, in_, r=(), w=(), eng="dve"):
        if eng == "act":
            return self.op("act", lambda e: e.copy(out=out, in_=in_), r, w)
        return self.op(eng, lambda e: e.tensor_copy(out=out, in_=in_), r, w)

    def memset(self, ap, val, w=(), eng="dve"):
        return self.op(eng, lambda e: e.memset(ap, val), (), w)

    def sb(self, es, name, shape, dt):
        self.uid = getattr(self, "uid", 0) + 1
        return es.enter_context(self.nc.sbuf_tensor("s%d_%s" % (self.uid, name), list(shape), dt))

    def ps(self, es, name, shape, dt=F32):
        self.uid = getattr(self, "uid", 0) + 1
        return es.enter_context(self.nc.psum_tensor("p%d_%s" % (self.uid, name), list(shape), dt))


def make_consts():
    i = np.arange(128)
    ident = np.eye(128, dtype=np.float32)
    J = ident[::-1].copy()
    U = (i[:, None] <= i[None, :]).astype(np.float32)
    L = (i[:, None] >= i[None, :]).astype(np.float32)
    ones = np.ones((128, 128), np.float32)
    nmf = np.where(i[:, None] <= i[None, :], 0.0, NEG).astype(np.float32)
    nmb = np.where(i[:, None] >= i[None, :], 0.0, NEG).astype(np.float32)
    ramp = np.tile(np.arange(1, 129, dtype=np.float32)[None, :], (128, 1))
    sel = np.zeros((128, 8 * 128), np.float32)
    for e in range(8):
        sel[e, e * 128:(e + 1) * 128] = 1.0
    return np.concatenate([ident, J, U, L, ones, nmf, nmb, ramp, sel], axis=1)


C_ID, C_J, C_U, C_L, C_ONE, C_NMF, C_NMB, C_RAMP, C_SEL = [k * 128 for k in range(9)]
NCONST = 16 * 128


def make_rope():
    pos = np.arange(NLAT)
    pr = (pos // 64).astype(np.float32)
    pc = (pos % 64).astype(np.float32)
    inv = (np.float32(10000.0) ** (-np.arange(16, dtype=np.float32) / np.float32(16))).astype(np.float32)
    ang = np.concatenate([pr[:, None] * inv, pc[:, None] * inv], axis=-1).astype(np.float32)
    t = np.zeros((S, 64), np.float32)
    t[:NCTX, :32] = 1.0
    t[NCTX:, :32] = np.cos(ang)
    t[NCTX:, 32:] = np.sin(ang)
    return t


class Ctx:
    pass


def build(dbg=None, layers=DEPTH, stop=None, skip=()):
    dbg = dbg or {}
    nc = bass.Bass("TRN2", target_bir_lowering=False)
    g = Ctx()
    g.nc = nc

    def din(name, shape, dt=F32):
        return nc.dram_tensor(name, list(shape), dt, kind="ExternalInput").ap()

    def dscr(name, shape, dt=F32):
        kind = "ExternalOutput" if name in dbg else "Internal"
        return nc.dram_tensor(name, list(shape), dt, kind=kind).ap()

    I = {}
    I["x"] = din("x", [NLAT, D])
    I["ctx"] = din("ctx", [NCTX, D])
    I["cc"] = din("cc", [16, 128])
    I["consts"] = din("consts", [128, NCONST])
    I["rope"] = din("rope", [S, 64])
    shapes = dict(
        norm1_g=[DEPTH, D], norm2_g=[DEPTH, D], ada_w=[DEPTH, D, 6 * D], ada_b=[DEPTH, 6 * D],
        w_in=[DEPTH, D, 6408], ssd_conv_w=[DEPTH, 5, 768], ssd_conv_b=[DEPTH, 768], ssd_a_log=[DEPTH, 2, 4],
        ssd_dt_bias=[DEPTH, 2, 4], ssd_d=[DEPTH, 4], ssd_norm_g=[DEPTH, 256], s5_lam_re=[DEPTH, 2, 16, 64],
        s5_lam_im=[DEPTH, 2, 16, 64], s5_log_step=[DEPTH, 2, 16], s5_b_re=[DEPTH, 2, 16, 64, 16],
        s5_b_im=[DEPTH, 2, 16, 64, 16], s5_c_re=[DEPTH, 2, 16, 16, 64], s5_c_im=[DEPTH, 2, 16, 16, 64],
        s5_d=[DEPTH, 256], s5_glu_w=[DEPTH, 256, 512], qk_norm_g=[DEPTH, 2, 64], swa_sink=[DEPTH, 4],
        w_branch=[DEPTH, 4, 256, D], w_out=[DEPTH, D, D], ffn_w_in=[2, D, 2 * FFN_DIM], ffn_w_out=[2, FFN_DIM, D],
        moe_router=[2, D, 8], moe_w_in=[2, 8, D, 2 * EXPERT_DIM], moe_w_out=[2, 8, EXPERT_DIM, D],
        final_norm_g=[D],
    )
    for k, shp in shapes.items():
        if k not in skip:
            I[k] = din(k, shp)
    out = nc.dram_tensor("out", [NLAT, D], F32, kind="ExternalOutput").ap()

    g.I = I
    g.xres = dscr("xres", [KC, 128, S])
    g.hT = dscr("hT", [KC, 128, S], BF16)
    g.ptm = dscr("ptm", [S, NPTM])
    g.xbcT = dscr("xbcT", [6, 128, S], BF16)
    g.xbctm = dscr("xbctm", [S, 512], BF16)
    g.yT = dscr("yT", [8, 128, S], BF16)
    g.wib = dscr("wib", [8, D, 2 * EXPERT_DIM], BF16)
    g.wob = dscr("wob", [8, EXPERT_DIM, D], BF16)

    with ExitStack() as es:
        p = Prog(nc, es)
        g.p = p
        blk = es.enter_context(nc.Block())
        g.cst = p.sb(es, "cst", [128, NCONST], F32)
        g.cstb = p.sb(es, "cstb", [128, NCONST], BF16)
        g.par = p.sb(es, "par", [128, 512], F32)
        g.mod = p.sb(es, "modv", [128, 48, 2], F32)
        g.AB = p.sb(es, "AB", [128, 4, 8, 2], F32)
        g.cact = p.sb(es, "cact", [128, 8, 2], F32)

        def body(_e):
            setup(g)
            for l in range(layers if stop != "setup" else 0):
                layer(g, l, stop)
                if stop is not None and stop[0] == l:
                    break
            final(g, out, stop is None)
            p.barrier()

        blk.gpsimd(body)
    return nc, p


def load_rows_T(g, es_ps, dst, src2d, n, key):
    p = g.p
    with ExitStack() as es:
        tmp = p.sb(es, "lrt_tmp", [128, 128], F32)
        pst = p.ps(es, "lrt_ps", [128, 128], F32)
        p.dma("sp", tmp[0:n, :], src2d, w=["lrt_tmp"])
        p.tr(pst[:, 0:n], tmp[0:n, :], g.cst[0:n, C_ID:C_ID + n], r=["lrt_tmp", "cst"], w=["lrt_ps"])
        p.cp(dst, pst[:, 0:n], r=["lrt_ps"], w=[key])
        p.barrier()


def setup(g):
    p, nc, I = g.p, g.nc, g.I
    p.dma("sp", g.cst[:, :], I["consts"][:, :], w=["cst"])
    p.cp(g.cstb[:, :], g.cst[:, :], r=["cst"], w=["cstb"])
    with ExitStack() as es:
        t = p.sb(es, "su_t", [128, 16], F32)
        load_rows_T(g, None, t[:, :], I["cc"][:, :], 16, "su_t")
        for i in range(2):
            p.act(g.cact[:, :, i], t[:, i * 8:(i + 1) * 8], AF.Silu, r=["su_t"], w=["cact"])
        p.barrier()
    with ExitStack() as es:
        xin = [p.sb(es, "su_x%d" % i, [128, D], F32) for i in range(2)]
        xo = [p.sb(es, "su_o%d" % i, [128, KC, 128], F32) for i in range(2)]
        pst = [p.ps(es, "su_ps%d" % i, [128, KC, 128], F32) for i in range(2)]
        for t in range(NT):
            b = t % 2
            src = I["ctx"][t * 128:(t + 1) * 128, :] if t < 2 else I["x"][(t - 2) * 128:(t - 1) * 128, :]
            p.dma("sp", xin[b][:, :], src, w=[("xin", b)])
            for c in range(KC):
                p.tr(pst[b][:, c, :], xin[b][:, c * 128:(c + 1) * 128], g.cst[:, C_ID:C_ID + 128],
                     r=[("xin", b), "cst"], w=[("xps", b)])
            p.cp(xo[b][:, :, :], pst[b][:, :, :], r=[("xps", b)], w=[("xo", b)], eng="act" if b else "dve")
            p.dma("sp", g.xres[:, :, t * 128:(t + 1) * 128].rearrange("c p s -> p c s"), xo[b][:, :, :],
                  r=[("xo", b)], w=["xres"])
        p.barrier()


def adaln(g, l):
    p, I = g.p, g.I
    with ExitStack() as es:
        wt = [p.sb(es, "ada_w%d" % i, [128, KC, 768], F32) for i in range(2)]
        adab = p.sb(es, "ada_b", [128, 48], F32)
        gn = p.sb(es, "ada_g", [128, 16], F32)
        mps = p.ps(es, "ada_ps", [128, 48, 2], F32)
        load_rows_T(g, None, adab[:, :], I["ada_b"][l].rearrange("(j q) -> j q", q=128), 48, "adab")
        load_rows_T(g, None, gn[:, 0:8], I["norm1_g"][l].rearrange("(j q) -> j q", q=128), 8, "gn")
        load_rows_T(g, None, gn[:, 8:16], I["norm2_g"][l].rearrange("(j q) -> j q", q=128), 8, "gn")
        wv = I["ada_w"][l].rearrange("(kc q) n -> q kc n", q=128)
        for ob in range(8):
            b = ob % 2
            for kc in range(KC):
                p.dma("sp" if kc % 2 == 0 else "act", wt[b][:, kc, :], wv[:, kc, ob * 768:(ob + 1) * 768], w=[("adaw", b, kc)])
            for jj in range(6):
                j = ob * 6 + jj
                for kc in range(KC):
                    p.mm(mps[:, j, :], wt[b][:, kc, jj * 128:(jj + 1) * 128], g.cact[:, kc, :],
                         start=(kc == 0), stop=(kc == KC - 1), r=[("adaw", b, kc), "cact"], w=["mps"])
        for i in range(2):
            p.tt(g.mod[:, :, i], mps[:, :, i], adab[:, :], ALU.add, r=["mps", "adab"], w=["mod"])
        for i in range(2):
            p.stt(g.AB[:, 0, :, i], g.mod[:, 8:16, i], 1.0, gn[:, 0:8], op0=ALU.add, op1=ALU.mult, r=["mod", "gn"], w=["AB"])
            p.cp(g.AB[:, 1, :, i], g.mod[:, 0:8, i], r=["mod"], w=["AB"])
            p.stt(g.AB[:, 2, :, i], g.mod[:, 32:40, i], 1.0, gn[:, 8:16], op0=ALU.add, op1=ALU.mult, r=["mod", "gn"], w=["AB"])
            p.cp(g.AB[:, 3, :, i], g.mod[:, 24:32, i], r=["mod"], w=["AB"])
        p.barrier()


def norm_tiles(g, which, sink, want32=False):
    p = g.p
    with ExitStack() as es:
        xt = [p.sb(es, "nm_x%d" % i, [128, KC, 512], F32) for i in range(2)]
        sq = p.sb(es, "nm_sq", [128, KC, 512], F32)
        rs = p.sb(es, "nm_rs", [128, 512], F32)
        ht = [p.sb(es, "nm_h%d" % i, [128, KC, 512], BF16) for i in range(2)]
        ssq = p.ps(es, "nm_ps", [128, 512], F32)
        for ti, (s0, n) in enumerate(FT):
            b = ti % 2
            ic = 1 if s0 == 0 else 0
            p.dma("sp", xt[b][:, :, 0:n], g.xres[:, :, s0:s0 + n].rearrange("c p s -> p c s"), r=["xres"], w=[("nmx", b)])
            for c in range(KC):
                p.act(sq[:, c, 0:n], xt[b][:, c, 0:n], AF.Square, r=[("nmx", b)], w=[("nmsq", c)])
                p.mm(ssq[:, 0:n], g.cst[:, C_ONE:C_ONE + 128], sq[:, c, 0:n], start=(c == 0), stop=(c == KC - 1),
                     r=[("nmsq", c), "cst"], w=["nmps"])
            p.act(rs[:, 0:n], ssq[:, 0:n], AF.Ln, bias=EPS, scale=1.0 / D, r=["nmps"], w=["nmrs"])
            p.act(rs[:, 0:n], rs[:, 0:n], AF.Exp, scale=-0.5, r=["nmrs"], w=["nmrs"])
            for c in range(KC):
                p.tt(sq[:, c, 0:n], xt[b][:, c, 0:n], rs[:, 0:n], ALU.mult, r=[("nmx", b), "nmrs"], w=[("nmsq", c)],
                     eng="dve" if c % 2 == 0 else "pool")
                if want32:
                    p.ts(sq[:, c, 0:n], sq[:, c, 0:n], g.AB[:, 2 * which, c, ic:ic + 1], g.AB[:, 2 * which + 1, c, ic:ic + 1],
                         op0=ALU.mult, op1=ALU.add, r=[("nmsq", c), "AB"], w=[("nmsq", c)], eng="dve" if c % 2 == 0 else "pool")
                    p.cp(ht[b][:, c, 0:n], sq[:, c, 0:n], r=[("nmsq", c)], w=[("nmh", b)], eng="dve" if c % 2 == 0 else "pool")
                else:
                    p.ts(ht[b][:, c, 0:n], sq[:, c, 0:n], g.AB[:, 2 * which, c, ic:ic + 1], g.AB[:, 2 * which + 1, c, ic:ic + 1],
                         op0=ALU.mult, op1=ALU.add, r=[("nmsq", c), "AB"], w=[("nmh", b)], eng="dve" if c % 2 == 0 else "pool")
            if want32:
                sink(ti, s0, n, ht[b], ("nmh", b), sq)
            else:
                sink(ti, s0, n, ht[b], ("nmh", b), None)
        p.barrier()


def phase_norm1(g, l):
    p = g.p

    def sink(ti, s0, n, ht, key, h32):
        p.dma("sp", g.hT[:, :, s0:s0 + n].rearrange("c p s -> p c s"), ht[:, :, 0:n], r=[key], w=["hT_d"])

    norm_tiles(g, 0, sink)


def phase_ptm(g, l):
    p, I = g.p, g.I
    wv = I["w_in"][l].rearrange("(kc q) n -> q kc n", q=128)
    with ExitStack() as es:
        w = p.sb(es, "ptm_w", [128, KC, NPTM], BF16)
        ht = [p.sb(es, "ptm_h%d" % i, [128, KC, 512], BF16) for i in range(2)]
        ob = [p.sb(es, "ptm_o%d" % i, [128, NPTM], F32) for i in range(2)]
        ps = [p.ps(es, "ptm_ps%d" % i, [128, 4, 512], F32) for i in range(2)]
        for kc in range(KC):
            p.dma("pool", w[:, kc, 0:256], wv[:, kc, 0:256], w=[("w", kc)])
            p.dma("pool", w[:, kc, 256:NPTM], wv[:, kc, 1024:2312], w=[("w", kc)])
        cols = [(0, 512), (512, 512), (1024, 512), (1536, 8)]
        cnt = 0
        for ti, (s0, n) in enumerate(FT):
            hb = ti % 2
            p.dma("sp", ht[hb][:, :, 0:n], g.hT[:, :, s0:s0 + n].rearrange("c p s -> p c s"), r=["hT_d"], w=[("h", hb)])
            for sub in range(n // 128):
                b = cnt % 2
                cnt += 1
                for kc in range(KC):
                    for bi, (c0, cn) in enumerate(cols):
                        p.mm(ps[b][:, bi, 0:cn], ht[hb][:, kc, sub * 128:(sub + 1) * 128], w[:, kc, c0:c0 + cn],
                             start=(kc == 0), stop=(kc == KC - 1), r=[("h", hb), ("w", kc)], w=[("ps", b, bi)])
                for bi, (c0, cn) in enumerate(cols):
                    p.cp(ob[b][:, c0:c0 + cn], ps[b][:, bi, 0:cn], r=[("ps", b, bi)], w=[("o", b)],
                         eng="act" if bi % 2 else "dve")
                t0 = s0 + sub * 128
                p.dma("sp", g.ptm[t0:t0 + 128, :], ob[b][:, :], r=[("o", b)], w=["ptm_d"])
        p.barrier()


def phase_xbc(g, l):
    p, I = g.p, g.I
    wv = I["w_in"][l].rearrange("(kc q) n -> q kc n", q=128)
    with ExitStack() as es:
        w = p.sb(es, "xb_w", [128, KC, 768], BF16)
        hT = p.sb(es, "xb_h", [128, KC, S], BF16)
        cw = p.sb(es, "xb_cw", [128, 36], F32)
        rawc = p.sb(es, "xb_rc", [128, NCTX + 4], F32)
        rawl = p.sb(es, "xb_rl", [128, NLAT + 4], F32)
        acc = p.sb(es, "xb_acc", [128, S], F32)
        xs = [p.sb(es, "xb_s%d" % i, [128, S], BF16) for i in range(2)]
        tmo = [p.sb(es, "xb_t%d" % i, [128, 4, 128], BF16) for i in range(2)]
        ps = [p.ps(es, "xb_ps%d" % i, [128, 512], F32) for i in range(2)]
        pst = [p.ps(es, "xb_pt%d" % i, [128, 4, 128], F32) for i in range(2)]
        load_rows_T(g, None, cw[:, 0:30], I["ssd_conv_w"][l].rearrange("k (c q) -> (k c) q", q=128), 30, "cw")
        load_rows_T(g, None, cw[:, 30:36], I["ssd_conv_b"][l].rearrange("(c q) -> c q", q=128), 6, "cw")
        for kc in range(KC):
            p.dma("pool", w[:, kc, :], wv[:, kc, 256:1024], w=[("w", kc)])
        for ti, (s0, n) in enumerate(FT):
            p.dma("sp", hT[:, :, s0:s0 + n], g.hT[:, :, s0:s0 + n].rearrange("c p s -> p c s"), r=["hT_d"], w=[("h", ti)])
        p.memset(rawc[:, :], 0.0, w=["rawc"])
        p.memset(rawl[:, :], 0.0, w=["rawl"], eng="pool")
        cnt = 0
        gcnt = 0
        for j in range(6):
            xb = xs[j % 2]
            for ti, (s0, n) in enumerate(FT):
                b = cnt % 2
                cnt += 1
                for kc in range(KC):
                    p.mm(ps[b][:, 0:n], w[:, kc, j * 128:(j + 1) * 128], hT[:, kc, s0:s0 + n], start=(kc == 0),
                         stop=(kc == KC - 1), r=[("w", kc), ("h", ti)], w=[("ps", b)])
                if s0 == 0:
                    p.cp(rawc[:, 2:2 + n], ps[b][:, 0:n], r=[("ps", b)], w=["rawc"], eng="act")
                else:
                    p.cp(rawl[:, 2 + s0 - NCTX:2 + s0 - NCTX + n], ps[b][:, 0:n], r=[("ps", b)], w=["rawl"], eng="act")
            if XBC_LEVEL < 1:
                continue
            pieces = [(rawc, "rawc", 0, 0, NCTX)] + [(rawl, "rawl", q * 1024, NCTX + q * 1024, 1024) for q in range(4)]
            for pi, (rw, rk, o, so, n) in enumerate(pieces):
                eng = "dve"
                a = acc[:, so:so + n]
                p.ts(a, rw[:, o:o + n], cw[:, j:j + 1], r=[rk, "cw"], w=[("acc", pi)], eng=eng)
                for k in range(1, 5):
                    p.stt(a, rw[:, o + k:o + k + n], cw[:, k * 6 + j:k * 6 + j + 1], a, r=[rk, "cw", ("acc", pi)],
                          w=[("acc", pi)], eng=eng)
                p.act(xb[:, so:so + n], a, AF.Silu, bias=cw[:, 30 + j:31 + j], r=[("acc", pi), "cw"], w=[("xs", j % 2, pi)])
            p.dma("sp", g.xbcT[j, :, :], xb[:, :], r=[("xs", j % 2, pi) for pi in range(5)], w=["xbcT_d"])
            if j < 4 and XBC_LEVEL >= 2:
                for t0 in range(0, NT, 4):
                    nt = min(4, NT - t0)
                    b = gcnt % 2
                    gcnt += 1
                    for tt_ in range(nt):
                        t = t0 + tt_
                        p.mm(pst[b][:, tt_, :], xb[:, t * 128:(t + 1) * 128], g.cstb[:, C_ID:C_ID + 128],
                             r=[("xs", j % 2, pi) for pi in range(5)] + ["cstb"], w=[("pt", b)])
                    p.cp(tmo[b][:, 0:nt, :], pst[b][:, 0:nt, :], r=[("pt", b)], w=[("tmo", b)], eng="dve" if b else "act")
                    p.dma("sp", g.xbctm[t0 * 128:(t0 + nt) * 128, j * 128:(j + 1) * 128].rearrange("(t q) c -> q t c", q=128),
                          tmo[b][:, 0:nt, :], r=[("tmo", b)], w=["xbctm_d"])
        p.barrier()


def phase_attn(g, l):
    p, I = g.p, g.I
    with ExitStack() as es:
        qT = p.sb(es, "at_qT", [128, 4, S], BF16)
        kT = p.sb(es, "at_kT", [128, 4, S], BF16)
        vv = p.sb(es, "at_v", [128, NT, 4, 128], BF16)
        yo = p.sb(es, "at_y", [128, 4, S], BF16)
        gbc = p.sb(es, "at_g", [128, 6, 64], F32)
        es_ = p.sb(es, "at_es", [128, 4], F32)
        with ExitStack() as es2:
            pin = [p.sb(es2, "at_in%d" % i, [128, 1024], F32) for i in range(2)]
            rp = [p.sb(es2, "at_rp%d" % i, [128, 64], F32) for i in range(2)]
            sq = p.sb(es2, "at_sq", [128, 384], F32)
            ms = p.sb(es2, "at_ms", [128, 6], F32)
            t1 = p.sb(es2, "at_t1", [128, 12, 2, 16], F32)
            t2 = p.sb(es2, "at_t2", [128, 12, 2, 16], F32)
            ro = p.sb(es2, "at_ro", [128, 12, 64], F32)
            tb = p.sb(es2, "at_tb", [128, 8, 128], BF16)
            pst = [p.ps(es2, "at_pt%d" % i, [128, 8, 128], F32) for i in range(2)]
            for i in range(4):
                p.dma("sp", gbc[:, i, :], I["qk_norm_g"][l, 0:1, :].partition_broadcast(128), w=["gbc"])
            for i in range(2):
                p.dma("sp", gbc[:, 4 + i, :], I["qk_norm_g"][l, 1:2, :].partition_broadcast(128), w=["gbc"])
            p.dma("sp", es_[:, :], I["swa_sink"][l:l + 1, :].partition_broadcast(128), w=["es"])
            p.act(es_[:, :], es_[:, :], AF.Exp, r=["es"], w=["es"])
            p.memset(vv[:, :, :, 64:128], 1.0, w=["vv1"])
            for t in range(NT):
                b = t % 2
                x = pin[b]
                p.dma("sp", x[:, :], g.ptm[t * 128:(t + 1) * 128, PCQ:PCQ + 1024], r=["ptm_d"], w=[("in", b)])
                p.dma("sp", rp[b][:, :], I["rope"][t * 128:(t + 1) * 128, :], w=[("rp", b)])
                p.act(sq[:, :], x[:, 0:384], AF.Square, r=[("in", b)], w=["sq"])
                p.op("dve", lambda e: e.tensor_reduce(out=ms[:, :], in_=sq[:, :].rearrange("p (h f) -> p h f", f=64), axis=AX.X, op=ALU.add),
                     r=["sq"], w=["ms"])
                p.act(ms[:, :], ms[:, :], AF.Ln, bias=EPS, scale=1.0 / 64, r=["ms"], w=["ms"])
                p.act(ms[:, :], ms[:, :], AF.Exp, scale=-0.5, r=["ms"], w=["ms"])
                xg = x[:, 0:384].rearrange("p (h f) -> p h f", f=64)
                p.tt(xg, xg, ms[:, :].unsqueeze(2).broadcast_to([128, 6, 64]), ALU.mult, r=[("in", b), "ms"], w=[("in", b)])
                p.tt(xg, xg, gbc[:, :, :], ALU.mult, r=[("in", b), "gbc"], w=[("in", b)])
                cosb = rp[b][:, 0:32].rearrange("p (a f) -> p a f", a=2)
                sinb = rp[b][:, 32:64].rearrange("p (a f) -> p a f", a=2)
                for (c0, h0, nh) in ((0, 0, 6), (512, 6, 6)):
                    xv = x[:, c0:c0 + nh * 64].rearrange("p (h a t f) -> p h a t f", a=2, t=2, f=16)
                    ov = ro[:, h0:h0 + nh, :].rearrange("p h (a t f) -> p h a t f", a=2, t=2, f=16)
                    for h in range(nh):
                        x1, x2 = xv[:, h, :, 0, :], xv[:, h, :, 1, :]
                        p.tt(t1[:, h0 + h, :, :], x1, cosb, ALU.mult, r=[("in", b), ("rp", b)], w=["t1"])
                        p.tt(t2[:, h0 + h, :, :], x2, sinb, ALU.mult, r=[("in", b), ("rp", b)], w=["t2"], eng="pool")
                        p.tt(ov[:, h, :, 0, :], t1[:, h0 + h, :, :], t2[:, h0 + h, :, :], ALU.subtract, r=["t1", "t2"], w=["ro"])
                        p.tt(t1[:, h0 + h, :, :], x2, cosb, ALU.mult, r=[("in", b), ("rp", b), "ro"], w=["t1"])
                        p.tt(t2[:, h0 + h, :, :], x1, sinb, ALU.mult, r=[("in", b), ("rp", b), "ro"], w=["t2"], eng="pool")
                        p.tt(ov[:, h, :, 1, :], t1[:, h0 + h, :, :], t2[:, h0 + h, :, :], ALU.add, r=["t1", "t2"], w=["ro"])
                rof = ro[:, :, :].rearrange("p h f -> p (h f)")
                p.cp(tb[:, 0:2, :], rof[:, 0:256].rearrange("p (c f) -> p c f", f=128), r=["ro"], w=["tb"])
                p.cp(tb[:, 4:6, :], rof[:, 384:640].rearrange("p (c f) -> p c f", f=128), r=["ro"], w=["tb"])
                for kv in range(2):
                    for d in range(2):
                        p.cp(tb[:, 2 + kv, d * 64:(d + 1) * 64], ro[:, 4 + kv, :], r=["ro"], w=["tb"], eng="pool")
                        p.cp(tb[:, 6 + kv, d * 64:(d + 1) * 64], ro[:, 10 + kv, :], r=["ro"], w=["tb"], eng="pool")
                p.cp(vv[:, t, 0:2, 0:64], x[:, 384:512].rearrange("p (k f) -> p k f", f=64), r=[("in", b)], w=[("vv", t)], eng="act")
                p.cp(vv[:, t, 2:4, 0:64], x[:, 896:1024].rearrange("p (k f) -> p k f", f=64), r=[("in", b)], w=[("vv", t)], eng="act")
                for c in range(8):
                    p.mm(pst[b][:, c, :], tb[:, c, :], g.cstb[:, C_ID:C_ID + 128], r=["tb", "cstb"], w=[("pt", b)])
                sl = slice(t * 128, (t + 1) * 128)
                ce = "act" if b else "dve"
                p.cp(qT[:, 0:2, sl], pst[b][:, 0:2, :], r=[("pt", b)], w=[("qk", t)], eng=ce)
                p.cp(kT[:, 0:2, sl], pst[b][:, 2:4, :], r=[("pt", b)], w=[("qk", t)], eng=ce)
                p.cp(qT[:, 2:4, sl], pst[b][:, 4:6, :], r=[("pt", b)], w=[("qk", t)], eng=ce)
                p.cp(kT[:, 2:4, sl], pst[b][:, 6:8, :], r=[("pt", b)], w=[("qk", t)], eng=ce)
            p.barrier()
        with ExitStack() as es2:
            pss = [p.ps(es2, "ga_s%d" % i, [128, 512], F32) for i in range(3)]
            pso = [p.ps(es2, "ga_o%d" % i, [128, 512], F32) for i in range(2)]
            pt = [p.sb(es2, "ga_p%d" % i, [128, 512], BF16) for i in range(3)]
            rd = p.sb(es2, "ga_rd", [128, 512], F32)
            cs = 0
            co = 0
            for ti, (s0, n) in enumerate(FT):
                kts = range(2) if s0 == 0 else range(NT)
                for h in range(4):
                    c, hp, kv = h // 2, (h % 2) * 64, h // 2
                    ob = co % 2
                    co += 1
                    for kt in kts:
                        sb_ = cs % 3
                        cs += 1
                        p.mm(pss[sb_][:, 0:n], kT[hp:hp + 64, c, kt * 128:(kt + 1) * 128], qT[hp:hp + 64, c, s0:s0 + n],
                             w=[("gs", sb_)])
                        p.act(pt[sb_][:, 0:n], pss[sb_][:, 0:n], AF.Exp, scale=0.125, r=[("gs", sb_)], w=[("gp", sb_)])
                        p.mm(pso[ob][:, 0:n], vv[:, kt, kv, :], pt[sb_][:, 0:n], start=(kt == kts[0]), stop=(kt == kts[-1]),
                             r=[("gp", sb_)], w=[("go", ob)])
                    p.op("dve", lambda e: e.reciprocal(out=rd[0:64, 0:n], in_=pso[ob][64:128, 0:n]), r=[("go", ob)], w=["rd"])
                    p.tt(yo[hp:hp + 64, c, s0:s0 + n], pso[ob][0:64, 0:n], rd[0:64, 0:n], ALU.mult, r=[("go", ob), "rd"], w=[("yo", c)])
            p.barrier()
        with ExitStack() as es2:
            pss = [p.ps(es2, "wa_s%d" % i, [128, 4, 128], F32) for i in range(3)]
            pso = [p.ps(es2, "wa_o%d" % i, [128, 4, 128], F32) for i in range(2)]
            pt = [p.sb(es2, "wa_p%d" % i, [128, 4, 128], BF16) for i in range(3)]
            dt_ = p.sb(es2, "wa_d", [128, 4, 128], F32)
            rd = p.sb(es2, "wa_rd", [128, 4, 128], F32)
            cs = 0
            co = 0
            for qb in range(NT):
                kl = [(0, None), (1, None)]
                if qb >= 2:
                    if qb - 1 >= 2:
                        kl.append((qb - 1, C_L))
                    kl.append((qb, None))
                    if qb + 1 < NT:
                        kl.append((qb + 1, C_U))
                qs = slice(qb * 128, (qb + 1) * 128)
                for h in range(4):
                    c, hp = 2 + h // 2, (h % 2) * 64
                    ob = co % 2
                    co += 1
                    for ki, (kt, msk) in enumerate(kl):
                        sb_ = cs % 3
                        cs += 1
                        p.mm(pss[sb_][:, 0, :], kT[hp:hp + 64, c, kt * 128:(kt + 1) * 128], qT[hp:hp + 64, c, qs], w=[("ws", sb_)])
                        p.act(pt[sb_][:, 0, :], pss[sb_][:, 0, :], AF.Exp, scale=0.125, r=[("ws", sb_)], w=[("wp", sb_)])
                        if msk is not None:
                            p.tt(pt[sb_][:, 0, :], pt[sb_][:, 0, :], g.cstb[:, msk:msk + 128], ALU.mult, r=[("wp", sb_), "cstb"],
                                 w=[("wp", sb_)])
                        p.mm(pso[ob][:, 0, :], vv[:, kt, 2 + h // 2, :], pt[sb_][:, 0, :], start=(ki == 0), stop=(ki == len(kl) - 1),
                             r=[("wp", sb_)], w=[("wo", ob)])
                    p.ts(dt_[64:128, 0, :], pso[ob][64:128, 0, :], es_[64:128, h:h + 1], op0=ALU.add, r=[("wo", ob), "es"], w=["wd"])
                    p.op("dve", lambda e: e.reciprocal(out=rd[0:64, 0, :], in_=dt_[64:128, 0, :]), r=["wd"], w=["wrd"])
                    p.tt(yo[hp:hp + 64, c, qs], pso[ob][0:64, 0, :], rd[0:64, 0, :], ALU.mult, r=[("wo", ob), "wrd"], w=[("yo", c)])
            p.barrier()
        for c in range(4):
            p.dma("sp", g.yT[4 + c, :, :], yo[:, c, :], w=["yT_d"])
        p.barrier()


def phase_ssd(g, l):
    p, I = g.p, g.I
    NC8 = NT * 8
    with ExitStack() as es:
        xsB = p.sb(es, "sd_xsB", [128, NT, 512], BF16)
        zdt = p.sb(es, "sd_zdt", [128, NT, 264], F32)
        ysum = p.sb(es, "sd_y", [128, NT, 256], F32)
        prm = p.sb(es, "sd_prm", [128, 20], F32)
        ng = p.sb(es, "sd_ng", [128, 256], F32)
        for t0 in range(0, NT, 4):
            nt = min(4, NT - t0)
            ts_ = slice(t0, t0 + nt)
            p.dma("sp", xsB[:, ts_, :], g.xbctm[t0 * 128:(t0 + nt) * 128, :].rearrange("(t q) c -> q t c", q=128),
                  r=["xbctm_d"], w=["xsB"])
            p.dma("act", zdt[:, ts_, :], g.ptm[t0 * 128:(t0 + nt) * 128, 0:264].rearrange("(t q) c -> q t c", q=128),
                  r=["ptm_d"], w=["zdt"])
        p.dma("sp", prm[:, 0:8], I["ssd_dt_bias"][l:l + 1].rearrange("o d h -> o (d h)").partition_broadcast(128), w=["prm"])
        p.dma("sp", prm[:, 8:16], I["ssd_a_log"][l:l + 1].rearrange("o d h -> o (d h)").partition_broadcast(128), w=["prm"])
        p.dma("sp", prm[:, 16:20], I["ssd_d"][l:l + 1, :].partition_broadcast(128), w=["prm"])
        p.dma("sp", ng[:, :], I["ssd_norm_g"][l:l + 1, :].partition_broadcast(128), w=["ng"])
        p.act(prm[:, 8:16], prm[:, 8:16], AF.Exp, r=["prm"], w=["prm"])
        p.ts(prm[:, 8:16], prm[:, 8:16], -1.0, r=["prm"], w=["prm"])
        with ExitStack() as es1:
            BCT = p.sb(es1, "sd_bct", [128, 4, S], BF16)
            dt = p.sb(es1, "sd_dt", [128, NT, 8], F32)
            la = p.sb(es1, "sd_la", [128, NT, 8], F32)
            cum = p.sb(es1, "sd_cum", [128, NT, 8], F32)
            tot = p.sb(es1, "sd_tot", [128, NT, 8], F32)
            eoff = p.sb(es1, "sd_eoff", [128, NT, 8], F32)
            dtdte = p.sb(es1, "sd_dte", [128, NT, 8], F32)
            etot = p.sb(es1, "sd_etot", [128, NT, 8], F32)
            ncum = p.sb(es1, "sd_ncum", [128, NT, 8], F32)
            nm4 = p.sb(es1, "sd_nm4", [128, 2, 4, 128], F32)
            laU = [p.sb(es1, "sd_laU%d" % i, [128, 4, 128], F32) for i in range(2)]
            dec = p.sb(es1, "sd_dec", [128, 4, 128], F32)
            WT = p.sb(es1, "sd_WT", [128, 4, 128], BF16)
            xdt = p.sb(es1, "sd_xdt", [128, 4, 64], BF16)
            xdte = p.sb(es1, "sd_xdte", [128, 4, 64], BF16)
            ydsb = p.sb(es1, "sd_yd", [128, 256], F32)
            Sst = p.sb(es1, "sd_S", [128, 4, 64], F32)
            Sb = p.sb(es1, "sd_Sb", [128, 4, 64], BF16)
            pc = p.ps(es1, "sd_pc", [128, NC8], F32)
            dps = [p.ps(es1, "sd_dps%d" % i, [128, 4, 128], F32) for i in range(2)]
            gps = p.ps(es1, "sd_gps", [128, 2, 128], F32)
            ydp = p.ps(es1, "sd_ydp", [128, 4, 64], F32)
            yop = p.ps(es1, "sd_yop", [128, 4, 64], F32)
            stp = p.ps(es1, "sd_stp", [128, 4, 64], F32)
            for c in range(4):
                p.dma("sp", BCT[:, c, :], g.xbcT[2 + c, :, :], r=["xbcT_d"], w=["BCT"])
            for d in range(2):
                for h in range(4):
                    nm = C_NMF if d == 0 else C_NMB
                    p.cp(nm4[:, d, h, :], g.cst[:, nm:nm + 128], r=["cst"], w=["nm4"], eng="pool")
            bc8 = lambda ap: ap.unsqueeze(1).broadcast_to([128, NT, 8])
            p.tt(dt[:, :, :], zdt[:, :, 256:264], bc8(prm[:, 0:8]), ALU.add, r=["zdt", "prm"], w=["dt"])
            p.act(dt[:, :, :], dt[:, :, :], AF.Exp, r=["dt"], w=["dt"])
            p.act(dt[:, :, :], dt[:, :, :], AF.Ln, bias=1.0, r=["dt"], w=["dt"])
            p.tt(la[:, :, :], dt[:, :, :], bc8(prm[:, 8:16]), ALU.mult, r=["dt", "prm"], w=["la"])
            laf = la[:, :, :].rearrange("p t c -> p (t c)")
            pc3 = pc[:, :].rearrange("p (t c) -> p t c", c=8)
            p.mm(pc[:, :], g.cst[:, C_U:C_U + 128], laf, r=["la", "cst"], w=["pc"])
            p.cp(cum[:, :, 0:4], pc3[:, :, 0:4], r=["pc"], w=["cum"])
            p.mm(pc[:, :], g.cst[:, C_L:C_L + 128], laf, r=["la", "cst"], w=["pc"])
            p.cp(cum[:, :, 4:8], pc3[:, :, 4:8], r=["pc"], w=["cum"])
            p.mm(pc[:, :], g.cst[:, C_ONE:C_ONE + 128], laf, r=["la", "cst"], w=["pc"])
            p.cp(tot[:, :, :], pc3, r=["pc"], w=["tot"])
            p.act(eoff[:, :, :], cum[:, :, :], AF.Exp, r=["cum"], w=["eoff"])
            p.act(etot[:, :, :], tot[:, :, :], AF.Exp, r=["tot"], w=["etot"])
            p.tt(dtdte[:, :, :], tot[:, :, :], cum[:, :, :], ALU.subtract, r=["tot", "cum"], w=["dtdte"])
            p.act(dtdte[:, :, :], dtdte[:, :, :], AF.Exp, r=["dtdte"], w=["dtdte"])
            p.tt(dtdte[:, :, :], dtdte[:, :, :], dt[:, :, :], ALU.mult, r=["dtdte", "dt"], w=["dtdte"])
            p.ts(ncum[:, :, :], cum[:, :, :], -1.0, r=["cum"], w=["ncum"])
            cnt = 0
            for d in range(min(2, SSD_LEVEL)):
                order = list(range(NT)) if d == 0 else [1, 0] + list(range(NT - 1, 1, -1))
                tri = C_U if d == 0 else C_L
                p.memset(Sst[:, :, :], 0.0, w=["S"])
                p.memset(Sb[:, :, :], 0.0, w=["Sb"])
                for t in order:
                    b = cnt % 2
                    cnt += 1
                    sl = slice(t * 128, (t + 1) * 128)
                    for h in range(4):
                        p.ts(laU[b][:, h, :], g.cst[:, tri:tri + 128], la[:, t, d * 4 + h:d * 4 + h + 1], r=["la", "cst"],
                             w=[("laU", b)], eng="pool" if h % 2 else "dve")
                    dflat = dps[b][:, :, :].rearrange("p h l -> p (h l)")
                    p.mm(dflat, g.cst[:, C_ONE:C_ONE + 128], laU[b][:, :, :].rearrange("p h l -> p (h l)"), start=True, stop=False,
                         r=[("laU", b), "cst"], w=[("dps", b)])
                    p.mm(dflat, g.cst[:, C_ID:C_ID + 128], nm4[:, d, :, :].rearrange("p h l -> p (h l)"), start=False, stop=True,
                         r=["nm4", "cst"], w=[("dps", b)])
                    for h in range(4):
                        p.act(dec[:, h, :], dps[b][:, h, :], AF.Exp, bias=ncum[:, t, d * 4 + h:d * 4 + h + 1],
                              r=[("dps", b), "ncum"], w=[("dec", h)])
                    for gq in range(2):
                        p.mm(gps[:, gq, :], BCT[:, gq, sl], BCT[:, 2 + gq, sl], r=["BCT"], w=[("gps", gq)])
                    for h in range(4):
                        p.tt(WT[:, h, :], dec[:, h, :], gps[:, h // 2, :], ALU.mult, r=[("dec", h), ("gps", h // 2)], w=[("WT", h)])
                    xs4 = xsB[:, t, 0:256].rearrange("p (h f) -> p h f", f=64)
                    p.tt(xdt[:, :, :], xs4, dt[:, t, d * 4:d * 4 + 4].unsqueeze(2).broadcast_to([128, 4, 64]), ALU.mult,
                         r=["xsB", "dt"], w=["xdt"], eng="pool")
                    p.tt(xdte[:, :, :], xs4, dtdte[:, t, d * 4:d * 4 + 4].unsqueeze(2).broadcast_to([128, 4, 64]), ALU.mult,
                         r=["xsB", "dtdte"], w=["xdte"], eng="pool")
                    for h in range(4):
                        p.mm(ydp[:, h, :], WT[:, h, :], xdt[:, h, :], r=[("WT", h), "xdt"], w=["ydp"])
                    for h in range(4):
                        p.mm(yop[:, h, :], BCT[:, 2 + h // 2, sl], Sb[:, h, :], r=["BCT", "Sb"], w=["yop"])
                    p.cp(ydsb[:, :], ydp[:, :, :].rearrange("p h f -> p (h f)"), r=["ydp"], w=["ydsb"], eng="act")
                    if d == 1:
                        p.tt(ydsb[:, :], ydsb[:, :], ysum[:, t, :], ALU.add, r=["ydsb", ("ysum", t)], w=["ydsb"])
                    for h in range(4):
                        p.stt(ysum[:, t, h * 64:(h + 1) * 64], yop[:, h, :], eoff[:, t, d * 4 + h:d * 4 + h + 1], ydsb[:, h * 64:(h + 1) * 64],
                              r=["yop", "eoff", "ydsb"], w=[("ysum", t)])
                    for h in range(4):
                        p.mm(stp[:, h, :], xsB[:, t, 256 + (h // 2) * 128:256 + (h // 2 + 1) * 128], xdte[:, h, :], r=["xsB", "xdte"], w=["stp"])
                    for h in range(4):
                        p.stt(Sst[:, h, :], Sst[:, h, :], etot[:, t, d * 4 + h:d * 4 + h + 1], stp[:, h, :], r=["S", "etot", "stp"], w=["S"])
                    p.cp(Sb[:, :, :], Sst[:, :, :], r=["S"], w=["Sb"], eng="act")
            p.barrier()
        with ExitStack() as es1:
          if SSD_LEVEL >= 3:
              sq = p.sb(es1, "sd_sq", [128, 256], F32)
              ms = p.sb(es1, "sd_ms", [128, NT], F32)
              yaT = p.sb(es1, "sd_yaT", [128, 2, S], BF16)
              yab = p.sb(es1, "sd_yab", [128, NT, 256], BF16)
              pst = [p.ps(es1, "sd_pt%d" % i, [128, 4, 128], F32) for i in range(2)]
              for t in range(NT):
                  e1 = "dve"
                  for h in range(4):
                      hs = slice(h * 64, (h + 1) * 64)
                      p.stt(ysum[:, t, hs], xsB[:, t, hs], prm[:, 16 + h:17 + h], ysum[:, t, hs], r=["xsB", "prm", ("ys", t)], w=[("ys", t)])
                  p.act(zdt[:, t, 0:256], zdt[:, t, 0:256], AF.Silu, r=[("z", t)], w=[("z", t)])
                  p.tt(ysum[:, t, :], ysum[:, t, :], zdt[:, t, 0:256], ALU.mult, r=[("z", t), ("ys", t)], w=[("ys", t)])
                  p.act(sq[:, :], ysum[:, t, :], AF.Square, r=[("ys", t)], w=["sq"])
                  p.op("dve", lambda e: e.tensor_reduce(out=ms[:, t:t + 1], in_=sq[:, :], axis=AX.X, op=ALU.add), r=["sq"], w=["ms"])
              p.act(ms[:, :], ms[:, :], AF.Ln, bias=EPS, scale=1.0 / 256, r=["ms"], w=["ms"])
              p.act(ms[:, :], ms[:, :], AF.Exp, scale=-0.5, r=["ms"], w=["ms"])
              for t in range(NT):
                  p.stt(yab[:, t, :], ysum[:, t, :], ms[:, t:t + 1], ng[:, :], op0=ALU.mult, op1=ALU.mult, r=[("ys", t), "ms", "ng"], w=["yab"])
              k = 0
              for c in (range(2) if SSD_LEVEL >= 4 else []):
                  for t0 in range(0, NT, 4):
                      nt = min(4, NT - t0)
                      b = k % 2
                      k += 1
                      for i in range(nt):
                          p.mm(pst[b][:, i, :], yab[:, t0 + i, c * 128:(c + 1) * 128], g.cstb[:, C_ID:C_ID + 128],
                               r=["yab", "cstb"], w=[("pt", b)])
                      p.cp(yaT[:, c, t0 * 128:(t0 + nt) * 128].rearrange("p (t s) -> p t s", t=nt), pst[b][:, 0:nt, :],
                           r=[("pt", b)], w=[("yaT", c)], eng="act" if b else "dve")
              for c in range(2):
                  p.dma("sp", g.yT[c, :, :], yaT[:, c, :], r=[("yaT", c)], w=["yT_d"])
              p.barrier()


def phase_s5(g, l):
    p, I = g.p, g.I
    TWO_PI = 2.0 * np.pi
    rt = lambda c: (1 - c) if c < 2 else (35 - c)
    with ExitStack() as es:
        u_tm = p.sb(es, "s5_utm", [128, NT, 256], F32)
        useq = p.sb(es, "s5_useq", [128, 2, S], F32)
        ysum = p.sb(es, "s5_ysum", [128, 2, S], F32)
        dcol = p.sb(es, "s5_d", [128, 2], F32)
        zero8 = p.sb(es, "s5_z8", [128, 8], F32)
        load_rows_T(g, None, dcol[:, :], I["s5_d"][l].rearrange("(c q) -> c q", q=128), 2, "dcol")
        p.memset(zero8[:, :], 0.0, w=["zero8"])
        for t0 in range(0, NT, 4):
            nt = min(4, NT - t0)
            p.dma("sp", u_tm[:, t0:t0 + nt, :], g.ptm[t0 * 128:(t0 + nt) * 128, PU:PU + 256].rearrange("(t q) c -> q t c", q=128),
                  r=["ptm_d"], w=["u_tm"])
        with ExitStack() as es1:
            pst = [p.ps(es1, "s5_pu%d" % i, [128, 4, 128], F32) for i in range(2)]
            bre = p.ps(es1, "s5_bre", [128, 4, 128], F32)
            bim = p.ps(es1, "s5_bim", [128, 4, 128], F32)
            yp = p.ps(es1, "s5_yp", [128, 128], F32)
            ytm = p.ps(es1, "s5_ytm", [128, 128], F32)
            prm = p.sb(es1, "s5_prm", [128, 16, 8], F32)
            T16 = p.sb(es1, "s5_T16", [128, 2, 16], F32)
            st16 = p.sb(es1, "s5_st16", [128, 16], F32)
            rr = p.sb(es1, "s5_r", [128, 8, 128], F32)
            ki = p.sb(es1, "s5_ki", [128, 8, 128], I32)
            kf = p.sb(es1, "s5_kf", [128, 8, 128], F32)
            cosT = p.sb(es1, "s5_cos", [128, 8, 128], F32)
            sinT = p.sb(es1, "s5_sin", [128, 8, 128], F32)
            Mre = p.sb(es1, "s5_Mre", [128, 8, 128], F32)
            Mim = p.sb(es1, "s5_Mim", [128, 8, 128], F32)
            Bre = p.sb(es1, "s5_Bre", [128, 8, 128], F32)
            Bim = p.sb(es1, "s5_Bim", [128, 8, 128], F32)
            Cre = p.sb(es1, "s5_Cre", [128, 8, 128], F32)
            Cim = p.sb(es1, "s5_Cim", [128, 8, 128], F32)
            tA = p.sb(es1, "s5_tA", [128, 4, 128], F32)
            tB = p.sb(es1, "s5_tB", [128, 4, 128], F32)
            vre = p.sb(es1, "s5_vre", [128, 4, 128], F32)
            vim = p.sb(es1, "s5_vim", [128, 4, 128], F32)
            wre = p.sb(es1, "s5_wre", [128, 4, 128], F32)
            wim = p.sb(es1, "s5_wim", [128, 4, 128], F32)
            xre = [p.sb(es1, "s5_xre%d" % i, [128, 8, 128], F32) for i in range(2)]
            xim = [p.sb(es1, "s5_xim%d" % i, [128, 8, 128], F32) for i in range(2)]
            ysb = p.sb(es1, "s5_ysb", [128, 128], F32)
            ident = g.cst[:, C_ID:C_ID + 128]
            LR, LI, ST, RHO, THP, CO, SI, ABR, ABI, DEN, FRE, FIM, TMP, TMP2 = range(14)
            for d in range(2):
                k = 0
                for c in (range(2) if S5_LEVEL >= 0 else []):
                    for t0 in range(0, NT, 4):
                        nt = min(4, NT - t0)
                        b = k % 2
                        k += 1
                        for i in range(nt):
                            src = t0 + i if d == 0 else rt(t0 + i)
                            rhs = ident if d == 0 else g.cst[:, C_J:C_J + 128]
                            p.mm(pst[b][:, i, :], u_tm[:, src, c * 128:(c + 1) * 128], rhs, r=["u_tm", "cst"], w=[("pu", b)])
                        dst = useq[:, c, t0 * 128:(t0 + nt) * 128].rearrange("p (t s) -> p t s", t=nt)
                        p.cp(dst, pst[b][:, 0:nt, :], r=[("pu", b)], w=["useq"], eng="act")
                        if d == 0:
                            yd_ = ysum[:, c, t0 * 128:(t0 + nt) * 128].rearrange("p (t s) -> p t s", t=nt)
                            p.ts(yd_, dst, dcol[:, c:c + 1], r=["useq", "dcol"], w=["ysum"])
                if S5_LEVEL < 1:
                    continue
                for which, nm in ((0, "s5_lam_re"), (1, "s5_lam_im")):
                    p.dma("sp", tA[0:16, 0, 0:64], I[nm][l, d, :, :], w=["tA"])
                    p.tr(yp[0:64, 0:16], tA[0:16, 0, 0:64], g.cst[0:16, C_ID:C_ID + 16], r=["tA", "cst"], w=["yp"])
                    p.cp(T16[0:64, which, :], yp[0:64, 0:16], r=["yp"], w=["T16"])
                    tv = T16[0:64, which, :].rearrange("p (gb gl) -> p gl gb", gl=2)
                    p.cp(prm[0:64, which, :], tv[:, 0, :], r=["T16"], w=["prm"])
                    p.cp(prm[64:128, which, :], tv[:, 1, :], r=["T16"], w=["prm"])
                p.dma("sp", st16[:, :], I["s5_log_step"][l, d:d + 1, :].partition_broadcast(128), w=["st16"])
                p.act(st16[:, :], st16[:, :], AF.Exp, r=["st16"], w=["st16"])
                sv = st16[:, :].rearrange("p (gb gl) -> p gl gb", gl=2)
                p.cp(prm[0:64, ST, :], sv[0:64, 0, :], r=["st16"], w=["prm"])
                p.cp(prm[64:128, ST, :], sv[64:128, 1, :], r=["st16"], w=["prm"])
                P = lambda i: prm[:, i, :]
                p.ts(P(LR), P(LR), -1e-4, op0=ALU.min, r=["prm"], w=["prm"])
                p.tt(P(TMP), P(LR), P(ST), ALU.mult, r=["prm"], w=["prm"])
                p.act(P(RHO), P(TMP), AF.Exp, r=["prm"], w=["prm"])
                p.tt(P(THP), P(LI), P(ST), ALU.mult, r=["prm"], w=["prm"])
                p.ts(P(THP), P(THP), 1.0 / TWO_PI, r=["prm"], w=["prm"])
                if S5_LEVEL < 2:
                    continue
                for gb in range(8):
                    p.ts(rr[:, gb, :], g.cst[:, C_RAMP:C_RAMP + 128], prm[:, THP, gb:gb + 1], r=["prm", "cst"], w=["rr"],
                         eng="pool" if gb % 2 else "dve")
                for (tab, shift) in ((sinT, 0.0), (cosT, 0.25)):
                    for hf in range(2):
                        hs = slice(hf * 4, hf * 4 + 4)
                        if shift:
                            p.ts(kf[:, hs, :], rr[:, hs, :], shift, op0=ALU.add, r=["rr"], w=["kf"])
                            src = kf
                        else:
                            src = rr
                        p.cp(ki[:, hs, :], src[:, hs, :], r=["rr", "kf"], w=["ki"])
                        p.cp(tab[:, hs, :], ki[:, hs, :], r=["ki"], w=["tab"])
                        p.tt(tab[:, hs, :], src[:, hs, :], tab[:, hs, :], ALU.subtract, r=["tab", "rr", "kf"], w=["tab"])
                        p.act(tab[:, hs, :], tab[:, hs, :], AF.Sin, scale=TWO_PI, r=["tab"], w=["tab"])
                p.cp(P(CO), cosT[:, :, 0], r=["tab"], w=["prm"])
                p.cp(P(SI), sinT[:, :, 0], r=["tab"], w=["prm"])
                p.tt(P(ABR), P(RHO), P(CO), ALU.mult, r=["prm"], w=["prm"])
                p.tt(P(ABI), P(RHO), P(SI), ALU.mult, r=["prm"], w=["prm"])
                p.tt(P(DEN), P(LR), P(LR), ALU.mult, r=["prm"], w=["prm"])
                p.tt(P(TMP), P(LI), P(LI), ALU.mult, r=["prm"], w=["prm"])
                p.tt(P(DEN), P(DEN), P(TMP), ALU.add, r=["prm"], w=["prm"])
                p.op("dve", lambda e: e.reciprocal(out=P(DEN), in_=P(DEN)), r=["prm"], w=["prm"])
                p.ts(P(ABR), P(ABR), -1.0, op0=ALU.add, r=["prm"], w=["prm"])
                p.tt(P(TMP), P(ABR), P(LR), ALU.mult, r=["prm"], w=["prm"])
                p.tt(P(TMP2), P(ABI), P(LI), ALU.mult, r=["prm"], w=["prm"])
                p.tt(P(FRE), P(TMP), P(TMP2), ALU.add, r=["prm"], w=["prm"])
                p.tt(P(FRE), P(FRE), P(DEN), ALU.mult, r=["prm"], w=["prm"])
                p.tt(P(TMP), P(ABI), P(LR), ALU.mult, r=["prm"], w=["prm"])
                p.tt(P(TMP2), P(ABR), P(LI), ALU.mult, r=["prm"], w=["prm"])
                p.tt(P(FIM), P(TMP), P(TMP2), ALU.subtract, r=["prm"], w=["prm"])
                p.tt(P(FIM), P(FIM), P(DEN), ALU.mult, r=["prm"], w=["prm"])
                if S5_LEVEL < 3:
                    continue
                for m_ in (Mre, Mim):
                    for hf in range(2):
                        p.memset(m_[:, hf * 4:hf * 4 + 4, :], 0.0, w=["M"], eng="pool" if hf else "dve")
                for gi in range(16):
                    gb, gl, gic = gi // 2, gi % 2, gi % 8
                    p.dma("sp", Mre[gl * 64:(gl + 1) * 64, gb, gic * 16:(gic + 1) * 16], I["s5_b_re"][l, d, gi, :, :], r=[], w=["M"])
                    p.dma("act", Mim[gl * 64:(gl + 1) * 64, gb, gic * 16:(gic + 1) * 16], I["s5_b_im"][l, d, gi, :, :], r=[], w=["M"])
                for gb in range(8):
                    fr, fi = prm[:, FRE, gb:gb + 1], prm[:, FIM, gb:gb + 1]
                    p.ts(tA[:, 0, :], Mim[:, gb, :], fi, r=["M", "prm"], w=["tA"])
                    p.stt(Bre[:, gb, :], Mre[:, gb, :], fr, tA[:, 0, :], op0=ALU.mult, op1=ALU.subtract, r=["M", "prm", "tA"], w=["B0"])
                    p.ts(tB[:, 0, :], Mre[:, gb, :], fi, r=["M", "prm"], w=["tB"])
                    p.stt(Bim[:, gb, :], Mim[:, gb, :], fr, tB[:, 0, :], op0=ALU.mult, op1=ALU.add, r=["M", "prm", "tB"], w=["B0"])
                for (src, dst, key) in ((Bre, Mre, "BT"), (Bim, Mim, "BT")):
                    for hf in range(2):
                        b = hf
                        for j in range(4):
                            p.tr(pst[b][:, j, :], src[:, hf * 4 + j, :], ident, r=["B0", "cst"], w=[("pu", b)])
                        p.cp(dst[:, hf * 4:hf * 4 + 4, :], pst[b][:, :, :], r=[("pu", b)], w=[key], eng="act")
                BTre, BTim = Mre, Mim
                for m_ in (Bre, Bim):
                    for hf in range(2):
                        p.memset(m_[:, hf * 4:hf * 4 + 4, :], 0.0, w=["B0"], eng="pool" if hf else "dve")
                for gi in range(16):
                    gb, gl, gic = gi // 2, gi % 2, gi % 8
                    p.dma("sp", Bre[gic * 16:(gic + 1) * 16, gb, gl * 64:(gl + 1) * 64], I["s5_c_re"][l, d, gi, :, :], r=[], w=["B0"])
                    p.dma("act", Bim[gic * 16:(gic + 1) * 16, gb, gl * 64:(gl + 1) * 64], I["s5_c_im"][l, d, gi, :, :], r=[], w=["B0"])
                for (src, dst, neg) in ((Bre, Cre, False), (Bim, Cim, True)):
                    for hf in range(2):
                        b = hf
                        for j in range(4):
                            p.tr(pst[b][:, j, :], src[:, hf * 4 + j, :], ident, r=["B0", "cst"], w=[("pu", b)])
                        if neg:
                            p.ts(dst[:, hf * 4:hf * 4 + 4, :], pst[b][:, :, :], -1.0, r=[("pu", b)], w=["CT"])
                        else:
                            p.cp(dst[:, hf * 4:hf * 4 + 4, :], pst[b][:, :, :], r=[("pu", b)], w=["CT"], eng="act")
                if S5_LEVEL < 4:
                    continue
                for c in range(NT):
                    cur, prv = c % 2, (c + 1) % 2
                    ps_ = slice(c * 128, (c + 1) * 128)
                    for hf in range(2):
                        hs = slice(hf * 4, hf * 4 + 4)
                        for j in range(4):
                            p.mm(bre[:, j, :], BTre[:, hf * 4 + j, :], useq[:, hf, ps_], r=["BT", "useq"], w=["bre"])
                        for j in range(4):
                            p.mm(bim[:, j, :], BTim[:, hf * 4 + j, :], useq[:, hf, ps_], r=["BT", "useq"], w=["bim"])
                        p.tt(tA[:, :, :], bre[:, :, :], cosT[:, hs, :], ALU.mult, r=["bre", "tab"], w=["tA"])
                        p.tt(tB[:, :, :], bim[:, :, :], sinT[:, hs, :], ALU.mult, r=["bim", "tab"], w=["tB"])
                        p.tt(vre[:, :, :], tA[:, :, :], tB[:, :, :], ALU.add, r=["tA", "tB"], w=["vre"], eng="pool")
                        p.tt(tA[:, :, :], bim[:, :, :], cosT[:, hs, :], ALU.mult, r=["bim", "tab", "vre"], w=["tA"])
                        p.tt(tB[:, :, :], bre[:, :, :], sinT[:, hs, :], ALU.mult, r=["bre", "tab", "vre"], w=["tB"])
                        p.tt(vim[:, :, :], tA[:, :, :], tB[:, :, :], ALU.subtract, r=["tA", "tB"], w=["vim"], eng="pool")
                        for j in range(4):
                            gb = hf * 4 + j
                            rho_b = prm[:, RHO, gb:gb + 1].broadcast_to([128, 128])
                            i_re = zero8[:, 0:1] if c == 0 else xre[prv][:, gb, 127:128]
                            i_im = zero8[:, 0:1] if c == 0 else xim[prv][:, gb, 127:128]
                            p.op("dve", lambda e, j=j, i_re=i_re, rho_b=rho_b: e.tensor_tensor_scan(
                                out=wre[:, j, :], data0=rho_b, data1=vre[:, j, :], initial=i_re, op0=ALU.mult, op1=ALU.add),
                                r=["vre", "prm", ("x", prv), "zero8"], w=["wre"])
                            p.op("dve", lambda e, j=j, i_im=i_im, rho_b=rho_b: e.tensor_tensor_scan(
                                out=wim[:, j, :], data0=rho_b, data1=vim[:, j, :], initial=i_im, op0=ALU.mult, op1=ALU.add),
                                r=["vim", "prm", ("x", prv), "zero8"], w=["wim"])
                        p.tt(tA[:, :, :], wre[:, :, :], cosT[:, hs, :], ALU.mult, r=["wre", "tab"], w=["tA"], eng="pool")
                        p.tt(tB[:, :, :], wim[:, :, :], sinT[:, hs, :], ALU.mult, r=["wim", "tab"], w=["tB"], eng="pool")
                        p.tt(xre[cur][:, hs, :], tA[:, :, :], tB[:, :, :], ALU.subtract, r=["tA", "tB"], w=[("x", cur)], eng="pool")
                        p.tt(tA[:, :, :], wre[:, :, :], sinT[:, hs, :], ALU.mult, r=["wre", "tab", ("x", cur)], w=["tA"])
                        p.tt(tB[:, :, :], wim[:, :, :], cosT[:, hs, :], ALU.mult, r=["wim", "tab", ("x", cur)], w=["tB"])
                        p.tt(xim[cur][:, hs, :], tA[:, :, :], tB[:, :, :], ALU.add, r=["tA", "tB"], w=[("x", cur)])
                        if d == 0:
                            for j in range(4):
                                gb = hf * 4 + j
                                p.mm(yp[:, :], Cre[:, gb, :], xre[cur][:, gb, :], start=(j == 0), stop=False, r=["CT", ("x", cur)], w=["yp"])
                                p.mm(yp[:, :], Cim[:, gb, :], xim[cur][:, gb, :], start=False, stop=(j == 3), r=["CT", ("x", cur)], w=["yp"])
                            p.tt(ysum[:, hf, ps_], yp[:, :], ysum[:, hf, ps_], ALU.add, r=["yp", "ysum"], w=["ysum"])
                        else:
                            for j in range(4):
                                gb = hf * 4 + j
                                p.mm(ytm[:, :], xre[cur][:, gb, :], Cre[:, gb, :], start=(j == 0), stop=False, r=["CT", ("x", cur)], w=["ytm"])
                                p.mm(ytm[:, :], xim[cur][:, gb, :], Cim[:, gb, :], start=False, stop=(j == 3), r=["CT", ("x", cur)], w=["ytm"])
                            p.cp(ysb[:, :], ytm[:, :], r=["ytm"], w=["ysb"], eng="act")
                            p.mm(yp[:, :], ysb[:, :], g.cst[:, C_J:C_J + 128], r=["ysb", "cst"], w=["yp"])
                            os_ = slice(rt(c) * 128, (rt(c) + 1) * 128)
                            p.tt(ysum[:, hf, os_], yp[:, :], ysum[:, hf, os_], ALU.add, r=["yp", "ysum"], w=["ysum"])
                p.barrier()
        with ExitStack() as es1:
            wg = p.sb(es1, "s5_wg", [128, 2, 512], BF16)
            vT = p.sb(es1, "s5_vT", [128, 2, 512], BF16)
            t1 = p.sb(es1, "s5_t1", [128, 512], F32)
            t2 = p.sb(es1, "s5_t2", [128, 512], F32)
            sg = p.sb(es1, "s5_sg", [128, 512], F32)
            yb = [p.sb(es1, "s5_yb%d" % i, [128, 2, 512], BF16) for i in range(2)]
            pv = [p.ps(es1, "s5_pv%d" % i, [128, 512], F32) for i in range(2)]
            pg = [p.ps(es1, "s5_pg%d" % i, [128, 512], F32) for i in range(2)]
            for kc in range(2):
                p.dma("pool", wg[:, kc, :], I["s5_glu_w"][l, kc * 128:(kc + 1) * 128, :], w=["wg"])
            k = 0
            for ti, (s0, n) in enumerate(FT if S5_LEVEL >= 5 else []):
                ob = ti % 2
                for c in range(2):
                    x = ysum[:, c, s0:s0 + n]
                    p.tt(t1[:, 0:n], x, x, ALU.mult, r=["ysum"], w=["t1"])
                    p.ts(t1[:, 0:n], t1[:, 0:n], 0.044715, 1.0, op0=ALU.mult, op1=ALU.add, r=["t1"], w=["t1"])
                    p.tt(t1[:, 0:n], t1[:, 0:n], x, ALU.mult, r=["t1", "ysum"], w=["t1"])
                    p.act(t2[:, 0:n], t1[:, 0:n], AF.Tanh, scale=0.7978845608028654, r=["t1"], w=["t2"])
                    p.ts(t2[:, 0:n], t2[:, 0:n], 0.5, 0.5, op0=ALU.mult, op1=ALU.add, r=["t2"], w=["t2"], eng="pool")
                    p.tt(vT[:, c, 0:n], t2[:, 0:n], x, ALU.mult, r=["t2", "ysum"], w=[("vT", c)])
                for c in range(2):
                    q = k % 2
                    k += 1
                    for kc in range(2):
                        p.mm(pv[q][:, 0:n], wg[:, kc, c * 128:(c + 1) * 128], vT[:, kc, 0:n], start=(kc == 0), stop=(kc == 1),
                             r=["wg", ("vT", kc)], w=[("pv", q)])
                    for kc in range(2):
                        p.mm(pg[q][:, 0:n], wg[:, kc, 256 + c * 128:256 + (c + 1) * 128], vT[:, kc, 0:n], start=(kc == 0), stop=(kc == 1),
                             r=["wg", ("vT", kc)], w=[("pg", q)])
                    p.act(sg[:, 0:n], pg[q][:, 0:n], AF.Sigmoid, r=[("pg", q)], w=["sg"])
                    p.tt(yb[ob][:, c, 0:n], pv[q][:, 0:n], sg[:, 0:n], ALU.mult, r=[("pv", q), "sg"], w=[("yb", ob)])
                p.dma("sp", g.yT[2:4, :, s0:s0 + n].rearrange("c p s -> p c s"), yb[ob][:, :, 0:n], r=[("yb", ob)], w=["yT_d"])
            p.barrier()


def phase_merge(g, l):
    p, I = g.p, g.I
    wv = I["w_in"][l].rearrange("(kc q) n -> q kc n", q=128)
    with ExitStack() as es:
        wg = p.sb(es, "mg_wg", [128, KC, 4096], BF16)
        wb = p.sb(es, "mg_wb", [128, 8, D], BF16)
        wo = p.sb(es, "mg_wo", [128, KC, D], BF16)
        ht = [p.sb(es, "mg_h%d" % i, [128, KC, 512], BF16) for i in range(2)]
        yt = [p.sb(es, "mg_y%d" % i, [128, 8, 512], BF16) for i in range(2)]
        xt = [p.sb(es, "mg_x%d" % i, [128, KC, 512], F32) for i in range(2)]
        sg = [p.sb(es, "mg_s%d" % i, [128, 512], F32) for i in range(2)]
        acc = p.sb(es, "mg_acc", [128, 512], F32)
        tmp = p.sb(es, "mg_tmp", [128, 512], F32)
        accT = p.sb(es, "mg_aT", [128, KC, 512], BF16)
        psg = [p.ps(es, "mg_pg%d" % i, [128, 512], F32) for i in range(2)]
        psb = [p.ps(es, "mg_pb%d" % i, [128, 512], F32) for i in range(2)]
        pso = [p.ps(es, "mg_po%d" % i, [128, 512], F32) for i in range(2)]
        for kc in range(KC):
            p.dma("pool", wg[:, kc, :], wv[:, kc, 2312:6408], w=[("wg", kc)])
            p.dma("pool", wo[:, kc, :], I["w_out"][l, kc * 128:(kc + 1) * 128, :], w=[("wo", kc)])
            p.dma("pool", wb[:, kc, :], I["w_branch"][l, kc // 2, (kc % 2) * 128:(kc % 2 + 1) * 128, :], w=[("wb", kc)])
        cg = 0
        co = 0
        for ti, (s0, n) in enumerate(FT):
            b = ti % 2
            ic = 1 if s0 == 0 else 0
            p.dma("sp", ht[b][:, :, 0:n], g.hT[:, :, s0:s0 + n].rearrange("c p s -> p c s"), r=["hT_d"], w=[("h", b)])
            p.dma("sp", yt[b][:, :, 0:n], g.yT[:, :, s0:s0 + n].rearrange("c p s -> p c s"), r=["yT_d"], w=[("y", b)])
            p.dma("sp", xt[b][:, :, 0:n], g.xres[:, :, s0:s0 + n].rearrange("c p s -> p c s"), r=["xres"], w=[("x", b)])
            for oc in range(KC):
                for br in range(4):
                    q = cg % 2
                    cg += 1
                    for kc in range(KC):
                        c0 = br * D + oc * 128
                        p.mm(psg[q][:, 0:n], wg[:, kc, c0:c0 + 128], ht[b][:, kc, 0:n], start=(kc == 0), stop=(kc == KC - 1),
                             r=[("wg", kc), ("h", b)], w=[("pg", q)])
                    for k2 in range(2):
                        p.mm(psb[q][:, 0:n], wb[:, br * 2 + k2, oc * 128:(oc + 1) * 128], yt[b][:, br * 2 + k2, 0:n], start=(k2 == 0),
                             stop=(k2 == 1), r=[("wb", br * 2 + k2), ("y", b)], w=[("pb", q)])
                    p.act(sg[q][:, 0:n], psg[q][:, 0:n], AF.Sigmoid, r=[("pg", q)], w=[("sg", q)])
                    if br == 0:
                        p.tt(acc[:, 0:n], psb[q][:, 0:n], sg[q][:, 0:n], ALU.mult, r=[("pb", q), ("sg", q)], w=["acc"])
                    else:
                        p.tt(tmp[:, 0:n], psb[q][:, 0:n], sg[q][:, 0:n], ALU.mult, r=[("pb", q), ("sg", q)], w=["tmp"])
                        p.tt(acc[:, 0:n], acc[:, 0:n], tmp[:, 0:n], ALU.add, r=["tmp", "acc"], w=["acc"], eng="pool")
                p.cp(accT[:, oc, 0:n], acc[:, 0:n], r=["acc"], w=[("aT", oc)], eng="act")
            for oc in range(KC):
                q = co % 2
                co += 1
                for kc in range(KC):
                    p.mm(pso[q][:, 0:n], wo[:, kc, oc * 128:(oc + 1) * 128], accT[:, kc, 0:n], start=(kc == 0), stop=(kc == KC - 1),
                         r=[("wo", kc), ("aT", kc)], w=[("po", q)])
                p.stt(xt[b][:, oc, 0:n], pso[q][:, 0:n], g.mod[:, 16 + oc, ic:ic + 1], xt[b][:, oc, 0:n], op0=ALU.mult, op1=ALU.add,
                      r=[("po", q), "mod", ("x", b)], w=[("x", b)])
            p.dma("sp", g.xres[:, :, s0:s0 + n].rearrange("c p s -> p c s"), xt[b][:, :, 0:n], r=[("x", b)], w=["xres"])
        p.barrier()


def phase_ffn(g, l):
    p, I = g.p, g.I
    moe = (l % 2 == 1)
    m = l // 2
    if moe:
        H, GS = EXPERT_DIM, 4
        experts = [(I["moe_w_in"][m, e], I["moe_w_out"][m, e]) for e in range(8)]
    else:
        H, GS = FFN_DIM, 2
        experts = [(I["ffn_w_in"][m], I["ffn_w_out"][m])]
    HC = H // 128
    NG = HC // GS
    with ExitStack() as es:
        wT = p.sb(es, "ff_wT", [8, S], F32)
        if moe:
            with ExitStack() as es0:
                rw = p.sb(es0, "ff_rw", [128, KC, 8], F32)
                lsb = p.sb(es0, "ff_lsb", [128, 8], F32)
                m8 = p.sb(es0, "ff_m8", [128, 8], F32)
                gt = p.sb(es0, "ff_gt", [128, 4], F32)
                e1 = p.sb(es0, "ff_e1", [128, 8], F32)
                e2 = p.sb(es0, "ff_e2", [128, 8], F32)
                lg = p.ps(es0, "ff_lg", [128, 8], F32)
                wtp = p.ps(es0, "ff_wtp", [8, 128], F32)
                p.dma("sp", rw[:, :, :], I["moe_router"][m].rearrange("(kc q) e -> q kc e", q=128), w=["rw"])

                def sink(ti, s0, n, ht, key, h32):
                    p.dma("sp", g.hT[:, :, s0:s0 + n].rearrange("c p s -> p c s"), ht[:, :, 0:n], r=[key], w=["hT_d"])
                    for sub in range(n // 128):
                        ss = slice(sub * 128, (sub + 1) * 128)
                        for kc in range(KC):
                            p.mm(lg[:, :], h32[:, kc, ss], rw[:, kc, :], start=(kc == 0), stop=(kc == KC - 1),
                                 r=[("nmsq", kc), "rw"], w=["lg"])
                        p.cp(lsb[:, :], lg[:, :], r=["lg"], w=["lsb"])
                        p.op("dve", lambda e: e.max(out=m8[:, :], in_=lsb[:, :]), r=["lsb"], w=["m8"])
                        p.tt(gt[:, 0:1], m8[:, 0:1], m8[:, 1:2], ALU.subtract, r=["m8"], w=["gt"])
                        p.act(gt[:, 1:2], gt[:, 0:1], AF.Sigmoid, r=["gt"], w=["gt1"])
                        p.act(gt[:, 2:3], gt[:, 0:1], AF.Sigmoid, scale=-1.0, r=["gt"], w=["gt2"])
                        p.ts(e1[:, :], lsb[:, :], m8[:, 0:1], gt[:, 1:2], op0=ALU.is_equal, op1=ALU.mult, r=["lsb", "m8", "gt1"], w=["e1"])
                        p.ts(e2[:, :], lsb[:, :], m8[:, 1:2], gt[:, 2:3], op0=ALU.is_equal, op1=ALU.mult, r=["lsb", "m8", "gt2"], w=["e2"])
                        p.tt(e1[:, :], e1[:, :], e2[:, :], ALU.add, r=["e1", "e2"], w=["e1"])
                        p.tr(wtp[:, :], e1[:, :], g.cst[:, C_ID:C_ID + 128], r=["e1", "cst"], w=["wtp"])
                        p.cp(wT[0:8, s0 + sub * 128:s0 + (sub + 1) * 128], wtp[:, :], r=["wtp"], w=["wT"])

                norm_tiles(g, 1, sink, want32=True)
        else:
            def sink(ti, s0, n, ht, key, h32):
                p.dma("sp", g.hT[:, :, s0:s0 + n].rearrange("c p s -> p c s"), ht[:, :, 0:n], r=[key], w=["hT_d"])

            norm_tiles(g, 1, sink)
        ht = [p.sb(es, "ff_h%d" % i, [128, KC, 512], BF16) for i in range(2)]
        wg = [p.sb(es, "ff_wg%d" % i, [128, KC, GS * 128], BF16) for i in range(2)]
        wu = [p.sb(es, "ff_wu%d" % i, [128, KC, GS * 128], BF16) for i in range(2)]
        wo = p.sb(es, "ff_wo", [128, HC, D], BF16)
        actT = p.sb(es, "ff_act", [128, HC, 512], BF16)
        oacc = p.sb(es, "ff_oacc", [128, KC, 512], F32)
        xt = p.sb(es, "ff_x", [128, KC, 512], F32)
        sgt = [p.sb(es, "ff_sg%d" % i, [128, 512], F32) for i in range(2)]
        tmp = [p.sb(es, "ff_tmp%d" % i, [128, 512], F32) for i in range(2)]
        wbc = p.sb(es, "ff_wbc", [128, 512], F32)
        psg = [p.ps(es, "ff_pg%d" % i, [128, 512], F32) for i in range(2)]
        psu = [p.ps(es, "ff_pu%d" % i, [128, 512], F32) for i in range(2)]
        pso = [p.ps(es, "ff_po%d" % i, [128, 512], F32) for i in range(2)]
        psw = p.ps(es, "ff_pw", [128, 512], F32)
        gcnt = 0
        jc = 0
        oc_ = 0
        for ti, (s0, n) in enumerate(FT):
            hb = ti % 2
            ic = 1 if s0 == 0 else 0
            p.dma("sp", ht[hb][:, :, 0:n], g.hT[:, :, s0:s0 + n].rearrange("c p s -> p c s"), r=["hT_d"], w=[("h", hb)])
            p.dma("sp", xt[:, :, 0:n], g.xres[:, :, s0:s0 + n].rearrange("c p s -> p c s"), r=["xres"], w=["x"])
            for ei, (w_in, w_out) in enumerate(experts):
                wiv = w_in.rearrange("(kc q) n -> q kc n", q=128)
                wov = w_out.rearrange("(hc q) n -> q hc n", q=128)
                if moe:
                    p.mm(psw[:, 0:n], g.cst[0:8, C_SEL + ei * 128:C_SEL + (ei + 1) * 128], wT[0:8, s0:s0 + n], r=["wT", "cst"], w=["psw"])
                    p.cp(wbc[:, 0:n], psw[:, 0:n], r=["psw"], w=["wbc"], eng="act")
                wibv = g.wib[ei, :, 0:2 * H].rearrange("(kc q) n -> q kc n", q=128)
                wobv = g.wob[ei, 0:H, :].rearrange("(hc q) n -> q hc n", q=128)
                for h0 in range(0, HC, 4):
                    hn = min(4, HC - h0)
                    if ti == 0:
                        p.dma("pool", wo[:, h0:h0 + hn, :], wov[:, h0:h0 + hn, :], w=[("wo", h0)])
                        p.dma("sp", wobv[:, h0:h0 + hn, :], wo[:, h0:h0 + hn, :], r=[("wo", h0)], w=[("wobd", ei, h0)])
                    else:
                        p.dma("sp", wo[:, h0:h0 + hn, :], wobv[:, h0:h0 + hn, :], r=[("wobd", ei, h0)], w=[("wo", h0)])
                for gi in range(NG):
                    gb = gcnt % 2
                    gcnt += 1
                    c0 = gi * GS * 128
                    if ti == 0:
                        p.dma("pool", wg[gb][:, :, :], wiv[:, :, c0:c0 + GS * 128], w=[("wg", gb)])
                        p.dma("pool", wu[gb][:, :, :], wiv[:, :, H + c0:H + c0 + GS * 128], w=[("wu", gb)])
                        p.dma("sp", wibv[:, :, c0:c0 + GS * 128], wg[gb][:, :, :], r=[("wg", gb)], w=[("wibd", ei, gi, 0)])
                        p.dma("sp", wibv[:, :, H + c0:H + c0 + GS * 128], wu[gb][:, :, :], r=[("wu", gb)], w=[("wibd", ei, gi, 1)])
                    else:
                        p.dma("sp", wg[gb][:, :, :], wibv[:, :, c0:c0 + GS * 128], r=[("wibd", ei, gi, 0)], w=[("wg", gb)])
                        p.dma("sp", wu[gb][:, :, :], wibv[:, :, H + c0:H + c0 + GS * 128], r=[("wibd", ei, gi, 1)], w=[("wu", gb)])
                    for j in range(GS):
                        q = jc % 2
                        jc += 1
                        hc = gi * GS + j
                        for kc in range(KC):
                            p.mm(psg[q][:, 0:n], wg[gb][:, kc, j * 128:(j + 1) * 128], ht[hb][:, kc, 0:n], start=(kc == 0),
                                 stop=(kc == KC - 1), r=[("wg", gb), ("h", hb)], w=[("pg", q)])
                        for kc in range(KC):
                            p.mm(psu[q][:, 0:n], wu[gb][:, kc, j * 128:(j + 1) * 128], ht[hb][:, kc, 0:n], start=(kc == 0),
                                 stop=(kc == KC - 1), r=[("wu", gb), ("h", hb)], w=[("pu", q)])
                        p.act(sgt[q][:, 0:n], psg[q][:, 0:n], AF.Silu, r=[("pg", q)], w=[("sg", q)])
                        if moe:
                            p.tt(tmp[q][:, 0:n], psu[q][:, 0:n], sgt[q][:, 0:n], ALU.mult, r=[("pu", q), ("sg", q)], w=[("tmp", q)])
                            p.tt(actT[:, hc, 0:n], tmp[q][:, 0:n], wbc[:, 0:n], ALU.mult, r=[("tmp", q), "wbc"], w=[("act", hc)], eng="pool")
                        else:
                            p.tt(actT[:, hc, 0:n], psu[q][:, 0:n], sgt[q][:, 0:n], ALU.mult, r=[("pu", q), ("sg", q)], w=[("act", hc)])
                for oc in range(KC):
                    q = oc_ % 2
                    oc_ += 1
                    for hc in range(HC):
                        p.mm(pso[q][:, 0:n], wo[:, hc, oc * 128:(oc + 1) * 128], actT[:, hc, 0:n], start=(hc == 0), stop=(hc == HC - 1),
                             r=[("wo", (hc // 4) * 4), ("act", hc)], w=[("po", q)])
                    if ei == 0:
                        p.cp(oacc[:, oc, 0:n], pso[q][:, 0:n], r=[("po", q)], w=[("oacc", oc)])
                    else:
                        p.tt(oacc[:, oc, 0:n], pso[q][:, 0:n], oacc[:, oc, 0:n], ALU.add, r=[("po", q), ("oacc", oc)], w=[("oacc", oc)])
            for oc in range(KC):
                p.stt(xt[:, oc, 0:n], oacc[:, oc, 0:n], g.mod[:, 40 + oc, ic:ic + 1], xt[:, oc, 0:n], op0=ALU.mult, op1=ALU.add,
                      r=[("oacc", oc), "mod", "x"], w=["x"])
            p.dma("sp", g.xres[:, :, s0:s0 + n].rearrange("c p s -> p c s"), xt[:, :, 0:n], r=["x"], w=["xres"])
        p.barrier()


def layer(g, l, stop):
    phases = [("ada", adaln), ("n1", phase_norm1), ("ptm", phase_ptm), ("xbc", phase_xbc), ("ssd", phase_ssd), ("s5", phase_s5), ("attn", phase_attn), ("merge", phase_merge), ("ffn", phase_ffn)]
    for name, fn in phases:
        fn(g, l)
        if stop is not None and stop == (l, name):
            return


def final(g, out, real):
    p, I = g.p, g.I
    if not real:
        with ExitStack() as es:
            z = p.sb(es, "fz", [128, D], F32)
            p.memset(z[:, :], 0.0, w=["fz"])
            p.dma("sp", out[0:128, :], z[:, :], r=["fz"], w=["out"])
            p.barrier()
        return
    with ExitStack() as es:
        gf = p.sb(es, "fn_g", [128, 8], F32)
        load_rows_T(g, None, gf[:, :], I["final_norm_g"].rearrange("(j q) -> j q", q=128), 8, "gf")
        xt = [p.sb(es, "fn_x%d" % i, [128, KC, 512], F32) for i in range(2)]
        sq = p.sb(es, "fn_sq", [128, KC, 512], F32)
        rs = p.sb(es, "fn_rs", [128, 512], F32)
        ot = [p.sb(es, "fn_o%d" % i, [128, KC, 128], F32) for i in range(2)]
        ssq = p.ps(es, "fn_ps", [128, 512], F32)
        pst = [p.ps(es, "fn_pt%d" % i, [128, KC, 128], F32) for i in range(2)]
        cnt = 0
        for ti, (s0, n) in enumerate(FT[1:]):
            b = ti % 2
            p.dma("sp", xt[b][:, :, 0:n], g.xres[:, :, s0:s0 + n].rearrange("c p s -> p c s"), r=["xres"], w=[("fx", b)])
            for c in range(KC):
                p.act(sq[:, c, 0:n], xt[b][:, c, 0:n], AF.Square, r=[("fx", b)], w=[("fsq", c)])
                p.mm(ssq[:, 0:n], g.cst[:, C_ONE:C_ONE + 128], sq[:, c, 0:n], start=(c == 0), stop=(c == KC - 1),
                     r=[("fsq", c), "cst"], w=["fps"])
            p.act(rs[:, 0:n], ssq[:, 0:n], AF.Ln, bias=EPS, scale=1.0 / D, r=["fps"], w=["frs"])
            p.act(rs[:, 0:n], rs[:, 0:n], AF.Exp, scale=-0.5, r=["frs"], w=["frs"])
            for c in range(KC):
                p.stt(sq[:, c, 0:n], xt[b][:, c, 0:n], gf[:, c:c + 1], rs[:, 0:n], op0=ALU.mult, op1=ALU.mult,
                      r=[("fx", b), "frs", "gf"], w=[("fsq", c)])
            for sub in range(n // 128):
                ob = cnt % 2
                cnt += 1
                for c in range(KC):
                    p.tr(pst[ob][:, c, :], sq[:, c, sub * 128:(sub + 1) * 128], g.cst[:, C_ID:C_ID + 128],
                         r=[("fsq", c), "cst"], w=[("fpt", ob)])
                p.cp(ot[ob][:, :, :], pst[ob][:, :, :], r=[("fpt", ob)], w=[("fo", ob)], eng="act" if ob else "dve")
                t0 = s0 - NCTX + sub * 128
                p.dma("sp", out[t0:t0 + 128, :], ot[ob][:, :, :].rearrange("p c f -> p (c f)"), r=[("fo", ob)], w=["out"])
        p.barrier()


_NC_CACHE = {}


def kernel(**inputs):
    x = np.asarray(inputs["x"], np.float32)
    nb = x.shape[0]
    if "nc" not in _NC_CACHE:
        _NC_CACHE["nc"] = build()[0]
    nc = _NC_CACHE["nc"]
    consts = make_consts()
    rope = make_rope()
    shared = {k: np.ascontiguousarray(np.asarray(v, np.float32)) for k, v in inputs.items() if k not in ("x", "c", "ctx", "c_ctx")}
    c = np.asarray(inputs["c"], np.float32)
    cctx = np.asarray(inputs["c_ctx"], np.float32)
    in_maps = []
    for b in range(nb):
        m = dict(shared)
        m["x"] = np.ascontiguousarray(x[b])
        m["ctx"] = np.ascontiguousarray(np.asarray(inputs["ctx"], np.float32)[b])
        m["cc"] = np.ascontiguousarray(np.concatenate([c[b].reshape(8, 128), cctx.reshape(8, 128)], 0))
        m["consts"] = consts
        m["rope"] = rope
        in_maps.append(m)
    res = run_bass_kernel_spmd(nc, in_maps, core_ids=list(range(nb)))
    return np.stack([r["out"] for r in res.results], axis=0).astype(np.float32)
```

```python
import numpy as np
from contextlib import ExitStack
import concourse.bass as bass
import concourse.mybir as mybir
from concourse.bass_utils import run_bass_kernel_spmd

F32 = mybir.dt.float32
BF16 = mybir.dt.bfloat16
I32 = mybir.dt.int32
ALU = mybir.AluOpType
AF = mybir.ActivationFunctionType
AX = mybir.AxisListType

D = 1024
KC = 8
NCTX = 256
NLAT = 4096
S = NCTX + NLAT
NT = S // 128
DEPTH = 4
FT = [(0, 256)] + [(256 + 512 * i, 512) for i in range(8)]
EPS = 1e-6
PZ, PDT, PU, PCQ, PCK, PCV, PDQ, PDK, PDV = 0, 256, 264, 520, 776, 904, 1032, 1288, 1416
NPTM = 1544
FFN_DIM = 2816
EXPERT_DIM = 3584
NEG = -30000.0
import os
SAME_ENG_SYNC = bool(int(os.environ.get('SAME_ENG_SYNC', '1')))
XBC_LEVEL = int(os.environ.get('XBC_LEVEL', '2'))
ATT_LEVEL = int(os.environ.get('ATT_LEVEL', '2'))
SSD_LEVEL = int(os.environ.get('SSD_LEVEL', '9'))
S5_LEVEL = int(os.environ.get('S5_LEVEL', '9'))


class Prog:
    def __init__(self, nc, es):
        self.nc = nc
        self.es = es
        self.E = {"pe": nc.tensor, "act": nc.scalar, "dve": nc.vector, "pool": nc.gpsimd, "sp": nc.sync}
        self.csem = {}
        self.ccnt = {}
        for e in ("pe", "act", "dve", "pool"):
            self.csem[e] = es.enter_context(nc.semaphore("c_" + e))
            self.ccnt[e] = 0
        self.dsem = {}
        self.dcnt = {}
        self.dnext = {}
        for q in ("sp", "pool", "act"):
            self.dsem[q] = [es.enter_context(nc.semaphore("d_%s%d" % (q, i))) for i in range(8)]
            self.dcnt[q] = [0] * 8
            self.dnext[q] = 0
        self.sems = {}
        for e in self.csem:
            self.sems[("c", e)] = self.csem[e]
        for q in self.dsem:
            for i, s in enumerate(self.dsem[q]):
                self.sems[("d", q, i)] = s
        self.waited = {e: {} for e in self.E}
        self.state = {}
        self.nins = 0

    def _deps(self, r, w):
        deps = {}

        def add(tok):
            if tok is None:
                return
            k, v = tok
            if deps.get(k, 0) < v:
                deps[k] = v

        for key in r:
            st = self.state.get(key)
            if st:
                add(st[0])
        for key in w:
            st = self.state.get(key)
            if st:
                add(st[0])
                for t in st[1]:
                    add(t)
        return deps

    def _wait(self, eng, deps):
        wd = self.waited[eng]
        for k, v in deps.items():
            if wd.get(k, 0) >= v:
                continue
            if k == ("c", eng) and (eng == "pe" or not SAME_ENG_SYNC):
                continue
            self.E[eng].wait_ge(self.sems[k], v)
            wd[k] = v
            self.nins += 1

    def _commit(self, tok, r, w):
        for key in r:
            st = self.state.setdefault(key, [None, []])
            st[1].append(tok)
            if len(st[1]) > 24:
                mx = {}
                for k, v in st[1]:
                    if mx.get(k, 0) < v:
                        mx[k] = v
                st[1] = list(mx.items())
        for key in w:
            self.state[key] = [tok, []]

    def op(self, eng, fn, r=(), w=()):
        self._wait(eng, self._deps(r, w))
        ins = fn(self.E[eng])
        self.ccnt[eng] += 1
        ins.then_inc(self.csem[eng], 1)
        tok = (("c", eng), self.ccnt[eng])
        self._commit(tok, r, w)
        self.nins += 1
        return tok

    def dma(self, q, out, in_, r=(), w=()):
        i = self.dnext[q]
        self.dnext[q] = (i + 1) % 8
        deps = self._deps(r, w)
        k = ("d", q, i)
        if self.dcnt[q][i] > 0:
            deps[k] = max(deps.get(k, 0), self.dcnt[q][i])
        self._wait(q, deps)
        self.dcnt[q][i] += 16
        self.E[q].dma_start(out=out, in_=in_).then_inc(self.dsem[q][i], 16)
        tok = (k, self.dcnt[q][i])
        self._commit(tok, r, w)
        self.nins += 1
        return tok

    def barrier(self):
        deps = {}
        for e in self.csem:
            if self.ccnt[e]:
                deps[("c", e)] = self.ccnt[e]
        for q in self.dsem:
            for i in range(8):
                if self.dcnt[q][i]:
                    deps[("d", q, i)] = self.dcnt[q][i]
        for e in self.E:
            d = {k: v for k, v in deps.items() if k != ("c", e)}
            self._wait(e, d)
        self.state = {}

    def mm(self, out, lhsT, rhs, start=True, stop=True, r=(), w=()):
        return self.op("pe", lambda e: e.matmul(out, lhsT, rhs, start=start, stop=stop), r, w)

    def tr(self, out, in_, ident, r=(), w=()):
        return self.op("pe", lambda e: e.transpose(out, in_, ident), r, w)

    def act(self, out, in_, func, bias=0.0, scale=1.0, r=(), w=(), eng="act"):
        return self.op(eng, lambda e: e.activation(out=out, in_=in_, func=func, bias=bias, scale=scale), r, w)

    def tt(self, out, in0, in1, op, r=(), w=(), eng="dve"):
        return self.op(eng, lambda e: e.tensor_tensor(out=out, in0=in0, in1=in1, op=op), r, w)

    def ts(self, out, in0, s1, s2=None, op0=ALU.mult, op1=None, r=(), w=(), eng="dve"):
        if op1 is None:
            return self.op(eng, lambda e: e.tensor_scalar(out=out, in0=in0, scalar1=s1, scalar2=None, op0=op0), r, w)
        return self.op(eng, lambda e: e.tensor_scalar(out=out, in0=in0, scalar1=s1, scalar2=s2, op0=op0, op1=op1), r, w)

    def stt(self, out, in0, scalar, in1, op0=ALU.mult, op1=ALU.add, r=(), w=(), eng="dve"):
        return self.op(eng, lambda e: e.scalar_tensor_tensor(out=out, in0=in0, scalar=scalar, in1=in1, op0=op0, op1=op1), r, w)

    def cp(self, out, in_, r=(), w=(), eng="dve"):
        if eng == "act":
            return self.op("act", lambda e: e.copy(out=out, in_=in_), r, w)
        return self.op(eng, lambda e: e.tensor_copy(out=out, in_=in_), r, w)

    def memset(self, ap, val, w=(), eng="dve"):
        return self.op(eng, lambda e: e.memset(ap, val), (), w)

    def sb(self, es, name, shape, dt):
        self.uid = getattr(self, "uid", 0) + 1
        return es.enter_context(self.nc.sbuf_tensor("s%d_%s" % (self.uid, name), list(shape), dt))

    def ps(self, es, name, shape, dt=F32):
        self.uid = getattr(self, "uid", 0) + 1
        return es.enter_context(self.nc.psum_tensor("p%d_%s" % (self.uid, name), list(shape), dt))


def make_consts():
    i = np.arange(128)
    ident = np.eye(128, dtype=np.float32)
    J = ident[::-1].copy()
    U = (i[:, None] <= i[None, :]).astype(np.float32)
    L = (i[:, None] >= i[None, :]).astype(np.float32)
    ones = np.ones((128, 128), np.float32)
    nmf = np.where(i[:, None] <= i[None, :], 0.0, NEG).astype(np.float32)
    nmb = np.where(i[:, None] >= i[None, :], 0.0, NEG).astype(np.float32)
    ramp = np.tile(np.arange(1, 129, dtype=np.float32)[None, :], (128, 1))
    sel = np.zeros((128, 8 * 128), np.float32)
    for e in range(8):
        sel[e, e * 128:(e + 1) * 128] = 1.0
    return np.concatenate([ident, J, U, L, ones, nmf, nmb, ramp, sel], axis=1)


C_ID, C_J, C_U, C_L, C_ONE, C_NMF, C_NMB, C_RAMP, C_SEL = [k * 128 for k in range(9)]
NCONST = 16 * 128


def make_rope():
    pos = np.arange(NLAT)
    pr = (pos // 64).astype(np.float32)
    pc = (pos % 64).astype(np.float32)
    inv = (np.float32(10000.0) ** (-np.arange(16, dtype=np.float32) / np.float32(16))).astype(np.float32)
    ang = np.concatenate([pr[:, None] * inv, pc[:, None] * inv], axis=-1).astype(np.float32)
    t = np.zeros((S, 64), np.float32)
    t[:NCTX, :32] = 1.0
    t[NCTX:, :32] = np.cos(ang)
    t[NCTX:, 32:] = np.sin(ang)
    return t


class Ctx:
    pass


def build(dbg=None, layers=DEPTH, stop=None, skip=()):
    dbg = dbg or {}
    nc = bass.Bass("TRN2", target_bir_lowering=False)
    g = Ctx()
    g.nc = nc

    def din(name, shape, dt=F32):
        return nc.dram_tensor(name, list(shape), dt, kind="ExternalInput").ap()

    def dscr(name, shape, dt=F32):
        kind = "ExternalOutput" if name in dbg else "Internal"
        return nc.dram_tensor(name, list(shape), dt, kind=kind).ap()

    I = {}
    I["x"] = din("x", [NLAT, D])
    I["ctx"] = din("ctx", [NCTX, D])
    I["cc"] = din("cc", [16, 128])
    I["consts"] = din("consts", [128, NCONST])
    I["rope"] = din("rope", [S, 64])
    shapes = dict(
        norm1_g=[DEPTH, D], norm2_g=[DEPTH, D], ada_w=[DEPTH, D, 6 * D], ada_b=[DEPTH, 6 * D],
        w_in=[DEPTH, D, 6408], ssd_conv_w=[DEPTH, 5, 768], ssd_conv_b=[DEPTH, 768], ssd_a_log=[DEPTH, 2, 4],
        ssd_dt_bias=[DEPTH, 2, 4], ssd_d=[DEPTH, 4], ssd_norm_g=[DEPTH, 256], s5_lam_re=[DEPTH, 2, 16, 64],
        s5_lam_im=[DEPTH, 2, 16, 64], s5_log_step=[DEPTH, 2, 16], s5_b_re=[DEPTH, 2, 16, 64, 16],
        s5_b_im=[DEPTH, 2, 16, 64, 16], s5_c_re=[DEPTH, 2, 16, 16, 64], s5_c_im=[DEPTH, 2, 16, 16, 64],
        s5_d=[DEPTH, 256], s5_glu_w=[DEPTH, 256, 512], qk_norm_g=[DEPTH, 2, 64], swa_sink=[DEPTH, 4],
        w_branch=[DEPTH, 4, 256, D], w_out=[DEPTH, D, D], ffn_w_in=[2, D, 2 * FFN_DIM], ffn_w_out=[2, FFN_DIM, D],
        moe_router=[2, D, 8], moe_w_in=[2, 8, D, 2 * EXPERT_DIM], moe_w_out=[2, 8, EXPERT_DIM, D],
        final_norm_g=[D],
    )
    for k, shp in shapes.items():
        if k not in skip:
            I[k] = din(k, shp)
    out = nc.dram_tensor("out", [NLAT, D], F32, kind="ExternalOutput").ap()

    g.I = I
    g.xres = dscr("xres", [KC, 128, S])
    g.hT = dscr("hT", [KC, 128, S], BF16)
    g.ptm = dscr("ptm", [S, NPTM])
    g.xbcT = dscr("xbcT", [6, 128, S], BF16)
    g.xbctm = dscr("xbctm", [S, 512], BF16)
    g.yT = dscr("yT", [8, 128, S], BF16)
    g.wib = dscr("wib", [8, D, 2 * EXPERT_DIM], BF16)
    g.wob = dscr("wob", [8, EXPERT_DIM, D], BF16)

    with ExitStack() as es:
        p = Prog(nc, es)
        g.p = p
        blk = es.enter_context(nc.Block())
        g.cst = p.sb(es, "cst", [128, NCONST], F32)
        g.cstb = p.sb(es, "cstb", [128, NCONST], BF16)
        g.par = p.sb(es, "par", [128, 512], F32)
        g.mod = p.sb(es, "modv", [128, 48, 2], F32)
        g.AB = p.sb(es, "AB", [128, 4, 8, 2], F32)
        g.cact = p.sb(es, "cact", [128, 8, 2], F32)

        def body(_e):
            setup(g)
            for l in range(layers if stop != "setup" else 0):
                layer(g, l, stop)
                if stop is not None and stop[0] == l:
                    break
            final(g, out, stop is None)
            p.barrier()

        blk.gpsimd(body)
    return nc, p


def load_rows_T(g, es_ps, dst, src2d, n, key):
    p = g.p
    with ExitStack() as es:
        tmp = p.sb(es, "lrt_tmp", [128, 128], F32)
        pst = p.ps(es, "lrt_ps", [128, 128], F32)
        p.dma("sp", tmp[0:n, :], src2d, w=["lrt_tmp"])
        p.tr(pst[:, 0:n], tmp[0:n, :], g.cst[0:n, C_ID:C_ID + n], r=["lrt_tmp", "cst"], w=["lrt_ps"])
        p.cp(dst, pst[:, 0:n], r=["lrt_ps"], w=[key])
        p.barrier()


def setup(g):
    p, nc, I = g.p, g.nc, g.I
    p.dma("sp", g.cst[:, :], I["consts"][:, :], w=["cst"])
    p.cp(g.cstb[:, :], g.cst[:, :], r=["cst"], w=["cstb"])
    with ExitStack() as es:
        t = p.sb(es, "su_t", [128, 16], F32)
        load_rows_T(g, None, t[:, :], I["cc"][:, :], 16, "su_t")
        for i in range(2):
            p.act(g.cact[:, :, i], t[:, i * 8:(i + 1) * 8], AF.Silu, r=["su_t"], w=["cact"])
        p.barrier()
    with ExitStack() as es:
        xin = [p.sb(es, "su_x%d" % i, [128, D], F32) for i in range(2)]
        xo = [p.sb(es, "su_o%d" % i, [128, KC, 128], F32) for i in range(2)]
        pst = [p.ps(es, "su_ps%d" % i, [128, KC, 128], F32) for i in range(2)]
        for t in range(NT):
            b = t % 2
            src = I["ctx"][t * 128:(t + 1) * 128, :] if t < 2 else I["x"][(t - 2) * 128:(t - 1) * 128, :]
            p.dma("sp", xin[b][:, :], src, w=[("xin", b)])
            for c in range(KC):
                p.tr(pst[b][:, c, :], xin[b][:, c * 128:(c + 1) * 128], g.cst[:, C_ID:C_ID + 128],
                     r=[("xin", b), "cst"], w=[("xps", b)])
            p.cp(xo[b][:, :, :], pst[b][:, :, :], r=[("xps", b)], w=[("xo", b)], eng="act" if b else "dve")
            p.dma("sp", g.xres[:, :, t * 128:(t + 1) * 128].rearrange("c p s -> p c s"), xo[b][:, :, :],
                  r=[("xo", b)], w=["xres"])
        p.barrier()


def adaln(g, l):
    p, I = g.p, g.I
    with ExitStack() as es:
        wt = [p.sb(es, "ada_w%d" % i, [128, KC, 768], F32) for i in range(2)]
        adab = p.sb(es, "ada_b", [128, 48], F32)
        gn = p.sb(es, "ada_g", [128, 16], F32)
        mps = p.ps(es, "ada_ps", [128, 48, 2], F32)
        load_rows_T(g, None, adab[:, :], I["ada_b"][l].rearrange("(j q) -> j q", q=128), 48, "adab")
        load_rows_T(g, None, gn[:, 0:8], I["norm1_g"][l].rearrange("(j q) -> j q", q=128), 8, "gn")
        load_rows_T(g, None, gn[:, 8:16], I["norm2_g"][l].rearrange("(j q) -> j q", q=128), 8, "gn")
        wv = I["ada_w"][l].rearrange("(kc q) n -> q kc n", q=128)
        for ob in range(8):
            b = ob % 2
            for kc in range(KC):
                p.dma("sp" if kc % 2 == 0 else "act", wt[b][:, kc, :], wv[:, kc, ob * 768:(ob + 1) * 768], w=[("adaw", b, kc)])
            for jj in range(6):
                j = ob * 6 + jj
                for kc in range(KC):
                    p.mm(mps[:, j, :], wt[b][:, kc, jj * 128:(jj + 1) * 128], g.cact[:, kc, :],
                         start=(kc == 0), stop=(kc == KC - 1), r=[("adaw", b, kc), "cact"], w=["mps"])
        for i in range(2):
            p.tt(g.mod[:, :, i], mps[:, :, i], adab[:, :], ALU.add, r=["mps", "adab"], w=["mod"])
        for i in range(2):
            p.stt(g.AB[:, 0, :, i], g.mod[:, 8:16, i], 1.0, gn[:, 0:8], op0=ALU.add, op1=ALU.mult, r=["mod", "gn"], w=["AB"])
            p.cp(g.AB[:, 1, :, i], g.mod[:, 0:8, i], r=["mod"], w=["AB"])
            p.stt(g.AB[:, 2, :, i], g.mod[:, 32:40, i], 1.0, gn[:, 8:16], op0=ALU.add, op1=ALU.mult, r=["mod", "gn"], w=["AB"])
            p.cp(g.AB[:, 3, :, i], g.mod[:, 24:32, i], r=["mod"], w=["AB"])
        p.barrier()


def norm_tiles(g, which, sink, want32=False):
    p = g.p
    with ExitStack() as es:
        xt = [p.sb(es, "nm_x%d" % i, [128, KC, 512], F32) for i in range(2)]
        sq = p.sb(es, "nm_sq", [128, KC, 512], F32)
        rs = p.sb(es, "nm_rs", [128, 512], F32)
        ht = [p.sb(es, "nm_h%d" % i, [128, KC, 512], BF16) for i in range(2)]
        ssq = p.ps(es, "nm_ps", [128, 512], F32)
        for ti, (s0, n) in enumerate(FT):
            b = ti % 2
            ic = 1 if s0 == 0 else 0
            p.dma("sp", xt[b][:, :, 0:n], g.xres[:, :, s0:s0 + n].rearrange("c p s -> p c s"), r=["xres"], w=[("nmx", b)])
            for c in range(KC):
                p.act(sq[:, c, 0:n], xt[b][:, c, 0:n], AF.Square, r=[("nmx", b)], w=[("nmsq", c)])
                p.mm(ssq[:, 0:n], g.cst[:, C_ONE:C_ONE + 128], sq[:, c, 0:n], start=(c == 0), stop=(c == KC - 1),
                     r=[("nmsq", c), "cst"], w=["nmps"])
            p.act(rs[:, 0:n], ssq[:, 0:n], AF.Ln, bias=EPS, scale=1.0 / D, r=["nmps"], w=["nmrs"])
            p.act(rs[:, 0:n], rs[:, 0:n], AF.Exp, scale=-0.5, r=["nmrs"], w=["nmrs"])
            for c in range(KC):
                p.tt(sq[:, c, 0:n], xt[b][:, c, 0:n], rs[:, 0:n], ALU.mult, r=[("nmx", b), "nmrs"], w=[("nmsq", c)],
                     eng="dve" if c % 2 == 0 else "pool")
                if want32:
                    p.ts(sq[:, c, 0:n], sq[:, c, 0:n], g.AB[:, 2 * which, c, ic:ic + 1], g.AB[:, 2 * which + 1, c, ic:ic + 1],
                         op0=ALU.mult, op1=ALU.add, r=[("nmsq", c), "AB"], w=[("nmsq", c)], eng="dve" if c % 2 == 0 else "pool")
                    p.cp(ht[b][:, c, 0:n], sq[:, c, 0:n], r=[("nmsq", c)], w=[("nmh", b)], eng="dve" if c % 2 == 0 else "pool")
                else:
                    p.ts(ht[b][:, c, 0:n], sq[:, c, 0:n], g.AB[:, 2 * which, c, ic:ic + 1], g.AB[:, 2 * which + 1, c, ic:ic + 1],
                         op0=ALU.mult, op1=ALU.add, r=[("nmsq", c), "AB"], w=[("nmh", b)], eng="dve" if c % 2 == 0 else "pool")
            if want32:
                sink(ti, s0, n, ht[b], ("nmh", b), sq)
            else:
                sink(ti, s0, n, ht[b], ("nmh", b), None)
        p.barrier()


def phase_norm1(g, l):
    p = g.p

    def sink(ti, s0, n, ht, key, h32):
        p.dma("sp", g.hT[:, :, s0:s0 + n].rearrange("c p s -> p c s"), ht[:, :, 0:n], r=[key], w=["hT_d"])

    norm_tiles(g, 0, sink)


def phase_ptm(g, l):
    p, I = g.p, g.I
    wv = I["w_in"][l].rearrange("(kc q) n -> q kc n", q=128)
    with ExitStack() as es:
        w = p.sb(es, "ptm_w", [128, KC, NPTM], BF16)
        ht = [p.sb(es, "ptm_h%d" % i, [128, KC, 512], BF16) for i in range(2)]
        ob = [p.sb(es, "ptm_o%d" % i, [128, NPTM], F32) for i in range(2)]
        ps = [p.ps(es, "ptm_ps%d" % i, [128, 4, 512], F32) for i in range(2)]
        for kc in range(KC):
            p.dma("pool", w[:, kc, 0:256], wv[:, kc, 0:256], w=[("w", kc)])
            p.dma("pool", w[:, kc, 256:NPTM], wv[:, kc, 1024:2312], w=[("w", kc)])
        cols = [(0, 512), (512, 512), (1024, 512), (1536, 8)]
        cnt = 0
        for ti, (s0, n) in enumerate(FT):
            hb = ti % 2
            p.dma("sp", ht[hb][:, :, 0:n], g.hT[:, :, s0:s0 + n].rearrange("c p s -> p c s"), r=["hT_d"], w=[("h", hb)])
            for sub in range(n // 128):
                b = cnt % 2
                cnt += 1
                for kc in range(KC):
                    for bi, (c0, cn) in enumerate(cols):
                        p.mm(ps[b][:, bi, 0:cn], ht[hb][:, kc, sub * 128:(sub + 1) * 128], w[:, kc, c0:c0 + cn],
                             start=(kc == 0), stop=(kc == KC - 1), r=[("h", hb), ("w", kc)], w=[("ps", b, bi)])
                for bi, (c0, cn) in enumerate(cols):
                    p.cp(ob[b][:, c0:c0 + cn], ps[b][:, bi, 0:cn], r=[("ps", b, bi)], w=[("o", b)],
                         eng="act" if bi % 2 else "dve")
                t0 = s0 + sub * 128
                p.dma("sp", g.ptm[t0:t0 + 128, :], ob[b][:, :], r=[("o", b)], w=["ptm_d"])
        p.barrier()


def phase_xbc(g, l):
    p, I = g.p, g.I
    wv = I["w_in"][l].rearrange("(kc q) n -> q kc n", q=128)
    with ExitStack() as es:
        w = p.sb(es, "xb_w", [128, KC, 768], BF16)
        hT = p.sb(es, "xb_h", [128, KC, S], BF16)
        cw = p.sb(es, "xb_cw", [128, 36], F32)
        rawc = p.sb(es, "xb_rc", [128, NCTX + 4], F32)
        rawl = p.sb(es, "xb_rl", [128, NLAT + 4], F32)
        acc = p.sb(es, "xb_acc", [128, S], F32)
        xs = [p.sb(es, "xb_s%d" % i, [128, S], BF16) for i in range(2)]
        tmo = [p.sb(es, "xb_t%d" % i, [128, 4, 128], BF16) for i in range(2)]
        ps = [p.ps(es, "xb_ps%d" % i, [128, 512], F32) for i in range(2)]
        pst = [p.ps(es, "xb_pt%d" % i, [128, 4, 128], F32) for i in range(2)]
        load_rows_T(g, None, cw[:, 0:30], I["ssd_conv_w"][l].rearrange("k (c q) -> (k c) q", q=128), 30, "cw")
        load_rows_T(g, None, cw[:, 30:36], I["ssd_conv_b"][l].rearrange("(c q) -> c q", q=128), 6, "cw")
        for kc in range(KC):
            p.dma("pool", w[:, kc, :], wv[:, kc, 256:1024], w=[("w", kc)])
        for ti, (s0, n) in enumerate(FT):
            p.dma("sp", hT[:, :, s0:s0 + n], g.hT[:, :, s0:s0 + n].rearrange("c p s -> p c s"), r=["hT_d"], w=[("h", ti)])
        p.memset(rawc[:, :], 0.0, w=["rawc"])
        p.memset(rawl[:, :], 0.0, w=["rawl"], eng="pool")
        cnt = 0
        gcnt = 0
        for j in range(6):
            xb = xs[j % 2]
            for ti, (s0, n) in enumerate(FT):
                b = cnt % 2
                cnt += 1
                for kc in range(KC):
                    p.mm(ps[b][:, 0:n], w[:, kc, j * 128:(j + 1) * 128], hT[:, kc, s0:s0 + n], start=(kc == 0),
                         stop=(kc == KC - 1), r=[("w", kc), ("h", ti)], w=[("ps", b)])
                if s0 == 0:
                    p.cp(rawc[:, 2:2 + n], ps[b][:, 0:n], r=[("ps", b)], w=["rawc"], eng="act")
                else:
                    p.cp(rawl[:, 2 + s0 - NCTX:2 + s0 - NCTX + n], ps[b][:, 0:n], r=[("ps", b)], w=["rawl"], eng="act")
            if XBC_LEVEL < 1:
                continue
            pieces = [(rawc, "rawc", 0, 0, NCTX)] + [(rawl, "rawl", q * 1024, NCTX + q * 1024, 1024) for q in range(4)]
            for pi, (rw, rk, o, so, n) in enumerate(pieces):
                eng = "dve"
                a = acc[:, so:so + n]
                p.ts(a, rw[:, o:o + n], cw[:, j:j + 1], r=[rk, "cw"], w=[("acc", pi)], eng=eng)
                for k in range(1, 5):
                    p.stt(a, rw[:, o + k:o + k + n], cw[:, k * 6 + j:k * 6 + j + 1], a, r=[rk, "cw", ("acc", pi)],
                          w=[("acc", pi)], eng=eng)
                p.act(xb[:, so:so + n], a, AF.Silu, bias=cw[:, 30 + j:31 + j], r=[("acc", pi), "cw"], w=[("xs", j % 2, pi)])
            p.dma("sp", g.xbcT[j, :, :], xb[:, :], r=[("xs", j % 2, pi) for pi in range(5)], w=["xbcT_d"])
            if j < 4 and XBC_LEVEL >= 2:
                for t0 in range(0, NT, 4):
                    nt = min(4, NT - t0)
                    b = gcnt % 2
                    gcnt += 1
                    for tt_ in range(nt):
                        t = t0 + tt_
                        p.mm(pst[b][:, tt_, :], xb[:, t * 128:(t + 1) * 128], g.cstb[:, C_ID:C_ID + 128],
                             r=[("xs", j % 2, pi) for pi in range(5)] + ["cstb"], w=[("pt", b)])
                    p.cp(tmo[b][:, 0:nt, :], pst[b][:, 0:nt, :], r=[("pt", b)], w=[("tmo", b)], eng="dve" if b else "act")
                    p.dma("sp", g.xbctm[t0 * 128:(t0 + nt) * 128, j * 128:(j + 1) * 128].rearrange("(t q) c -> q t c", q=128),
                          tmo[b][:, 0:nt, :], r=[("tmo", b)], w=["xbctm_d"])
        p.barrier()


def phase_attn(g, l):
    p, I = g.p, g.I
    with ExitStack() as es:
        qT = p.sb(es, "at_qT", [128, 4, S], BF16)
        kT = p.sb(es, "at_kT", [128, 4, S], BF16)
        vv = p.sb(es, "at_v", [128, NT, 4, 128], BF16)
        yo = p.sb(es, "at_y", [128, 4, S], BF16)
        gbc = p.sb(es, "at_g", [128, 6, 64], F32)
        es_ = p.sb(es, "at_es", [128, 4], F32)
        with ExitStack() as es2:
            pin = [p.sb(es2, "at_in%d" % i, [128, 1024], F32) for i in range(2)]
            rp = [p.sb(es2, "at_rp%d" % i, [128, 64], F32) for i in range(2)]
            sq = p.sb(es2, "at_sq", [128, 384], F32)
            ms = p.sb(es2, "at_ms", [128, 6], F32)
            t1 = p.sb(es2, "at_t1", [128, 12, 2, 16], F32)
            t2 = p.sb(es2, "at_t2", [128, 12, 2, 16], F32)
            t3 = p.sb(es2, "at_t3", [128, 12, 2, 16], F32)
            t4 = p.sb(es2, "at_t4", [128, 12, 2, 16], F32)
            ro = p.sb(es2, "at_ro", [128, 12, 64], F32)
            tb = p.sb(es2, "at_tb", [128, 8, 128], BF16)
            pst = [p.ps(es2, "at_pt%d" % i, [128, 8, 128], F32) for i in range(2)]
            for i in range(4):
                p.dma("sp", gbc[:, i, :], I["qk_norm_g"][l, 0:1, :].partition_broadcast(128), w=["gbc"])
            for i in range(2):
                p.dma("sp", gbc[:, 4 + i, :], I["qk_norm_g"][l, 1:2, :].partition_broadcast(128), w=["gbc"])
            p.dma("sp", es_[:, :], I["swa_sink"][l:l + 1, :].partition_broadcast(128), w=["es"])
            p.act(es_[:, :], es_[:, :], AF.Exp, r=["es"], w=["es"])
            p.memset(vv[:, :, :, 64:128], 1.0, w=["vv1"])
            for t in range(NT):
                b = t % 2
                x = pin[b]
                p.dma("sp", x[:, :], g.ptm[t * 128:(t + 1) * 128, PCQ:PCQ + 1024], r=["ptm_d"], w=[("in", b)])
                p.dma("sp", rp[b][:, :], I["rope"][t * 128:(t + 1) * 128, :], w=[("rp", b)])
                p.act(sq[:, :], x[:, 0:384], AF.Square, r=[("in", b)], w=["sq"])
                p.op("dve", lambda e: e.tensor_reduce(out=ms[:, :], in_=sq[:, :].rearrange("p (h f) -> p h f", f=64), axis=AX.X, op=ALU.add),
                     r=["sq"], w=["ms"])
                p.act(ms[:, :], ms[:, :], AF.Ln, bias=EPS, scale=1.0 / 64, r=["ms"], w=["ms"])
                p.act(ms[:, :], ms[:, :], AF.Exp, scale=-0.5, r=["ms"], w=["ms"])
                xg = x[:, 0:384].rearrange("p (h f) -> p h f", f=64)
                p.tt(xg, xg, ms[:, :].unsqueeze(2).broadcast_to([128, 6, 64]), ALU.mult, r=[("in", b), "ms"], w=[("in", b)])
                p.tt(xg, xg, gbc[:, :, :], ALU.mult, r=[("in", b), "gbc"], w=[("in", b)])
                cosb = rp[b][:, 0:32].rearrange("p (a f) -> p a f", a=2)
                sinb = rp[b][:, 32:64].rearrange("p (a f) -> p a f", a=2)
                cos6 = cosb.unsqueeze(1).broadcast_to([128, 6, 2, 16])
                sin6 = sinb.unsqueeze(1).broadcast_to([128, 6, 2, 16])
                for (c0, h0, nh) in ((0, 0, 6), (512, 6, 6)):
                    xv = x[:, c0:c0 + nh * 64].rearrange("p (h a t f) -> p h a t f", a=2, t=2, f=16)
                    ov = ro[:, h0:h0 + nh, :].rearrange("p h (a t f) -> p h a t f", a=2, t=2, f=16)
                    x1, x2 = xv[:, :, :, 0, :], xv[:, :, :, 1, :]
                    hs = slice(h0, h0 + nh)
                    p.tt(t1[:, hs, :, :], x1, cos6, ALU.mult, r=[("in", b), ("rp", b)], w=[("t1", h0)])
                    p.tt(t2[:, hs, :, :], x2, sin6, ALU.mult, r=[("in", b), ("rp", b)], w=[("t2", h0)], eng="pool")
                    p.tt(ov[:, :, :, 0, :], t1[:, hs, :, :], t2[:, hs, :, :], ALU.subtract, r=[("t1", h0), ("t2", h0)], w=["ro"])
                    p.tt(t3[:, hs, :, :], x2, cos6, ALU.mult, r=[("in", b), ("rp", b)], w=[("t3", h0)], eng="pool")
                    p.tt(t4[:, hs, :, :], x1, sin6, ALU.mult, r=[("in", b), ("rp", b)], w=[("t4", h0)])
                    p.tt(ov[:, :, :, 1, :], t3[:, hs, :, :], t4[:, hs, :, :], ALU.add, r=[("t3", h0), ("t4", h0)], w=["ro"], eng="pool")
                rof = ro[:, :, :].rearrange("p h f -> p (h f)")
                p.cp(tb[:, 0:2, :], rof[:, 0:256].rearrange("p (c f) -> p c f", f=128), r=["ro"], w=["tb"])
                p.cp(tb[:, 4:6, :], rof[:, 384:640].rearrange("p (c f) -> p c f", f=128), r=["ro"], w=["tb"])
                for kv in range(2):
                    for d in range(2):
                        p.cp(tb[:, 2 + kv, d * 64:(d + 1) * 64], ro[:, 4 + kv, :], r=["ro"], w=["tb"], eng="pool")
                        p.cp(tb[:, 6 + kv, d * 64:(d + 1) * 64], ro[:, 10 + kv, :], r=["ro"], w=["tb"], eng="pool")
                p.cp(vv[:, t, 0:2, 0:64], x[:, 384:512].rearrange("p (k f) -> p k f", f=64), r=[("in", b)], w=[("vv", t)], eng="act")
                p.cp(vv[:, t, 2:4, 0:64], x[:, 896:1024].rearrange("p (k f) -> p k f", f=64), r=[("in", b)], w=[("vv", t)], eng="act")
                for c in range(8):
                    p.mm(pst[b][:, c, :], tb[:, c, :], g.cstb[:, C_ID:C_ID + 128], r=["tb", "cstb"], w=[("pt", b)])
                sl = slice(t * 128, (t + 1) * 128)
                ce = "act" if b else "dve"
                p.cp(qT[:, 0:2, sl], pst[b][:, 0:2, :], r=[("pt", b)], w=[("qk", t)], eng=ce)
                p.cp(kT[:, 0:2, sl], pst[b][:, 2:4, :], r=[("pt", b)], w=[("qk", t)], eng=ce)
                p.cp(qT[:, 2:4, sl], pst[b][:, 4:6, :], r=[("pt", b)], w=[("qk", t)], eng=ce)
                p.cp(kT[:, 2:4, sl], pst[b][:, 6:8, :], r=[("pt", b)], w=[("qk", t)], eng=ce)
            p.barrier()
        with ExitStack() as es2:
            pss = [p.ps(es2, "ga_s%d" % i, [128, 512], F32) for i in range(3)]
            pso = [p.ps(es2, "ga_o%d" % i, [128, 512], F32) for i in range(2)]
            pt = [p.sb(es2, "ga_p%d" % i, [128, 512], BF16) for i in range(3)]
            rd = p.sb(es2, "ga_rd", [128, 512], F32)
            cs = 0
            co = 0
            for ti, (s0, n) in enumerate(FT):
                kts = range(2) if s0 == 0 else range(NT)
                for h in range(4):
                    c, hp, kv = h // 2, (h % 2) * 64, h // 2
                    ob = co % 2
                    co += 1
                    for kt in kts:
                        sb_ = cs % 3
                        cs += 1
                        p.mm(pss[sb_][:, 0:n], kT[hp:hp + 64, c, kt * 128:(kt + 1) * 128], qT[hp:hp + 64, c, s0:s0 + n],
                             w=[("gs", sb_)])
                        p.act(pt[sb_][:, 0:n], pss[sb_][:, 0:n], AF.Exp, scale=0.125, r=[("gs", sb_)], w=[("gp", sb_)])
                        p.mm(pso[ob][:, 0:n], vv[:, kt, kv, :], pt[sb_][:, 0:n], start=(kt == kts[0]), stop=(kt == kts[-1]),
                             r=[("gp", sb_)], w=[("go", ob)])
                    p.op("dve", lambda e: e.reciprocal(out=rd[0:64, 0:n], in_=pso[ob][64:128, 0:n]), r=[("go", ob)], w=["rd"])
                    p.tt(yo[hp:hp + 64, c, s0:s0 + n], pso[ob][0:64, 0:n], rd[0:64, 0:n], ALU.mult, r=[("go", ob), "rd"], w=[("yo", c)])
            p.barrier()
        with ExitStack() as es2:
            pss = [p.ps(es2, "wa_s%d" % i, [128, 8, 128], F32) for i in range(2)]
            pso = [p.ps(es2, "wa_o%d" % i, [128, 4, 128], F32) for i in range(2)]
            pt = [p.sb(es2, "wa_p%d" % i, [128, 8, 128], BF16) for i in range(2)]
            dt_ = p.sb(es2, "wa_d", [128, 4, 128], F32)
            rd = p.sb(es2, "wa_rd", [128, 4, 128], F32)
            co = 0
            for qb in range(NT):
                kl = [(0, None), (1, None)]
                if qb >= 2:
                    if qb - 1 >= 2:
                        kl.append((qb - 1, C_L))
                    kl.append((qb, None))
                    if qb + 1 < NT:
                        kl.append((qb + 1, C_U))
                nk = len(kl)
                qs = slice(qb * 128, (qb + 1) * 128)
                for h in range(4):
                    c, hp = 2 + h // 2, (h % 2) * 64
                    ob = co % 2
                    co += 1
                    for ki, (kt, msk) in enumerate(kl):
                        p.mm(pss[ob][:, ki, :], kT[hp:hp + 64, c, kt * 128:(kt + 1) * 128], qT[hp:hp + 64, c, qs], w=[("ws", ob)])
                    p.act(pt[ob][:, 0:nk, :], pss[ob][:, 0:nk, :], AF.Exp, scale=0.125, r=[("ws", ob)], w=[("wp", ob)])
                    for ki, (kt, msk) in enumerate(kl):
                        if msk is not None:
                            p.tt(pt[ob][:, ki, :], pt[ob][:, ki, :], g.cstb[:, msk:msk + 128], ALU.mult, r=[("wp", ob), "cstb"],
                                 w=[("wp", ob)], eng="pool")
                    for ki, (kt, msk) in enumerate(kl):
                        p.mm(pso[ob][:, 0, :], vv[:, kt, 2 + h // 2, :], pt[ob][:, ki, :], start=(ki == 0), stop=(ki == nk - 1),
                             r=[("wp", ob)], w=[("wo", ob)])
                    p.ts(dt_[64:128, 0, :], pso[ob][64:128, 0, :], es_[64:128, h:h + 1], op0=ALU.add, r=[("wo", ob), "es"], w=["wd"])
                    p.op("dve", lambda e: e.reciprocal(out=rd[0:64, 0, :], in_=dt_[64:128, 0, :]), r=["wd"], w=["wrd"])
                    p.tt(yo[hp:hp + 64, c, qs], pso[ob][0:64, 0, :], rd[0:64, 0, :], ALU.mult, r=[("wo", ob), "wrd"], w=[("yo", c)])
            p.barrier()
        for c in range(4):
            p.dma("sp", g.yT[4 + c, :, :], yo[:, c, :], w=["yT_d"])
        p.barrier()


def phase_ssd(g, l):
    p, I = g.p, g.I
    NC8 = NT * 8
    with ExitStack() as es:
        xsB = p.sb(es, "sd_xsB", [128, NT, 512], BF16)
        zdt = p.sb(es, "sd_zdt", [128, NT, 264], F32)
        ysum = p.sb(es, "sd_y", [128, NT, 256], F32)
        prm = p.sb(es, "sd_prm", [128, 20], F32)
        ng = p.sb(es, "sd_ng", [128, 256], F32)
        for t0 in range(0, NT, 4):
            nt = min(4, NT - t0)
            ts_ = slice(t0, t0 + nt)
            p.dma("sp", xsB[:, ts_, :], g.xbctm[t0 * 128:(t0 + nt) * 128, :].rearrange("(t q) c -> q t c", q=128),
                  r=["xbctm_d"], w=["xsB"])
            p.dma("act", zdt[:, ts_, :], g.ptm[t0 * 128:(t0 + nt) * 128, 0:264].rearrange("(t q) c -> q t c", q=128),
                  r=["ptm_d"], w=["zdt"])
        p.dma("sp", prm[:, 0:8], I["ssd_dt_bias"][l:l + 1].rearrange("o d h -> o (d h)").partition_broadcast(128), w=["prm"])
        p.dma("sp", prm[:, 8:16], I["ssd_a_log"][l:l + 1].rearrange("o d h -> o (d h)").partition_broadcast(128), w=["prm"])
        p.dma("sp", prm[:, 16:20], I["ssd_d"][l:l + 1, :].partition_broadcast(128), w=["prm"])
        p.dma("sp", ng[:, :], I["ssd_norm_g"][l:l + 1, :].partition_broadcast(128), w=["ng"])
        p.act(prm[:, 8:16], prm[:, 8:16], AF.Exp, r=["prm"], w=["prm"])
        p.ts(prm[:, 8:16], prm[:, 8:16], -1.0, r=["prm"], w=["prm"])
        with ExitStack() as es1:
            BCT = p.sb(es1, "sd_bct", [128, 4, S], BF16)
            dt = p.sb(es1, "sd_dt", [128, NT, 8], F32)
            la = p.sb(es1, "sd_la", [128, NT, 8], F32)
            cum = p.sb(es1, "sd_cum", [128, NT, 8], F32)
            tot = p.sb(es1, "sd_tot", [128, NT, 8], F32)
            eoff = p.sb(es1, "sd_eoff", [128, NT, 8], F32)
            dtdte = p.sb(es1, "sd_dte", [128, NT, 8], F32)
            etot = p.sb(es1, "sd_etot", [128, NT, 8], F32)
            ncum = p.sb(es1, "sd_ncum", [128, NT, 8], F32)
            nm4 = p.sb(es1, "sd_nm4", [128, 2, 4, 128], F32)
            laU = [p.sb(es1, "sd_laU%d" % i, [128, 4, 128], F32) for i in range(2)]
            dec = p.sb(es1, "sd_dec", [128, 4, 128], F32)
            WT = p.sb(es1, "sd_WT", [128, 4, 128], BF16)
            xdt = p.sb(es1, "sd_xdt", [128, 4, 64], BF16)
            xdte = p.sb(es1, "sd_xdte", [128, 4, 64], BF16)
            ydsb = p.sb(es1, "sd_yd", [128, 256], F32)
            Sst = p.sb(es1, "sd_S", [128, 4, 64], F32)
            Sb = p.sb(es1, "sd_Sb", [128, 4, 64], BF16)
            pc = p.ps(es1, "sd_pc", [128, NC8], F32)
            dps = [p.ps(es1, "sd_dps%d" % i, [128, 4, 128], F32) for i in range(2)]
            gps = p.ps(es1, "sd_gps", [128, 2, 128], F32)
            ydp = p.ps(es1, "sd_ydp", [128, 4, 64], F32)
            yop = p.ps(es1, "sd_yop", [128, 4, 64], F32)
            stp = p.ps(es1, "sd_stp", [128, 4, 64], F32)
            for c in range(4):
                p.dma("sp", BCT[:, c, :], g.xbcT[2 + c, :, :], r=["xbcT_d"], w=["BCT"])
            for d in range(2):
                for h in range(4):
                    nm = C_NMF if d == 0 else C_NMB
                    p.cp(nm4[:, d, h, :], g.cst[:, nm:nm + 128], r=["cst"], w=["nm4"], eng="pool")
            bc8 = lambda ap: ap.unsqueeze(1).broadcast_to([128, NT, 8])
            p.tt(dt[:, :, :], zdt[:, :, 256:264], bc8(prm[:, 0:8]), ALU.add, r=["zdt", "prm"], w=["dt"])
            p.act(dt[:, :, :], dt[:, :, :], AF.Exp, r=["dt"], w=["dt"])
            p.act(dt[:, :, :], dt[:, :, :], AF.Ln, bias=1.0, r=["dt"], w=["dt"])
            p.tt(la[:, :, :], dt[:, :, :], bc8(prm[:, 8:16]), ALU.mult, r=["dt", "prm"], w=["la"])
            laf = la[:, :, :].rearrange("p t c -> p (t c)")
            pc3 = pc[:, :].rearrange("p (t c) -> p t c", c=8)
            p.mm(pc[:, :], g.cst[:, C_U:C_U + 128], laf, r=["la", "cst"], w=["pc"])
            p.cp(cum[:, :, 0:4], pc3[:, :, 0:4], r=["pc"], w=["cum"])
            p.mm(pc[:, :], g.cst[:, C_L:C_L + 128], laf, r=["la", "cst"], w=["pc"])
            p.cp(cum[:, :, 4:8], pc3[:, :, 4:8], r=["pc"], w=["cum"])
            p.mm(pc[:, :], g.cst[:, C_ONE:C_ONE + 128], laf, r=["la", "cst"], w=["pc"])
            p.cp(tot[:, :, :], pc3, r=["pc"], w=["tot"])
            p.act(eoff[:, :, :], cum[:, :, :], AF.Exp, r=["cum"], w=["eoff"])
            p.act(etot[:, :, :], tot[:, :, :], AF.Exp, r=["tot"], w=["etot"])
            p.tt(dtdte[:, :, :], tot[:, :, :], cum[:, :, :], ALU.subtract, r=["tot", "cum"], w=["dtdte"])
            p.act(dtdte[:, :, :], dtdte[:, :, :], AF.Exp, r=["dtdte"], w=["dtdte"])
            p.tt(dtdte[:, :, :], dtdte[:, :, :], dt[:, :, :], ALU.mult, r=["dtdte", "dt"], w=["dtdte"])
            p.ts(ncum[:, :, :], cum[:, :, :], -1.0, r=["cum"], w=["ncum"])
            cnt = 0
            for d in range(min(2, SSD_LEVEL)):
                order = list(range(NT)) if d == 0 else [1, 0] + list(range(NT - 1, 1, -1))
                tri = C_U if d == 0 else C_L
                p.memset(Sst[:, :, :], 0.0, w=["S"])
                p.memset(Sb[:, :, :], 0.0, w=["Sb"])
                for t in order:
                    b = cnt % 2
                    cnt += 1
                    sl = slice(t * 128, (t + 1) * 128)
                    for h in range(4):
                        p.ts(laU[b][:, h, :], g.cst[:, tri:tri + 128], la[:, t, d * 4 + h:d * 4 + h + 1], r=["la", "cst"],
                             w=[("laU", b)], eng="pool" if h % 2 else "dve")
                    dflat = dps[b][:, :, :].rearrange("p h l -> p (h l)")
                    p.mm(dflat, g.cst[:, C_ONE:C_ONE + 128], laU[b][:, :, :].rearrange("p h l -> p (h l)"), start=True, stop=False,
                         r=[("laU", b), "cst"], w=[("dps", b)])
                    p.mm(dflat, g.cst[:, C_ID:C_ID + 128], nm4[:, d, :, :].rearrange("p h l -> p (h l)"), start=False, stop=True,
                         r=["nm4", "cst"], w=[("dps", b)])
                    for h in range(4):
                        p.act(dec[:, h, :], dps[b][:, h, :], AF.Exp, bias=ncum[:, t, d * 4 + h:d * 4 + h + 1],
                              r=[("dps", b), "ncum"], w=[("dec", h)])
                    for gq in range(2):
                        p.mm(gps[:, gq, :], BCT[:, gq, sl], BCT[:, 2 + gq, sl], r=["BCT"], w=[("gps", gq)])
                    for h in range(4):
                        p.tt(WT[:, h, :], dec[:, h, :], gps[:, h // 2, :], ALU.mult, r=[("dec", h), ("gps", h // 2)], w=[("WT", h)])
                    xs4 = xsB[:, t, 0:256].rearrange("p (h f) -> p h f", f=64)
                    p.tt(xdt[:, :, :], xs4, dt[:, t, d * 4:d * 4 + 4].unsqueeze(2).broadcast_to([128, 4, 64]), ALU.mult,
                         r=["xsB", "dt"], w=["xdt"], eng="pool")
                    p.tt(xdte[:, :, :], xs4, dtdte[:, t, d * 4:d * 4 + 4].unsqueeze(2).broadcast_to([128, 4, 64]), ALU.mult,
                         r=["xsB", "dtdte"], w=["xdte"], eng="pool")
                    for h in range(4):
                        p.mm(ydp[:, h, :], WT[:, h, :], xdt[:, h, :], r=[("WT", h), "xdt"], w=["ydp"])
                    for h in range(4):
                        p.mm(yop[:, h, :], BCT[:, 2 + h // 2, sl], Sb[:, h, :], r=["BCT", "Sb"], w=["yop"])
                    p.cp(ydsb[:, :], ydp[:, :, :].rearrange("p h f -> p (h f)"), r=["ydp"], w=["ydsb"], eng="act")
                    if d == 1:
                        p.tt(ydsb[:, :], ydsb[:, :], ysum[:, t, :], ALU.add, r=["ydsb", ("ysum", t)], w=["ydsb"])
                    for h in range(4):
                        p.stt(ysum[:, t, h * 64:(h + 1) * 64], yop[:, h, :], eoff[:, t, d * 4 + h:d * 4 + h + 1], ydsb[:, h * 64:(h + 1) * 64],
                              r=["yop", "eoff", "ydsb"], w=[("ysum", t)])
                    for h in range(4):
                        p.mm(stp[:, h, :], xsB[:, t, 256 + (h // 2) * 128:256 + (h // 2 + 1) * 128], xdte[:, h, :], r=["xsB", "xdte"], w=["stp"])
                    for h in range(4):
                        p.stt(Sst[:, h, :], Sst[:, h, :], etot[:, t, d * 4 + h:d * 4 + h + 1], stp[:, h, :], r=["S", "etot", "stp"], w=["S"])
                    p.cp(Sb[:, :, :], Sst[:, :, :], r=["S"], w=["Sb"], eng="act")
            p.barrier()
        with ExitStack() as es1:
          if SSD_LEVEL >= 3:
              sq = p.sb(es1, "sd_sq", [128, 256], F32)
              ms = p.sb(es1, "sd_ms", [128, NT], F32)
              yaT = p.sb(es1, "sd_yaT", [128, 2, S], BF16)
              yab = p.sb(es1, "sd_yab", [128, NT, 256], BF16)
              pst = [p.ps(es1, "sd_pt%d" % i, [128, 4, 128], F32) for i in range(2)]
              for t in range(NT):
                  e1 = "dve"
                  for h in range(4):
                      hs = slice(h * 64, (h + 1) * 64)
                      p.stt(ysum[:, t, hs], xsB[:, t, hs], prm[:, 16 + h:17 + h], ysum[:, t, hs], r=["xsB", "prm", ("ys", t)], w=[("ys", t)])
                  p.act(zdt[:, t, 0:256], zdt[:, t, 0:256], AF.Silu, r=[("z", t)], w=[("z", t)])
                  p.tt(ysum[:, t, :], ysum[:, t, :], zdt[:, t, 0:256], ALU.mult, r=[("z", t), ("ys", t)], w=[("ys", t)])
                  p.act(sq[:, :], ysum[:, t, :], AF.Square, r=[("ys", t)], w=["sq"])
                  p.op("dve", lambda e: e.tensor_reduce(out=ms[:, t:t + 1], in_=sq[:, :], axis=AX.X, op=ALU.add), r=["sq"], w=["ms"])
              p.act(ms[:, :], ms[:, :], AF.Ln, bias=EPS, scale=1.0 / 256, r=["ms"], w=["ms"])
              p.act(ms[:, :], ms[:, :], AF.Exp, scale=-0.5, r=["ms"], w=["ms"])
              for t in range(NT):
                  p.stt(yab[:, t, :], ysum[:, t, :], ms[:, t:t + 1], ng[:, :], op0=ALU.mult, op1=ALU.mult, r=[("ys", t), "ms", "ng"], w=["yab"])
              k = 0
              for c in (range(2) if SSD_LEVEL >= 4 else []):
                  for t0 in range(0, NT, 4):
                      nt = min(4, NT - t0)
                      b = k % 2
                      k += 1
                      for i in range(nt):
                          p.mm(pst[b][:, i, :], yab[:, t0 + i, c * 128:(c + 1) * 128], g.cstb[:, C_ID:C_ID + 128],
                               r=["yab", "cstb"], w=[("pt", b)])
                      p.cp(yaT[:, c, t0 * 128:(t0 + nt) * 128].rearrange("p (t s) -> p t s", t=nt), pst[b][:, 0:nt, :],
                           r=[("pt", b)], w=[("yaT", c)], eng="act" if b else "dve")
              for c in range(2):
                  p.dma("sp", g.yT[c, :, :], yaT[:, c, :], r=[("yaT", c)], w=["yT_d"])
              p.barrier()


def phase_s5(g, l):
    p, I = g.p, g.I
    TWO_PI = 2.0 * np.pi
    rt = lambda c: (1 - c) if c < 2 else (35 - c)
    with ExitStack() as es:
        u_tm = p.sb(es, "s5_utm", [128, NT, 256], F32)
        useq = p.sb(es, "s5_useq", [128, 2, S], F32)
        ysum = p.sb(es, "s5_ysum", [128, 2, S], F32)
        dcol = p.sb(es, "s5_d", [128, 2], F32)
        zero8 = p.sb(es, "s5_z8", [128, 8], F32)
        load_rows_T(g, None, dcol[:, :], I["s5_d"][l].rearrange("(c q) -> c q", q=128), 2, "dcol")
        p.memset(zero8[:, :], 0.0, w=["zero8"])
        for t0 in range(0, NT, 4):
            nt = min(4, NT - t0)
            p.dma("sp", u_tm[:, t0:t0 + nt, :], g.ptm[t0 * 128:(t0 + nt) * 128, PU:PU + 256].rearrange("(t q) c -> q t c", q=128),
                  r=["ptm_d"], w=["u_tm"])
        with ExitStack() as es1:
            bre = p.ps(es1, "s5_bre", [128, 8, 128], F32)
            bim = p.ps(es1, "s5_bim", [128, 8, 128], F32)
            pst = [bre, bim]
            ypl = [p.ps(es1, "s5_yp%d" % i, [128, 128], F32) for i in range(2)]
            ytl = [p.ps(es1, "s5_ytm%d" % i, [128, 128], F32) for i in range(2)]
            yp = ypl[0]
            prm = p.sb(es1, "s5_prm", [128, 16, 8], F32)
            T16 = p.sb(es1, "s5_T16", [128, 2, 16], F32)
            st16 = p.sb(es1, "s5_st16", [128, 16], F32)
            cosT = p.sb(es1, "s5_cos", [128, 8, 128], F32)
            sinT = p.sb(es1, "s5_sin", [128, 8, 128], F32)
            rhoT = p.sb(es1, "s5_rhoT", [128, 8, 128], F32)
            Mre = p.sb(es1, "s5_Mre", [128, 8, 128], F32)
            Mim = p.sb(es1, "s5_Mim", [128, 8, 128], F32)
            Bre = p.sb(es1, "s5_Bre", [128, 8, 128], F32)
            Bim = p.sb(es1, "s5_Bim", [128, 8, 128], F32)
            Cre = p.sb(es1, "s5_Cre", [128, 8, 128], F32)
            Cim = p.sb(es1, "s5_Cim", [128, 8, 128], F32)
            tA = p.sb(es1, "s5_tA", [128, 8, 128], F32)
            tB = p.sb(es1, "s5_tB", [128, 8, 128], F32)
            tC = p.sb(es1, "s5_tC", [128, 8, 128], F32)
            tD = p.sb(es1, "s5_tD", [128, 8, 128], F32)
            vre = p.sb(es1, "s5_vre", [128, 8, 128], F32)
            vim = p.sb(es1, "s5_vim", [128, 8, 128], F32)
            wre = p.sb(es1, "s5_wre", [128, 8, 128], F32)
            wim = p.sb(es1, "s5_wim", [128, 8, 128], F32)
            c8 = p.sb(es1, "s5_c8", [128, 2, 8], F32)
            rr, kf = wre, vre
            ki = wim[:, :, :].bitcast(I32)
            xre = [p.sb(es1, "s5_xre%d" % i, [128, 8, 128], F32) for i in range(2)]
            xim = [p.sb(es1, "s5_xim%d" % i, [128, 8, 128], F32) for i in range(2)]
            ysb = [p.sb(es1, "s5_ysb%d" % i, [128, 128], F32) for i in range(2)]
            ident = g.cst[:, C_ID:C_ID + 128]
            LR, LI, ST, RHO, THP, CO, SI, ABR, ABI, DEN, FRE, FIM, TMP, TMP2 = range(14)
            for d in range(2):
                k = 0
                for c in (range(2) if S5_LEVEL >= 0 else []):
                    for t0 in range(0, NT, 4):
                        nt = min(4, NT - t0)
                        b = k % 2
                        k += 1
                        for i in range(nt):
                            src = t0 + i if d == 0 else rt(t0 + i)
                            rhs = ident if d == 0 else g.cst[:, C_J:C_J + 128]
                            p.mm(pst[b][:, i, :], u_tm[:, src, c * 128:(c + 1) * 128], rhs, r=["u_tm", "cst"], w=[("pu", b)])
                        dst = useq[:, c, t0 * 128:(t0 + nt) * 128].rearrange("p (t s) -> p t s", t=nt)
                        p.cp(dst, pst[b][:, 0:nt, :], r=[("pu", b)], w=["useq"], eng="act")
                        if d == 0:
                            yd_ = ysum[:, c, t0 * 128:(t0 + nt) * 128].rearrange("p (t s) -> p t s", t=nt)
                            p.ts(yd_, dst, dcol[:, c:c + 1], r=["useq", "dcol"], w=["ysum"])
                if S5_LEVEL < 1:
                    continue
                for which, nm in ((0, "s5_lam_re"), (1, "s5_lam_im")):
                    p.dma("sp", tA[0:16, 0, 0:64], I[nm][l, d, :, :], w=["tA"])
                    p.tr(yp[0:64, 0:16], tA[0:16, 0, 0:64], g.cst[0:16, C_ID:C_ID + 16], r=["tA", "cst"], w=["yp"])
                    p.cp(T16[0:64, which, :], yp[0:64, 0:16], r=["yp"], w=["T16"])
                    tv = T16[0:64, which, :].rearrange("p (gb gl) -> p gl gb", gl=2)
                    p.cp(prm[0:64, which, :], tv[:, 0, :], r=["T16"], w=["prm"])
                    p.cp(prm[64:128, which, :], tv[:, 1, :], r=["T16"], w=["prm"])
                p.dma("sp", st16[:, :], I["s5_log_step"][l, d:d + 1, :].partition_broadcast(128), w=["st16"])
                p.act(st16[:, :], st16[:, :], AF.Exp, r=["st16"], w=["st16"])
                sv = st16[:, :].rearrange("p (gb gl) -> p gl gb", gl=2)
                p.cp(prm[0:64, ST, :], sv[0:64, 0, :], r=["st16"], w=["prm"])
                p.cp(prm[64:128, ST, :], sv[64:128, 1, :], r=["st16"], w=["prm"])
                P = lambda i: prm[:, i, :]
                p.ts(P(LR), P(LR), -1e-4, op0=ALU.min, r=["prm"], w=["prm"])
                p.tt(P(TMP), P(LR), P(ST), ALU.mult, r=["prm"], w=["prm"])
                p.act(P(RHO), P(TMP), AF.Exp, r=["prm"], w=["prm"])
                p.tt(P(THP), P(LI), P(ST), ALU.mult, r=["prm"], w=["prm"])
                p.ts(P(THP), P(THP), 1.0 / TWO_PI, r=["prm"], w=["prm"])
                if S5_LEVEL < 2:
                    continue
                for gb in range(8):
                    p.ts(rr[:, gb, :], g.cst[:, C_RAMP:C_RAMP + 128], prm[:, THP, gb:gb + 1], r=["prm", "cst"], w=["rr"],
                         eng="pool" if gb % 2 else "dve")
                for (tab, shift) in ((sinT, 0.0), (cosT, 0.25)):
                    for hf in range(2):
                        hs = slice(hf * 4, hf * 4 + 4)
                        if shift:
                            p.ts(kf[:, hs, :], rr[:, hs, :], shift, op0=ALU.add, r=["rr"], w=["kf"])
                            src = kf
                        else:
                            src = rr
                        p.cp(ki[:, hs, :], src[:, hs, :], r=["rr", "kf"], w=["ki"])
                        p.cp(tab[:, hs, :], ki[:, hs, :], r=["ki"], w=["tab"])
                        p.tt(tab[:, hs, :], src[:, hs, :], tab[:, hs, :], ALU.subtract, r=["tab", "rr", "kf"], w=["tab"])
                        p.act(tab[:, hs, :], tab[:, hs, :], AF.Sin, scale=TWO_PI, r=["tab"], w=["tab"])
                p.cp(P(CO), cosT[:, :, 0], r=["tab"], w=["prm"])
                p.cp(P(SI), sinT[:, :, 0], r=["tab"], w=["prm"])
                p.tt(P(ABR), P(RHO), P(CO), ALU.mult, r=["prm"], w=["prm"])
                p.tt(P(ABI), P(RHO), P(SI), ALU.mult, r=["prm"], w=["prm"])
                p.tt(P(DEN), P(LR), P(LR), ALU.mult, r=["prm"], w=["prm"])
                p.tt(P(TMP), P(LI), P(LI), ALU.mult, r=["prm"], w=["prm"])
                p.tt(P(DEN), P(DEN), P(TMP), ALU.add, r=["prm"], w=["prm"])
                p.op("dve", lambda e: e.reciprocal(out=P(DEN), in_=P(DEN)), r=["prm"], w=["prm"])
                p.ts(P(ABR), P(ABR), -1.0, op0=ALU.add, r=["prm"], w=["prm"])
                p.tt(P(TMP), P(ABR), P(LR), ALU.mult, r=["prm"], w=["prm"])
                p.tt(P(TMP2), P(ABI), P(LI), ALU.mult, r=["prm"], w=["prm"])
                p.tt(P(FRE), P(TMP), P(TMP2), ALU.add, r=["prm"], w=["prm"])
                p.tt(P(FRE), P(FRE), P(DEN), ALU.mult, r=["prm"], w=["prm"])
                p.tt(P(TMP), P(ABI), P(LR), ALU.mult, r=["prm"], w=["prm"])
                p.tt(P(TMP2), P(ABR), P(LI), ALU.mult, r=["prm"], w=["prm"])
                p.tt(P(FIM), P(TMP), P(TMP2), ALU.subtract, r=["prm"], w=["prm"])
                p.tt(P(FIM), P(FIM), P(DEN), ALU.mult, r=["prm"], w=["prm"])
                if S5_LEVEL < 3:
                    continue
                for m_ in (Mre, Mim):
                    for hf in range(2):
                        p.memset(m_[:, hf * 4:hf * 4 + 4, :], 0.0, w=["M"], eng="pool" if hf else "dve")
                for gi in range(16):
                    gb, gl, gic = gi // 2, gi % 2, gi % 8
                    p.dma("sp", Mre[gl * 64:(gl + 1) * 64, gb, gic * 16:(gic + 1) * 16], I["s5_b_re"][l, d, gi, :, :], r=[], w=["M"])
                    p.dma("act", Mim[gl * 64:(gl + 1) * 64, gb, gic * 16:(gic + 1) * 16], I["s5_b_im"][l, d, gi, :, :], r=[], w=["M"])
                for gb in range(8):
                    fr, fi = prm[:, FRE, gb:gb + 1], prm[:, FIM, gb:gb + 1]
                    p.ts(tA[:, 0, :], Mim[:, gb, :], fi, r=["M", "prm"], w=["tA"])
                    p.stt(Bre[:, gb, :], Mre[:, gb, :], fr, tA[:, 0, :], op0=ALU.mult, op1=ALU.subtract, r=["M", "prm", "tA"], w=["B0"])
                    p.ts(tB[:, 0, :], Mre[:, gb, :], fi, r=["M", "prm"], w=["tB"])
                    p.stt(Bim[:, gb, :], Mim[:, gb, :], fr, tB[:, 0, :], op0=ALU.mult, op1=ALU.add, r=["M", "prm", "tB"], w=["B0"])
                for (src, dst, key) in ((Bre, Mre, "BT"), (Bim, Mim, "BT")):
                    for hf in range(2):
                        b = hf
                        for j in range(4):
                            p.tr(pst[b][:, j, :], src[:, hf * 4 + j, :], ident, r=["B0", "cst"], w=[("pu", b)])
                        p.cp(dst[:, hf * 4:hf * 4 + 4, :], pst[b][:, 0:4, :], r=[("pu", b)], w=[key], eng="act")
                BTre, BTim = Mre, Mim
                for m_ in (Bre, Bim):
                    for hf in range(2):
                        p.memset(m_[:, hf * 4:hf * 4 + 4, :], 0.0, w=["B0"], eng="pool" if hf else "dve")
                for gi in range(16):
                    gb, gl, gic = gi // 2, gi % 2, gi % 8
                    p.dma("sp", Bre[gic * 16:(gic + 1) * 16, gb, gl * 64:(gl + 1) * 64], I["s5_c_re"][l, d, gi, :, :], r=[], w=["B0"])
                    p.dma("act", Bim[gic * 16:(gic + 1) * 16, gb, gl * 64:(gl + 1) * 64], I["s5_c_im"][l, d, gi, :, :], r=[], w=["B0"])
                for (src, dst, neg) in ((Bre, Cre, False), (Bim, Cim, True)):
                    for hf in range(2):
                        b = hf
                        for j in range(4):
                            p.tr(pst[b][:, j, :], src[:, hf * 4 + j, :], ident, r=["B0", "cst"], w=[("pu", b)])
                        if neg:
                            p.ts(dst[:, hf * 4:hf * 4 + 4, :], pst[b][:, 0:4, :], -1.0, r=[("pu", b)], w=["CT"])
                        else:
                            p.cp(dst[:, hf * 4:hf * 4 + 4, :], pst[b][:, 0:4, :], r=[("pu", b)], w=["CT"], eng="act")
                if S5_LEVEL < 4:
                    continue
                for gb in range(8):
                    p.ts(rhoT[:, gb, :], g.cst[:, C_ONE:C_ONE + 128], prm[:, RHO, gb:gb + 1], r=["prm", "cst"], w=["rhoT"],
                         eng="pool" if gb % 2 else "dve")
                p.memset(rhoT[:, :, 0:1], 0.0, w=["rhoT"])
                p.barrier()
                fl = lambda t_: t_[:, :, :].rearrange("p g s -> p (g s)")
                for c in range(NT):
                    cur, prv = c % 2, (c + 1) % 2
                    ps_ = slice(c * 128, (c + 1) * 128)
                    for gb in range(8):
                        p.mm(bre[:, gb, :], BTre[:, gb, :], useq[:, gb // 4, ps_], r=["BT", "useq"], w=["bre"])
                    for gb in range(8):
                        p.mm(bim[:, gb, :], BTim[:, gb, :], useq[:, gb // 4, ps_], r=["BT", "useq"], w=["bim"])
                    p.tt(tA[:, :, :], bre[:, :, :], cosT[:, :, :], ALU.mult, r=["bre", "tab"], w=["tA"])
                    p.tt(tB[:, :, :], bim[:, :, :], sinT[:, :, :], ALU.mult, r=["bim", "tab"], w=["tB"])
                    p.tt(vre[:, :, :], tA[:, :, :], tB[:, :, :], ALU.add, r=["tA", "tB"], w=["vre"], eng="pool")
                    p.tt(tC[:, :, :], bim[:, :, :], cosT[:, :, :], ALU.mult, r=["bim", "tab"], w=["tC"])
                    p.tt(tD[:, :, :], bre[:, :, :], sinT[:, :, :], ALU.mult, r=["bre", "tab"], w=["tD"])
                    p.tt(vim[:, :, :], tC[:, :, :], tD[:, :, :], ALU.subtract, r=["tC", "tD"], w=["vim"], eng="pool")
                    if c > 0:
                        p.tt(c8[:, 0, :], xre[prv][:, :, 127], prm[:, RHO, :], ALU.mult, r=[("x", prv), "prm"], w=["c8"], eng="pool")
                        p.tt(vre[:, :, 0], vre[:, :, 0], c8[:, 0, :], ALU.add, r=["c8", "vre"], w=["vre"], eng="pool")
                        p.tt(c8[:, 1, :], xim[prv][:, :, 127], prm[:, RHO, :], ALU.mult, r=[("x", prv), "prm"], w=["c8b"], eng="pool")
                        p.tt(vim[:, :, 0], vim[:, :, 0], c8[:, 1, :], ALU.add, r=["c8b", "vim"], w=["vim"], eng="pool")
                    p.op("dve", lambda e: e.tensor_tensor_scan(out=fl(wre), data0=fl(rhoT), data1=fl(vre), initial=0.0,
                                                               op0=ALU.mult, op1=ALU.add), r=["vre", "rhoT"], w=["wre"])
                    p.op("dve", lambda e: e.tensor_tensor_scan(out=fl(wim), data0=fl(rhoT), data1=fl(vim), initial=0.0,
                                                               op0=ALU.mult, op1=ALU.add), r=["vim", "rhoT"], w=["wim"])
                    p.tt(tA[:, :, :], wre[:, :, :], cosT[:, :, :], ALU.mult, r=["wre", "tab"], w=["tA"], eng="pool")
                    p.tt(tB[:, :, :], wim[:, :, :], sinT[:, :, :], ALU.mult, r=["wim", "tab"], w=["tB"], eng="pool")
                    p.tt(xre[cur][:, :, :], tA[:, :, :], tB[:, :, :], ALU.subtract, r=["tA", "tB"], w=[("x", cur)], eng="pool")
                    p.tt(tC[:, :, :], wre[:, :, :], sinT[:, :, :], ALU.mult, r=["wre", "tab"], w=["tC"])
                    p.tt(tD[:, :, :], wim[:, :, :], cosT[:, :, :], ALU.mult, r=["wim", "tab"], w=["tD"])
                    p.tt(xim[cur][:, :, :], tC[:, :, :], tD[:, :, :], ALU.add, r=["tC", "tD"], w=[("x", cur)])
                    for hf in range(2):
                        if d == 0:
                            ypt = ypl[hf]
                            for j in range(4):
                                gb = hf * 4 + j
                                p.mm(ypt[:, :], Cre[:, gb, :], xre[cur][:, gb, :], start=(j == 0), stop=False, r=["CT", ("x", cur)], w=[("yp", hf)])
                                p.mm(ypt[:, :], Cim[:, gb, :], xim[cur][:, gb, :], start=False, stop=(j == 3), r=["CT", ("x", cur)], w=[("yp", hf)])
                            p.tt(ysum[:, hf, ps_], ypt[:, :], ysum[:, hf, ps_], ALU.add, r=[("yp", hf), "ysum"], w=["ysum"])
                        else:
                            ytm, ypt = ytl[hf], ypl[hf]
                            for j in range(4):
                                gb = hf * 4 + j
                                p.mm(ytm[:, :], xre[cur][:, gb, :], Cre[:, gb, :], start=(j == 0), stop=False, r=["CT", ("x", cur)], w=[("ytm", hf)])
                                p.mm(ytm[:, :], xim[cur][:, gb, :], Cim[:, gb, :], start=False, stop=(j == 3), r=["CT", ("x", cur)], w=[("ytm", hf)])
                            p.cp(ysb[hf][:, :], ytm[:, :], r=[("ytm", hf)], w=[("ysb", hf)], eng="act")
                            p.mm(ypt[:, :], ysb[hf][:, :], g.cst[:, C_J:C_J + 128], r=[("ysb", hf), "cst"], w=[("yp", hf)])
                            os_ = slice(rt(c) * 128, (rt(c) + 1) * 128)
                            p.tt(ysum[:, hf, os_], ypt[:, :], ysum[:, hf, os_], ALU.add, r=[("yp", hf), "ysum"], w=["ysum"])
                p.barrier()
        with ExitStack() as es1:
            wg = p.sb(es1, "s5_wg", [128, 2, 512], BF16)
            vT = p.sb(es1, "s5_vT", [128, 2, 512], BF16)
            t1 = p.sb(es1, "s5_t1", [128, 512], F32)
            t2 = p.sb(es1, "s5_t2", [128, 512], F32)
            sg = p.sb(es1, "s5_sg", [128, 512], F32)
            yb = [p.sb(es1, "s5_yb%d" % i, [128, 2, 512], BF16) for i in range(2)]
            pv = [p.ps(es1, "s5_pv%d" % i, [128, 512], F32) for i in range(2)]
            pg = [p.ps(es1, "s5_pg%d" % i, [128, 512], F32) for i in range(2)]
            for kc in range(2):
                p.dma("pool", wg[:, kc, :], I["s5_glu_w"][l, kc * 128:(kc + 1) * 128, :], w=["wg"])
            k = 0
            for ti, (s0, n) in enumerate(FT if S5_LEVEL >= 5 else []):
                ob = ti % 2
                for c in range(2):
                    x = ysum[:, c, s0:s0 + n]
                    p.tt(t1[:, 0:n], x, x, ALU.mult, r=["ysum"], w=["t1"])
                    p.ts(t1[:, 0:n], t1[:, 0:n], 0.044715, 1.0, op0=ALU.mult, op1=ALU.add, r=["t1"], w=["t1"])
                    p.tt(t1[:, 0:n], t1[:, 0:n], x, ALU.mult, r=["t1", "ysum"], w=["t1"])
                    p.act(t2[:, 0:n], t1[:, 0:n], AF.Tanh, scale=0.7978845608028654, r=["t1"], w=["t2"])
                    p.ts(t2[:, 0:n], t2[:, 0:n], 0.5, 0.5, op0=ALU.mult, op1=ALU.add, r=["t2"], w=["t2"], eng="pool")
                    p.tt(vT[:, c, 0:n], t2[:, 0:n], x, ALU.mult, r=["t2", "ysum"], w=[("vT", c)])
                for c in range(2):
                    q = k % 2
                    k += 1
                    for kc in range(2):
                        p.mm(pv[q][:, 0:n], wg[:, kc, c * 128:(c + 1) * 128], vT[:, kc, 0:n], start=(kc == 0), stop=(kc == 1),
                             r=["wg", ("vT", kc)], w=[("pv", q)])
                    for kc in range(2):
                        p.mm(pg[q][:, 0:n], wg[:, kc, 256 + c * 128:256 + (c + 1) * 128], vT[:, kc, 0:n], start=(kc == 0), stop=(kc == 1),
                             r=["wg", ("vT", kc)], w=[("pg", q)])
                    p.act(sg[:, 0:n], pg[q][:, 0:n], AF.Sigmoid, r=[("pg", q)], w=["sg"])
                    p.tt(yb[ob][:, c, 0:n], pv[q][:, 0:n], sg[:, 0:n], ALU.mult, r=[("pv", q), "sg"], w=[("yb", ob)])
                p.dma("sp", g.yT[2:4, :, s0:s0 + n].rearrange("c p s -> p c s"), yb[ob][:, :, 0:n], r=[("yb", ob)], w=["yT_d"])
            p.barrier()


def phase_merge(g, l):
    p, I = g.p, g.I
    wv = I["w_in"][l].rearrange("(kc q) n -> q kc n", q=128)
    with ExitStack() as es:
        wg = p.sb(es, "mg_wg", [128, KC, 4096], BF16)
        wb = p.sb(es, "mg_wb", [128, 8, D], BF16)
        wo = p.sb(es, "mg_wo", [128, KC, D], BF16)
        ht = [p.sb(es, "mg_h%d" % i, [128, KC, 512], BF16) for i in range(2)]
        yt = [p.sb(es, "mg_y%d" % i, [128, 8, 512], BF16) for i in range(2)]
        xt = [p.sb(es, "mg_x%d" % i, [128, KC, 512], F32) for i in range(2)]
        sg = [p.sb(es, "mg_s%d" % i, [128, 512], F32) for i in range(2)]
        acc = p.sb(es, "mg_acc", [128, 512], F32)
        tmp = p.sb(es, "mg_tmp", [128, 512], F32)
        accT = p.sb(es, "mg_aT", [128, KC, 512], BF16)
        psg = [p.ps(es, "mg_pg%d" % i, [128, 512], F32) for i in range(2)]
        psb = [p.ps(es, "mg_pb%d" % i, [128, 512], F32) for i in range(2)]
        pso = [p.ps(es, "mg_po%d" % i, [128, 512], F32) for i in range(2)]
        for kc in range(KC):
            p.dma("pool", wg[:, kc, :], wv[:, kc, 2312:6408], w=[("wg", kc)])
            p.dma("pool", wo[:, kc, :], I["w_out"][l, kc * 128:(kc + 1) * 128, :], w=[("wo", kc)])
            p.dma("pool", wb[:, kc, :], I["w_branch"][l, kc // 2, (kc % 2) * 128:(kc % 2 + 1) * 128, :], w=[("wb", kc)])
        cg = 0
        co = 0
        for ti, (s0, n) in enumerate(FT):
            b = ti % 2
            ic = 1 if s0 == 0 else 0
            p.dma("sp", ht[b][:, :, 0:n], g.hT[:, :, s0:s0 + n].rearrange("c p s -> p c s"), r=["hT_d"], w=[("h", b)])
            p.dma("sp", yt[b][:, :, 0:n], g.yT[:, :, s0:s0 + n].rearrange("c p s -> p c s"), r=["yT_d"], w=[("y", b)])
            p.dma("sp", xt[b][:, :, 0:n], g.xres[:, :, s0:s0 + n].rearrange("c p s -> p c s"), r=["xres"], w=[("x", b)])
            for oc in range(KC):
                for br in range(4):
                    q = cg % 2
                    cg += 1
                    for kc in range(KC):
                        c0 = br * D + oc * 128
                        p.mm(psg[q][:, 0:n], wg[:, kc, c0:c0 + 128], ht[b][:, kc, 0:n], start=(kc == 0), stop=(kc == KC - 1),
                             r=[("wg", kc), ("h", b)], w=[("pg", q)])
                    for k2 in range(2):
                        p.mm(psb[q][:, 0:n], wb[:, br * 2 + k2, oc * 128:(oc + 1) * 128], yt[b][:, br * 2 + k2, 0:n], start=(k2 == 0),
                             stop=(k2 == 1), r=[("wb", br * 2 + k2), ("y", b)], w=[("pb", q)])
                    p.act(sg[q][:, 0:n], psg[q][:, 0:n], AF.Sigmoid, r=[("pg", q)], w=[("sg", q)])
                    if br == 0:
                        p.tt(acc[:, 0:n], psb[q][:, 0:n], sg[q][:, 0:n], ALU.mult, r=[("pb", q), ("sg", q)], w=["acc"])
                    else:
                        p.tt(tmp[:, 0:n], psb[q][:, 0:n], sg[q][:, 0:n], ALU.mult, r=[("pb", q), ("sg", q)], w=["tmp"])
                        p.tt(acc[:, 0:n], acc[:, 0:n], tmp[:, 0:n], ALU.add, r=["tmp", "acc"], w=["acc"], eng="pool")
                p.cp(accT[:, oc, 0:n], acc[:, 0:n], r=["acc"], w=[("aT", oc)], eng="act")
            for oc in range(KC):
                q = co % 2
                co += 1
                for kc in range(KC):
                    p.mm(pso[q][:, 0:n], wo[:, kc, oc * 128:(oc + 1) * 128], accT[:, kc, 0:n], start=(kc == 0), stop=(kc == KC - 1),
                         r=[("wo", kc), ("aT", kc)], w=[("po", q)])
                p.stt(xt[b][:, oc, 0:n], pso[q][:, 0:n], g.mod[:, 16 + oc, ic:ic + 1], xt[b][:, oc, 0:n], op0=ALU.mult, op1=ALU.add,
                      r=[("po", q), "mod", ("x", b)], w=[("x", b)])
            p.dma("sp", g.xres[:, :, s0:s0 + n].rearrange("c p s -> p c s"), xt[b][:, :, 0:n], r=[("x", b)], w=["xres"])
        p.barrier()


def phase_ffn(g, l):
    p, I = g.p, g.I
    moe = (l % 2 == 1)
    m = l // 2
    if moe:
        H, GS = EXPERT_DIM, 4
        experts = [(I["moe_w_in"][m, e], I["moe_w_out"][m, e]) for e in range(8)]
    else:
        H, GS = FFN_DIM, 2
        experts = [(I["ffn_w_in"][m], I["ffn_w_out"][m])]
    HC = H // 128
    NG = HC // GS
    with ExitStack() as es:
        wT = p.sb(es, "ff_wT", [8, S], F32)
        if moe:
            with ExitStack() as es0:
                rw = p.sb(es0, "ff_rw", [128, KC, 8], F32)
                lsb = p.sb(es0, "ff_lsb", [128, 8], F32)
                m8 = p.sb(es0, "ff_m8", [128, 8], F32)
                gt = p.sb(es0, "ff_gt", [128, 4], F32)
                e1 = p.sb(es0, "ff_e1", [128, 8], F32)
                e2 = p.sb(es0, "ff_e2", [128, 8], F32)
                lg = p.ps(es0, "ff_lg", [128, 8], F32)
                wtp = p.ps(es0, "ff_wtp", [8, 128], F32)
                p.dma("sp", rw[:, :, :], I["moe_router"][m].rearrange("(kc q) e -> q kc e", q=128), w=["rw"])

                def sink(ti, s0, n, ht, key, h32):
                    p.dma("sp", g.hT[:, :, s0:s0 + n].rearrange("c p s -> p c s"), ht[:, :, 0:n], r=[key], w=["hT_d"])
                    for sub in range(n // 128):
                        ss = slice(sub * 128, (sub + 1) * 128)
                        for kc in range(KC):
                            p.mm(lg[:, :], h32[:, kc, ss], rw[:, kc, :], start=(kc == 0), stop=(kc == KC - 1),
                                 r=[("nmsq", kc), "rw"], w=["lg"])
                        p.cp(lsb[:, :], lg[:, :], r=["lg"], w=["lsb"])
                        p.op("dve", lambda e: e.max(out=m8[:, :], in_=lsb[:, :]), r=["lsb"], w=["m8"])
                        p.tt(gt[:, 0:1], m8[:, 0:1], m8[:, 1:2], ALU.subtract, r=["m8"], w=["gt"])
                        p.act(gt[:, 1:2], gt[:, 0:1], AF.Sigmoid, r=["gt"], w=["gt1"])
                        p.act(gt[:, 2:3], gt[:, 0:1], AF.Sigmoid, scale=-1.0, r=["gt"], w=["gt2"])
                        p.ts(e1[:, :], lsb[:, :], m8[:, 0:1], gt[:, 1:2], op0=ALU.is_equal, op1=ALU.mult, r=["lsb", "m8", "gt1"], w=["e1"])
                        p.ts(e2[:, :], lsb[:, :], m8[:, 1:2], gt[:, 2:3], op0=ALU.is_equal, op1=ALU.mult, r=["lsb", "m8", "gt2"], w=["e2"])
                        p.tt(e1[:, :], e1[:, :], e2[:, :], ALU.add, r=["e1", "e2"], w=["e1"])
                        p.tr(wtp[:, :], e1[:, :], g.cst[:, C_ID:C_ID + 128], r=["e1", "cst"], w=["wtp"])
                        p.cp(wT[0:8, s0 + sub * 128:s0 + (sub + 1) * 128], wtp[:, :], r=["wtp"], w=["wT"])

                norm_tiles(g, 1, sink, want32=True)
        else:
            def sink(ti, s0, n, ht, key, h32):
                p.dma("sp", g.hT[:, :, s0:s0 + n].rearrange("c p s -> p c s"), ht[:, :, 0:n], r=[key], w=["hT_d"])

            norm_tiles(g, 1, sink)
        ht = [p.sb(es, "ff_h%d" % i, [128, KC, 512], BF16) for i in range(2)]
        wg = [p.sb(es, "ff_wg%d" % i, [128, KC, GS * 128], BF16) for i in range(2)]
        wu = [p.sb(es, "ff_wu%d" % i, [128, KC, GS * 128], BF16) for i in range(2)]
        wo = p.sb(es, "ff_wo", [128, HC, D], BF16)
        actT = p.sb(es, "ff_act", [128, HC, 512], BF16)
        oacc = p.sb(es, "ff_oacc", [128, KC, 512], F32)
        xt = p.sb(es, "ff_x", [128, KC, 512], F32)
        sgt = [p.sb(es, "ff_sg%d" % i, [128, 512], F32) for i in range(2)]
        tmp = [p.sb(es, "ff_tmp%d" % i, [128, 512], F32) for i in range(2)]
        wbc = p.sb(es, "ff_wbc", [128, 512], F32)
        psg = [p.ps(es, "ff_pg%d" % i, [128, 512], F32) for i in range(2)]
        psu = [p.ps(es, "ff_pu%d" % i, [128, 512], F32) for i in range(2)]
        pso = [p.ps(es, "ff_po%d" % i, [128, 512], F32) for i in range(2)]
        psw = p.ps(es, "ff_pw", [128, 512], F32)
        gcnt = 0
        jc = 0
        oc_ = 0
        for ti, (s0, n) in enumerate(FT):
            hb = ti % 2
            ic = 1 if s0 == 0 else 0
            p.dma("sp", ht[hb][:, :, 0:n], g.hT[:, :, s0:s0 + n].rearrange("c p s -> p c s"), r=["hT_d"], w=[("h", hb)])
            p.dma("sp", xt[:, :, 0:n], g.xres[:, :, s0:s0 + n].rearrange("c p s -> p c s"), r=["xres"], w=["x"])
            for ei, (w_in, w_out) in enumerate(experts):
                wiv = w_in.rearrange("(kc q) n -> q kc n", q=128)
                wov = w_out.rearrange("(hc q) n -> q hc n", q=128)
                if moe:
                    p.mm(psw[:, 0:n], g.cst[0:8, C_SEL + ei * 128:C_SEL + (ei + 1) * 128], wT[0:8, s0:s0 + n], r=["wT", "cst"], w=["psw"])
                    p.cp(wbc[:, 0:n], psw[:, 0:n], r=["psw"], w=["wbc"], eng="act")
                wibv = g.wib[ei, :, 0:2 * H].rearrange("(kc q) n -> q kc n", q=128)
                wobv = g.wob[ei, 0:H, :].rearrange("(hc q) n -> q hc n", q=128)
                for h0 in range(0, HC, 4):
                    hn = min(4, HC - h0)
                    if ti == 0:
                        p.dma("pool", wo[:, h0:h0 + hn, :], wov[:, h0:h0 + hn, :], w=[("wo", h0)])
                        p.dma("sp", wobv[:, h0:h0 + hn, :], wo[:, h0:h0 + hn, :], r=[("wo", h0)], w=[("wobd", ei, h0)])
                    else:
                        p.dma("sp", wo[:, h0:h0 + hn, :], wobv[:, h0:h0 + hn, :], r=[("wobd", ei, h0)], w=[("wo", h0)])
                for gi in range(NG):
                    gb = gcnt % 2
                    gcnt += 1
                    c0 = gi * GS * 128
                    if ti == 0:
                        p.dma("pool", wg[gb][:, :, :], wiv[:, :, c0:c0 + GS * 128], w=[("wg", gb)])
                        p.dma("pool", wu[gb][:, :, :], wiv[:, :, H + c0:H + c0 + GS * 128], w=[("wu", gb)])
                        p.dma("sp", wibv[:, :, c0:c0 + GS * 128], wg[gb][:, :, :], r=[("wg", gb)], w=[("wibd", ei, gi, 0)])
                        p.dma("sp", wibv[:, :, H + c0:H + c0 + GS * 128], wu[gb][:, :, :], r=[("wu", gb)], w=[("wibd", ei, gi, 1)])
                    else:
                        p.dma("sp", wg[gb][:, :, :], wibv[:, :, c0:c0 + GS * 128], r=[("wibd", ei, gi, 0)], w=[("wg", gb)])
                        p.dma("sp", wu[gb][:, :, :], wibv[:, :, H + c0:H + c0 + GS * 128], r=[("wibd", ei, gi, 1)], w=[("wu", gb)])
                    for j in range(GS):
                        q = jc % 2
                        jc += 1
                        hc = gi * GS + j
                        for kc in range(KC):
                            p.mm(psg[q][:, 0:n], wg[gb][:, kc, j * 128:(j + 1) * 128], ht[hb][:, kc, 0:n], start=(kc == 0),
                                 stop=(kc == KC - 1), r=[("wg", gb), ("h", hb)], w=[("pg", q)])
                        for kc in range(KC):
                            p.mm(psu[q][:, 0:n], wu[gb][:, kc, j * 128:(j + 1) * 128], ht[hb][:, kc, 0:n], start=(kc == 0),
                                 stop=(kc == KC - 1), r=[("wu", gb), ("h", hb)], w=[("pu", q)])
                        p.act(sgt[q][:, 0:n], psg[q][:, 0:n], AF.Silu, r=[("pg", q)], w=[("sg", q)])
                        if moe:
                            p.tt(tmp[q][:, 0:n], psu[q][:, 0:n], sgt[q][:, 0:n], ALU.mult, r=[("pu", q), ("sg", q)], w=[("tmp", q)])
                            p.tt(actT[:, hc, 0:n], tmp[q][:, 0:n], wbc[:, 0:n], ALU.mult, r=[("tmp", q), "wbc"], w=[("act", hc)], eng="pool")
                        else:
                            p.tt(actT[:, hc, 0:n], psu[q][:, 0:n], sgt[q][:, 0:n], ALU.mult, r=[("pu", q), ("sg", q)], w=[("act", hc)])
                for oc in range(KC):
                    q = oc_ % 2
                    oc_ += 1
                    for hc in range(HC):
                        p.mm(pso[q][:, 0:n], wo[:, hc, oc * 128:(oc + 1) * 128], actT[:, hc, 0:n], start=(hc == 0), stop=(hc == HC - 1),
                             r=[("wo", (hc // 4) * 4), ("act", hc)], w=[("po", q)])
                    if ei == 0:
                        p.cp(oacc[:, oc, 0:n], pso[q][:, 0:n], r=[("po", q)], w=[("oacc", oc)])
                    else:
                        p.tt(oacc[:, oc, 0:n], pso[q][:, 0:n], oacc[:, oc, 0:n], ALU.add, r=[("po", q), ("oacc", oc)], w=[("oacc", oc)])
            for oc in range(KC):
                p.stt(xt[:, oc, 0:n], oacc[:, oc, 0:n], g.mod[:, 40 + oc, ic:ic + 1], xt[:, oc, 0:n], op0=ALU.mult, op1=ALU.add,
                      r=[("oacc", oc), "mod", "x"], w=["x"])
            p.dma("sp", g.xres[:, :, s0:s0 + n].rearrange("c p s -> p c s"), xt[:, :, 0:n], r=["x"], w=["xres"])
        p.barrier()


def layer(g, l, stop):
    phases = [("ada", adaln), ("n1", phase_norm1), ("ptm", phase_ptm), ("xbc", phase_xbc), ("ssd", phase_ssd), ("s5", phase_s5), ("attn", phase_attn), ("merge", phase_merge), ("ffn", phase_ffn)]
    for name, fn in phases:
        fn(g, l)
        if stop is not None and stop == (l, name):
            return


def final(g, out, real):
    p, I = g.p, g.I
    if not real:
        with ExitStack() as es:
            z = p.sb(es, "fz", [128, D], F32)
            p.memset(z[:, :], 0.0, w=["fz"])
            p.dma("sp", out[0:128, :], z[:, :], r=["fz"], w=["out"])
            p.barrier()
        return
    with ExitStack() as es:
        gf = p.sb(es, "fn_g", [128, 8], F32)
        load_rows_T(g, None, gf[:, :], I["final_norm_g"].rearrange("(j q) -> j q", q=128), 8, "gf")
        xt = [p.sb(es, "fn_x%d" % i, [128, KC, 512], F32) for i in range(2)]
        sq = p.sb(es, "fn_sq", [128, KC, 512], F32)
        rs = p.sb(es, "fn_rs", [128, 512], F32)
        ot = [p.sb(es, "fn_o%d" % i, [128, KC, 128], F32) for i in range(2)]
        ssq = p.ps(es, "fn_ps", [128, 512], F32)
        pst = [p.ps(es, "fn_pt%d" % i, [128, KC, 128], F32) for i in range(2)]
        cnt = 0
        for ti, (s0, n) in enumerate(FT[1:]):
            b = ti % 2
            p.dma("sp", xt[b][:, :, 0:n], g.xres[:, :, s0:s0 + n].rearrange("c p s -> p c s"), r=["xres"], w=[("fx", b)])
            for c in range(KC):
                p.act(sq[:, c, 0:n], xt[b][:, c, 0:n], AF.Square, r=[("fx", b)], w=[("fsq", c)])
                p.mm(ssq[:, 0:n], g.cst[:, C_ONE:C_ONE + 128], sq[:, c, 0:n], start=(c == 0), stop=(c == KC - 1),
                     r=[("fsq", c), "cst"], w=["fps"])
            p.act(rs[:, 0:n], ssq[:, 0:n], AF.Ln, bias=EPS, scale=1.0 / D, r=["fps"], w=["frs"])
            p.act(rs[:, 0:n], rs[:, 0:n], AF.Exp, scale=-0.5, r=["frs"], w=["frs"])
            for c in range(KC):
                p.stt(sq[:, c, 0:n], xt[b][:, c, 0:n], gf[:, c:c + 1], rs[:, 0:n], op0=ALU.mult, op1=ALU.mult,
                      r=[("fx", b), "frs", "gf"], w=[("fsq", c)])
            for sub in range(n // 128):
                ob = cnt % 2
                cnt += 1
                for c in range(KC):
                    p.tr(pst[ob][:, c, :], sq[:, c, sub * 128:(sub + 1) * 128], g.cst[:, C_ID:C_ID + 128],
                         r=[("fsq", c), "cst"], w=[("fpt", ob)])
                p.cp(ot[ob][:, :, :], pst[ob][:, :, :], r=[("fpt", ob)], w=[("fo", ob)], eng="act" if ob else "dve")
                t0 = s0 - NCTX + sub * 128
                p.dma("sp", out[t0:t0 + 128, :], ot[ob][:, :, :].rearrange("p c f -> p (c f)"), r=[("fo", ob)], w=["out"])
        p.barrier()


_NC_CACHE = {}


def kernel(**inputs):
    x = np.asarray(inputs["x"], np.float32)
    nb = x.shape[0]
    if "nc" not in _NC_CACHE:
        _NC_CACHE["nc"] = build()[0]
    nc = _NC_CACHE["nc"]
    consts = make_consts()
    rope = make_rope()
    shared = {k: np.ascontiguousarray(np.asarray(v, np.float32)) for k, v in inputs.items() if k not in ("x", "c", "ctx", "c_ctx")}
    c = np.asarray(inputs["c"], np.float32)
    cctx = np.asarray(inputs["c_ctx"], np.float32)
    in_maps = []
    for b in range(nb):
        m = dict(shared)
        m["x"] = np.ascontiguousarray(x[b])
        m["ctx"] = np.ascontiguousarray(np.asarray(inputs["ctx"], np.float32)[b])
        m["cc"] = np.ascontiguousarray(np.concatenate([c[b].reshape(8, 128), cctx.reshape(8, 128)], 0))
        m["consts"] = consts
        m["rope"] = rope
        in_maps.append(m)
    res = run_bass_kernel_spmd(nc, in_maps, core_ids=list(range(nb)))
    return np.stack([r["out"] for r in res.results], axis=0).astype(np.float32)
```

```python
import numpy as np
from contextlib import ExitStack
import concourse.bass as bass
import concourse.mybir as mybir
from concourse.bass_utils import run_bass_kernel_spmd

F32 = mybir.dt.float32
BF16 = mybir.dt.bfloat16
I32 = mybir.dt.int32
ALU = mybir.AluOpType
AF = mybir.ActivationFunctionType
AX = mybir.AxisListType

D = 1024
KC = 8
NCTX = 256
NLAT = 4096
S = NCTX + NLAT
NT = S // 128
DEPTH = 4
FT = [(0, 256)] + [(256 + 512 * i, 512) for i in range(8)]
EPS = 1e-6
PZ, PDT, PU, PCQ, PCK, PCV, PDQ, PDK, PDV = 0, 256, 264, 520, 776, 904, 1032, 1288, 1416
NPTM = 1544
FFN_DIM = 2816
EXPERT_DIM = 3584
NEG = -30000.0
import os
SAME_ENG_SYNC = bool(int(os.environ.get('SAME_ENG_SYNC', '1')))
XBC_LEVEL = int(os.environ.get('XBC_LEVEL', '2'))
ATT_LEVEL = int(os.environ.get('ATT_LEVEL', '2'))
SSD_LEVEL = int(os.environ.get('SSD_LEVEL', '9'))
S5_LEVEL = int(os.environ.get('S5_LEVEL', '9'))


class Prog:
    def __init__(self, nc, es):
        self.nc = nc
        self.es = es
        self.E = {"pe": nc.tensor, "act": nc.scalar, "dve": nc.vector, "pool": nc.gpsimd, "sp": nc.sync}
        self.csem = {}
        self.ccnt = {}
        for e in ("pe", "act", "dve", "pool"):
            self.csem[e] = es.enter_context(nc.semaphore("c_" + e))
            self.ccnt[e] = 0
        self.dsem = {}
        self.dcnt = {}
        self.dnext = {}
        for q in ("sp", "pool", "act"):
            self.dsem[q] = [es.enter_context(nc.semaphore("d_%s%d" % (q, i))) for i in range(8)]
            self.dcnt[q] = [0] * 8
            self.dnext[q] = 0
        self.sems = {}
        for e in self.csem:
            self.sems[("c", e)] = self.csem[e]
        for q in self.dsem:
            for i, s in enumerate(self.dsem[q]):
                self.sems[("d", q, i)] = s
        self.waited = {e: {} for e in self.E}
        self.state = {}
        self.nins = 0

    def _deps(self, r, w):
        deps = {}

        def add(tok):
            if tok is None:
                return
            k, v = tok
            if deps.get(k, 0) < v:
                deps[k] = v

        for key in r:
            st = self.state.get(key)
            if st:
                add(st[0])
        for key in w:
            st = self.state.get(key)
            if st:
                add(st[0])
                for t in st[1]:
                    add(t)
        return deps

    def _wait(self, eng, deps):
        wd = self.waited[eng]
        for k, v in deps.items():
            if wd.get(k, 0) >= v:
                continue
            if k == ("c", eng) and (eng == "pe" or not SAME_ENG_SYNC):
                continue
            self.E[eng].wait_ge(self.sems[k], v)
            wd[k] = v
            self.nins += 1

    def _commit(self, tok, r, w):
        for key in r:
            st = self.state.setdefault(key, [None, []])
            st[1].append(tok)
            if len(st[1]) > 24:
                mx = {}
                for k, v in st[1]:
                    if mx.get(k, 0) < v:
                        mx[k] = v
                st[1] = list(mx.items())
        for key in w:
            self.state[key] = [tok, []]

    def op(self, eng, fn, r=(), w=()):
        self._wait(eng, self._deps(r, w))
        ins = fn(self.E[eng])
        self.ccnt[eng] += 1
        ins.then_inc(self.csem[eng], 1)
        tok = (("c", eng), self.ccnt[eng])
        self._commit(tok, r, w)
        self.nins += 1
        return tok

    def dma(self, q, out, in_, r=(), w=()):
        i = self.dnext[q]
        self.dnext[q] = (i + 1) % 8
        deps = self._deps(r, w)
        k = ("d", q, i)
        if self.dcnt[q][i] > 0:
            deps[k] = max(deps.get(k, 0), self.dcnt[q][i])
        self._wait(q, deps)
        self.dcnt[q][i] += 16
        self.E[q].dma_start(out=out, in_=in_).then_inc(self.dsem[q][i], 16)
        tok = (k, self.dcnt[q][i])
        self._commit(tok, r, w)
        self.nins += 1
        return tok

    def barrier(self):
        deps = {}
        for e in self.csem:
            if self.ccnt[e]:
                deps[("c", e)] = self.ccnt[e]
        for q in self.dsem:
            for i in range(8):
                if self.dcnt[q][i]:
                    deps[("d", q, i)] = self.dcnt[q][i]
        for e in self.E:
            d = {k: v for k, v in deps.items() if k != ("c", e)}
            self._wait(e, d)
        self.state = {}

    def mm(self, out, lhsT, rhs, start=True, stop=True, r=(), w=()):
        return self.op("pe", lambda e: e.matmul(out, lhsT, rhs, start=start, stop=stop), r, w)

    def tr(self, out, in_, ident, r=(), w=()):
        return self.op("pe", lambda e: e.transpose(out, in_, ident), r, w)

    def act(self, out, in_, func, bias=0.0, scale=1.0, r=(), w=(), eng="act"):
        return self.op(eng, lambda e: e.activation(out=out, in_=in_, func=func, bias=bias, scale=scale), r, w)

    def tt(self, out, in0, in1, op, r=(), w=(), eng="dve"):
        return self.op(eng, lambda e: e.tensor_tensor(out=out, in0=in0, in1=in1, op=op), r, w)

    def ts(self, out, in0, s1, s2=None, op0=ALU.mult, op1=None, r=(), w=(), eng="dve"):
        if op1 is None:
            return self.op(eng, lambda e: e.tensor_scalar(out=out, in0=in0, scalar1=s1, scalar2=None, op0=op0), r, w)
        return self.op(eng, lambda e: e.tensor_scalar(out=out, in0=in0, scalar1=s1, scalar2=s2, op0=op0, op1=op1), r, w)

    def stt(self, out, in0, scalar, in1, op0=ALU.mult, op1=ALU.add, r=(), w=(), eng="dve"):
        return self.op(eng, lambda e: e.scalar_tensor_tensor(out=out, in0=in0, scalar=scalar, in1=in1, op0=op0, op1=op1), r, w)

    def cp(self, out, in_, r=(), w=(), eng="dve"):
        if eng == "act":
            return self.op("act", lambda e: e.copy(out=out, in_=in_), r, w)
        return self.op(eng, lambda e: e.tensor_copy(out=out, in_=in_), r, w)

    def memset(self, ap, val, w=(), eng="dve"):
        return self.op(eng, lambda e: e.memset(ap, val), (), w)

    def sb(self, es, name, shape, dt):
        self.uid = getattr(self, "uid", 0) + 1
        return es.enter_context(self.nc.sbuf_tensor("s%d_%s" % (self.uid, name), list(shape), dt))

    def ps(self, es, name, shape, dt=F32):
        self.uid = getattr(self, "uid", 0) + 1
        return es.enter_context(self.nc.psum_tensor("p%d_%s" % (self.uid, name), list(shape), dt))


def make_consts():
    i = np.arange(128)
    ident = np.eye(128, dtype=np.float32)
    J = ident[::-1].copy()
    U = (i[:, None] <= i[None, :]).astype(np.float32)
    L = (i[:, None] >= i[None, :]).astype(np.float32)
    ones = np.ones((128, 128), np.float32)
    nmf = np.where(i[:, None] <= i[None, :], 0.0, NEG).astype(np.float32)
    nmb = np.where(i[:, None] >= i[None, :], 0.0, NEG).astype(np.float32)
    ramp = np.tile(np.arange(1, 129, dtype=np.float32)[None, :], (128, 1))
    sel = np.zeros((128, 8 * 128), np.float32)
    for e in range(8):
        sel[e, e * 128:(e + 1) * 128] = 1.0
    return np.concatenate([ident, J, U, L, ones, nmf, nmb, ramp, sel], axis=1)


C_ID, C_J, C_U, C_L, C_ONE, C_NMF, C_NMB, C_RAMP, C_SEL = [k * 128 for k in range(9)]
NCONST = 16 * 128


def make_rope():
    pos = np.arange(NLAT)
    pr = (pos // 64).astype(np.float32)
    pc = (pos % 64).astype(np.float32)
    inv = (np.float32(10000.0) ** (-np.arange(16, dtype=np.float32) / np.float32(16))).astype(np.float32)
    ang = np.concatenate([pr[:, None] * inv, pc[:, None] * inv], axis=-1).astype(np.float32)
    t = np.zeros((S, 64), np.float32)
    t[:NCTX, :32] = 1.0
    t[NCTX:, :32] = np.cos(ang)
    t[NCTX:, 32:] = np.sin(ang)
    return t


class Ctx:
    pass


def build(dbg=None, layers=DEPTH, stop=None, skip=()):
    dbg = dbg or {}
    nc = bass.Bass("TRN2", target_bir_lowering=False)
    g = Ctx()
    g.nc = nc

    def din(name, shape, dt=F32):
        return nc.dram_tensor(name, list(shape), dt, kind="ExternalInput").ap()

    def dscr(name, shape, dt=F32):
        kind = "ExternalOutput" if name in dbg else "Internal"
        return nc.dram_tensor(name, list(shape), dt, kind=kind).ap()

    I = {}
    I["x"] = din("x", [NLAT, D])
    I["ctx"] = din("ctx", [NCTX, D])
    I["cc"] = din("cc", [16, 128])
    I["consts"] = din("consts", [128, NCONST])
    I["rope"] = din("rope", [S, 64])
    shapes = dict(
        norm1_g=[DEPTH, D], norm2_g=[DEPTH, D], ada_w=[DEPTH, D, 6 * D], ada_b=[DEPTH, 6 * D],
        w_in=[DEPTH, D, 6408], ssd_conv_w=[DEPTH, 5, 768], ssd_conv_b=[DEPTH, 768], ssd_a_log=[DEPTH, 2, 4],
        ssd_dt_bias=[DEPTH, 2, 4], ssd_d=[DEPTH, 4], ssd_norm_g=[DEPTH, 256], s5_lam_re=[DEPTH, 2, 16, 64],
        s5_lam_im=[DEPTH, 2, 16, 64], s5_log_step=[DEPTH, 2, 16], s5_b_re=[DEPTH, 2, 16, 64, 16],
        s5_b_im=[DEPTH, 2, 16, 64, 16], s5_c_re=[DEPTH, 2, 16, 16, 64], s5_c_im=[DEPTH, 2, 16, 16, 64],
        s5_d=[DEPTH, 256], s5_glu_w=[DEPTH, 256, 512], qk_norm_g=[DEPTH, 2, 64], swa_sink=[DEPTH, 4],
        w_branch=[DEPTH, 4, 256, D], w_out=[DEPTH, D, D], ffn_w_in=[2, D, 2 * FFN_DIM], ffn_w_out=[2, FFN_DIM, D],
        moe_router=[2, D, 8], moe_w_in=[2, 8, D, 2 * EXPERT_DIM], moe_w_out=[2, 8, EXPERT_DIM, D],
        final_norm_g=[D],
    )
    for k, shp in shapes.items():
        if k not in skip:
            I[k] = din(k, shp)
    out = nc.dram_tensor("out", [NLAT, D], F32, kind="ExternalOutput").ap()

    g.I = I
    g.xres = dscr("xres", [KC, 128, S])
    g.hT = dscr("hT", [KC, 128, S], BF16)
    g.ptm = dscr("ptm", [S, NPTM])
    g.xbcT = dscr("xbcT", [6, 128, S], BF16)
    g.xbctm = dscr("xbctm", [S, 512], BF16)
    g.yT = dscr("yT", [8, 128, S], BF16)
    g.wib = dscr("wib", [8, D, 2 * EXPERT_DIM], BF16)
    g.wob = dscr("wob", [8, EXPERT_DIM, D], BF16)

    with ExitStack() as es:
        p = Prog(nc, es)
        g.p = p
        blk = es.enter_context(nc.Block())
        g.cst = p.sb(es, "cst", [128, NCONST], F32)
        g.cstb = p.sb(es, "cstb", [128, NCONST], BF16)
        g.par = p.sb(es, "par", [128, 512], F32)
        g.mod = p.sb(es, "modv", [128, 48, 2], F32)
        g.AB = p.sb(es, "AB", [128, 4, 8, 2], F32)
        g.cact = p.sb(es, "cact", [128, 8, 2], F32)

        def body(_e):
            setup(g)
            for l in range(layers if stop != "setup" else 0):
                layer(g, l, stop)
                if stop is not None and stop[0] == l:
                    break
            final(g, out, stop is None)
            p.barrier()

        blk.gpsimd(body)
    return nc, p


def load_rows_T(g, es_ps, dst, src2d, n, key):
    p = g.p
    with ExitStack() as es:
        tmp = p.sb(es, "lrt_tmp", [128, 128], F32)
        pst = p.ps(es, "lrt_ps", [128, 128], F32)
        p.dma("sp", tmp[0:n, :], src2d, w=["lrt_tmp"])
        p.tr(pst[:, 0:n], tmp[0:n, :], g.cst[0:n, C_ID:C_ID + n], r=["lrt_tmp", "cst"], w=["lrt_ps"])
        p.cp(dst, pst[:, 0:n], r=["lrt_ps"], w=[key])
        p.barrier()


def setup(g):
    p, nc, I = g.p, g.nc, g.I
    p.dma("sp", g.cst[:, :], I["consts"][:, :], w=["cst"])
    p.cp(g.cstb[:, :], g.cst[:, :], r=["cst"], w=["cstb"])
    with ExitStack() as es:
        t = p.sb(es, "su_t", [128, 16], F32)
        load_rows_T(g, None, t[:, :], I["cc"][:, :], 16, "su_t")
        for i in range(2):
            p.act(g.cact[:, :, i], t[:, i * 8:(i + 1) * 8], AF.Silu, r=["su_t"], w=["cact"])
        p.barrier()
    with ExitStack() as es:
        xin = [p.sb(es, "su_x%d" % i, [128, D], F32) for i in range(2)]
        xo = [p.sb(es, "su_o%d" % i, [128, KC, 128], F32) for i in range(2)]
        pst = [p.ps(es, "su_ps%d" % i, [128, KC, 128], F32) for i in range(2)]
        for t in range(NT):
            b = t % 2
            src = I["ctx"][t * 128:(t + 1) * 128, :] if t < 2 else I["x"][(t - 2) * 128:(t - 1) * 128, :]
            p.dma("sp", xin[b][:, :], src, w=[("xin", b)])
            for c in range(KC):
                p.tr(pst[b][:, c, :], xin[b][:, c * 128:(c + 1) * 128], g.cst[:, C_ID:C_ID + 128],
                     r=[("xin", b), "cst"], w=[("xps", b)])
            p.cp(xo[b][:, :, :], pst[b][:, :, :], r=[("xps", b)], w=[("xo", b)], eng="act" if b else "dve")
            p.dma("sp", g.xres[:, :, t * 128:(t + 1) * 128].rearrange("c p s -> p c s"), xo[b][:, :, :],
                  r=[("xo", b)], w=["xres"])
        p.barrier()


def adaln(g, l):
    p, I = g.p, g.I
    with ExitStack() as es:
        wt = [p.sb(es, "ada_w%d" % i, [128, KC, 768], F32) for i in range(2)]
        adab = p.sb(es, "ada_b", [128, 48], F32)
        gn = p.sb(es, "ada_g", [128, 16], F32)
        mps = p.ps(es, "ada_ps", [128, 48, 2], F32)
        load_rows_T(g, None, adab[:, :], I["ada_b"][l].rearrange("(j q) -> j q", q=128), 48, "adab")
        load_rows_T(g, None, gn[:, 0:8], I["norm1_g"][l].rearrange("(j q) -> j q", q=128), 8, "gn")
        load_rows_T(g, None, gn[:, 8:16], I["norm2_g"][l].rearrange("(j q) -> j q", q=128), 8, "gn")
        wv = I["ada_w"][l].rearrange("(kc q) n -> q kc n", q=128)
        for ob in range(8):
            b = ob % 2
            for kc in range(KC):
                p.dma("sp" if kc % 2 == 0 else "act", wt[b][:, kc, :], wv[:, kc, ob * 768:(ob + 1) * 768], w=[("adaw", b, kc)])
            for jj in range(6):
                j = ob * 6 + jj
                for kc in range(KC):
                    p.mm(mps[:, j, :], wt[b][:, kc, jj * 128:(jj + 1) * 128], g.cact[:, kc, :],
                         start=(kc == 0), stop=(kc == KC - 1), r=[("adaw", b, kc), "cact"], w=["mps"])
        for i in range(2):
            p.tt(g.mod[:, :, i], mps[:, :, i], adab[:, :], ALU.add, r=["mps", "adab"], w=["mod"])
        for i in range(2):
            p.stt(g.AB[:, 0, :, i], g.mod[:, 8:16, i], 1.0, gn[:, 0:8], op0=ALU.add, op1=ALU.mult, r=["mod", "gn"], w=["AB"])
            p.cp(g.AB[:, 1, :, i], g.mod[:, 0:8, i], r=["mod"], w=["AB"])
            p.stt(g.AB[:, 2, :, i], g.mod[:, 32:40, i], 1.0, gn[:, 8:16], op0=ALU.add, op1=ALU.mult, r=["mod", "gn"], w=["AB"])
            p.cp(g.AB[:, 3, :, i], g.mod[:, 24:32, i], r=["mod"], w=["AB"])
        p.barrier()


def norm_tiles(g, which, sink, want32=False):
    p = g.p
    with ExitStack() as es:
        xt = [p.sb(es, "nm_x%d" % i, [128, KC, 512], F32) for i in range(2)]
        sq = p.sb(es, "nm_sq", [128, KC, 512], F32)
        rs = p.sb(es, "nm_rs", [128, 512], F32)
        ht = [p.sb(es, "nm_h%d" % i, [128, KC, 512], BF16) for i in range(2)]
        ssq = p.ps(es, "nm_ps", [128, 512], F32)
        for ti, (s0, n) in enumerate(FT):
            b = ti % 2
            ic = 1 if s0 == 0 else 0
            p.dma("sp", xt[b][:, :, 0:n], g.xres[:, :, s0:s0 + n].rearrange("c p s -> p c s"), r=["xres"], w=[("nmx", b)])
            for c in range(KC):
                p.act(sq[:, c, 0:n], xt[b][:, c, 0:n], AF.Square, r=[("nmx", b)], w=[("nmsq", c)])
                p.mm(ssq[:, 0:n], g.cst[:, C_ONE:C_ONE + 128], sq[:, c, 0:n], start=(c == 0), stop=(c == KC - 1),
                     r=[("nmsq", c), "cst"], w=["nmps"])
            p.act(rs[:, 0:n], ssq[:, 0:n], AF.Ln, bias=EPS, scale=1.0 / D, r=["nmps"], w=["nmrs"])
            p.act(rs[:, 0:n], rs[:, 0:n], AF.Exp, scale=-0.5, r=["nmrs"], w=["nmrs"])
            for c in range(KC):
                p.tt(sq[:, c, 0:n], xt[b][:, c, 0:n], rs[:, 0:n], ALU.mult, r=[("nmx", b), "nmrs"], w=[("nmsq", c)],
                     eng="dve" if c % 2 == 0 else "pool")
                if want32:
                    p.ts(sq[:, c, 0:n], sq[:, c, 0:n], g.AB[:, 2 * which, c, ic:ic + 1], g.AB[:, 2 * which + 1, c, ic:ic + 1],
                         op0=ALU.mult, op1=ALU.add, r=[("nmsq", c), "AB"], w=[("nmsq", c)], eng="dve" if c % 2 == 0 else "pool")
                    p.cp(ht[b][:, c, 0:n], sq[:, c, 0:n], r=[("nmsq", c)], w=[("nmh", b)], eng="dve" if c % 2 == 0 else "pool")
                else:
                    p.ts(ht[b][:, c, 0:n], sq[:, c, 0:n], g.AB[:, 2 * which, c, ic:ic + 1], g.AB[:, 2 * which + 1, c, ic:ic + 1],
                         op0=ALU.mult, op1=ALU.add, r=[("nmsq", c), "AB"], w=[("nmh", b)], eng="dve" if c % 2 == 0 else "pool")
            if want32:
                sink(ti, s0, n, ht[b], ("nmh", b), sq)
            else:
                sink(ti, s0, n, ht[b], ("nmh", b), None)
        p.barrier()


def phase_norm1(g, l):
    p = g.p

    def sink(ti, s0, n, ht, key, h32):
        p.dma("sp", g.hT[:, :, s0:s0 + n].rearrange("c p s -> p c s"), ht[:, :, 0:n], r=[key], w=["hT_d"])

    norm_tiles(g, 0, sink)


def phase_ptm(g, l):
    p, I = g.p, g.I
    wv = I["w_in"][l].rearrange("(kc q) n -> q kc n", q=128)
    with ExitStack() as es:
        w = p.sb(es, "ptm_w", [128, KC, NPTM], BF16)
        ht = [p.sb(es, "ptm_h%d" % i, [128, KC, 512], BF16) for i in range(2)]
        ob = [p.sb(es, "ptm_o%d" % i, [128, NPTM], F32) for i in range(2)]
        ps = [p.ps(es, "ptm_ps%d" % i, [128, 4, 512], F32) for i in range(2)]
        for kc in range(KC):
            p.dma("pool", w[:, kc, 0:256], wv[:, kc, 0:256], w=[("w", kc)])
            p.dma("pool", w[:, kc, 256:NPTM], wv[:, kc, 1024:2312], w=[("w", kc)])
        cols = [(0, 512), (512, 512), (1024, 512), (1536, 8)]
        cnt = 0
        for ti, (s0, n) in enumerate(FT):
            hb = ti % 2
            p.dma("sp", ht[hb][:, :, 0:n], g.hT[:, :, s0:s0 + n].rearrange("c p s -> p c s"), r=["hT_d"], w=[("h", hb)])
            for sub in range(n // 128):
                b = cnt % 2
                cnt += 1
                for kc in range(KC):
                    for bi, (c0, cn) in enumerate(cols):
                        p.mm(ps[b][:, bi, 0:cn], ht[hb][:, kc, sub * 128:(sub + 1) * 128], w[:, kc, c0:c0 + cn],
                             start=(kc == 0), stop=(kc == KC - 1), r=[("h", hb), ("w", kc)], w=[("ps", b, bi)])
                for bi, (c0, cn) in enumerate(cols):
                    p.cp(ob[b][:, c0:c0 + cn], ps[b][:, bi, 0:cn], r=[("ps", b, bi)], w=[("o", b)],
                         eng="act" if bi % 2 else "dve")
                t0 = s0 + sub * 128
                p.dma("sp", g.ptm[t0:t0 + 128, :], ob[b][:, :], r=[("o", b)], w=["ptm_d"])
        p.barrier()


def phase_xbc(g, l):
    p, I = g.p, g.I
    wv = I["w_in"][l].rearrange("(kc q) n -> q kc n", q=128)
    with ExitStack() as es:
        w = p.sb(es, "xb_w", [128, KC, 768], BF16)
        hT = p.sb(es, "xb_h", [128, KC, S], BF16)
        cw = p.sb(es, "xb_cw", [128, 36], F32)
        rawc = p.sb(es, "xb_rc", [128, NCTX + 4], F32)
        rawl = p.sb(es, "xb_rl", [128, NLAT + 4], F32)
        acc = p.sb(es, "xb_acc", [128, S], F32)
        xs = [p.sb(es, "xb_s%d" % i, [128, S], BF16) for i in range(2)]
        tmo = [p.sb(es, "xb_t%d" % i, [128, 4, 128], BF16) for i in range(2)]
        ps = [p.ps(es, "xb_ps%d" % i, [128, 512], F32) for i in range(2)]
        pst = [p.ps(es, "xb_pt%d" % i, [128, 4, 128], F32) for i in range(2)]
        load_rows_T(g, None, cw[:, 0:30], I["ssd_conv_w"][l].rearrange("k (c q) -> (k c) q", q=128), 30, "cw")
        load_rows_T(g, None, cw[:, 30:36], I["ssd_conv_b"][l].rearrange("(c q) -> c q", q=128), 6, "cw")
        for kc in range(KC):
            p.dma("pool", w[:, kc, :], wv[:, kc, 256:1024], w=[("w", kc)])
        for ti, (s0, n) in enumerate(FT):
            p.dma("sp", hT[:, :, s0:s0 + n], g.hT[:, :, s0:s0 + n].rearrange("c p s -> p c s"), r=["hT_d"], w=[("h", ti)])
        p.memset(rawc[:, :], 0.0, w=["rawc"])
        p.memset(rawl[:, :], 0.0, w=["rawl"], eng="pool")
        cnt = 0
        gcnt = 0
        for j in range(6):
            xb = xs[j % 2]
            for ti, (s0, n) in enumerate(FT):
                b = cnt % 2
                cnt += 1
                for kc in range(KC):
                    p.mm(ps[b][:, 0:n], w[:, kc, j * 128:(j + 1) * 128], hT[:, kc, s0:s0 + n], start=(kc == 0),
                         stop=(kc == KC - 1), r=[("w", kc), ("h", ti)], w=[("ps", b)])
                if s0 == 0:
                    p.cp(rawc[:, 2:2 + n], ps[b][:, 0:n], r=[("ps", b)], w=["rawc"], eng="act")
                else:
                    p.cp(rawl[:, 2 + s0 - NCTX:2 + s0 - NCTX + n], ps[b][:, 0:n], r=[("ps", b)], w=["rawl"], eng="act")
            if XBC_LEVEL < 1:
                continue
            pieces = [(rawc, "rawc", 0, 0, NCTX)] + [(rawl, "rawl", q * 1024, NCTX + q * 1024, 1024) for q in range(4)]
            for pi, (rw, rk, o, so, n) in enumerate(pieces):
                eng = "dve"
                a = acc[:, so:so + n]
                p.ts(a, rw[:, o:o + n], cw[:, j:j + 1], r=[rk, "cw"], w=[("acc", pi)], eng=eng)
                for k in range(1, 5):
                    p.stt(a, rw[:, o + k:o + k + n], cw[:, k * 6 + j:k * 6 + j + 1], a, r=[rk, "cw", ("acc", pi)],
                          w=[("acc", pi)], eng=eng)
                p.act(xb[:, so:so + n], a, AF.Silu, bias=cw[:, 30 + j:31 + j], r=[("acc", pi), "cw"], w=[("xs", j % 2, pi)])
            p.dma("sp", g.xbcT[j, :, :], xb[:, :], r=[("xs", j % 2, pi) for pi in range(5)], w=["xbcT_d"])
            if j < 4 and XBC_LEVEL >= 2:
                for t0 in range(0, NT, 4):
                    nt = min(4, NT - t0)
                    b = gcnt % 2
                    gcnt += 1
                    for tt_ in range(nt):
                        t = t0 + tt_
                        p.mm(pst[b][:, tt_, :], xb[:, t * 128:(t + 1) * 128], g.cstb[:, C_ID:C_ID + 128],
                             r=[("xs", j % 2, pi) for pi in range(5)] + ["cstb"], w=[("pt", b)])
                    p.cp(tmo[b][:, 0:nt, :], pst[b][:, 0:nt, :], r=[("pt", b)], w=[("tmo", b)], eng="dve" if b else "act")
                    p.dma("sp", g.xbctm[t0 * 128:(t0 + nt) * 128, j * 128:(j + 1) * 128].rearrange("(t q) c -> q t c", q=128),
                          tmo[b][:, 0:nt, :], r=[("tmo", b)], w=["xbctm_d"])
        p.barrier()


def phase_attn(g, l):
    p, I = g.p, g.I
    with ExitStack() as es:
        qT = p.sb(es, "at_qT", [128, 4, S], BF16)
        kT = p.sb(es, "at_kT", [128, 4, S], BF16)
        vv = p.sb(es, "at_v", [128, NT, 4, 128], BF16)
        yo = p.sb(es, "at_y", [128, 4, S], BF16)
        gbc = p.sb(es, "at_g", [128, 6, 64], F32)
        es_ = p.sb(es, "at_es", [128, 4], F32)
        with ExitStack() as es2:
            pin = [p.sb(es2, "at_in%d" % i, [128, 1024], F32) for i in range(2)]
            rp = [p.sb(es2, "at_rp%d" % i, [128, 64], F32) for i in range(2)]
            sq = p.sb(es2, "at_sq", [128, 384], F32)
            ms = p.sb(es2, "at_ms", [128, 6], F32)
            t1 = p.sb(es2, "at_t1", [128, 12, 2, 16], F32)
            t2 = p.sb(es2, "at_t2", [128, 12, 2, 16], F32)
            t3 = p.sb(es2, "at_t3", [128, 12, 2, 16], F32)
            t4 = p.sb(es2, "at_t4", [128, 12, 2, 16], F32)
            ro = p.sb(es2, "at_ro", [128, 12, 64], F32)
            tb = p.sb(es2, "at_tb", [128, 8, 128], BF16)
            pst = [p.ps(es2, "at_pt%d" % i, [128, 8, 128], F32) for i in range(2)]
            for i in range(4):
                p.dma("sp", gbc[:, i, :], I["qk_norm_g"][l, 0:1, :].partition_broadcast(128), w=["gbc"])
            for i in range(2):
                p.dma("sp", gbc[:, 4 + i, :], I["qk_norm_g"][l, 1:2, :].partition_broadcast(128), w=["gbc"])
            p.dma("sp", es_[:, :], I["swa_sink"][l:l + 1, :].partition_broadcast(128), w=["es"])
            p.act(es_[:, :], es_[:, :], AF.Exp, r=["es"], w=["es"])
            p.memset(vv[:, :, :, 64:128], 1.0, w=["vv1"])
            for t in range(NT):
                b = t % 2
                x = pin[b]
                p.dma("sp", x[:, :], g.ptm[t * 128:(t + 1) * 128, PCQ:PCQ + 1024], r=["ptm_d"], w=[("in", b)])
                p.dma("sp", rp[b][:, :], I["rope"][t * 128:(t + 1) * 128, :], w=[("rp", b)])
                p.act(sq[:, :], x[:, 0:384], AF.Square, r=[("in", b)], w=["sq"])
                p.op("dve", lambda e: e.tensor_reduce(out=ms[:, :], in_=sq[:, :].rearrange("p (h f) -> p h f", f=64), axis=AX.X, op=ALU.add),
                     r=["sq"], w=["ms"])
                p.act(ms[:, :], ms[:, :], AF.Ln, bias=EPS, scale=1.0 / 64, r=["ms"], w=["ms"])
                p.act(ms[:, :], ms[:, :], AF.Exp, scale=-0.5, r=["ms"], w=["ms"])
                xg = x[:, 0:384].rearrange("p (h f) -> p h f", f=64)
                p.tt(xg, xg, ms[:, :].unsqueeze(2).broadcast_to([128, 6, 64]), ALU.mult, r=[("in", b), "ms"], w=[("in", b)])
                p.tt(xg, xg, gbc[:, :, :], ALU.mult, r=[("in", b), "gbc"], w=[("in", b)])
                cosb = rp[b][:, 0:32].rearrange("p (a f) -> p a f", a=2)
                sinb = rp[b][:, 32:64].rearrange("p (a f) -> p a f", a=2)
                cos6 = cosb.unsqueeze(1).broadcast_to([128, 6, 2, 16])
                sin6 = sinb.unsqueeze(1).broadcast_to([128, 6, 2, 16])
                for (c0, h0, nh) in ((0, 0, 6), (512, 6, 6)):
                    xv = x[:, c0:c0 + nh * 64].rearrange("p (h a t f) -> p h a t f", a=2, t=2, f=16)
                    ov = ro[:, h0:h0 + nh, :].rearrange("p h (a t f) -> p h a t f", a=2, t=2, f=16)
                    x1, x2 = xv[:, :, :, 0, :], xv[:, :, :, 1, :]
                    hs = slice(h0, h0 + nh)
                    p.tt(t1[:, hs, :, :], x1, cos6, ALU.mult, r=[("in", b), ("rp", b)], w=[("t1", h0)])
                    p.tt(t2[:, hs, :, :], x2, sin6, ALU.mult, r=[("in", b), ("rp", b)], w=[("t2", h0)], eng="pool")
                    p.tt(ov[:, :, :, 0, :], t1[:, hs, :, :], t2[:, hs, :, :], ALU.subtract, r=[("t1", h0), ("t2", h0)], w=["ro"])
                    p.tt(t3[:, hs, :, :], x2, cos6, ALU.mult, r=[("in", b), ("rp", b)], w=[("t3", h0)], eng="pool")
                    p.tt(t4[:, hs, :, :], x1, sin6, ALU.mult, r=[("in", b), ("rp", b)], w=[("t4", h0)])
                    p.tt(ov[:, :, :, 1, :], t3[:, hs, :, :], t4[:, hs, :, :], ALU.add, r=[("t3", h0), ("t4", h0)], w=["ro"], eng="pool")
                rof = ro[:, :, :].rearrange("p h f -> p (h f)")
                p.cp(tb[:, 0:2, :], rof[:, 0:256].rearrange("p (c f) -> p c f", f=128), r=["ro"], w=["tb"])
                p.cp(tb[:, 4:6, :], rof[:, 384:640].rearrange("p (c f) -> p c f", f=128), r=["ro"], w=["tb"])
                for kv in range(2):
                    for d in range(2):
                        p.cp(tb[:, 2 + kv, d * 64:(d + 1) * 64], ro[:, 4 + kv, :], r=["ro"], w=["tb"], eng="pool")
                        p.cp(tb[:, 6 + kv, d * 64:(d + 1) * 64], ro[:, 10 + kv, :], r=["ro"], w=["tb"], eng="pool")
                p.cp(vv[:, t, 0:2, 0:64], x[:, 384:512].rearrange("p (k f) -> p k f", f=64), r=[("in", b)], w=[("vv", t)], eng="act")
                p.cp(vv[:, t, 2:4, 0:64], x[:, 896:1024].rearrange("p (k f) -> p k f", f=64), r=[("in", b)], w=[("vv", t)], eng="act")
                for c in range(8):
                    p.mm(pst[b][:, c, :], tb[:, c, :], g.cstb[:, C_ID:C_ID + 128], r=["tb", "cstb"], w=[("pt", b)])
                sl = slice(t * 128, (t + 1) * 128)
                ce = "act" if b else "dve"
                p.cp(qT[:, 0:2, sl], pst[b][:, 0:2, :], r=[("pt", b)], w=[("qk", t)], eng=ce)
                p.cp(kT[:, 0:2, sl], pst[b][:, 2:4, :], r=[("pt", b)], w=[("qk", t)], eng=ce)
                p.cp(qT[:, 2:4, sl], pst[b][:, 4:6, :], r=[("pt", b)], w=[("qk", t)], eng=ce)
                p.cp(kT[:, 2:4, sl], pst[b][:, 6:8, :], r=[("pt", b)], w=[("qk", t)], eng=ce)
            p.barrier()
        with ExitStack() as es2:
            pss = [p.ps(es2, "ga_s%d" % i, [128, 512], F32) for i in range(3)]
            pso = [p.ps(es2, "ga_o%d" % i, [128, 512], F32) for i in range(2)]
            pt = [p.sb(es2, "ga_p%d" % i, [128, 512], BF16) for i in range(3)]
            rd = p.sb(es2, "ga_rd", [128, 512], F32)
            cs = 0
            co = 0
            for ti, (s0, n) in enumerate(FT):
                kts = range(2) if s0 == 0 else range(NT)
                for h in range(4):
                    c, hp, kv = h // 2, (h % 2) * 64, h // 2
                    ob = co % 2
                    co += 1
                    for kt in kts:
                        sb_ = cs % 3
                        cs += 1
                        p.mm(pss[sb_][:, 0:n], kT[hp:hp + 64, c, kt * 128:(kt + 1) * 128], qT[hp:hp + 64, c, s0:s0 + n],
                             w=[("gs", sb_)])
                        p.act(pt[sb_][:, 0:n], pss[sb_][:, 0:n], AF.Exp, scale=0.125, r=[("gs", sb_)], w=[("gp", sb_)])
                        p.mm(pso[ob][:, 0:n], vv[:, kt, kv, :], pt[sb_][:, 0:n], start=(kt == kts[0]), stop=(kt == kts[-1]),
                             r=[("gp", sb_)], w=[("go", ob)])
                    p.op("dve", lambda e: e.reciprocal(out=rd[0:64, 0:n], in_=pso[ob][64:128, 0:n]), r=[("go", ob)], w=["rd"])
                    p.tt(yo[hp:hp + 64, c, s0:s0 + n], pso[ob][0:64, 0:n], rd[0:64, 0:n], ALU.mult, r=[("go", ob), "rd"], w=[("yo", c)])
            p.barrier()
        with ExitStack() as es2:
            pss = [p.ps(es2, "wa_s%d" % i, [128, 8, 128], F32) for i in range(2)]
            pso = [p.ps(es2, "wa_o%d" % i, [128, 4, 128], F32) for i in range(2)]
            pt = [p.sb(es2, "wa_p%d" % i, [128, 8, 128], BF16) for i in range(2)]
            dt_ = p.sb(es2, "wa_d", [128, 4, 128], F32)
            rd = p.sb(es2, "wa_rd", [128, 4, 128], F32)
            co = 0
            for qb in range(NT):
                kl = [(0, None), (1, None)]
                if qb >= 2:
                    if qb - 1 >= 2:
                        kl.append((qb - 1, C_L))
                    kl.append((qb, None))
                    if qb + 1 < NT:
                        kl.append((qb + 1, C_U))
                nk = len(kl)
                qs = slice(qb * 128, (qb + 1) * 128)
                for h in range(4):
                    c, hp = 2 + h // 2, (h % 2) * 64
                    ob = co % 2
                    co += 1
                    for ki, (kt, msk) in enumerate(kl):
                        p.mm(pss[ob][:, ki, :], kT[hp:hp + 64, c, kt * 128:(kt + 1) * 128], qT[hp:hp + 64, c, qs], w=[("ws", ob)])
                    p.act(pt[ob][:, 0:nk, :], pss[ob][:, 0:nk, :], AF.Exp, scale=0.125, r=[("ws", ob)], w=[("wp", ob)])
                    for ki, (kt, msk) in enumerate(kl):
                        if msk is not None:
                            p.tt(pt[ob][:, ki, :], pt[ob][:, ki, :], g.cstb[:, msk:msk + 128], ALU.mult, r=[("wp", ob), "cstb"],
                                 w=[("wp", ob)], eng="pool")
                    for ki, (kt, msk) in enumerate(kl):
                        p.mm(pso[ob][:, 0, :], vv[:, kt, 2 + h // 2, :], pt[ob][:, ki, :], start=(ki == 0), stop=(ki == nk - 1),
                             r=[("wp", ob)], w=[("wo", ob)])
                    p.ts(dt_[64:128, 0, :], pso[ob][64:128, 0, :], es_[64:128, h:h + 1], op0=ALU.add, r=[("wo", ob), "es"], w=["wd"])
                    p.op("dve", lambda e: e.reciprocal(out=rd[0:64, 0, :], in_=dt_[64:128, 0, :]), r=["wd"], w=["wrd"])
                    p.tt(yo[hp:hp + 64, c, qs], pso[ob][0:64, 0, :], rd[0:64, 0, :], ALU.mult, r=[("wo", ob), "wrd"], w=[("yo", c)])
            p.barrier()
        for c in range(4):
            p.dma("sp", g.yT[4 + c, :, :], yo[:, c, :], w=["yT_d"])
        p.barrier()


def phase_ssd(g, l):
    p, I = g.p, g.I
    NC8 = NT * 8
    with ExitStack() as es:
        xsB = p.sb(es, "sd_xsB", [128, NT, 512], BF16)
        zdt = p.sb(es, "sd_zdt", [128, NT, 264], F32)
        ysum = p.sb(es, "sd_y", [128, NT, 256], F32)
        prm = p.sb(es, "sd_prm", [128, 20], F32)
        ng = p.sb(es, "sd_ng", [128, 256], F32)
        for t0 in range(0, NT, 4):
            nt = min(4, NT - t0)
            ts_ = slice(t0, t0 + nt)
            p.dma("sp", xsB[:, ts_, :], g.xbctm[t0 * 128:(t0 + nt) * 128, :].rearrange("(t q) c -> q t c", q=128),
                  r=["xbctm_d"], w=["xsB"])
            p.dma("act", zdt[:, ts_, :], g.ptm[t0 * 128:(t0 + nt) * 128, 0:264].rearrange("(t q) c -> q t c", q=128),
                  r=["ptm_d"], w=["zdt"])
        p.dma("sp", prm[:, 0:8], I["ssd_dt_bias"][l:l + 1].rearrange("o d h -> o (d h)").partition_broadcast(128), w=["prm"])
        p.dma("sp", prm[:, 8:16], I["ssd_a_log"][l:l + 1].rearrange("o d h -> o (d h)").partition_broadcast(128), w=["prm"])
        p.dma("sp", prm[:, 16:20], I["ssd_d"][l:l + 1, :].partition_broadcast(128), w=["prm"])
        p.dma("sp", ng[:, :], I["ssd_norm_g"][l:l + 1, :].partition_broadcast(128), w=["ng"])
        p.act(prm[:, 8:16], prm[:, 8:16], AF.Exp, r=["prm"], w=["prm"])
        p.ts(prm[:, 8:16], prm[:, 8:16], -1.0, r=["prm"], w=["prm"])
        with ExitStack() as es1:
            BCT = p.sb(es1, "sd_bct", [128, 4, S], BF16)
            dt = p.sb(es1, "sd_dt", [128, NT, 8], F32)
            la = p.sb(es1, "sd_la", [128, NT, 8], F32)
            cum = p.sb(es1, "sd_cum", [128, NT, 8], F32)
            tot = p.sb(es1, "sd_tot", [128, NT, 8], F32)
            eoff = p.sb(es1, "sd_eoff", [128, NT, 8], F32)
            dtdte = p.sb(es1, "sd_dte", [128, NT, 8], F32)
            etot = p.sb(es1, "sd_etot", [128, NT, 8], F32)
            ncum = p.sb(es1, "sd_ncum", [128, NT, 8], F32)
            nm4 = p.sb(es1, "sd_nm4", [128, 2, 4, 128], F32)
            laU = [p.sb(es1, "sd_laU%d" % i, [128, 4, 128], F32) for i in range(2)]
            dec = p.sb(es1, "sd_dec", [128, 4, 128], F32)
            WT = p.sb(es1, "sd_WT", [128, 4, 128], BF16)
            xdt = p.sb(es1, "sd_xdt", [128, 4, 64], BF16)
            xdte = p.sb(es1, "sd_xdte", [128, 4, 64], BF16)
            ydsb = p.sb(es1, "sd_yd", [128, 256], F32)
            Sst = p.sb(es1, "sd_S", [128, 4, 64], F32)
            Sb = p.sb(es1, "sd_Sb", [128, 4, 64], BF16)
            pc = p.ps(es1, "sd_pc", [128, NC8], F32)
            dps = [p.ps(es1, "sd_dps%d" % i, [128, 4, 128], F32) for i in range(2)]
            gps = p.ps(es1, "sd_gps", [128, 2, 128], F32)
            ydp = p.ps(es1, "sd_ydp", [128, 4, 64], F32)
            yop = p.ps(es1, "sd_yop", [128, 4, 64], F32)
            stp = p.ps(es1, "sd_stp", [128, 4, 64], F32)
            for c in range(4):
                p.dma("sp", BCT[:, c, :], g.xbcT[2 + c, :, :], r=["xbcT_d"], w=["BCT"])
            for d in range(2):
                for h in range(4):
                    nm = C_NMF if d == 0 else C_NMB
                    p.cp(nm4[:, d, h, :], g.cst[:, nm:nm + 128], r=["cst"], w=["nm4"], eng="pool")
            bc8 = lambda ap: ap.unsqueeze(1).broadcast_to([128, NT, 8])
            p.tt(dt[:, :, :], zdt[:, :, 256:264], bc8(prm[:, 0:8]), ALU.add, r=["zdt", "prm"], w=["dt"])
            p.act(dt[:, :, :], dt[:, :, :], AF.Exp, r=["dt"], w=["dt"])
            p.act(dt[:, :, :], dt[:, :, :], AF.Ln, bias=1.0, r=["dt"], w=["dt"])
            p.tt(la[:, :, :], dt[:, :, :], bc8(prm[:, 8:16]), ALU.mult, r=["dt", "prm"], w=["la"])
            laf = la[:, :, :].rearrange("p t c -> p (t c)")
            pc3 = pc[:, :].rearrange("p (t c) -> p t c", c=8)
            p.mm(pc[:, :], g.cst[:, C_U:C_U + 128], laf, r=["la", "cst"], w=["pc"])
            p.cp(cum[:, :, 0:4], pc3[:, :, 0:4], r=["pc"], w=["cum"])
            p.mm(pc[:, :], g.cst[:, C_L:C_L + 128], laf, r=["la", "cst"], w=["pc"])
            p.cp(cum[:, :, 4:8], pc3[:, :, 4:8], r=["pc"], w=["cum"])
            p.mm(pc[:, :], g.cst[:, C_ONE:C_ONE + 128], laf, r=["la", "cst"], w=["pc"])
            p.cp(tot[:, :, :], pc3, r=["pc"], w=["tot"])
            p.act(eoff[:, :, :], cum[:, :, :], AF.Exp, r=["cum"], w=["eoff"])
            p.act(etot[:, :, :], tot[:, :, :], AF.Exp, r=["tot"], w=["etot"])
            p.tt(dtdte[:, :, :], tot[:, :, :], cum[:, :, :], ALU.subtract, r=["tot", "cum"], w=["dtdte"])
            p.act(dtdte[:, :, :], dtdte[:, :, :], AF.Exp, r=["dtdte"], w=["dtdte"])
            p.tt(dtdte[:, :, :], dtdte[:, :, :], dt[:, :, :], ALU.mult, r=["dtdte", "dt"], w=["dtdte"])
            p.ts(ncum[:, :, :], cum[:, :, :], -1.0, r=["cum"], w=["ncum"])
            cnt = 0
            for d in range(min(2, SSD_LEVEL)):
                order = list(range(NT)) if d == 0 else [1, 0] + list(range(NT - 1, 1, -1))
                tri = C_U if d == 0 else C_L
                p.memset(Sst[:, :, :], 0.0, w=["S"])
                p.memset(Sb[:, :, :], 0.0, w=["Sb"])
                for t in order:
                    b = cnt % 2
                    cnt += 1
                    sl = slice(t * 128, (t + 1) * 128)
                    for h in range(4):
                        p.ts(laU[b][:, h, :], g.cst[:, tri:tri + 128], la[:, t, d * 4 + h:d * 4 + h + 1], r=["la", "cst"],
                             w=[("laU", b)], eng="pool" if h % 2 else "dve")
                    dflat = dps[b][:, :, :].rearrange("p h l -> p (h l)")
                    p.mm(dflat, g.cst[:, C_ONE:C_ONE + 128], laU[b][:, :, :].rearrange("p h l -> p (h l)"), start=True, stop=False,
                         r=[("laU", b), "cst"], w=[("dps", b)])
                    p.mm(dflat, g.cst[:, C_ID:C_ID + 128], nm4[:, d, :, :].rearrange("p h l -> p (h l)"), start=False, stop=True,
                         r=["nm4", "cst"], w=[("dps", b)])
                    for h in range(4):
                        p.act(dec[:, h, :], dps[b][:, h, :], AF.Exp, bias=ncum[:, t, d * 4 + h:d * 4 + h + 1],
                              r=[("dps", b), "ncum"], w=[("dec", h)])
                    for gq in range(2):
                        p.mm(gps[:, gq, :], BCT[:, gq, sl], BCT[:, 2 + gq, sl], r=["BCT"], w=[("gps", gq)])
                    for h in range(4):
                        p.tt(WT[:, h, :], dec[:, h, :], gps[:, h // 2, :], ALU.mult, r=[("dec", h), ("gps", h // 2)], w=[("WT", h)])
                    xs4 = xsB[:, t, 0:256].rearrange("p (h f) -> p h f", f=64)
                    p.tt(xdt[:, :, :], xs4, dt[:, t, d * 4:d * 4 + 4].unsqueeze(2).broadcast_to([128, 4, 64]), ALU.mult,
                         r=["xsB", "dt"], w=["xdt"], eng="pool")
                    p.tt(xdte[:, :, :], xs4, dtdte[:, t, d * 4:d * 4 + 4].unsqueeze(2).broadcast_to([128, 4, 64]), ALU.mult,
                         r=["xsB", "dtdte"], w=["xdte"], eng="pool")
                    for h in range(4):
                        p.mm(ydp[:, h, :], WT[:, h, :], xdt[:, h, :], r=[("WT", h), "xdt"], w=["ydp"])
                    for h in range(4):
                        p.mm(yop[:, h, :], BCT[:, 2 + h // 2, sl], Sb[:, h, :], r=["BCT", "Sb"], w=["yop"])
                    p.cp(ydsb[:, :], ydp[:, :, :].rearrange("p h f -> p (h f)"), r=["ydp"], w=["ydsb"], eng="act")
                    if d == 1:
                        p.tt(ydsb[:, :], ydsb[:, :], ysum[:, t, :], ALU.add, r=["ydsb", ("ysum", t)], w=["ydsb"])
                    for h in range(4):
                        p.stt(ysum[:, t, h * 64:(h + 1) * 64], yop[:, h, :], eoff[:, t, d * 4 + h:d * 4 + h + 1], ydsb[:, h * 64:(h + 1) * 64],
                              r=["yop", "eoff", "ydsb"], w=[("ysum", t)])
                    for h in range(4):
                        p.mm(stp[:, h, :], xsB[:, t, 256 + (h // 2) * 128:256 + (h // 2 + 1) * 128], xdte[:, h, :], r=["xsB", "xdte"], w=["stp"])
                    for h in range(4):
                        p.stt(Sst[:, h, :], Sst[:, h, :], etot[:, t, d * 4 + h:d * 4 + h + 1], stp[:, h, :], r=["S", "etot", "stp"], w=["S"])
                    p.cp(Sb[:, :, :], Sst[:, :, :], r=["S"], w=["Sb"], eng="act")
            p.barrier()
        with ExitStack() as es1:
          if SSD_LEVEL >= 3:
              sq = p.sb(es1, "sd_sq", [128, 256], F32)
              ms = p.sb(es1, "sd_ms", [128, NT], F32)
              yaT = p.sb(es1, "sd_yaT", [128, 2, S], BF16)
              yab = p.sb(es1, "sd_yab", [128, NT, 256], BF16)
              pst = [p.ps(es1, "sd_pt%d" % i, [128, 4, 128], F32) for i in range(2)]
              for t in range(NT):
                  e1 = "dve"
                  for h in range(4):
                      hs = slice(h * 64, (h + 1) * 64)
                      p.stt(ysum[:, t, hs], xsB[:, t, hs], prm[:, 16 + h:17 + h], ysum[:, t, hs], r=["xsB", "prm", ("ys", t)], w=[("ys", t)])
                  p.act(zdt[:, t, 0:256], zdt[:, t, 0:256], AF.Silu, r=[("z", t)], w=[("z", t)])
                  p.tt(ysum[:, t, :], ysum[:, t, :], zdt[:, t, 0:256], ALU.mult, r=[("z", t), ("ys", t)], w=[("ys", t)])
                  p.act(sq[:, :], ysum[:, t, :], AF.Square, r=[("ys", t)], w=["sq"])
                  p.op("dve", lambda e: e.tensor_reduce(out=ms[:, t:t + 1], in_=sq[:, :], axis=AX.X, op=ALU.add), r=["sq"], w=["ms"])
              p.act(ms[:, :], ms[:, :], AF.Ln, bias=EPS, scale=1.0 / 256, r=["ms"], w=["ms"])
              p.act(ms[:, :], ms[:, :], AF.Exp, scale=-0.5, r=["ms"], w=["ms"])
              for t in range(NT):
                  p.stt(yab[:, t, :], ysum[:, t, :], ms[:, t:t + 1], ng[:, :], op0=ALU.mult, op1=ALU.mult, r=[("ys", t), "ms", "ng"], w=["yab"])
              k = 0
              for c in (range(2) if SSD_LEVEL >= 4 else []):
                  for t0 in range(0, NT, 4):
                      nt = min(4, NT - t0)
                      b = k % 2
                      k += 1
                      for i in range(nt):
                          p.mm(pst[b][:, i, :], yab[:, t0 + i, c * 128:(c + 1) * 128], g.cstb[:, C_ID:C_ID + 128],
                               r=["yab", "cstb"], w=[("pt", b)])
                      p.cp(yaT[:, c, t0 * 128:(t0 + nt) * 128].rearrange("p (t s) -> p t s", t=nt), pst[b][:, 0:nt, :],
                           r=[("pt", b)], w=[("yaT", c)], eng="act" if b else "dve")
              for c in range(2):
                  p.dma("sp", g.yT[c, :, :], yaT[:, c, :], r=[("yaT", c)], w=["yT_d"])
              p.barrier()


def phase_s5(g, l):
    p, I = g.p, g.I
    TWO_PI = 2.0 * np.pi
    rt = lambda c: (1 - c) if c < 2 else (35 - c)
    with ExitStack() as es:
        u_tm = p.sb(es, "s5_utm", [128, NT, 256], F32)
        useq = p.sb(es, "s5_useq", [128, 2, S], F32)
        ysum = p.sb(es, "s5_ysum", [128, 2, S], F32)
        dcol = p.sb(es, "s5_d", [128, 2], F32)
        zero8 = p.sb(es, "s5_z8", [128, 8], F32)
        load_rows_T(g, None, dcol[:, :], I["s5_d"][l].rearrange("(c q) -> c q", q=128), 2, "dcol")
        p.memset(zero8[:, :], 0.0, w=["zero8"])
        for t0 in range(0, NT, 4):
            nt = min(4, NT - t0)
            p.dma("sp", u_tm[:, t0:t0 + nt, :], g.ptm[t0 * 128:(t0 + nt) * 128, PU:PU + 256].rearrange("(t q) c -> q t c", q=128),
                  r=["ptm_d"], w=["u_tm"])
        with ExitStack() as es1:
            bre = p.ps(es1, "s5_bre", [128, 8, 128], F32)
            bim = p.ps(es1, "s5_bim", [128, 8, 128], F32)
            pst = [bre, bim]
            ypl = [p.ps(es1, "s5_yp%d" % i, [128, 128], F32) for i in range(2)]
            ytl = [p.ps(es1, "s5_ytm%d" % i, [128, 128], F32) for i in range(2)]
            yp = ypl[0]
            prm = p.sb(es1, "s5_prm", [128, 16, 8], F32)
            T16 = p.sb(es1, "s5_T16", [128, 2, 16], F32)
            st16 = p.sb(es1, "s5_st16", [128, 16], F32)
            cosT = p.sb(es1, "s5_cos", [128, 8, 128], F32)
            sinT = p.sb(es1, "s5_sin", [128, 8, 128], F32)
            rhoT = p.sb(es1, "s5_rhoT", [128, 8, 128], F32)
            Mre = p.sb(es1, "s5_Mre", [128, 8, 128], F32)
            Mim = p.sb(es1, "s5_Mim", [128, 8, 128], F32)
            Bre = p.sb(es1, "s5_Bre", [128, 8, 128], F32)
            Bim = p.sb(es1, "s5_Bim", [128, 8, 128], F32)
            Cre = p.sb(es1, "s5_Cre", [128, 8, 128], F32)
            Cim = p.sb(es1, "s5_Cim", [128, 8, 128], F32)
            tA = p.sb(es1, "s5_tA", [128, 8, 128], F32)
            tB = p.sb(es1, "s5_tB", [128, 8, 128], F32)
            tC = p.sb(es1, "s5_tC", [128, 8, 128], F32)
            tD = p.sb(es1, "s5_tD", [128, 8, 128], F32)
            vre = p.sb(es1, "s5_vre", [128, 8, 128], F32)
            vim = p.sb(es1, "s5_vim", [128, 8, 128], F32)
            wre = p.sb(es1, "s5_wre", [128, 8, 128], F32)
            wim = p.sb(es1, "s5_wim", [128, 8, 128], F32)
            c8 = p.sb(es1, "s5_c8", [128, 2, 8], F32)
            rr, kf = wre, vre
            ki = wim[:, :, :].bitcast(I32)
            xre = [p.sb(es1, "s5_xre%d" % i, [128, 8, 128], F32) for i in range(2)]
            xim = [p.sb(es1, "s5_xim%d" % i, [128, 8, 128], F32) for i in range(2)]
            ysb = [p.sb(es1, "s5_ysb%d" % i, [128, 128], F32) for i in range(2)]
            ident = g.cst[:, C_ID:C_ID + 128]
            LR, LI, ST, RHO, THP, CO, SI, ABR, ABI, DEN, FRE, FIM, TMP, TMP2 = range(14)
            for d in range(2):
                k = 0
                for c in (range(2) if S5_LEVEL >= 0 else []):
                    for t0 in range(0, NT, 4):
                        nt = min(4, NT - t0)
                        b = k % 2
                        k += 1
                        for i in range(nt):
                            src = t0 + i if d == 0 else rt(t0 + i)
                            rhs = ident if d == 0 else g.cst[:, C_J:C_J + 128]
                            p.mm(pst[b][:, i, :], u_tm[:, src, c * 128:(c + 1) * 128], rhs, r=["u_tm", "cst"], w=[("pu", b)])
                        dst = useq[:, c, t0 * 128:(t0 + nt) * 128].rearrange("p (t s) -> p t s", t=nt)
                        p.cp(dst, pst[b][:, 0:nt, :], r=[("pu", b)], w=["useq"], eng="act")
                        if d == 0:
                            yd_ = ysum[:, c, t0 * 128:(t0 + nt) * 128].rearrange("p (t s) -> p t s", t=nt)
                            p.ts(yd_, dst, dcol[:, c:c + 1], r=["useq", "dcol"], w=["ysum"])
                if S5_LEVEL < 1:
                    continue
                for which, nm in ((0, "s5_lam_re"), (1, "s5_lam_im")):
                    p.dma("sp", tA[0:16, 0, 0:64], I[nm][l, d, :, :], w=["tA"])
                    p.tr(yp[0:64, 0:16], tA[0:16, 0, 0:64], g.cst[0:16, C_ID:C_ID + 16], r=["tA", "cst"], w=["yp"])
                    p.cp(T16[0:64, which, :], yp[0:64, 0:16], r=["yp"], w=["T16"])
                    tv = T16[0:64, which, :].rearrange("p (gb gl) -> p gl gb", gl=2)
                    p.cp(prm[0:64, which, :], tv[:, 0, :], r=["T16"], w=["prm"])
                    p.cp(prm[64:128, which, :], tv[:, 1, :], r=["T16"], w=["prm"])
                p.dma("sp", st16[:, :], I["s5_log_step"][l, d:d + 1, :].partition_broadcast(128), w=["st16"])
                p.act(st16[:, :], st16[:, :], AF.Exp, r=["st16"], w=["st16"])
                sv = st16[:, :].rearrange("p (gb gl) -> p gl gb", gl=2)
                p.cp(prm[0:64, ST, :], sv[0:64, 0, :], r=["st16"], w=["prm"])
                p.cp(prm[64:128, ST, :], sv[64:128, 1, :], r=["st16"], w=["prm"])
                P = lambda i: prm[:, i, :]
                p.ts(P(LR), P(LR), -1e-4, op0=ALU.min, r=["prm"], w=["prm"])
                p.tt(P(TMP), P(LR), P(ST), ALU.mult, r=["prm"], w=["prm"])
                p.act(P(RHO), P(TMP), AF.Exp, r=["prm"], w=["prm"])
                p.tt(P(THP), P(LI), P(ST), ALU.mult, r=["prm"], w=["prm"])
                p.ts(P(THP), P(THP), 1.0 / TWO_PI, r=["prm"], w=["prm"])
                if S5_LEVEL < 2:
                    continue
                for gb in range(8):
                    p.ts(rr[:, gb, :], g.cst[:, C_RAMP:C_RAMP + 128], prm[:, THP, gb:gb + 1], r=["prm", "cst"], w=["rr"],
                         eng="pool" if gb % 2 else "dve")
                for (tab, shift) in ((sinT, 0.0), (cosT, 0.25)):
                    for hf in range(2):
                        hs = slice(hf * 4, hf * 4 + 4)
                        if shift:
                            p.ts(kf[:, hs, :], rr[:, hs, :], shift, op0=ALU.add, r=["rr"], w=["kf"])
                            src = kf
                        else:
                            src = rr
                        p.cp(ki[:, hs, :], src[:, hs, :], r=["rr", "kf"], w=["ki"])
                        p.cp(tab[:, hs, :], ki[:, hs, :], r=["ki"], w=["tab"])
                        p.tt(tab[:, hs, :], src[:, hs, :], tab[:, hs, :], ALU.subtract, r=["tab", "rr", "kf"], w=["tab"])
                        p.act(tab[:, hs, :], tab[:, hs, :], AF.Sin, scale=TWO_PI, r=["tab"], w=["tab"])
                p.cp(P(CO), cosT[:, :, 0], r=["tab"], w=["prm"])
                p.cp(P(SI), sinT[:, :, 0], r=["tab"], w=["prm"])
                p.tt(P(ABR), P(RHO), P(CO), ALU.mult, r=["prm"], w=["prm"])
                p.tt(P(ABI), P(RHO), P(SI), ALU.mult, r=["prm"], w=["prm"])
                p.tt(P(DEN), P(LR), P(LR), ALU.mult, r=["prm"], w=["prm"])
                p.tt(P(TMP), P(LI), P(LI), ALU.mult, r=["prm"], w=["prm"])
                p.tt(P(DEN), P(DEN), P(TMP), ALU.add, r=["prm"], w=["prm"])
                p.op("dve", lambda e: e.reciprocal(out=P(DEN), in_=P(DEN)), r=["prm"], w=["prm"])
                p.ts(P(ABR), P(ABR), -1.0, op0=ALU.add, r=["prm"], w=["prm"])
                p.tt(P(TMP), P(ABR), P(LR), ALU.mult, r=["prm"], w=["prm"])
                p.tt(P(TMP2), P(ABI), P(LI), ALU.mult, r=["prm"], w=["prm"])
                p.tt(P(FRE), P(TMP), P(TMP2), ALU.add, r=["prm"], w=["prm"])
                p.tt(P(FRE), P(FRE), P(DEN), ALU.mult, r=["prm"], w=["prm"])
                p.tt(P(TMP), P(ABI), P(LR), ALU.mult, r=["prm"], w=["prm"])
                p.tt(P(TMP2), P(ABR), P(LI), ALU.mult, r=["prm"], w=["prm"])
                p.tt(P(FIM), P(TMP), P(TMP2), ALU.subtract, r=["prm"], w=["prm"])
                p.tt(P(FIM), P(FIM), P(DEN), ALU.mult, r=["prm"], w=["prm"])
                if S5_LEVEL < 3:
                    continue
                for m_ in (Mre, Mim):
                    for hf in range(2):
                        p.memset(m_[:, hf * 4:hf * 4 + 4, :], 0.0, w=["M"], eng="pool" if hf else "dve")
                for gi in range(16):
                    gb, gl, gic = gi // 2, gi % 2, gi % 8
                    p.dma("sp", Mre[gl * 64:(gl + 1) * 64, gb, gic * 16:(gic + 1) * 16], I["s5_b_re"][l, d, gi, :, :], r=[], w=["M"])
                    p.dma("act", Mim[gl * 64:(gl + 1) * 64, gb, gic * 16:(gic + 1) * 16], I["s5_b_im"][l, d, gi, :, :], r=[], w=["M"])
                for gb in range(8):
                    fr, fi = prm[:, FRE, gb:gb + 1], prm[:, FIM, gb:gb + 1]
                    p.ts(tA[:, 0, :], Mim[:, gb, :], fi, r=["M", "prm"], w=["tA"])
                    p.stt(Bre[:, gb, :], Mre[:, gb, :], fr, tA[:, 0, :], op0=ALU.mult, op1=ALU.subtract, r=["M", "prm", "tA"], w=["B0"])
                    p.ts(tB[:, 0, :], Mre[:, gb, :], fi, r=["M", "prm"], w=["tB"])
                    p.stt(Bim[:, gb, :], Mim[:, gb, :], fr, tB[:, 0, :], op0=ALU.mult, op1=ALU.add, r=["M", "prm", "tB"], w=["B0"])
                for (src, dst, key) in ((Bre, Mre, "BT"), (Bim, Mim, "BT")):
                    for hf in range(2):
                        b = hf
                        for j in range(4):
                            p.tr(pst[b][:, j, :], src[:, hf * 4 + j, :], ident, r=["B0", "cst"], w=[("pu", b)])
                        p.cp(dst[:, hf * 4:hf * 4 + 4, :], pst[b][:, 0:4, :], r=[("pu", b)], w=[key], eng="act")
                BTre, BTim = Mre, Mim
                for m_ in (Bre, Bim):
                    for hf in range(2):
                        p.memset(m_[:, hf * 4:hf * 4 + 4, :], 0.0, w=["B0"], eng="pool" if hf else "dve")
                for gi in range(16):
                    gb, gl, gic = gi // 2, gi % 2, gi % 8
                    p.dma("sp", Bre[gic * 16:(gic + 1) * 16, gb, gl * 64:(gl + 1) * 64], I["s5_c_re"][l, d, gi, :, :], r=[], w=["B0"])
                    p.dma("act", Bim[gic * 16:(gic + 1) * 16, gb, gl * 64:(gl + 1) * 64], I["s5_c_im"][l, d, gi, :, :], r=[], w=["B0"])
                for (src, dst, neg) in ((Bre, Cre, False), (Bim, Cim, True)):
                    for hf in range(2):
                        b = hf
                        for j in range(4):
                            p.tr(pst[b][:, j, :], src[:, hf * 4 + j, :], ident, r=["B0", "cst"], w=[("pu", b)])
                        if neg:
                            p.ts(dst[:, hf * 4:hf * 4 + 4, :], pst[b][:, 0:4, :], -1.0, r=[("pu", b)], w=["CT"])
                        else:
                            p.cp(dst[:, hf * 4:hf * 4 + 4, :], pst[b][:, 0:4, :], r=[("pu", b)], w=["CT"], eng="act")
                if S5_LEVEL < 4:
                    continue
                for gb in range(8):
                    p.ts(rhoT[:, gb, :], g.cst[:, C_ONE:C_ONE + 128], prm[:, RHO, gb:gb + 1], r=["prm", "cst"], w=["rhoT"],
                         eng="pool" if gb % 2 else "dve")
                p.memset(rhoT[:, :, 0:1], 0.0, w=["rhoT"])
                p.barrier()
                fl = lambda t_: t_[:, :, :].rearrange("p g s -> p (g s)")
                for c in range(NT):
                    cur, prv = c % 2, (c + 1) % 2
                    ps_ = slice(c * 128, (c + 1) * 128)
                    for gb in range(8):
                        p.mm(bre[:, gb, :], BTre[:, gb, :], useq[:, gb // 4, ps_], r=["BT", "useq"], w=["bre"])
                    for gb in range(8):
                        p.mm(bim[:, gb, :], BTim[:, gb, :], useq[:, gb // 4, ps_], r=["BT", "useq"], w=["bim"])
                    p.tt(tA[:, :, :], bre[:, :, :], cosT[:, :, :], ALU.mult, r=["bre", "tab"], w=["tA"])
                    p.tt(tB[:, :, :], bim[:, :, :], sinT[:, :, :], ALU.mult, r=["bim", "tab"], w=["tB"])
                    p.tt(vre[:, :, :], tA[:, :, :], tB[:, :, :], ALU.add, r=["tA", "tB"], w=["vre"], eng="pool")
                    p.tt(tC[:, :, :], bim[:, :, :], cosT[:, :, :], ALU.mult, r=["bim", "tab"], w=["tC"])
                    p.tt(tD[:, :, :], bre[:, :, :], sinT[:, :, :], ALU.mult, r=["bre", "tab"], w=["tD"])
                    p.tt(vim[:, :, :], tC[:, :, :], tD[:, :, :], ALU.subtract, r=["tC", "tD"], w=["vim"], eng="pool")
                    if c > 0:
                        p.tt(c8[:, 0, :], xre[prv][:, :, 127], prm[:, RHO, :], ALU.mult, r=[("x", prv), "prm"], w=["c8"], eng="pool")
                        p.tt(vre[:, :, 0], vre[:, :, 0], c8[:, 0, :], ALU.add, r=["c8", "vre"], w=["vre"], eng="pool")
                        p.tt(c8[:, 1, :], xim[prv][:, :, 127], prm[:, RHO, :], ALU.mult, r=[("x", prv), "prm"], w=["c8b"], eng="pool")
                        p.tt(vim[:, :, 0], vim[:, :, 0], c8[:, 1, :], ALU.add, r=["c8b", "vim"], w=["vim"], eng="pool")
                    p.op("dve", lambda e: e.tensor_tensor_scan(out=fl(wre), data0=fl(rhoT), data1=fl(vre), initial=0.0,
                                                               op0=ALU.mult, op1=ALU.add), r=["vre", "rhoT"], w=["wre"])
                    p.op("dve", lambda e: e.tensor_tensor_scan(out=fl(wim), data0=fl(rhoT), data1=fl(vim), initial=0.0,
                                                               op0=ALU.mult, op1=ALU.add), r=["vim", "rhoT"], w=["wim"])
                    p.tt(tA[:, :, :], wre[:, :, :], cosT[:, :, :], ALU.mult, r=["wre", "tab"], w=["tA"], eng="pool")
                    p.tt(tB[:, :, :], wim[:, :, :], sinT[:, :, :], ALU.mult, r=["wim", "tab"], w=["tB"], eng="pool")
                    p.tt(xre[cur][:, :, :], tA[:, :, :], tB[:, :, :], ALU.subtract, r=["tA", "tB"], w=[("x", cur)], eng="pool")
                    p.tt(tC[:, :, :], wre[:, :, :], sinT[:, :, :], ALU.mult, r=["wre", "tab"], w=["tC"])
                    p.tt(tD[:, :, :], wim[:, :, :], cosT[:, :, :], ALU.mult, r=["wim", "tab"], w=["tD"])
                    p.tt(xim[cur][:, :, :], tC[:, :, :], tD[:, :, :], ALU.add, r=["tC", "tD"], w=[("x", cur)])
                    for hf in range(2):
                        if d == 0:
                            ypt = ypl[hf]
                            for j in range(4):
                                gb = hf * 4 + j
                                p.mm(ypt[:, :], Cre[:, gb, :], xre[cur][:, gb, :], start=(j == 0), stop=False, r=["CT", ("x", cur)], w=[("yp", hf)])
                                p.mm(ypt[:, :], Cim[:, gb, :], xim[cur][:, gb, :], start=False, stop=(j == 3), r=["CT", ("x", cur)], w=[("yp", hf)])
                            p.tt(ysum[:, hf, ps_], ypt[:, :], ysum[:, hf, ps_], ALU.add, r=[("yp", hf), "ysum"], w=["ysum"])
                        else:
                            ytm, ypt = ytl[hf], ypl[hf]
                            for j in range(4):
                                gb = hf * 4 + j
                                p.mm(ytm[:, :], xre[cur][:, gb, :], Cre[:, gb, :], start=(j == 0), stop=False, r=["CT", ("x", cur)], w=[("ytm", hf)])
                                p.mm(ytm[:, :], xim[cur][:, gb, :], Cim[:, gb, :], start=False, stop=(j == 3), r=["CT", ("x", cur)], w=[("ytm", hf)])
                            p.cp(ysb[hf][:, :], ytm[:, :], r=[("ytm", hf)], w=[("ysb", hf)], eng="act")
                            p.mm(ypt[:, :], ysb[hf][:, :], g.cst[:, C_J:C_J + 128], r=[("ysb", hf), "cst"], w=[("yp", hf)])
                            os_ = slice(rt(c) * 128, (rt(c) + 1) * 128)
                            p.tt(ysum[:, hf, os_], ypt[:, :], ysum[:, hf, os_], ALU.add, r=[("yp", hf), "ysum"], w=["ysum"])
                p.barrier()
        with ExitStack() as es1:
            wg = p.sb(es1, "s5_wg", [128, 2, 512], BF16)
            vT = p.sb(es1, "s5_vT", [128, 2, 512], BF16)
            t1 = p.sb(es1, "s5_t1", [128, 512], F32)
            t2 = p.sb(es1, "s5_t2", [128, 512], F32)
            sg = p.sb(es1, "s5_sg", [128, 512], F32)
            yb = [p.sb(es1, "s5_yb%d" % i, [128, 2, 512], BF16) for i in range(2)]
            pv = [p.ps(es1, "s5_pv%d" % i, [128, 512], F32) for i in range(2)]
            pg = [p.ps(es1, "s5_pg%d" % i, [128, 512], F32) for i in range(2)]
            for kc in range(2):
                p.dma("pool", wg[:, kc, :], I["s5_glu_w"][l, kc * 128:(kc + 1) * 128, :], w=["wg"])
            k = 0
            for ti, (s0, n) in enumerate(FT if S5_LEVEL >= 5 else []):
                ob = ti % 2
                for c in range(2):
                    x = ysum[:, c, s0:s0 + n]
                    p.tt(t1[:, 0:n], x, x, ALU.mult, r=["ysum"], w=["t1"])
                    p.ts(t1[:, 0:n], t1[:, 0:n], 0.044715, 1.0, op0=ALU.mult, op1=ALU.add, r=["t1"], w=["t1"])
                    p.tt(t1[:, 0:n], t1[:, 0:n], x, ALU.mult, r=["t1", "ysum"], w=["t1"])
                    p.act(t2[:, 0:n], t1[:, 0:n], AF.Tanh, scale=0.7978845608028654, r=["t1"], w=["t2"])
                    p.ts(t2[:, 0:n], t2[:, 0:n], 0.5, 0.5, op0=ALU.mult, op1=ALU.add, r=["t2"], w=["t2"], eng="pool")
                    p.tt(vT[:, c, 0:n], t2[:, 0:n], x, ALU.mult, r=["t2", "ysum"], w=[("vT", c)])
                for c in range(2):
                    q = k % 2
                    k += 1
                    for kc in range(2):
                        p.mm(pv[q][:, 0:n], wg[:, kc, c * 128:(c + 1) * 128], vT[:, kc, 0:n], start=(kc == 0), stop=(kc == 1),
                             r=["wg", ("vT", kc)], w=[("pv", q)])
                    for kc in range(2):
                        p.mm(pg[q][:, 0:n], wg[:, kc, 256 + c * 128:256 + (c + 1) * 128], vT[:, kc, 0:n], start=(kc == 0), stop=(kc == 1),
                             r=["wg", ("vT", kc)], w=[("pg", q)])
                    p.act(sg[:, 0:n], pg[q][:, 0:n], AF.Sigmoid, r=[("pg", q)], w=["sg"])
                    p.tt(yb[ob][:, c, 0:n], pv[q][:, 0:n], sg[:, 0:n], ALU.mult, r=[("pv", q), "sg"], w=[("yb", ob)])
                p.dma("sp", g.yT[2:4, :, s0:s0 + n].rearrange("c p s -> p c s"), yb[ob][:, :, 0:n], r=[("yb", ob)], w=["yT_d"])
            p.barrier()


def phase_merge(g, l):
    p, I = g.p, g.I
    wv = I["w_in"][l].rearrange("(kc q) n -> q kc n", q=128)
    with ExitStack() as es:
        wg = p.sb(es, "mg_wg", [128, KC, 4096], BF16)
        wb = p.sb(es, "mg_wb", [128, 8, D], BF16)
        wo = p.sb(es, "mg_wo", [128, KC, D], BF16)
        ht = [p.sb(es, "mg_h%d" % i, [128, KC, 512], BF16) for i in range(2)]
        yt = [p.sb(es, "mg_y%d" % i, [128, 8, 512], BF16) for i in range(2)]
        xt = [p.sb(es, "mg_x%d" % i, [128, KC, 512], F32) for i in range(2)]
        sg = [p.sb(es, "mg_s%d" % i, [128, 512], F32) for i in range(2)]
        acc = p.sb(es, "mg_acc", [128, 512], F32)
        tmp = p.sb(es, "mg_tmp", [128, 512], F32)
        accT = p.sb(es, "mg_aT", [128, KC, 512], BF16)
        psg = [p.ps(es, "mg_pg%d" % i, [128, 512], F32) for i in range(2)]
        psb = [p.ps(es, "mg_pb%d" % i, [128, 512], F32) for i in range(2)]
        pso = [p.ps(es, "mg_po%d" % i, [128, 512], F32) for i in range(2)]
        for kc in range(KC):
            p.dma("pool", wg[:, kc, :], wv[:, kc, 2312:6408], w=[("wg", kc)])
            p.dma("pool", wo[:, kc, :], I["w_out"][l, kc * 128:(kc + 1) * 128, :], w=[("wo", kc)])
            p.dma("pool", wb[:, kc, :], I["w_branch"][l, kc // 2, (kc % 2) * 128:(kc % 2 + 1) * 128, :], w=[("wb", kc)])
        cg = 0
        co = 0
        for ti, (s0, n) in enumerate(FT[1:] if l == DEPTH - 1 else FT):
            b = ti % 2
            ic = 1 if s0 == 0 else 0
            p.dma("sp", ht[b][:, :, 0:n], g.hT[:, :, s0:s0 + n].rearrange("c p s -> p c s"), r=["hT_d"], w=[("h", b)])
            p.dma("sp", yt[b][:, :, 0:n], g.yT[:, :, s0:s0 + n].rearrange("c p s -> p c s"), r=["yT_d"], w=[("y", b)])
            p.dma("sp", xt[b][:, :, 0:n], g.xres[:, :, s0:s0 + n].rearrange("c p s -> p c s"), r=["xres"], w=[("x", b)])
            for oc in range(KC):
                for br in range(4):
                    q = cg % 2
                    cg += 1
                    for kc in range(KC):
                        c0 = br * D + oc * 128
                        p.mm(psg[q][:, 0:n], wg[:, kc, c0:c0 + 128], ht[b][:, kc, 0:n], start=(kc == 0), stop=(kc == KC - 1),
                             r=[("wg", kc), ("h", b)], w=[("pg", q)])
                    for k2 in range(2):
                        p.mm(psb[q][:, 0:n], wb[:, br * 2 + k2, oc * 128:(oc + 1) * 128], yt[b][:, br * 2 + k2, 0:n], start=(k2 == 0),
                             stop=(k2 == 1), r=[("wb", br * 2 + k2), ("y", b)], w=[("pb", q)])
                    p.act(sg[q][:, 0:n], psg[q][:, 0:n], AF.Sigmoid, r=[("pg", q)], w=[("sg", q)])
                    if br == 0:
                        p.tt(acc[:, 0:n], psb[q][:, 0:n], sg[q][:, 0:n], ALU.mult, r=[("pb", q), ("sg", q)], w=["acc"])
                    else:
                        p.tt(tmp[:, 0:n], psb[q][:, 0:n], sg[q][:, 0:n], ALU.mult, r=[("pb", q), ("sg", q)], w=["tmp"])
                        p.tt(acc[:, 0:n], acc[:, 0:n], tmp[:, 0:n], ALU.add, r=["tmp", "acc"], w=["acc"], eng="pool")
                p.cp(accT[:, oc, 0:n], acc[:, 0:n], r=["acc"], w=[("aT", oc)], eng="act")
            for oc in range(KC):
                q = co % 2
                co += 1
                for kc in range(KC):
                    p.mm(pso[q][:, 0:n], wo[:, kc, oc * 128:(oc + 1) * 128], accT[:, kc, 0:n], start=(kc == 0), stop=(kc == KC - 1),
                         r=[("wo", kc), ("aT", kc)], w=[("po", q)])
                p.stt(xt[b][:, oc, 0:n], pso[q][:, 0:n], g.mod[:, 16 + oc, ic:ic + 1], xt[b][:, oc, 0:n], op0=ALU.mult, op1=ALU.add,
                      r=[("po", q), "mod", ("x", b)], w=[("x", b)])
            p.dma("sp", g.xres[:, :, s0:s0 + n].rearrange("c p s -> p c s"), xt[b][:, :, 0:n], r=[("x", b)], w=["xres"])
        p.barrier()


def phase_ffn(g, l):
    p, I = g.p, g.I
    moe = (l % 2 == 1)
    m = l // 2
    if moe:
        H, GS = EXPERT_DIM, 4
        experts = [(I["moe_w_in"][m, e], I["moe_w_out"][m, e]) for e in range(8)]
    else:
        H, GS = FFN_DIM, 2
        experts = [(I["ffn_w_in"][m], I["ffn_w_out"][m])]
    HC = H // 128
    NG = HC // GS
    with ExitStack() as es:
        wT = p.sb(es, "ff_wT", [8, S], F32)
        if moe:
            with ExitStack() as es0:
                rw = p.sb(es0, "ff_rw", [128, KC, 8], F32)
                lsb = p.sb(es0, "ff_lsb", [128, 8], F32)
                m8 = p.sb(es0, "ff_m8", [128, 8], F32)
                gt = p.sb(es0, "ff_gt", [128, 4], F32)
                e1 = p.sb(es0, "ff_e1", [128, 8], F32)
                e2 = p.sb(es0, "ff_e2", [128, 8], F32)
                lg = p.ps(es0, "ff_lg", [128, 8], F32)
                wtp = p.ps(es0, "ff_wtp", [8, 128], F32)
                p.dma("sp", rw[:, :, :], I["moe_router"][m].rearrange("(kc q) e -> q kc e", q=128), w=["rw"])

                def sink(ti, s0, n, ht, key, h32):
                    p.dma("sp", g.hT[:, :, s0:s0 + n].rearrange("c p s -> p c s"), ht[:, :, 0:n], r=[key], w=["hT_d"])
                    for sub in range(n // 128):
                        ss = slice(sub * 128, (sub + 1) * 128)
                        for kc in range(KC):
                            p.mm(lg[:, :], h32[:, kc, ss], rw[:, kc, :], start=(kc == 0), stop=(kc == KC - 1),
                                 r=[("nmsq", kc), "rw"], w=["lg"])
                        p.cp(lsb[:, :], lg[:, :], r=["lg"], w=["lsb"])
                        p.op("dve", lambda e: e.max(out=m8[:, :], in_=lsb[:, :]), r=["lsb"], w=["m8"])
                        p.tt(gt[:, 0:1], m8[:, 0:1], m8[:, 1:2], ALU.subtract, r=["m8"], w=["gt"])
                        p.act(gt[:, 1:2], gt[:, 0:1], AF.Sigmoid, r=["gt"], w=["gt1"])
                        p.act(gt[:, 2:3], gt[:, 0:1], AF.Sigmoid, scale=-1.0, r=["gt"], w=["gt2"])
                        p.ts(e1[:, :], lsb[:, :], m8[:, 0:1], gt[:, 1:2], op0=ALU.is_equal, op1=ALU.mult, r=["lsb", "m8", "gt1"], w=["e1"])
                        p.ts(e2[:, :], lsb[:, :], m8[:, 1:2], gt[:, 2:3], op0=ALU.is_equal, op1=ALU.mult, r=["lsb", "m8", "gt2"], w=["e2"])
                        p.tt(e1[:, :], e1[:, :], e2[:, :], ALU.add, r=["e1", "e2"], w=["e1"])
                        p.tr(wtp[:, :], e1[:, :], g.cst[:, C_ID:C_ID + 128], r=["e1", "cst"], w=["wtp"])
                        p.cp(wT[0:8, s0 + sub * 128:s0 + (sub + 1) * 128], wtp[:, :], r=["wtp"], w=["wT"])

                norm_tiles(g, 1, sink, want32=True)
        else:
            def sink(ti, s0, n, ht, key, h32):
                p.dma("sp", g.hT[:, :, s0:s0 + n].rearrange("c p s -> p c s"), ht[:, :, 0:n], r=[key], w=["hT_d"])

            norm_tiles(g, 1, sink)
        ht = [p.sb(es, "ff_h%d" % i, [128, KC, 512], BF16) for i in range(2)]
        wg = [p.sb(es, "ff_wg%d" % i, [128, KC, GS * 128], BF16) for i in range(2)]
        wu = [p.sb(es, "ff_wu%d" % i, [128, KC, GS * 128], BF16) for i in range(2)]
        wo = p.sb(es, "ff_wo", [128, HC, D], BF16)
        actT = p.sb(es, "ff_act", [128, HC, 512], BF16)
        oacc = p.sb(es, "ff_oacc", [128, KC, 512], F32)
        xt = p.sb(es, "ff_x", [128, KC, 512], F32)
        sgt = [p.sb(es, "ff_sg%d" % i, [128, 512], F32) for i in range(2)]
        tmp = [p.sb(es, "ff_tmp%d" % i, [128, 512], F32) for i in range(2)]
        wbc = p.sb(es, "ff_wbc", [128, 512], F32)
        psg = [p.ps(es, "ff_pg%d" % i, [128, 512], F32) for i in range(2)]
        psu = [p.ps(es, "ff_pu%d" % i, [128, 512], F32) for i in range(2)]
        pso = [p.ps(es, "ff_po%d" % i, [128, 512], F32) for i in range(2)]
        psw = p.ps(es, "ff_pw", [128, 512], F32)
        gcnt = 0
        jc = 0
        oc_ = 0
        tiles = FT[1:] if l == DEPTH - 1 else FT
        for ti, (s0, n) in enumerate(tiles):
            hb = ti % 2
            ic = 1 if s0 == 0 else 0
            p.dma("sp", ht[hb][:, :, 0:n], g.hT[:, :, s0:s0 + n].rearrange("c p s -> p c s"), r=["hT_d"], w=[("h", hb)])
            p.dma("sp", xt[:, :, 0:n], g.xres[:, :, s0:s0 + n].rearrange("c p s -> p c s"), r=["xres"], w=["x"])
            for ei, (w_in, w_out) in enumerate(experts):
                wiv = w_in.rearrange("(kc q) n -> q kc n", q=128)
                wov = w_out.rearrange("(hc q) n -> q hc n", q=128)
                if moe:
                    p.mm(psw[:, 0:n], g.cst[0:8, C_SEL + ei * 128:C_SEL + (ei + 1) * 128], wT[0:8, s0:s0 + n], r=["wT", "cst"], w=["psw"])
                    p.cp(wbc[:, 0:n], psw[:, 0:n], r=["psw"], w=["wbc"], eng="act")
                wibv = g.wib[ei, :, 0:2 * H].rearrange("(kc q) n -> q kc n", q=128)
                wobv = g.wob[ei, 0:H, :].rearrange("(hc q) n -> q hc n", q=128)
                for h0 in range(0, HC, 4):
                    hn = min(4, HC - h0)
                    if ti == 0:
                        p.dma("pool", wo[:, h0:h0 + hn, :], wov[:, h0:h0 + hn, :], w=[("wo", h0)])
                        p.dma("sp", wobv[:, h0:h0 + hn, :], wo[:, h0:h0 + hn, :], r=[("wo", h0)], w=[("wobd", ei, h0)])
                    else:
                        p.dma("sp", wo[:, h0:h0 + hn, :], wobv[:, h0:h0 + hn, :], r=[("wobd", ei, h0)], w=[("wo", h0)])
                for gi in range(NG):
                    gb = gcnt % 2
                    gcnt += 1
                    c0 = gi * GS * 128
                    if ti == 0:
                        p.dma("pool", wg[gb][:, :, :], wiv[:, :, c0:c0 + GS * 128], w=[("wg", gb)])
                        p.dma("pool", wu[gb][:, :, :], wiv[:, :, H + c0:H + c0 + GS * 128], w=[("wu", gb)])
                        p.dma("sp", wibv[:, :, c0:c0 + GS * 128], wg[gb][:, :, :], r=[("wg", gb)], w=[("wibd", ei, gi, 0)])
                        p.dma("sp", wibv[:, :, H + c0:H + c0 + GS * 128], wu[gb][:, :, :], r=[("wu", gb)], w=[("wibd", ei, gi, 1)])
                    else:
                        p.dma("sp", wg[gb][:, :, :], wibv[:, :, c0:c0 + GS * 128], r=[("wibd", ei, gi, 0)], w=[("wg", gb)])
                        p.dma("sp", wu[gb][:, :, :], wibv[:, :, H + c0:H + c0 + GS * 128], r=[("wibd", ei, gi, 1)], w=[("wu", gb)])
                    for j in range(GS):
                        q = jc % 2
                        jc += 1
                        hc = gi * GS + j
                        for kc in range(KC):
                            p.mm(psg[q][:, 0:n], wg[gb][:, kc, j * 128:(j + 1) * 128], ht[hb][:, kc, 0:n], start=(kc == 0),
                                 stop=(kc == KC - 1), r=[("wg", gb), ("h", hb)], w=[("pg", q)])
                        for kc in range(KC):
                            p.mm(psu[q][:, 0:n], wu[gb][:, kc, j * 128:(j + 1) * 128], ht[hb][:, kc, 0:n], start=(kc == 0),
                                 stop=(kc == KC - 1), r=[("wu", gb), ("h", hb)], w=[("pu", q)])
                        p.act(sgt[q][:, 0:n], psg[q][:, 0:n], AF.Silu, r=[("pg", q)], w=[("sg", q)])
                        if moe:
                            p.tt(tmp[q][:, 0:n], psu[q][:, 0:n], sgt[q][:, 0:n], ALU.mult, r=[("pu", q), ("sg", q)], w=[("tmp", q)])
                            p.tt(actT[:, hc, 0:n], tmp[q][:, 0:n], wbc[:, 0:n], ALU.mult, r=[("tmp", q), "wbc"], w=[("act", hc)], eng="pool")
                        else:
                            p.tt(actT[:, hc, 0:n], psu[q][:, 0:n], sgt[q][:, 0:n], ALU.mult, r=[("pu", q), ("sg", q)], w=[("act", hc)])
                for oc in range(KC):
                    q = oc_ % 2
                    oc_ += 1
                    for hc in range(HC):
                        p.mm(pso[q][:, 0:n], wo[:, hc, oc * 128:(oc + 1) * 128], actT[:, hc, 0:n], start=(hc == 0), stop=(hc == HC - 1),
                             r=[("wo", (hc // 4) * 4), ("act", hc)], w=[("po", q)])
                    if ei == 0:
                        p.cp(oacc[:, oc, 0:n], pso[q][:, 0:n], r=[("po", q)], w=[("oacc", oc)])
                    else:
                        p.tt(oacc[:, oc, 0:n], pso[q][:, 0:n], oacc[:, oc, 0:n], ALU.add, r=[("po", q), ("oacc", oc)], w=[("oacc", oc)])
            for oc in range(KC):
                p.stt(xt[:, oc, 0:n], oacc[:, oc, 0:n], g.mod[:, 40 + oc, ic:ic + 1], xt[:, oc, 0:n], op0=ALU.mult, op1=ALU.add,
                      r=[("oacc", oc), "mod", "x"], w=["x"])
            p.dma("sp", g.xres[:, :, s0:s0 + n].rearrange("c p s -> p c s"), xt[:, :, 0:n], r=["x"], w=["xres"])
        p.barrier()


def layer(g, l, stop):
    phases = [("ada", adaln), ("n1", phase_norm1), ("ptm", phase_ptm), ("xbc", phase_xbc), ("ssd", phase_ssd), ("s5", phase_s5), ("attn", phase_attn), ("merge", phase_merge), ("ffn", phase_ffn)]
    for name, fn in phases:
        fn(g, l)
        if stop is not None and stop == (l, name):
            return


def final(g, out, real):
    p, I = g.p, g.I
    if not real:
        with ExitStack() as es:
            z = p.sb(es, "fz", [128, D], F32)
            p.memset(z[:, :], 0.0, w=["fz"])
            p.dma("sp", out[0:128, :], z[:, :], r=["fz"], w=["out"])
            p.barrier()
        return
    with ExitStack() as es:
        gf = p.sb(es, "fn_g", [128, 8], F32)
        load_rows_T(g, None, gf[:, :], I["final_norm_g"].rearrange("(j q) -> j q", q=128), 8, "gf")
        xt = [p.sb(es, "fn_x%d" % i, [128, KC, 512], F32) for i in range(2)]
        sq = p.sb(es, "fn_sq", [128, KC, 512], F32)
        rs = p.sb(es, "fn_rs", [128, 512], F32)
        ot = [p.sb(es, "fn_o%d" % i, [128, KC, 128], F32) for i in range(2)]
        ssq = p.ps(es, "fn_ps", [128, 512], F32)
        pst = [p.ps(es, "fn_pt%d" % i, [128, KC, 128], F32) for i in range(2)]
        cnt = 0
        for ti, (s0, n) in enumerate(FT[1:]):
            b = ti % 2
            p.dma("sp", xt[b][:, :, 0:n], g.xres[:, :, s0:s0 + n].rearrange("c p s -> p c s"), r=["xres"], w=[("fx", b)])
            for c in range(KC):
                p.act(sq[:, c, 0:n], xt[b][:, c, 0:n], AF.Square, r=[("fx", b)], w=[("fsq", c)])
                p.mm(ssq[:, 0:n], g.cst[:, C_ONE:C_ONE + 128], sq[:, c, 0:n], start=(c == 0), stop=(c == KC - 1),
                     r=[("fsq", c), "cst"], w=["fps"])
            p.act(rs[:, 0:n], ssq[:, 0:n], AF.Ln, bias=EPS, scale=1.0 / D, r=["fps"], w=["frs"])
            p.act(rs[:, 0:n], rs[:, 0:n], AF.Exp, scale=-0.5, r=["frs"], w=["frs"])
            for c in range(KC):
                p.stt(sq[:, c, 0:n], xt[b][:, c, 0:n], gf[:, c:c + 1], rs[:, 0:n], op0=ALU.mult, op1=ALU.mult,
                      r=[("fx", b), "frs", "gf"], w=[("fsq", c)])
            for sub in range(n // 128):
                ob = cnt % 2
                cnt += 1
                for c in range(KC):
                    p.tr(pst[ob][:, c, :], sq[:, c, sub * 128:(sub + 1) * 128], g.cst[:, C_ID:C_ID + 128],
                         r=[("fsq", c), "cst"], w=[("fpt", ob)])
                p.cp(ot[ob][:, :, :], pst[ob][:, :, :], r=[("fpt", ob)], w=[("fo", ob)], eng="act" if ob else "dve")
                t0 = s0 - NCTX + sub * 128
                p.dma("sp", out[t0:t0 + 128, :], ot[ob][:, :, :].rearrange("p c f -> p (c f)"), r=[("fo", ob)], w=["out"])
        p.barrier()


_NC_CACHE = {}


def kernel(**inputs):
    x = np.asarray(inputs["x"], np.float32)
    nb = x.shape[0]
    if "nc" not in _NC_CACHE:
        _NC_CACHE["nc"] = build()[0]
    nc = _NC_CACHE["nc"]
    consts = make_consts()
    rope = make_rope()
    shared = {k: np.ascontiguousarray(np.asarray(v, np.float32)) for k, v in inputs.items() if k not in ("x", "c", "ctx", "c_ctx")}
    c = np.asarray(inputs["c"], np.float32)
    cctx = np.asarray(inputs["c_ctx"], np.float32)
    in_maps = []
    for b in range(nb):
        m = dict(shared)
        m["x"] = np.ascontiguousarray(x[b])
        m["ctx"] = np.ascontiguousarray(np.asarray(inputs["ctx"], np.float32)[b])
        m["cc"] = np.ascontiguousarray(np.concatenate([c[b].reshape(8, 128), cctx.reshape(8, 128)], 0))
        m["consts"] = consts
        m["rope"] = rope
        in_maps.append(m)
    res = run_bass_kernel_spmd(nc, in_maps, core_ids=list(range(nb)))
    return np.stack([r["out"] for r in res.results], axis=0).astype(np.float32)
```

```python
import numpy as np
from contextlib import ExitStack
import concourse.bass as bass
import concourse.mybir as mybir
from concourse.bass_utils import run_bass_kernel_spmd

F32 = mybir.dt.float32
BF16 = mybir.dt.bfloat16
I32 = mybir.dt.int32
ALU = mybir.AluOpType
AF = mybir.ActivationFunctionType
AX = mybir.AxisListType

D = 1024
KC = 8
NCTX = 256
NLAT = 4096
S = NCTX + NLAT
NT = S // 128
DEPTH = 4
FT = [(0, 256)] + [(256 + 512 * i, 512) for i in range(8)]
EPS = 1e-6
PZ, PDT, PU, PCQ, PCK, PCV, PDQ, PDK, PDV = 0, 256, 264, 520, 776, 904, 1032, 1288, 1416
NPTM = 1544
FFN_DIM = 2816
EXPERT_DIM = 3584
NEG = -30000.0
import os
SAME_ENG_SYNC = bool(int(os.environ.get('SAME_ENG_SYNC', '1')))
XBC_LEVEL = int(os.environ.get('XBC_LEVEL', '2'))
ATT_LEVEL = int(os.environ.get('ATT_LEVEL', '2'))
SSD_LEVEL = int(os.environ.get('SSD_LEVEL', '9'))
S5_LEVEL = int(os.environ.get('S5_LEVEL', '9'))


class Prog:
    def __init__(self, nc, es):
        self.nc = nc
        self.es = es
        self.E = {"pe": nc.tensor, "act": nc.scalar, "dve": nc.vector, "pool": nc.gpsimd, "sp": nc.sync}
        self.csem = {}
        self.ccnt = {}
        for e in ("pe", "act", "dve", "pool"):
            self.csem[e] = es.enter_context(nc.semaphore("c_" + e))
            self.ccnt[e] = 0
        self.dsem = {}
        self.dcnt = {}
        self.dnext = {}
        for q in ("sp", "pool", "act"):
            self.dsem[q] = [es.enter_context(nc.semaphore("d_%s%d" % (q, i))) for i in range(8)]
            self.dcnt[q] = [0] * 8
            self.dnext[q] = 0
        self.sems = {}
        for e in self.csem:
            self.sems[("c", e)] = self.csem[e]
        for q in self.dsem:
            for i, s in enumerate(self.dsem[q]):
                self.sems[("d", q, i)] = s
        self.waited = {e: {} for e in self.E}
        self.state = {}
        self.nins = 0

    def _deps(self, r, w):
        deps = {}

        def add(tok):
            if tok is None:
                return
            k, v = tok
            if deps.get(k, 0) < v:
                deps[k] = v

        for key in r:
            st = self.state.get(key)
            if st:
                add(st[0])
        for key in w:
            st = self.state.get(key)
            if st:
                add(st[0])
                for t in st[1]:
                    add(t)
        return deps

    def _wait(self, eng, deps):
        wd = self.waited[eng]
        for k, v in deps.items():
            if wd.get(k, 0) >= v:
                continue
            if k == ("c", eng) and (eng == "pe" or not SAME_ENG_SYNC):
                continue
            self.E[eng].wait_ge(self.sems[k], v)
            wd[k] = v
            self.nins += 1

    def _commit(self, tok, r, w):
        for key in r:
            st = self.state.setdefault(key, [None, []])
            st[1].append(tok)
            if len(st[1]) > 24:
                mx = {}
                for k, v in st[1]:
                    if mx.get(k, 0) < v:
                        mx[k] = v
                st[1] = list(mx.items())
        for key in w:
            self.state[key] = [tok, []]

    def op(self, eng, fn, r=(), w=()):
        self._wait(eng, self._deps(r, w))
        ins = fn(self.E[eng])
        self.ccnt[eng] += 1
        ins.then_inc(self.csem[eng], 1)
        tok = (("c", eng), self.ccnt[eng])
        self._commit(tok, r, w)
        self.nins += 1
        return tok

    def dma(self, q, out, in_, r=(), w=()):
        i = self.dnext[q]
        self.dnext[q] = (i + 1) % 8
        deps = self._deps(r, w)
        k = ("d", q, i)
        if self.dcnt[q][i] > 0:
            deps[k] = max(deps.get(k, 0), self.dcnt[q][i])
        self._wait(q, deps)
        self.dcnt[q][i] += 16
        self.E[q].dma_start(out=out, in_=in_).then_inc(self.dsem[q][i], 16)
        tok = (k, self.dcnt[q][i])
        self._commit(tok, r, w)
        self.nins += 1
        return tok

    def barrier(self):
        deps = {}
        for e in self.csem:
            if self.ccnt[e]:
                deps[("c", e)] = self.ccnt[e]
        for q in self.dsem:
            for i in range(8):
                if self.dcnt[q][i]:
                    deps[("d", q, i)] = self.dcnt[q][i]
        for e in self.E:
            d = {k: v for k, v in deps.items() if k != ("c", e)}
            self._wait(e, d)
        self.state = {}

    def mm(self, out, lhsT, rhs, start=True, stop=True, r=(), w=()):
        return self.op("pe", lambda e: e.matmul(out, lhsT, rhs, start=start, stop=stop), r, w)

    def tr(self, out, in_, ident, r=(), w=()):
        return self.op("pe", lambda e: e.transpose(out, in_, ident), r, w)

    def act(self, out, in_, func, bias=0.0, scale=1.0, r=(), w=(), eng="act"):
        return self.op(eng, lambda e: e.activation(out=out, in_=in_, func=func, bias=bias, scale=scale), r, w)

    def tt(self, out, in0, in1, op, r=(), w=(), eng="dve"):
        return self.op(eng, lambda e: e.tensor_tensor(out=out, in0=in0, in1=in1, op=op), r, w)

    def ts(self, out, in0, s1, s2=None, op0=ALU.mult, op1=None, r=(), w=(), eng="dve"):
        if op1 is None:
            return self.op(eng, lambda e: e.tensor_scalar(out=out, in0=in0, scalar1=s1, scalar2=None, op0=op0), r, w)
        return self.op(eng, lambda e: e.tensor_scalar(out=out, in0=in0, scalar1=s1, scalar2=s2, op0=op0, op1=op1), r, w)

    def stt(self, out, in0, scalar, in1, op0=ALU.mult, op1=ALU.add, r=(), w=(), eng="dve"):
        return self.op(eng, lambda e: e.scalar_tensor_tensor(out=out, in0=in0, scalar=scalar, in1=in1, op0=op0, op1=op1), r, w)

    def cp(self, out, in_, r=(), w=(), eng="dve"):
        if eng == "act":
            return self.op("act", lambda e: e.copy(out=out, in_=in_), r, w)
        return self.op(eng, lambda e: e.tensor_copy(out=out, in_=in_), r, w)

    def memset(self, ap, val, w=(), eng="dve"):
        return self.op(eng, lambda e: e.memset(ap, val), (), w)

    def sb(self, es, name, shape, dt):
        self.uid = getattr(self, "uid", 0) + 1
        return es.enter_context(self.nc.sbuf_tensor("s%d_%s" % (self.uid, name), list(shape), dt))

    def ps(self, es, name, shape, dt=F32):
        self.uid = getattr(self, "uid", 0) + 1
        esz = 4 if dt == F32 else 2
        n = 1
        for d_ in shape[1:]:
            n *= d_
        per_bank = 2048 // esz
        padded = ((n + per_bank - 1) // per_bank) * per_bank
        t = es.enter_context(self.nc.psum_tensor("p%d_%s" % (self.uid, name), [128, padded], dt))
        v = t[0:shape[0], 0:n]
        if len(shape) == 3:
            v = v.rearrange("p (a b) -> p a b", a=shape[1])
        elif len(shape) == 4:
            v = v.rearrange("p (a b c) -> p a b c", a=shape[1], b=shape[2])
        return v


def make_consts():
    i = np.arange(128)
    ident = np.eye(128, dtype=np.float32)
    J = ident[::-1].copy()
    U = (i[:, None] <= i[None, :]).astype(np.float32)
    L = (i[:, None] >= i[None, :]).astype(np.float32)
    ones = np.ones((128, 128), np.float32)
    nmf = np.where(i[:, None] <= i[None, :], 0.0, NEG).astype(np.float32)
    nmb = np.where(i[:, None] >= i[None, :], 0.0, NEG).astype(np.float32)
    ramp = np.tile(np.arange(1, 129, dtype=np.float32)[None, :], (128, 1))
    sel = np.zeros((128, 8 * 128), np.float32)
    for e in range(8):
        sel[e, e * 128:(e + 1) * 128] = 1.0
    return np.concatenate([ident, J, U, L, ones, nmf, nmb, ramp, sel], axis=1)


C_ID, C_J, C_U, C_L, C_ONE, C_NMF, C_NMB, C_RAMP, C_SEL = [k * 128 for k in range(9)]
NCONST = 16 * 128


def make_rope():
    pos = np.arange(NLAT)
    pr = (pos // 64).astype(np.float32)
    pc = (pos % 64).astype(np.float32)
    inv = (np.float32(10000.0) ** (-np.arange(16, dtype=np.float32) / np.float32(16))).astype(np.float32)
    ang = np.concatenate([pr[:, None] * inv, pc[:, None] * inv], axis=-1).astype(np.float32)
    t = np.zeros((S, 64), np.float32)
    t[:NCTX, :32] = 1.0
    t[NCTX:, :32] = np.cos(ang)
    t[NCTX:, 32:] = np.sin(ang)
    return t


class Ctx:
    pass


def build(dbg=None, layers=DEPTH, stop=None, skip=()):
    dbg = dbg or {}
    nc = bass.Bass("TRN2", target_bir_lowering=False)
    g = Ctx()
    g.nc = nc

    def din(name, shape, dt=F32):
        return nc.dram_tensor(name, list(shape), dt, kind="ExternalInput").ap()

    def dscr(name, shape, dt=F32):
        kind = "ExternalOutput" if name in dbg else "Internal"
        return nc.dram_tensor(name, list(shape), dt, kind=kind).ap()

    I = {}
    I["x"] = din("x", [NLAT, D])
    I["ctx"] = din("ctx", [NCTX, D])
    I["cc"] = din("cc", [16, 128])
    I["consts"] = din("consts", [128, NCONST])
    I["rope"] = din("rope", [S, 64])
    shapes = dict(
        norm1_g=[DEPTH, D], norm2_g=[DEPTH, D], ada_w=[DEPTH, D, 6 * D], ada_b=[DEPTH, 6 * D],
        w_in=[DEPTH, D, 6408], ssd_conv_w=[DEPTH, 5, 768], ssd_conv_b=[DEPTH, 768], ssd_a_log=[DEPTH, 2, 4],
        ssd_dt_bias=[DEPTH, 2, 4], ssd_d=[DEPTH, 4], ssd_norm_g=[DEPTH, 256], s5_lam_re=[DEPTH, 2, 16, 64],
        s5_lam_im=[DEPTH, 2, 16, 64], s5_log_step=[DEPTH, 2, 16], s5_b_re=[DEPTH, 2, 16, 64, 16],
        s5_b_im=[DEPTH, 2, 16, 64, 16], s5_c_re=[DEPTH, 2, 16, 16, 64], s5_c_im=[DEPTH, 2, 16, 16, 64],
        s5_d=[DEPTH, 256], s5_glu_w=[DEPTH, 256, 512], qk_norm_g=[DEPTH, 2, 64], swa_sink=[DEPTH, 4],
        w_branch=[DEPTH, 4, 256, D], w_out=[DEPTH, D, D], ffn_w_in=[2, D, 2 * FFN_DIM], ffn_w_out=[2, FFN_DIM, D],
        moe_router=[2, D, 8], moe_w_in=[2, 8, D, 2 * EXPERT_DIM], moe_w_out=[2, 8, EXPERT_DIM, D],
        final_norm_g=[D],
    )
    for k, shp in shapes.items():
        if k not in skip:
            I[k] = din(k, shp)
    out = nc.dram_tensor("out", [NLAT, D], F32, kind="ExternalOutput").ap()

    g.I = I
    g.xres = dscr("xres", [KC, 128, S])
    g.hT = dscr("hT", [KC, 128, S], BF16)
    g.ptm = dscr("ptm", [S, NPTM])
    g.xbcT = dscr("xbcT", [6, 128, S], BF16)
    g.xbctm = dscr("xbctm", [S, 512], BF16)
    g.yT = dscr("yT", [8, 128, S], BF16)
    g.wib = dscr("wib", [8, D, 2 * EXPERT_DIM], BF16)
    g.wob = dscr("wob", [8, EXPERT_DIM, D], BF16)

    with ExitStack() as es:
        p = Prog(nc, es)
        g.p = p
        blk = es.enter_context(nc.Block())
        g.cst = p.sb(es, "cst", [128, NCONST], F32)
        g.cstb = p.sb(es, "cstb", [128, NCONST], BF16)
        g.par = p.sb(es, "par", [128, 512], F32)
        g.mod = p.sb(es, "modv", [128, 48, 2], F32)
        g.AB = p.sb(es, "AB", [128, 4, 8, 2], F32)
        g.cact = p.sb(es, "cact", [128, 8, 2], F32)

        def body(_e):
            setup(g)
            for l in range(layers if stop != "setup" else 0):
                layer(g, l, stop)
                if stop is not None and stop[0] == l:
                    break
            final(g, out, stop is None)
            p.barrier()

        blk.gpsimd(body)
    return nc, p


def load_rows_T(g, es_ps, dst, src2d, n, key):
    p = g.p
    with ExitStack() as es:
        tmp = p.sb(es, "lrt_tmp", [128, 128], F32)
        pst = p.ps(es, "lrt_ps", [128, 128], F32)
        p.dma("sp", tmp[0:n, :], src2d, w=["lrt_tmp"])
        p.tr(pst[:, 0:n], tmp[0:n, :], g.cst[0:n, C_ID:C_ID + n], r=["lrt_tmp", "cst"], w=["lrt_ps"])
        p.cp(dst, pst[:, 0:n], r=["lrt_ps"], w=[key])
        p.barrier()


def setup(g):
    p, nc, I = g.p, g.nc, g.I
    p.dma("sp", g.cst[:, :], I["consts"][:, :], w=["cst"])
    p.cp(g.cstb[:, :], g.cst[:, :], r=["cst"], w=["cstb"])
    with ExitStack() as es:
        t = p.sb(es, "su_t", [128, 16], F32)
        load_rows_T(g, None, t[:, :], I["cc"][:, :], 16, "su_t")
        for i in range(2):
            p.act(g.cact[:, :, i], t[:, i * 8:(i + 1) * 8], AF.Silu, r=["su_t"], w=["cact"])
        p.barrier()
    with ExitStack() as es:
        xin = [p.sb(es, "su_x%d" % i, [128, D], F32) for i in range(2)]
        xo = [p.sb(es, "su_o%d" % i, [128, KC, 128], F32) for i in range(2)]
        pst = [p.ps(es, "su_ps%d" % i, [128, KC, 128], F32) for i in range(2)]
        for t in range(NT):
            b = t % 2
            src = I["ctx"][t * 128:(t + 1) * 128, :] if t < 2 else I["x"][(t - 2) * 128:(t - 1) * 128, :]
            p.dma("sp", xin[b][:, :], src, w=[("xin", b)])
            for c in range(KC):
                p.tr(pst[b][:, c, :], xin[b][:, c * 128:(c + 1) * 128], g.cst[:, C_ID:C_ID + 128],
                     r=[("xin", b), "cst"], w=[("xps", b)])
            p.cp(xo[b][:, :, :], pst[b][:, :, :], r=[("xps", b)], w=[("xo", b)], eng="act" if b else "dve")
            p.dma("sp", g.xres[:, :, t * 128:(t + 1) * 128].rearrange("c p s -> p c s"), xo[b][:, :, :],
                  r=[("xo", b)], w=["xres"])
        p.barrier()


def adaln(g, l):
    p, I = g.p, g.I
    with ExitStack() as es:
        wt = [p.sb(es, "ada_w%d" % i, [128, KC, 768], F32) for i in range(2)]
        adab = p.sb(es, "ada_b", [128, 48], F32)
        gn = p.sb(es, "ada_g", [128, 16], F32)
        mps = p.ps(es, "ada_ps", [128, 48, 2], F32)
        load_rows_T(g, None, adab[:, :], I["ada_b"][l].rearrange("(j q) -> j q", q=128), 48, "adab")
        load_rows_T(g, None, gn[:, 0:8], I["norm1_g"][l].rearrange("(j q) -> j q", q=128), 8, "gn")
        load_rows_T(g, None, gn[:, 8:16], I["norm2_g"][l].rearrange("(j q) -> j q", q=128), 8, "gn")
        wv = I["ada_w"][l].rearrange("(kc q) n -> q kc n", q=128)
        for ob in range(8):
            b = ob % 2
            for kc in range(KC):
                p.dma("sp" if kc % 2 == 0 else "act", wt[b][:, kc, :], wv[:, kc, ob * 768:(ob + 1) * 768], w=[("adaw", b, kc)])
            for jj in range(6):
                j = ob * 6 + jj
                for kc in range(KC):
                    p.mm(mps[:, j, :], wt[b][:, kc, jj * 128:(jj + 1) * 128], g.cact[:, kc, :],
                         start=(kc == 0), stop=(kc == KC - 1), r=[("adaw", b, kc), "cact"], w=["mps"])
        for i in range(2):
            p.tt(g.mod[:, :, i], mps[:, :, i], adab[:, :], ALU.add, r=["mps", "adab"], w=["mod"])
        for i in range(2):
            p.stt(g.AB[:, 0, :, i], g.mod[:, 8:16, i], 1.0, gn[:, 0:8], op0=ALU.add, op1=ALU.mult, r=["mod", "gn"], w=["AB"])
            p.cp(g.AB[:, 1, :, i], g.mod[:, 0:8, i], r=["mod"], w=["AB"])
            p.stt(g.AB[:, 2, :, i], g.mod[:, 32:40, i], 1.0, gn[:, 8:16], op0=ALU.add, op1=ALU.mult, r=["mod", "gn"], w=["AB"])
            p.cp(g.AB[:, 3, :, i], g.mod[:, 24:32, i], r=["mod"], w=["AB"])
        p.barrier()


def norm_tiles(g, which, sink, want32=False):
    p = g.p
    with ExitStack() as es:
        xt = [p.sb(es, "nm_x%d" % i, [128, KC, 512], F32) for i in range(2)]
        sq = p.sb(es, "nm_sq", [128, KC, 512], F32)
        rs = p.sb(es, "nm_rs", [128, 512], F32)
        ht = [p.sb(es, "nm_h%d" % i, [128, KC, 512], BF16) for i in range(2)]
        ssq = p.ps(es, "nm_ps", [128, 512], F32)
        for ti, (s0, n) in enumerate(FT):
            b = ti % 2
            ic = 1 if s0 == 0 else 0
            p.dma("sp", xt[b][:, :, 0:n], g.xres[:, :, s0:s0 + n].rearrange("c p s -> p c s"), r=["xres"], w=[("nmx", b)])
            for c in range(KC):
                p.act(sq[:, c, 0:n], xt[b][:, c, 0:n], AF.Square, r=[("nmx", b)], w=[("nmsq", c)])
                p.mm(ssq[:, 0:n], g.cst[:, C_ONE:C_ONE + 128], sq[:, c, 0:n], start=(c == 0), stop=(c == KC - 1),
                     r=[("nmsq", c), "cst"], w=["nmps"])
            p.act(rs[:, 0:n], ssq[:, 0:n], AF.Ln, bias=EPS, scale=1.0 / D, r=["nmps"], w=["nmrs"])
            p.act(rs[:, 0:n], rs[:, 0:n], AF.Exp, scale=-0.5, r=["nmrs"], w=["nmrs"])
            for c in range(KC):
                p.tt(sq[:, c, 0:n], xt[b][:, c, 0:n], rs[:, 0:n], ALU.mult, r=[("nmx", b), "nmrs"], w=[("nmsq", c)],
                     eng="dve" if c % 2 == 0 else "pool")
                if want32:
                    p.ts(sq[:, c, 0:n], sq[:, c, 0:n], g.AB[:, 2 * which, c, ic:ic + 1], g.AB[:, 2 * which + 1, c, ic:ic + 1],
                         op0=ALU.mult, op1=ALU.add, r=[("nmsq", c), "AB"], w=[("nmsq", c)], eng="dve" if c % 2 == 0 else "pool")
                    p.cp(ht[b][:, c, 0:n], sq[:, c, 0:n], r=[("nmsq", c)], w=[("nmh", b)], eng="dve" if c % 2 == 0 else "pool")
                else:
                    p.ts(ht[b][:, c, 0:n], sq[:, c, 0:n], g.AB[:, 2 * which, c, ic:ic + 1], g.AB[:, 2 * which + 1, c, ic:ic + 1],
                         op0=ALU.mult, op1=ALU.add, r=[("nmsq", c), "AB"], w=[("nmh", b)], eng="dve" if c % 2 == 0 else "pool")
            if want32:
                sink(ti, s0, n, ht[b], ("nmh", b), sq)
            else:
                sink(ti, s0, n, ht[b], ("nmh", b), None)
        p.barrier()


def phase_norm1(g, l):
    p = g.p

    def sink(ti, s0, n, ht, key, h32):
        p.dma("sp", g.hT[:, :, s0:s0 + n].rearrange("c p s -> p c s"), ht[:, :, 0:n], r=[key], w=["hT_d"])

    norm_tiles(g, 0, sink)


def phase_ptm(g, l):
    p, I = g.p, g.I
    wv = I["w_in"][l].rearrange("(kc q) n -> q kc n", q=128)
    with ExitStack() as es:
        w = p.sb(es, "ptm_w", [128, KC, NPTM], BF16)
        ht = [p.sb(es, "ptm_h%d" % i, [128, KC, 512], BF16) for i in range(2)]
        ob = [p.sb(es, "ptm_o%d" % i, [128, NPTM], F32) for i in range(2)]
        ps = [p.ps(es, "ptm_ps%d" % i, [128, 4, 512], F32) for i in range(2)]
        for kc in range(KC):
            p.dma("pool", w[:, kc, 0:256], wv[:, kc, 0:256], w=[("w", kc)])
            p.dma("pool", w[:, kc, 256:NPTM], wv[:, kc, 1024:2312], w=[("w", kc)])
        cols = [(0, 512), (512, 512), (1024, 512), (1536, 8)]
        cnt = 0
        for ti, (s0, n) in enumerate(FT):
            hb = ti % 2
            p.dma("sp", ht[hb][:, :, 0:n], g.hT[:, :, s0:s0 + n].rearrange("c p s -> p c s"), r=["hT_d"], w=[("h", hb)])
            for sub in range(n // 128):
                b = cnt % 2
                cnt += 1
                for kc in range(KC):
                    for bi, (c0, cn) in enumerate(cols):
                        p.mm(ps[b][:, bi, 0:cn], ht[hb][:, kc, sub * 128:(sub + 1) * 128], w[:, kc, c0:c0 + cn],
                             start=(kc == 0), stop=(kc == KC - 1), r=[("h", hb), ("w", kc)], w=[("ps", b, bi)])
                for bi, (c0, cn) in enumerate(cols):
                    p.cp(ob[b][:, c0:c0 + cn], ps[b][:, bi, 0:cn], r=[("ps", b, bi)], w=[("o", b)],
                         eng="act" if bi % 2 else "dve")
                t0 = s0 + sub * 128
                p.dma("sp", g.ptm[t0:t0 + 128, :], ob[b][:, :], r=[("o", b)], w=["ptm_d"])
        p.barrier()


def phase_xbc(g, l):
    p, I = g.p, g.I
    wv = I["w_in"][l].rearrange("(kc q) n -> q kc n", q=128)
    with ExitStack() as es:
        w = p.sb(es, "xb_w", [128, KC, 768], BF16)
        hT = p.sb(es, "xb_h", [128, KC, S], BF16)
        cw = p.sb(es, "xb_cw", [128, 36], F32)
        rawc = p.sb(es, "xb_rc", [128, NCTX + 4], F32)
        rawl = p.sb(es, "xb_rl", [128, NLAT + 4], F32)
        acc = p.sb(es, "xb_acc", [128, S], F32)
        xs = [p.sb(es, "xb_s%d" % i, [128, S], BF16) for i in range(2)]
        tmo = [p.sb(es, "xb_t%d" % i, [128, 4, 128], BF16) for i in range(2)]
        ps = [p.ps(es, "xb_ps%d" % i, [128, 512], F32) for i in range(2)]
        pst = [p.ps(es, "xb_pt%d" % i, [128, 4, 128], F32) for i in range(2)]
        load_rows_T(g, None, cw[:, 0:30], I["ssd_conv_w"][l].rearrange("k (c q) -> (k c) q", q=128), 30, "cw")
        load_rows_T(g, None, cw[:, 30:36], I["ssd_conv_b"][l].rearrange("(c q) -> c q", q=128), 6, "cw")
        for kc in range(KC):
            p.dma("pool", w[:, kc, :], wv[:, kc, 256:1024], w=[("w", kc)])
        for ti, (s0, n) in enumerate(FT):
            p.dma("sp", hT[:, :, s0:s0 + n], g.hT[:, :, s0:s0 + n].rearrange("c p s -> p c s"), r=["hT_d"], w=[("h", ti)])
        p.memset(rawc[:, :], 0.0, w=["rawc"])
        p.memset(rawl[:, :], 0.0, w=["rawl"], eng="pool")
        cnt = 0
        gcnt = 0
        for j in range(6):
            xb = xs[j % 2]
            for ti, (s0, n) in enumerate(FT):
                b = cnt % 2
                cnt += 1
                for kc in range(KC):
                    p.mm(ps[b][:, 0:n], w[:, kc, j * 128:(j + 1) * 128], hT[:, kc, s0:s0 + n], start=(kc == 0),
                         stop=(kc == KC - 1), r=[("w", kc), ("h", ti)], w=[("ps", b)])
                if s0 == 0:
                    p.cp(rawc[:, 2:2 + n], ps[b][:, 0:n], r=[("ps", b)], w=["rawc"], eng="act")
                else:
                    p.cp(rawl[:, 2 + s0 - NCTX:2 + s0 - NCTX + n], ps[b][:, 0:n], r=[("ps", b)], w=["rawl"], eng="act")
            if XBC_LEVEL < 1:
                continue
            pieces = [(rawc, "rawc", 0, 0, NCTX)] + [(rawl, "rawl", q * 1024, NCTX + q * 1024, 1024) for q in range(4)]
            for pi, (rw, rk, o, so, n) in enumerate(pieces):
                eng = "dve"
                a = acc[:, so:so + n]
                p.ts(a, rw[:, o:o + n], cw[:, j:j + 1], r=[rk, "cw"], w=[("acc", pi)], eng=eng)
                for k in range(1, 5):
                    p.stt(a, rw[:, o + k:o + k + n], cw[:, k * 6 + j:k * 6 + j + 1], a, r=[rk, "cw", ("acc", pi)],
                          w=[("acc", pi)], eng=eng)
                p.act(xb[:, so:so + n], a, AF.Silu, bias=cw[:, 30 + j:31 + j], r=[("acc", pi), "cw"], w=[("xs", j % 2, pi)])
            p.dma("sp", g.xbcT[j, :, :], xb[:, :], r=[("xs", j % 2, pi) for pi in range(5)], w=["xbcT_d"])
            if j < 4 and XBC_LEVEL >= 2:
                for t0 in range(0, NT, 4):
                    nt = min(4, NT - t0)
                    b = gcnt % 2
                    gcnt += 1
                    for tt_ in range(nt):
                        t = t0 + tt_
                        p.mm(pst[b][:, tt_, :], xb[:, t * 128:(t + 1) * 128], g.cstb[:, C_ID:C_ID + 128],
                             r=[("xs", j % 2, pi) for pi in range(5)] + ["cstb"], w=[("pt", b)])
                    p.cp(tmo[b][:, 0:nt, :], pst[b][:, 0:nt, :], r=[("pt", b)], w=[("tmo", b)], eng="dve" if b else "act")
                    p.dma("sp", g.xbctm[t0 * 128:(t0 + nt) * 128, j * 128:(j + 1) * 128].rearrange("(t q) c -> q t c", q=128),
                          tmo[b][:, 0:nt, :], r=[("tmo", b)], w=["xbctm_d"])
        p.barrier()


def phase_attn(g, l):
    p, I = g.p, g.I
    with ExitStack() as es:
        qT = p.sb(es, "at_qT", [128, 4, S], BF16)
        kT = p.sb(es, "at_kT", [128, 4, S], BF16)
        vv = p.sb(es, "at_v", [128, NT, 4, 128], BF16)
        yo = p.sb(es, "at_y", [128, 4, S], BF16)
        gbc = p.sb(es, "at_g", [128, 6, 64], F32)
        es_ = p.sb(es, "at_es", [128, 4], F32)
        with ExitStack() as es2:
            pin = [p.sb(es2, "at_in%d" % i, [128, 1024], F32) for i in range(2)]
            rp = [p.sb(es2, "at_rp%d" % i, [128, 64], F32) for i in range(2)]
            sq = p.sb(es2, "at_sq", [128, 384], F32)
            ms = p.sb(es2, "at_ms", [128, 6], F32)
            t1 = p.sb(es2, "at_t1", [128, 12, 2, 16], F32)
            t2 = p.sb(es2, "at_t2", [128, 12, 2, 16], F32)
            t3 = p.sb(es2, "at_t3", [128, 12, 2, 16], F32)
            t4 = p.sb(es2, "at_t4", [128, 12, 2, 16], F32)
            ro = p.sb(es2, "at_ro", [128, 12, 64], F32)
            tb = p.sb(es2, "at_tb", [128, 8, 128], BF16)
            pst = [p.ps(es2, "at_pt%d" % i, [128, 8, 128], F32) for i in range(2)]
            for i in range(4):
                p.dma("sp", gbc[:, i, :], I["qk_norm_g"][l, 0:1, :].partition_broadcast(128), w=["gbc"])
            for i in range(2):
                p.dma("sp", gbc[:, 4 + i, :], I["qk_norm_g"][l, 1:2, :].partition_broadcast(128), w=["gbc"])
            p.dma("sp", es_[:, :], I["swa_sink"][l:l + 1, :].partition_broadcast(128), w=["es"])
            p.act(es_[:, :], es_[:, :], AF.Exp, r=["es"], w=["es"])
            p.memset(vv[:, :, :, 64:128], 1.0, w=["vv1"])
            for t in range(NT):
                b = t % 2
                x = pin[b]
                p.dma("sp", x[:, :], g.ptm[t * 128:(t + 1) * 128, PCQ:PCQ + 1024], r=["ptm_d"], w=[("in", b)])
                p.dma("sp", rp[b][:, :], I["rope"][t * 128:(t + 1) * 128, :], w=[("rp", b)])
                p.act(sq[:, :], x[:, 0:384], AF.Square, r=[("in", b)], w=["sq"])
                p.op("dve", lambda e: e.tensor_reduce(out=ms[:, :], in_=sq[:, :].rearrange("p (h f) -> p h f", f=64), axis=AX.X, op=ALU.add),
                     r=["sq"], w=["ms"])
                p.act(ms[:, :], ms[:, :], AF.Ln, bias=EPS, scale=1.0 / 64, r=["ms"], w=["ms"])
                p.act(ms[:, :], ms[:, :], AF.Exp, scale=-0.5, r=["ms"], w=["ms"])
                xg = x[:, 0:384].rearrange("p (h f) -> p h f", f=64)
                p.tt(xg, xg, ms[:, :].unsqueeze(2).broadcast_to([128, 6, 64]), ALU.mult, r=[("in", b), "ms"], w=[("in", b)])
                p.tt(xg, xg, gbc[:, :, :], ALU.mult, r=[("in", b), "gbc"], w=[("in", b)])
                cosb = rp[b][:, 0:32].rearrange("p (a f) -> p a f", a=2)
                sinb = rp[b][:, 32:64].rearrange("p (a f) -> p a f", a=2)
                cos6 = cosb.unsqueeze(1).broadcast_to([128, 6, 2, 16])
                sin6 = sinb.unsqueeze(1).broadcast_to([128, 6, 2, 16])
                for (c0, h0, nh) in ((0, 0, 6), (512, 6, 6)):
                    xv = x[:, c0:c0 + nh * 64].rearrange("p (h a t f) -> p h a t f", a=2, t=2, f=16)
                    ov = ro[:, h0:h0 + nh, :].rearrange("p h (a t f) -> p h a t f", a=2, t=2, f=16)
                    x1, x2 = xv[:, :, :, 0, :], xv[:, :, :, 1, :]
                    hs = slice(h0, h0 + nh)
                    p.tt(t1[:, hs, :, :], x1, cos6, ALU.mult, r=[("in", b), ("rp", b)], w=[("t1", h0)])
                    p.tt(t2[:, hs, :, :], x2, sin6, ALU.mult, r=[("in", b), ("rp", b)], w=[("t2", h0)], eng="pool")
                    p.tt(ov[:, :, :, 0, :], t1[:, hs, :, :], t2[:, hs, :, :], ALU.subtract, r=[("t1", h0), ("t2", h0)], w=["ro"])
                    p.tt(t3[:, hs, :, :], x2, cos6, ALU.mult, r=[("in", b), ("rp", b)], w=[("t3", h0)], eng="pool")
                    p.tt(t4[:, hs, :, :], x1, sin6, ALU.mult, r=[("in", b), ("rp", b)], w=[("t4", h0)])
                    p.tt(ov[:, :, :, 1, :], t3[:, hs, :, :], t4[:, hs, :, :], ALU.add, r=[("t3", h0), ("t4", h0)], w=["ro"], eng="pool")
                rof = ro[:, :, :].rearrange("p h f -> p (h f)")
                p.cp(tb[:, 0:2, :], rof[:, 0:256].rearrange("p (c f) -> p c f", f=128), r=["ro"], w=["tb"])
                p.cp(tb[:, 4:6, :], rof[:, 384:640].rearrange("p (c f) -> p c f", f=128), r=["ro"], w=["tb"])
                for kv in range(2):
                    for d in range(2):
                        p.cp(tb[:, 2 + kv, d * 64:(d + 1) * 64], ro[:, 4 + kv, :], r=["ro"], w=["tb"], eng="pool")
                        p.cp(tb[:, 6 + kv, d * 64:(d + 1) * 64], ro[:, 10 + kv, :], r=["ro"], w=["tb"], eng="pool")
                p.cp(vv[:, t, 0:2, 0:64], x[:, 384:512].rearrange("p (k f) -> p k f", f=64), r=[("in", b)], w=[("vv", t)], eng="act")
                p.cp(vv[:, t, 2:4, 0:64], x[:, 896:1024].rearrange("p (k f) -> p k f", f=64), r=[("in", b)], w=[("vv", t)], eng="act")
                for c in range(8):
                    p.mm(pst[b][:, c, :], tb[:, c, :], g.cstb[:, C_ID:C_ID + 128], r=["tb", "cstb"], w=[("pt", b)])
                sl = slice(t * 128, (t + 1) * 128)
                ce = "act" if b else "dve"
                p.cp(qT[:, 0:2, sl], pst[b][:, 0:2, :], r=[("pt", b)], w=[("qk", t)], eng=ce)
                p.cp(kT[:, 0:2, sl], pst[b][:, 2:4, :], r=[("pt", b)], w=[("qk", t)], eng=ce)
                p.cp(qT[:, 2:4, sl], pst[b][:, 4:6, :], r=[("pt", b)], w=[("qk", t)], eng=ce)
                p.cp(kT[:, 2:4, sl], pst[b][:, 6:8, :], r=[("pt", b)], w=[("qk", t)], eng=ce)
            p.barrier()
        with ExitStack() as es2:
            pss = [p.ps(es2, "ga_s%d" % i, [128, 512], F32) for i in range(3)]
            pso = [p.ps(es2, "ga_o%d" % i, [128, 512], F32) for i in range(2)]
            pt = [p.sb(es2, "ga_p%d" % i, [128, 512], BF16) for i in range(3)]
            rd = p.sb(es2, "ga_rd", [128, 512], F32)
            cs = 0
            co = 0
            for ti, (s0, n) in enumerate(FT):
                kts = range(2) if s0 == 0 else range(NT)
                for h in range(4):
                    c, hp, kv = h // 2, (h % 2) * 64, h // 2
                    ob = co % 2
                    co += 1
                    for kt in kts:
                        sb_ = cs % 3
                        cs += 1
                        p.mm(pss[sb_][:, 0:n], kT[hp:hp + 64, c, kt * 128:(kt + 1) * 128], qT[hp:hp + 64, c, s0:s0 + n],
                             w=[("gs", sb_)])
                        p.act(pt[sb_][:, 0:n], pss[sb_][:, 0:n], AF.Exp, scale=0.125, r=[("gs", sb_)], w=[("gp", sb_)])
                        p.mm(pso[ob][:, 0:n], vv[:, kt, kv, :], pt[sb_][:, 0:n], start=(kt == kts[0]), stop=(kt == kts[-1]),
                             r=[("gp", sb_)], w=[("go", ob)])
                    p.op("dve", lambda e: e.reciprocal(out=rd[0:64, 0:n], in_=pso[ob][64:128, 0:n]), r=[("go", ob)], w=["rd"])
                    p.tt(yo[hp:hp + 64, c, s0:s0 + n], pso[ob][0:64, 0:n], rd[0:64, 0:n], ALU.mult, r=[("go", ob), "rd"], w=[("yo", c)])
            p.barrier()
        with ExitStack() as es2:
            pss = [p.ps(es2, "wa_s%d" % i, [128, 8, 128], F32) for i in range(2)]
            pso = [p.ps(es2, "wa_o%d" % i, [128, 4, 128], F32) for i in range(2)]
            pt = [p.sb(es2, "wa_p%d" % i, [128, 8, 128], BF16) for i in range(2)]
            dt_ = p.sb(es2, "wa_d", [128, 4, 128], F32)
            rd = p.sb(es2, "wa_rd", [128, 4, 128], F32)
            co = 0
            for qb in range(NT):
                kl = [(0, None), (1, None)]
                if qb >= 2:
                    if qb - 1 >= 2:
                        kl.append((qb - 1, C_L))
                    kl.append((qb, None))
                    if qb + 1 < NT:
                        kl.append((qb + 1, C_U))
                nk = len(kl)
                qs = slice(qb * 128, (qb + 1) * 128)
                for h in range(4):
                    c, hp = 2 + h // 2, (h % 2) * 64
                    ob = co % 2
                    co += 1
                    for ki, (kt, msk) in enumerate(kl):
                        p.mm(pss[ob][:, ki, :], kT[hp:hp + 64, c, kt * 128:(kt + 1) * 128], qT[hp:hp + 64, c, qs], w=[("ws", ob)])
                    p.act(pt[ob][:, 0:nk, :], pss[ob][:, 0:nk, :], AF.Exp, scale=0.125, r=[("ws", ob)], w=[("wp", ob)])
                    for ki, (kt, msk) in enumerate(kl):
                        if msk is not None:
                            p.tt(pt[ob][:, ki, :], pt[ob][:, ki, :], g.cstb[:, msk:msk + 128], ALU.mult, r=[("wp", ob), "cstb"],
                                 w=[("wp", ob)], eng="pool")
                    for ki, (kt, msk) in enumerate(kl):
                        p.mm(pso[ob][:, 0, :], vv[:, kt, 2 + h // 2, :], pt[ob][:, ki, :], start=(ki == 0), stop=(ki == nk - 1),
                             r=[("wp", ob)], w=[("wo", ob)])
                    p.ts(dt_[64:128, 0, :], pso[ob][64:128, 0, :], es_[64:128, h:h + 1], op0=ALU.add, r=[("wo", ob), "es"], w=["wd"])
                    p.op("dve", lambda e: e.reciprocal(out=rd[0:64, 0, :], in_=dt_[64:128, 0, :]), r=["wd"], w=["wrd"])
                    p.tt(yo[hp:hp + 64, c, qs], pso[ob][0:64, 0, :], rd[0:64, 0, :], ALU.mult, r=[("wo", ob), "wrd"], w=[("yo", c)])
            p.barrier()
        for c in range(4):
            p.dma("sp", g.yT[4 + c, :, :], yo[:, c, :], w=["yT_d"])
        p.barrier()


def phase_ssd(g, l):
    p, I = g.p, g.I
    NC8 = NT * 8
    with ExitStack() as es:
        xsB = p.sb(es, "sd_xsB", [128, NT, 512], BF16)
        zdt = p.sb(es, "sd_zdt", [128, NT, 264], F32)
        ysum = p.sb(es, "sd_y", [128, NT, 256], F32)
        prm = p.sb(es, "sd_prm", [128, 20], F32)
        ng = p.sb(es, "sd_ng", [128, 256], F32)
        for t0 in range(0, NT, 4):
            nt = min(4, NT - t0)
            ts_ = slice(t0, t0 + nt)
            p.dma("sp", xsB[:, ts_, :], g.xbctm[t0 * 128:(t0 + nt) * 128, :].rearrange("(t q) c -> q t c", q=128),
                  r=["xbctm_d"], w=["xsB"])
            p.dma("act", zdt[:, ts_, :], g.ptm[t0 * 128:(t0 + nt) * 128, 0:264].rearrange("(t q) c -> q t c", q=128),
                  r=["ptm_d"], w=["zdt"])
        p.dma("sp", prm[:, 0:8], I["ssd_dt_bias"][l:l + 1].rearrange("o d h -> o (d h)").partition_broadcast(128), w=["prm"])
        p.dma("sp", prm[:, 8:16], I["ssd_a_log"][l:l + 1].rearrange("o d h -> o (d h)").partition_broadcast(128), w=["prm"])
        p.dma("sp", prm[:, 16:20], I["ssd_d"][l:l + 1, :].partition_broadcast(128), w=["prm"])
        p.dma("sp", ng[:, :], I["ssd_norm_g"][l:l + 1, :].partition_broadcast(128), w=["ng"])
        p.act(prm[:, 8:16], prm[:, 8:16], AF.Exp, r=["prm"], w=["prm"])
        p.ts(prm[:, 8:16], prm[:, 8:16], -1.0, r=["prm"], w=["prm"])
        with ExitStack() as es1:
            BCT = p.sb(es1, "sd_bct", [128, 4, S], BF16)
            dt = p.sb(es1, "sd_dt", [128, NT, 8], F32)
            la = p.sb(es1, "sd_la", [128, NT, 8], F32)
            cum = p.sb(es1, "sd_cum", [128, NT, 8], F32)
            tot = p.sb(es1, "sd_tot", [128, NT, 8], F32)
            eoff = p.sb(es1, "sd_eoff", [128, NT, 8], F32)
            dtdte = p.sb(es1, "sd_dte", [128, NT, 8], F32)
            etot = p.sb(es1, "sd_etot", [128, NT, 8], F32)
            ncum = p.sb(es1, "sd_ncum", [128, NT, 8], F32)
            nm4 = p.sb(es1, "sd_nm4", [128, 2, 4, 128], F32)
            laU = [p.sb(es1, "sd_laU%d" % i, [128, 4, 128], F32) for i in range(2)]
            dec = p.sb(es1, "sd_dec", [128, 4, 128], F32)
            WT = p.sb(es1, "sd_WT", [128, 4, 128], BF16)
            xdt = p.sb(es1, "sd_xdt", [128, 4, 64], BF16)
            xdte = p.sb(es1, "sd_xdte", [128, 4, 64], BF16)
            ydsb = p.sb(es1, "sd_yd", [128, 256], F32)
            Sst = p.sb(es1, "sd_S", [128, 4, 64], F32)
            Sb = p.sb(es1, "sd_Sb", [128, 4, 64], BF16)
            pc = p.ps(es1, "sd_pc", [128, NC8], F32)
            dps = [p.ps(es1, "sd_dps%d" % i, [128, 4, 128], F32) for i in range(2)]
            gps = p.ps(es1, "sd_gps", [128, 2, 128], F32)
            ydp = p.ps(es1, "sd_ydp", [128, 4, 64], F32)
            yop = p.ps(es1, "sd_yop", [128, 4, 64], F32)
            stp = p.ps(es1, "sd_stp", [128, 4, 64], F32)
            for c in range(4):
                p.dma("sp", BCT[:, c, :], g.xbcT[2 + c, :, :], r=["xbcT_d"], w=["BCT"])
            for d in range(2):
                for h in range(4):
                    nm = C_NMF if d == 0 else C_NMB
                    p.cp(nm4[:, d, h, :], g.cst[:, nm:nm + 128], r=["cst"], w=["nm4"], eng="pool")
            bc8 = lambda ap: ap.unsqueeze(1).broadcast_to([128, NT, 8])
            p.tt(dt[:, :, :], zdt[:, :, 256:264], bc8(prm[:, 0:8]), ALU.add, r=["zdt", "prm"], w=["dt"])
            p.act(dt[:, :, :], dt[:, :, :], AF.Exp, r=["dt"], w=["dt"])
            p.act(dt[:, :, :], dt[:, :, :], AF.Ln, bias=1.0, r=["dt"], w=["dt"])
            p.tt(la[:, :, :], dt[:, :, :], bc8(prm[:, 8:16]), ALU.mult, r=["dt", "prm"], w=["la"])
            laf = la[:, :, :].rearrange("p t c -> p (t c)")
            pc3 = pc[:, :].rearrange("p (t c) -> p t c", c=8)
            p.mm(pc[:, :], g.cst[:, C_U:C_U + 128], laf, r=["la", "cst"], w=["pc"])
            p.cp(cum[:, :, 0:4], pc3[:, :, 0:4], r=["pc"], w=["cum"])
            p.mm(pc[:, :], g.cst[:, C_L:C_L + 128], laf, r=["la", "cst"], w=["pc"])
            p.cp(cum[:, :, 4:8], pc3[:, :, 4:8], r=["pc"], w=["cum"])
            p.mm(pc[:, :], g.cst[:, C_ONE:C_ONE + 128], laf, r=["la", "cst"], w=["pc"])
            p.cp(tot[:, :, :], pc3, r=["pc"], w=["tot"])
            p.act(eoff[:, :, :], cum[:, :, :], AF.Exp, r=["cum"], w=["eoff"])
            p.act(etot[:, :, :], tot[:, :, :], AF.Exp, r=["tot"], w=["etot"])
            p.tt(dtdte[:, :, :], tot[:, :, :], cum[:, :, :], ALU.subtract, r=["tot", "cum"], w=["dtdte"])
            p.act(dtdte[:, :, :], dtdte[:, :, :], AF.Exp, r=["dtdte"], w=["dtdte"])
            p.tt(dtdte[:, :, :], dtdte[:, :, :], dt[:, :, :], ALU.mult, r=["dtdte", "dt"], w=["dtdte"])
            p.ts(ncum[:, :, :], cum[:, :, :], -1.0, r=["cum"], w=["ncum"])
            cnt = 0
            for d in range(min(2, SSD_LEVEL)):
                order = list(range(NT)) if d == 0 else [1, 0] + list(range(NT - 1, 1, -1))
                tri = C_U if d == 0 else C_L
                p.memset(Sst[:, :, :], 0.0, w=["S"])
                p.memset(Sb[:, :, :], 0.0, w=["Sb"])
                for t in order:
                    b = cnt % 2
                    cnt += 1
                    sl = slice(t * 128, (t + 1) * 128)
                    for h in range(4):
                        p.ts(laU[b][:, h, :], g.cst[:, tri:tri + 128], la[:, t, d * 4 + h:d * 4 + h + 1], r=["la", "cst"],
                             w=[("laU", b)], eng="pool" if h % 2 else "dve")
                    dflat = dps[b][:, :, :].rearrange("p h l -> p (h l)")
                    p.mm(dflat, g.cst[:, C_ONE:C_ONE + 128], laU[b][:, :, :].rearrange("p h l -> p (h l)"), start=True, stop=False,
                         r=[("laU", b), "cst"], w=[("dps", b)])
                    p.mm(dflat, g.cst[:, C_ID:C_ID + 128], nm4[:, d, :, :].rearrange("p h l -> p (h l)"), start=False, stop=True,
                         r=["nm4", "cst"], w=[("dps", b)])
                    for h in range(4):
                        p.act(dec[:, h, :], dps[b][:, h, :], AF.Exp, bias=ncum[:, t, d * 4 + h:d * 4 + h + 1],
                              r=[("dps", b), "ncum"], w=[("dec", h)])
                    for gq in range(2):
                        p.mm(gps[:, gq, :], BCT[:, gq, sl], BCT[:, 2 + gq, sl], r=["BCT"], w=[("gps", gq)])
                    for h in range(4):
                        p.tt(WT[:, h, :], dec[:, h, :], gps[:, h // 2, :], ALU.mult, r=[("dec", h), ("gps", h // 2)], w=[("WT", h)])
                    xs4 = xsB[:, t, 0:256].rearrange("p (h f) -> p h f", f=64)
                    p.tt(xdt[:, :, :], xs4, dt[:, t, d * 4:d * 4 + 4].unsqueeze(2).broadcast_to([128, 4, 64]), ALU.mult,
                         r=["xsB", "dt"], w=["xdt"], eng="pool")
                    p.tt(xdte[:, :, :], xs4, dtdte[:, t, d * 4:d * 4 + 4].unsqueeze(2).broadcast_to([128, 4, 64]), ALU.mult,
                         r=["xsB", "dtdte"], w=["xdte"], eng="pool")
                    for h in range(4):
                        p.mm(ydp[:, h, :], WT[:, h, :], xdt[:, h, :], r=[("WT", h), "xdt"], w=["ydp"])
                    for h in range(4):
                        p.mm(yop[:, h, :], BCT[:, 2 + h // 2, sl], Sb[:, h, :], r=["BCT", "Sb"], w=["yop"])
                    p.cp(ydsb[:, :], ydp[:, :, :].rearrange("p h f -> p (h f)"), r=["ydp"], w=["ydsb"], eng="act")
                    if d == 1:
                        p.tt(ydsb[:, :], ydsb[:, :], ysum[:, t, :], ALU.add, r=["ydsb", ("ysum", t)], w=["ydsb"])
                    for h in range(4):
                        p.stt(ysum[:, t, h * 64:(h + 1) * 64], yop[:, h, :], eoff[:, t, d * 4 + h:d * 4 + h + 1], ydsb[:, h * 64:(h + 1) * 64],
                              r=["yop", "eoff", "ydsb"], w=[("ysum", t)])
                    for h in range(4):
                        p.mm(stp[:, h, :], xsB[:, t, 256 + (h // 2) * 128:256 + (h // 2 + 1) * 128], xdte[:, h, :], r=["xsB", "xdte"], w=["stp"])
                    for h in range(4):
                        p.stt(Sst[:, h, :], Sst[:, h, :], etot[:, t, d * 4 + h:d * 4 + h + 1], stp[:, h, :], r=["S", "etot", "stp"], w=["S"])
                    p.cp(Sb[:, :, :], Sst[:, :, :], r=["S"], w=["Sb"], eng="act")
            p.barrier()
        with ExitStack() as es1:
          if SSD_LEVEL >= 3:
              sq = p.sb(es1, "sd_sq", [128, 256], F32)
              ms = p.sb(es1, "sd_ms", [128, NT], F32)
              yaT = p.sb(es1, "sd_yaT", [128, 2, S], BF16)
              yab = p.sb(es1, "sd_yab", [128, NT, 256], BF16)
              pst = [p.ps(es1, "sd_pt%d" % i, [128, 4, 128], F32) for i in range(2)]
              for t in range(NT):
                  e1 = "dve"
                  for h in range(4):
                      hs = slice(h * 64, (h + 1) * 64)
                      p.stt(ysum[:, t, hs], xsB[:, t, hs], prm[:, 16 + h:17 + h], ysum[:, t, hs], r=["xsB", "prm", ("ys", t)], w=[("ys", t)])
                  p.act(zdt[:, t, 0:256], zdt[:, t, 0:256], AF.Silu, r=[("z", t)], w=[("z", t)])
                  p.tt(ysum[:, t, :], ysum[:, t, :], zdt[:, t, 0:256], ALU.mult, r=[("z", t), ("ys", t)], w=[("ys", t)])
                  p.act(sq[:, :], ysum[:, t, :], AF.Square, r=[("ys", t)], w=["sq"])
                  p.op("dve", lambda e: e.tensor_reduce(out=ms[:, t:t + 1], in_=sq[:, :], axis=AX.X, op=ALU.add), r=["sq"], w=["ms"])
              p.act(ms[:, :], ms[:, :], AF.Ln, bias=EPS, scale=1.0 / 256, r=["ms"], w=["ms"])
              p.act(ms[:, :], ms[:, :], AF.Exp, scale=-0.5, r=["ms"], w=["ms"])
              for t in range(NT):
                  p.stt(yab[:, t, :], ysum[:, t, :], ms[:, t:t + 1], ng[:, :], op0=ALU.mult, op1=ALU.mult, r=[("ys", t), "ms", "ng"], w=["yab"])
              k = 0
              for c in (range(2) if SSD_LEVEL >= 4 else []):
                  for t0 in range(0, NT, 4):
                      nt = min(4, NT - t0)
                      b = k % 2
                      k += 1
                      for i in range(nt):
                          p.mm(pst[b][:, i, :], yab[:, t0 + i, c * 128:(c + 1) * 128], g.cstb[:, C_ID:C_ID + 128],
                               r=["yab", "cstb"], w=[("pt", b)])
                      p.cp(yaT[:, c, t0 * 128:(t0 + nt) * 128].rearrange("p (t s) -> p t s", t=nt), pst[b][:, 0:nt, :],
                           r=[("pt", b)], w=[("yaT", c)], eng="act" if b else "dve")
              for c in range(2):
                  p.dma("sp", g.yT[c, :, :], yaT[:, c, :], r=[("yaT", c)], w=["yT_d"])
              p.barrier()


def phase_s5(g, l):
    p, I = g.p, g.I
    TWO_PI = 2.0 * np.pi
    rt = lambda c: (1 - c) if c < 2 else (35 - c)
    with ExitStack() as es:
        u_tm = p.sb(es, "s5_utm", [128, NT, 256], F32)
        useq = p.sb(es, "s5_useq", [128, 2, S], F32)
        ysum = p.sb(es, "s5_ysum", [128, 2, S], F32)
        dcol = p.sb(es, "s5_d", [128, 2], F32)
        zero8 = p.sb(es, "s5_z8", [128, 8], F32)
        load_rows_T(g, None, dcol[:, :], I["s5_d"][l].rearrange("(c q) -> c q", q=128), 2, "dcol")
        p.memset(zero8[:, :], 0.0, w=["zero8"])
        for t0 in range(0, NT, 4):
            nt = min(4, NT - t0)
            p.dma("sp", u_tm[:, t0:t0 + nt, :], g.ptm[t0 * 128:(t0 + nt) * 128, PU:PU + 256].rearrange("(t q) c -> q t c", q=128),
                  r=["ptm_d"], w=["u_tm"])
        with ExitStack() as es1:
            bre = p.ps(es1, "s5_bre", [128, 8, 128], F32)
            bim = p.ps(es1, "s5_bim", [128, 8, 128], F32)
            pst = [bre, bim]
            ypl = [p.ps(es1, "s5_yp%d" % i, [128, 128], F32) for i in range(2)]
            ytl = [p.ps(es1, "s5_ytm%d" % i, [128, 128], F32) for i in range(2)]
            yp = ypl[0]
            prm = p.sb(es1, "s5_prm", [128, 16, 8], F32)
            T16 = p.sb(es1, "s5_T16", [128, 2, 16], F32)
            st16 = p.sb(es1, "s5_st16", [128, 16], F32)
            cosT = p.sb(es1, "s5_cos", [128, 8, 128], F32)
            sinT = p.sb(es1, "s5_sin", [128, 8, 128], F32)
            rhoT = p.sb(es1, "s5_rhoT", [128, 8, 128], F32)
            Mre = p.sb(es1, "s5_Mre", [128, 8, 128], F32)
            Mim = p.sb(es1, "s5_Mim", [128, 8, 128], F32)
            Bre = p.sb(es1, "s5_Bre", [128, 8, 128], F32)
            Bim = p.sb(es1, "s5_Bim", [128, 8, 128], F32)
            Cre = p.sb(es1, "s5_Cre", [128, 8, 128], F32)
            Cim = p.sb(es1, "s5_Cim", [128, 8, 128], F32)
            tA = p.sb(es1, "s5_tA", [128, 8, 128], F32)
            tB = p.sb(es1, "s5_tB", [128, 8, 128], F32)
            tC = p.sb(es1, "s5_tC", [128, 8, 128], F32)
            tD = p.sb(es1, "s5_tD", [128, 8, 128], F32)
            vre = p.sb(es1, "s5_vre", [128, 8, 128], F32)
            vim = p.sb(es1, "s5_vim", [128, 8, 128], F32)
            wre = p.sb(es1, "s5_wre", [128, 8, 128], F32)
            wim = p.sb(es1, "s5_wim", [128, 8, 128], F32)
            c8 = p.sb(es1, "s5_c8", [128, 2, 8], F32)
            rr, kf = wre, vre
            ki = wim[:, :, :].bitcast(I32)
            xre = [p.sb(es1, "s5_xre%d" % i, [128, 8, 128], F32) for i in range(2)]
            xim = [p.sb(es1, "s5_xim%d" % i, [128, 8, 128], F32) for i in range(2)]
            ysb = [p.sb(es1, "s5_ysb%d" % i, [128, 128], F32) for i in range(2)]
            ident = g.cst[:, C_ID:C_ID + 128]
            LR, LI, ST, RHO, THP, CO, SI, ABR, ABI, DEN, FRE, FIM, TMP, TMP2 = range(14)
            for d in range(2):
                k = 0
                for c in (range(2) if S5_LEVEL >= 0 else []):
                    for t0 in range(0, NT, 4):
                        nt = min(4, NT - t0)
                        b = k % 2
                        k += 1
                        for i in range(nt):
                            src = t0 + i if d == 0 else rt(t0 + i)
                            rhs = ident if d == 0 else g.cst[:, C_J:C_J + 128]
                            p.mm(pst[b][:, i, :], u_tm[:, src, c * 128:(c + 1) * 128], rhs, r=["u_tm", "cst"], w=[("pu", b)])
                        dst = useq[:, c, t0 * 128:(t0 + nt) * 128].rearrange("p (t s) -> p t s", t=nt)
                        p.cp(dst, pst[b][:, 0:nt, :], r=[("pu", b)], w=["useq"], eng="act")
                        if d == 0:
                            yd_ = ysum[:, c, t0 * 128:(t0 + nt) * 128].rearrange("p (t s) -> p t s", t=nt)
                            p.ts(yd_, dst, dcol[:, c:c + 1], r=["useq", "dcol"], w=["ysum"])
                if S5_LEVEL < 1:
                    continue
                for which, nm in ((0, "s5_lam_re"), (1, "s5_lam_im")):
                    p.dma("sp", tA[0:16, 0, 0:64], I[nm][l, d, :, :], w=["tA"])
                    p.tr(yp[0:64, 0:16], tA[0:16, 0, 0:64], g.cst[0:16, C_ID:C_ID + 16], r=["tA", "cst"], w=["yp"])
                    p.cp(T16[0:64, which, :], yp[0:64, 0:16], r=["yp"], w=["T16"])
                    tv = T16[0:64, which, :].rearrange("p (gb gl) -> p gl gb", gl=2)
                    p.cp(prm[0:64, which, :], tv[:, 0, :], r=["T16"], w=["prm"])
                    p.cp(prm[64:128, which, :], tv[:, 1, :], r=["T16"], w=["prm"])
                p.dma("sp", st16[:, :], I["s5_log_step"][l, d:d + 1, :].partition_broadcast(128), w=["st16"])
                p.act(st16[:, :], st16[:, :], AF.Exp, r=["st16"], w=["st16"])
                sv = st16[:, :].rearrange("p (gb gl) -> p gl gb", gl=2)
                p.cp(prm[0:64, ST, :], sv[0:64, 0, :], r=["st16"], w=["prm"])
                p.cp(prm[64:128, ST, :], sv[64:128, 1, :], r=["st16"], w=["prm"])
                P = lambda i: prm[:, i, :]
                p.ts(P(LR), P(LR), -1e-4, op0=ALU.min, r=["prm"], w=["prm"])
                p.tt(P(TMP), P(LR), P(ST), ALU.mult, r=["prm"], w=["prm"])
                p.act(P(RHO), P(TMP), AF.Exp, r=["prm"], w=["prm"])
                p.tt(P(THP), P(LI), P(ST), ALU.mult, r=["prm"], w=["prm"])
                p.ts(P(THP), P(THP), 1.0 / TWO_PI, r=["prm"], w=["prm"])
                if S5_LEVEL < 2:
                    continue
                for gb in range(8):
                    p.ts(rr[:, gb, :], g.cst[:, C_RAMP:C_RAMP + 128], prm[:, THP, gb:gb + 1], r=["prm", "cst"], w=["rr"],
                         eng="pool" if gb % 2 else "dve")
                for (tab, shift) in ((sinT, 0.0), (cosT, 0.25)):
                    for hf in range(2):
                        hs = slice(hf * 4, hf * 4 + 4)
                        if shift:
                            p.ts(kf[:, hs, :], rr[:, hs, :], shift, op0=ALU.add, r=["rr"], w=["kf"])
                            src = kf
                        else:
                            src = rr
                        p.cp(ki[:, hs, :], src[:, hs, :], r=["rr", "kf"], w=["ki"])
                        p.cp(tab[:, hs, :], ki[:, hs, :], r=["ki"], w=["tab"])
                        p.tt(tab[:, hs, :], src[:, hs, :], tab[:, hs, :], ALU.subtract, r=["tab", "rr", "kf"], w=["tab"])
                        p.act(tab[:, hs, :], tab[:, hs, :], AF.Sin, scale=TWO_PI, r=["tab"], w=["tab"])
                p.cp(P(CO), cosT[:, :, 0], r=["tab"], w=["prm"])
                p.cp(P(SI), sinT[:, :, 0], r=["tab"], w=["prm"])
                p.tt(P(ABR), P(RHO), P(CO), ALU.mult, r=["prm"], w=["prm"])
                p.tt(P(ABI), P(RHO), P(SI), ALU.mult, r=["prm"], w=["prm"])
                p.tt(P(DEN), P(LR), P(LR), ALU.mult, r=["prm"], w=["prm"])
                p.tt(P(TMP), P(LI), P(LI), ALU.mult, r=["prm"], w=["prm"])
                p.tt(P(DEN), P(DEN), P(TMP), ALU.add, r=["prm"], w=["prm"])
                p.op("dve", lambda e: e.reciprocal(out=P(DEN), in_=P(DEN)), r=["prm"], w=["prm"])
                p.ts(P(ABR), P(ABR), -1.0, op0=ALU.add, r=["prm"], w=["prm"])
                p.tt(P(TMP), P(ABR), P(LR), ALU.mult, r=["prm"], w=["prm"])
                p.tt(P(TMP2), P(ABI), P(LI), ALU.mult, r=["prm"], w=["prm"])
                p.tt(P(FRE), P(TMP), P(TMP2), ALU.add, r=["prm"], w=["prm"])
                p.tt(P(FRE), P(FRE), P(DEN), ALU.mult, r=["prm"], w=["prm"])
                p.tt(P(TMP), P(ABI), P(LR), ALU.mult, r=["prm"], w=["prm"])
                p.tt(P(TMP2), P(ABR), P(LI), ALU.mult, r=["prm"], w=["prm"])
                p.tt(P(FIM), P(TMP), P(TMP2), ALU.subtract, r=["prm"], w=["prm"])
                p.tt(P(FIM), P(FIM), P(DEN), ALU.mult, r=["prm"], w=["prm"])
                if S5_LEVEL < 3:
                    continue
                for m_ in (Mre, Mim):
                    for hf in range(2):
                        p.memset(m_[:, hf * 4:hf * 4 + 4, :], 0.0, w=["M"], eng="pool" if hf else "dve")
                for gi in range(16):
                    gb, gl, gic = gi // 2, gi % 2, gi % 8
                    p.dma("sp", Mre[gl * 64:(gl + 1) * 64, gb, gic * 16:(gic + 1) * 16], I["s5_b_re"][l, d, gi, :, :], r=[], w=["M"])
                    p.dma("act", Mim[gl * 64:(gl + 1) * 64, gb, gic * 16:(gic + 1) * 16], I["s5_b_im"][l, d, gi, :, :], r=[], w=["M"])
                for gb in range(8):
                    fr, fi = prm[:, FRE, gb:gb + 1], prm[:, FIM, gb:gb + 1]
                    p.ts(tA[:, 0, :], Mim[:, gb, :], fi, r=["M", "prm"], w=["tA"])
                    p.stt(Bre[:, gb, :], Mre[:, gb, :], fr, tA[:, 0, :], op0=ALU.mult, op1=ALU.subtract, r=["M", "prm", "tA"], w=["B0"])
                    p.ts(tB[:, 0, :], Mre[:, gb, :], fi, r=["M", "prm"], w=["tB"])
                    p.stt(Bim[:, gb, :], Mim[:, gb, :], fr, tB[:, 0, :], op0=ALU.mult, op1=ALU.add, r=["M", "prm", "tB"], w=["B0"])
                for (src, dst, key) in ((Bre, Mre, "BT"), (Bim, Mim, "BT")):
                    for hf in range(2):
                        b = hf
                        for j in range(4):
                            p.tr(pst[b][:, j, :], src[:, hf * 4 + j, :], ident, r=["B0", "cst"], w=[("pu", b)])
                        p.cp(dst[:, hf * 4:hf * 4 + 4, :], pst[b][:, 0:4, :], r=[("pu", b)], w=[key], eng="act")
                BTre, BTim = Mre, Mim
                for m_ in (Bre, Bim):
                    for hf in range(2):
                        p.memset(m_[:, hf * 4:hf * 4 + 4, :], 0.0, w=["B0"], eng="pool" if hf else "dve")
                for gi in range(16):
                    gb, gl, gic = gi // 2, gi % 2, gi % 8
                    p.dma("sp", Bre[gic * 16:(gic + 1) * 16, gb, gl * 64:(gl + 1) * 64], I["s5_c_re"][l, d, gi, :, :], r=[], w=["B0"])
                    p.dma("act", Bim[gic * 16:(gic + 1) * 16, gb, gl * 64:(gl + 1) * 64], I["s5_c_im"][l, d, gi, :, :], r=[], w=["B0"])
                for (src, dst, neg) in ((Bre, Cre, False), (Bim, Cim, True)):
                    for hf in range(2):
                        b = hf
                        for j in range(4):
                            p.tr(pst[b][:, j, :], src[:, hf * 4 + j, :], ident, r=["B0", "cst"], w=[("pu", b)])
                        if neg:
                            p.ts(dst[:, hf * 4:hf * 4 + 4, :], pst[b][:, 0:4, :], -1.0, r=[("pu", b)], w=["CT"])
                        else:
                            p.cp(dst[:, hf * 4:hf * 4 + 4, :], pst[b][:, 0:4, :], r=[("pu", b)], w=["CT"], eng="act")
                if S5_LEVEL < 4:
                    continue
                for gb in range(8):
                    p.ts(rhoT[:, gb, :], g.cst[:, C_ONE:C_ONE + 128], prm[:, RHO, gb:gb + 1], r=["prm", "cst"], w=["rhoT"],
                         eng="pool" if gb % 2 else "dve")
                p.memset(rhoT[:, :, 0:1], 0.0, w=["rhoT"])
                p.barrier()
                fl = lambda t_: t_[:, :, :].rearrange("p g s -> p (g s)")
                for c in range(NT):
                    cur, prv = c % 2, (c + 1) % 2
                    ps_ = slice(c * 128, (c + 1) * 128)
                    for gb in range(8):
                        p.mm(bre[:, gb, :], BTre[:, gb, :], useq[:, gb // 4, ps_], r=["BT", "useq"], w=["bre"])
                    for gb in range(8):
                        p.mm(bim[:, gb, :], BTim[:, gb, :], useq[:, gb // 4, ps_], r=["BT", "useq"], w=["bim"])
                    p.tt(tA[:, :, :], bre[:, :, :], cosT[:, :, :], ALU.mult, r=["bre", "tab"], w=["tA"])
                    p.tt(tB[:, :, :], bim[:, :, :], sinT[:, :, :], ALU.mult, r=["bim", "tab"], w=["tB"])
                    p.tt(vre[:, :, :], tA[:, :, :], tB[:, :, :], ALU.add, r=["tA", "tB"], w=["vre"], eng="pool")
                    p.tt(tC[:, :, :], bim[:, :, :], cosT[:, :, :], ALU.mult, r=["bim", "tab"], w=["tC"])
                    p.tt(tD[:, :, :], bre[:, :, :], sinT[:, :, :], ALU.mult, r=["bre", "tab"], w=["tD"])
                    p.tt(vim[:, :, :], tC[:, :, :], tD[:, :, :], ALU.subtract, r=["tC", "tD"], w=["vim"], eng="pool")
                    if c > 0:
                        p.tt(c8[:, 0, :], xre[prv][:, :, 127], prm[:, RHO, :], ALU.mult, r=[("x", prv), "prm"], w=["c8"], eng="pool")
                        p.tt(vre[:, :, 0], vre[:, :, 0], c8[:, 0, :], ALU.add, r=["c8", "vre"], w=["vre"], eng="pool")
                        p.tt(c8[:, 1, :], xim[prv][:, :, 127], prm[:, RHO, :], ALU.mult, r=[("x", prv), "prm"], w=["c8b"], eng="pool")
                        p.tt(vim[:, :, 0], vim[:, :, 0], c8[:, 1, :], ALU.add, r=["c8b", "vim"], w=["vim"], eng="pool")
                    p.op("dve", lambda e: e.tensor_tensor_scan(out=fl(wre), data0=fl(rhoT), data1=fl(vre), initial=0.0,
                                                               op0=ALU.mult, op1=ALU.add), r=["vre", "rhoT"], w=["wre"])
                    p.op("dve", lambda e: e.tensor_tensor_scan(out=fl(wim), data0=fl(rhoT), data1=fl(vim), initial=0.0,
                                                               op0=ALU.mult, op1=ALU.add), r=["vim", "rhoT"], w=["wim"])
                    p.tt(tA[:, :, :], wre[:, :, :], cosT[:, :, :], ALU.mult, r=["wre", "tab"], w=["tA"], eng="pool")
                    p.tt(tB[:, :, :], wim[:, :, :], sinT[:, :, :], ALU.mult, r=["wim", "tab"], w=["tB"], eng="pool")
                    p.tt(xre[cur][:, :, :], tA[:, :, :], tB[:, :, :], ALU.subtract, r=["tA", "tB"], w=[("x", cur)], eng="pool")
                    p.tt(tC[:, :, :], wre[:, :, :], sinT[:, :, :], ALU.mult, r=["wre", "tab"], w=["tC"])
                    p.tt(tD[:, :, :], wim[:, :, :], cosT[:, :, :], ALU.mult, r=["wim", "tab"], w=["tD"])
                    p.tt(xim[cur][:, :, :], tC[:, :, :], tD[:, :, :], ALU.add, r=["tC", "tD"], w=[("x", cur)])
                    for hf in range(2):
                        if d == 0:
                            ypt = ypl[hf]
                            for j in range(4):
                                gb = hf * 4 + j
                                p.mm(ypt[:, :], Cre[:, gb, :], xre[cur][:, gb, :], start=(j == 0), stop=False, r=["CT", ("x", cur)], w=[("yp", hf)])
                                p.mm(ypt[:, :], Cim[:, gb, :], xim[cur][:, gb, :], start=False, stop=(j == 3), r=["CT", ("x", cur)], w=[("yp", hf)])
                            p.tt(ysum[:, hf, ps_], ypt[:, :], ysum[:, hf, ps_], ALU.add, r=[("yp", hf), "ysum"], w=["ysum"])
                        else:
                            ytm, ypt = ytl[hf], ypl[hf]
                            for j in range(4):
                                gb = hf * 4 + j
                                p.mm(ytm[:, :], xre[cur][:, gb, :], Cre[:, gb, :], start=(j == 0), stop=False, r=["CT", ("x", cur)], w=[("ytm", hf)])
                                p.mm(ytm[:, :], xim[cur][:, gb, :], Cim[:, gb, :], start=False, stop=(j == 3), r=["CT", ("x", cur)], w=[("ytm", hf)])
                            p.cp(ysb[hf][:, :], ytm[:, :], r=[("ytm", hf)], w=[("ysb", hf)], eng="act")
                            p.mm(ypt[:, :], ysb[hf][:, :], g.cst[:, C_J:C_J + 128], r=[("ysb", hf), "cst"], w=[("yp", hf)])
                            os_ = slice(rt(c) * 128, (rt(c) + 1) * 128)
                            p.tt(ysum[:, hf, os_], ypt[:, :], ysum[:, hf, os_], ALU.add, r=[("yp", hf), "ysum"], w=["ysum"])
                p.barrier()
        with ExitStack() as es1:
            wg = p.sb(es1, "s5_wg", [128, 2, 512], BF16)
            vT = p.sb(es1, "s5_vT", [128, 2, 512], BF16)
            t1 = p.sb(es1, "s5_t1", [128, 512], F32)
            t2 = p.sb(es1, "s5_t2", [128, 512], F32)
            sg = p.sb(es1, "s5_sg", [128, 512], F32)
            yb = [p.sb(es1, "s5_yb%d" % i, [128, 2, 512], BF16) for i in range(2)]
            pv = [p.ps(es1, "s5_pv%d" % i, [128, 512], F32) for i in range(2)]
            pg = [p.ps(es1, "s5_pg%d" % i, [128, 512], F32) for i in range(2)]
            for kc in range(2):
                p.dma("pool", wg[:, kc, :], I["s5_glu_w"][l, kc * 128:(kc + 1) * 128, :], w=["wg"])
            k = 0
            for ti, (s0, n) in enumerate(FT if S5_LEVEL >= 5 else []):
                ob = ti % 2
                for c in range(2):
                    x = ysum[:, c, s0:s0 + n]
                    p.tt(t1[:, 0:n], x, x, ALU.mult, r=["ysum"], w=["t1"])
                    p.ts(t1[:, 0:n], t1[:, 0:n], 0.044715, 1.0, op0=ALU.mult, op1=ALU.add, r=["t1"], w=["t1"])
                    p.tt(t1[:, 0:n], t1[:, 0:n], x, ALU.mult, r=["t1", "ysum"], w=["t1"])
                    p.act(t2[:, 0:n], t1[:, 0:n], AF.Tanh, scale=0.7978845608028654, r=["t1"], w=["t2"])
                    p.ts(t2[:, 0:n], t2[:, 0:n], 0.5, 0.5, op0=ALU.mult, op1=ALU.add, r=["t2"], w=["t2"], eng="pool")
                    p.tt(vT[:, c, 0:n], t2[:, 0:n], x, ALU.mult, r=["t2", "ysum"], w=[("vT", c)])
                for c in range(2):
                    q = k % 2
                    k += 1
                    for kc in range(2):
                        p.mm(pv[q][:, 0:n], wg[:, kc, c * 128:(c + 1) * 128], vT[:, kc, 0:n], start=(kc == 0), stop=(kc == 1),
                             r=["wg", ("vT", kc)], w=[("pv", q)])
                    for kc in range(2):
                        p.mm(pg[q][:, 0:n], wg[:, kc, 256 + c * 128:256 + (c + 1) * 128], vT[:, kc, 0:n], start=(kc == 0), stop=(kc == 1),
                             r=["wg", ("vT", kc)], w=[("pg", q)])
                    p.act(sg[:, 0:n], pg[q][:, 0:n], AF.Sigmoid, r=[("pg", q)], w=["sg"])
                    p.tt(yb[ob][:, c, 0:n], pv[q][:, 0:n], sg[:, 0:n], ALU.mult, r=[("pv", q), "sg"], w=[("yb", ob)])
                p.dma("sp", g.yT[2:4, :, s0:s0 + n].rearrange("c p s -> p c s"), yb[ob][:, :, 0:n], r=[("yb", ob)], w=["yT_d"])
            p.barrier()


def phase_merge(g, l):
    p, I = g.p, g.I
    wv = I["w_in"][l].rearrange("(kc q) n -> q kc n", q=128)
    with ExitStack() as es:
        wg = p.sb(es, "mg_wg", [128, KC, 4096], BF16)
        wb = p.sb(es, "mg_wb", [128, 8, D], BF16)
        wo = p.sb(es, "mg_wo", [128, KC, D], BF16)
        ht = [p.sb(es, "mg_h%d" % i, [128, KC, 512], BF16) for i in range(2)]
        yt = [p.sb(es, "mg_y%d" % i, [128, 8, 512], BF16) for i in range(2)]
        xt = [p.sb(es, "mg_x%d" % i, [128, KC, 512], F32) for i in range(2)]
        sg = [p.sb(es, "mg_s%d" % i, [128, 512], F32) for i in range(2)]
        acc = p.sb(es, "mg_acc", [128, 512], F32)
        tmp = p.sb(es, "mg_tmp", [128, 512], F32)
        accT = p.sb(es, "mg_aT", [128, KC, 512], BF16)
        psg = [p.ps(es, "mg_pg%d" % i, [128, 512], F32) for i in range(2)]
        psb = [p.ps(es, "mg_pb%d" % i, [128, 512], F32) for i in range(2)]
        pso = [p.ps(es, "mg_po%d" % i, [128, 512], F32) for i in range(2)]
        for kc in range(KC):
            p.dma("pool", wg[:, kc, :], wv[:, kc, 2312:6408], w=[("wg", kc)])
            p.dma("pool", wo[:, kc, :], I["w_out"][l, kc * 128:(kc + 1) * 128, :], w=[("wo", kc)])
            p.dma("pool", wb[:, kc, :], I["w_branch"][l, kc // 2, (kc % 2) * 128:(kc % 2 + 1) * 128, :], w=[("wb", kc)])
        cg = 0
        co = 0
        for ti, (s0, n) in enumerate(FT[1:] if l == DEPTH - 1 else FT):
            b = ti % 2
            ic = 1 if s0 == 0 else 0
            p.dma("sp", ht[b][:, :, 0:n], g.hT[:, :, s0:s0 + n].rearrange("c p s -> p c s"), r=["hT_d"], w=[("h", b)])
            p.dma("sp", yt[b][:, :, 0:n], g.yT[:, :, s0:s0 + n].rearrange("c p s -> p c s"), r=["yT_d"], w=[("y", b)])
            p.dma("sp", xt[b][:, :, 0:n], g.xres[:, :, s0:s0 + n].rearrange("c p s -> p c s"), r=["xres"], w=[("x", b)])
            for oc in range(KC):
                for br in range(4):
                    q = cg % 2
                    cg += 1
                    for kc in range(KC):
                        c0 = br * D + oc * 128
                        p.mm(psg[q][:, 0:n], wg[:, kc, c0:c0 + 128], ht[b][:, kc, 0:n], start=(kc == 0), stop=(kc == KC - 1),
                             r=[("wg", kc), ("h", b)], w=[("pg", q)])
                    for k2 in range(2):
                        p.mm(psb[q][:, 0:n], wb[:, br * 2 + k2, oc * 128:(oc + 1) * 128], yt[b][:, br * 2 + k2, 0:n], start=(k2 == 0),
                             stop=(k2 == 1), r=[("wb", br * 2 + k2), ("y", b)], w=[("pb", q)])
                    p.act(sg[q][:, 0:n], psg[q][:, 0:n], AF.Sigmoid, r=[("pg", q)], w=[("sg", q)])
                    if br == 0:
                        p.tt(acc[:, 0:n], psb[q][:, 0:n], sg[q][:, 0:n], ALU.mult, r=[("pb", q), ("sg", q)], w=["acc"])
                    else:
                        p.tt(tmp[:, 0:n], psb[q][:, 0:n], sg[q][:, 0:n], ALU.mult, r=[("pb", q), ("sg", q)], w=["tmp"])
                        p.tt(acc[:, 0:n], acc[:, 0:n], tmp[:, 0:n], ALU.add, r=["tmp", "acc"], w=["acc"], eng="pool")
                p.cp(accT[:, oc, 0:n], acc[:, 0:n], r=["acc"], w=[("aT", oc)], eng="act")
            for oc in range(KC):
                q = co % 2
                co += 1
                for kc in range(KC):
                    p.mm(pso[q][:, 0:n], wo[:, kc, oc * 128:(oc + 1) * 128], accT[:, kc, 0:n], start=(kc == 0), stop=(kc == KC - 1),
                         r=[("wo", kc), ("aT", kc)], w=[("po", q)])
                p.stt(xt[b][:, oc, 0:n], pso[q][:, 0:n], g.mod[:, 16 + oc, ic:ic + 1], xt[b][:, oc, 0:n], op0=ALU.mult, op1=ALU.add,
                      r=[("po", q), "mod", ("x", b)], w=[("x", b)])
            p.dma("sp", g.xres[:, :, s0:s0 + n].rearrange("c p s -> p c s"), xt[b][:, :, 0:n], r=[("x", b)], w=["xres"])
        p.barrier()


def phase_ffn(g, l):
    p, I = g.p, g.I
    moe = (l % 2 == 1)
    m = l // 2
    if moe:
        H, GS = EXPERT_DIM, 4
        experts = [(I["moe_w_in"][m, e], I["moe_w_out"][m, e]) for e in range(8)]
    else:
        H, GS = FFN_DIM, 2
        experts = [(I["ffn_w_in"][m], I["ffn_w_out"][m])]
    HC = H // 128
    NG = HC // GS
    with ExitStack() as es:
        wT = p.sb(es, "ff_wT", [8, S], F32)
        if moe:
            with ExitStack() as es0:
                rw = p.sb(es0, "ff_rw", [128, KC, 8], F32)
                lsb = p.sb(es0, "ff_lsb", [128, 8], F32)
                m8 = p.sb(es0, "ff_m8", [128, 8], F32)
                gt = p.sb(es0, "ff_gt", [128, 4], F32)
                e1 = p.sb(es0, "ff_e1", [128, 8], F32)
                e2 = p.sb(es0, "ff_e2", [128, 8], F32)
                lg = p.ps(es0, "ff_lg", [128, 8], F32)
                wtp = p.ps(es0, "ff_wtp", [8, 128], F32)
                p.dma("sp", rw[:, :, :], I["moe_router"][m].rearrange("(kc q) e -> q kc e", q=128), w=["rw"])

                def sink(ti, s0, n, ht, key, h32):
                    p.dma("sp", g.hT[:, :, s0:s0 + n].rearrange("c p s -> p c s"), ht[:, :, 0:n], r=[key], w=["hT_d"])
                    for sub in range(n // 128):
                        ss = slice(sub * 128, (sub + 1) * 128)
                        for kc in range(KC):
                            p.mm(lg[:, :], h32[:, kc, ss], rw[:, kc, :], start=(kc == 0), stop=(kc == KC - 1),
                                 r=[("nmsq", kc), "rw"], w=["lg"])
                        p.cp(lsb[:, :], lg[:, :], r=["lg"], w=["lsb"])
                        p.op("dve", lambda e: e.max(out=m8[:, :], in_=lsb[:, :]), r=["lsb"], w=["m8"])
                        p.tt(gt[:, 0:1], m8[:, 0:1], m8[:, 1:2], ALU.subtract, r=["m8"], w=["gt"])
                        p.act(gt[:, 1:2], gt[:, 0:1], AF.Sigmoid, r=["gt"], w=["gt1"])
                        p.act(gt[:, 2:3], gt[:, 0:1], AF.Sigmoid, scale=-1.0, r=["gt"], w=["gt2"])
                        p.ts(e1[:, :], lsb[:, :], m8[:, 0:1], gt[:, 1:2], op0=ALU.is_equal, op1=ALU.mult, r=["lsb", "m8", "gt1"], w=["e1"])
                        p.ts(e2[:, :], lsb[:, :], m8[:, 1:2], gt[:, 2:3], op0=ALU.is_equal, op1=ALU.mult, r=["lsb", "m8", "gt2"], w=["e2"])
                        p.tt(e1[:, :], e1[:, :], e2[:, :], ALU.add, r=["e1", "e2"], w=["e1"])
                        p.tr(wtp[:, :], e1[:, :], g.cst[:, C_ID:C_ID + 128], r=["e1", "cst"], w=["wtp"])
                        p.cp(wT[0:8, s0 + sub * 128:s0 + (sub + 1) * 128], wtp[:, :], r=["wtp"], w=["wT"])

                norm_tiles(g, 1, sink, want32=True)
        else:
            def sink(ti, s0, n, ht, key, h32):
                p.dma("sp", g.hT[:, :, s0:s0 + n].rearrange("c p s -> p c s"), ht[:, :, 0:n], r=[key], w=["hT_d"])

            norm_tiles(g, 1, sink)
        ht = [p.sb(es, "ff_h%d" % i, [128, KC, 512], BF16) for i in range(2)]
        wg = [p.sb(es, "ff_wg%d" % i, [128, KC, GS * 128], BF16) for i in range(2)]
        wu = [p.sb(es, "ff_wu%d" % i, [128, KC, GS * 128], BF16) for i in range(2)]
        wo = p.sb(es, "ff_wo", [128, HC, D], BF16)
        actT = p.sb(es, "ff_act", [128, HC, 512], BF16)
        oacc = p.sb(es, "ff_oacc", [128, KC, 512], F32)
        xt = p.sb(es, "ff_x", [128, KC, 512], F32)
        sgt = [p.sb(es, "ff_sg%d" % i, [128, 512], F32) for i in range(2)]
        tmp = [p.sb(es, "ff_tmp%d" % i, [128, 512], F32) for i in range(2)]
        wbc = p.sb(es, "ff_wbc", [128, 512], F32)
        psg = [p.ps(es, "ff_pg%d" % i, [128, 512], F32) for i in range(2)]
        psu = [p.ps(es, "ff_pu%d" % i, [128, 512], F32) for i in range(2)]
        pso = [p.ps(es, "ff_po%d" % i, [128, 512], F32) for i in range(2)]
        psw = p.ps(es, "ff_pw", [128, 512], F32)
        gcnt = 0
        jc = 0
        oc_ = 0
        tiles = FT[1:] if l == DEPTH - 1 else FT
        for ti, (s0, n) in enumerate(tiles):
            hb = ti % 2
            ic = 1 if s0 == 0 else 0
            p.dma("sp", ht[hb][:, :, 0:n], g.hT[:, :, s0:s0 + n].rearrange("c p s -> p c s"), r=["hT_d"], w=[("h", hb)])
            p.dma("sp", xt[:, :, 0:n], g.xres[:, :, s0:s0 + n].rearrange("c p s -> p c s"), r=["xres"], w=["x"])
            for ei, (w_in, w_out) in enumerate(experts):
                wiv = w_in.rearrange("(kc q) n -> q kc n", q=128)
                wov = w_out.rearrange("(hc q) n -> q hc n", q=128)
                if moe:
                    p.mm(psw[:, 0:n], g.cst[0:8, C_SEL + ei * 128:C_SEL + (ei + 1) * 128], wT[0:8, s0:s0 + n], r=["wT", "cst"], w=["psw"])
                    p.cp(wbc[:, 0:n], psw[:, 0:n], r=["psw"], w=["wbc"], eng="act")
                wibv = g.wib[ei, :, 0:2 * H].rearrange("(kc q) n -> q kc n", q=128)
                wobv = g.wob[ei, 0:H, :].rearrange("(hc q) n -> q hc n", q=128)
                for h0 in range(0, HC, 4):
                    hn = min(4, HC - h0)
                    if ti == 0:
                        p.dma("pool", wo[:, h0:h0 + hn, :], wov[:, h0:h0 + hn, :], w=[("wo", h0)])
                        p.dma("sp", wobv[:, h0:h0 + hn, :], wo[:, h0:h0 + hn, :], r=[("wo", h0)], w=[("wobd", ei, h0)])
                    else:
                        p.dma("sp", wo[:, h0:h0 + hn, :], wobv[:, h0:h0 + hn, :], r=[("wobd", ei, h0)], w=[("wo", h0)])
                for gi in range(NG):
                    gb = gcnt % 2
                    gcnt += 1
                    c0 = gi * GS * 128
                    if ti == 0:
                        p.dma("pool", wg[gb][:, :, :], wiv[:, :, c0:c0 + GS * 128], w=[("wg", gb)])
                        p.dma("pool", wu[gb][:, :, :], wiv[:, :, H + c0:H + c0 + GS * 128], w=[("wu", gb)])
                        p.dma("sp", wibv[:, :, c0:c0 + GS * 128], wg[gb][:, :, :], r=[("wg", gb)], w=[("wibd", ei, gi, 0)])
                        p.dma("sp", wibv[:, :, H + c0:H + c0 + GS * 128], wu[gb][:, :, :], r=[("wu", gb)], w=[("wibd", ei, gi, 1)])
                    else:
                        p.dma("sp", wg[gb][:, :, :], wibv[:, :, c0:c0 + GS * 128], r=[("wibd", ei, gi, 0)], w=[("wg", gb)])
                        p.dma("sp", wu[gb][:, :, :], wibv[:, :, H + c0:H + c0 + GS * 128], r=[("wibd", ei, gi, 1)], w=[("wu", gb)])
                    for j in range(GS):
                        q = jc % 2
                        jc += 1
                        hc = gi * GS + j
                        for kc in range(KC):
                            p.mm(psg[q][:, 0:n], wg[gb][:, kc, j * 128:(j + 1) * 128], ht[hb][:, kc, 0:n], start=(kc == 0),
                                 stop=(kc == KC - 1), r=[("wg", gb), ("h", hb)], w=[("pg", q)])
                        for kc in range(KC):
                            p.mm(psu[q][:, 0:n], wu[gb][:, kc, j * 128:(j + 1) * 128], ht[hb][:, kc, 0:n], start=(kc == 0),
                                 stop=(kc == KC - 1), r=[("wu", gb), ("h", hb)], w=[("pu", q)])
                        p.act(sgt[q][:, 0:n], psg[q][:, 0:n], AF.Silu, r=[("pg", q)], w=[("sg", q)])
                        if moe:
                            p.tt(tmp[q][:, 0:n], psu[q][:, 0:n], sgt[q][:, 0:n], ALU.mult, r=[("pu", q), ("sg", q)], w=[("tmp", q)])
                            p.tt(actT[:, hc, 0:n], tmp[q][:, 0:n], wbc[:, 0:n], ALU.mult, r=[("tmp", q), "wbc"], w=[("act", hc)], eng="pool")
                        else:
                            p.tt(actT[:, hc, 0:n], psu[q][:, 0:n], sgt[q][:, 0:n], ALU.mult, r=[("pu", q), ("sg", q)], w=[("act", hc)])
                for oc in range(KC):
                    q = oc_ % 2
                    oc_ += 1
                    for hc in range(HC):
                        p.mm(pso[q][:, 0:n], wo[:, hc, oc * 128:(oc + 1) * 128], actT[:, hc, 0:n], start=(hc == 0), stop=(hc == HC - 1),
                             r=[("wo", (hc // 4) * 4), ("act", hc)], w=[("po", q)])
                    if ei == 0:
                        p.cp(oacc[:, oc, 0:n], pso[q][:, 0:n], r=[("po", q)], w=[("oacc", oc)])
                    else:
                        p.tt(oacc[:, oc, 0:n], pso[q][:, 0:n], oacc[:, oc, 0:n], ALU.add, r=[("po", q), ("oacc", oc)], w=[("oacc", oc)])
            for oc in range(KC):
                p.stt(xt[:, oc, 0:n], oacc[:, oc, 0:n], g.mod[:, 40 + oc, ic:ic + 1], xt[:, oc, 0:n], op0=ALU.mult, op1=ALU.add,
                      r=[("oacc", oc), "mod", "x"], w=["x"])
            p.dma("sp", g.xres[:, :, s0:s0 + n].rearrange("c p s -> p c s"), xt[:, :, 0:n], r=["x"], w=["xres"])
        p.barrier()


def layer(g, l, stop):
    phases = [("ada", adaln), ("n1", phase_norm1), ("ptm", phase_ptm), ("xbc", phase_xbc), ("ssd", phase_ssd), ("s5", phase_s5), ("attn", phase_attn), ("merge", phase_merge), ("ffn", phase_ffn)]
    for name, fn in phases:
        fn(g, l)
        if stop is not None and stop == (l, name):
            return


def final(g, out, real):
    p, I = g.p, g.I
    if not real:
        with ExitStack() as es:
            z = p.sb(es, "fz", [128, D], F32)
            p.memset(z[:, :], 0.0, w=["fz"])
            p.dma("sp", out[0:128, :], z[:, :], r=["fz"], w=["out"])
            p.barrier()
        return
    with ExitStack() as es:
        gf = p.sb(es, "fn_g", [128, 8], F32)
        load_rows_T(g, None, gf[:, :], I["final_norm_g"].rearrange("(j q) -> j q", q=128), 8, "gf")
        xt = [p.sb(es, "fn_x%d" % i, [128, KC, 512], F32) for i in range(2)]
        sq = p.sb(es, "fn_sq", [128, KC, 512], F32)
        rs = p.sb(es, "fn_rs", [128, 512], F32)
        ot = [p.sb(es, "fn_o%d" % i, [128, KC, 128], F32) for i in range(2)]
        ssq = p.ps(es, "fn_ps", [128, 512], F32)
        pst = [p.ps(es, "fn_pt%d" % i, [128, KC, 128], F32) for i in range(2)]
        cnt = 0
        for ti, (s0, n) in enumerate(FT[1:]):
            b = ti % 2
            p.dma("sp", xt[b][:, :, 0:n], g.xres[:, :, s0:s0 + n].rearrange("c p s -> p c s"), r=["xres"], w=[("fx", b)])
            for c in range(KC):
                p.act(sq[:, c, 0:n], xt[b][:, c, 0:n], AF.Square, r=[("fx", b)], w=[("fsq", c)])
                p.mm(ssq[:, 0:n], g.cst[:, C_ONE:C_ONE + 128], sq[:, c, 0:n], start=(c == 0), stop=(c == KC - 1),
                     r=[("fsq", c), "cst"], w=["fps"])
            p.act(rs[:, 0:n], ssq[:, 0:n], AF.Ln, bias=EPS, scale=1.0 / D, r=["fps"], w=["frs"])
            p.act(rs[:, 0:n], rs[:, 0:n], AF.Exp, scale=-0.5, r=["frs"], w=["frs"])
            for c in range(KC):
                p.stt(sq[:, c, 0:n], xt[b][:, c, 0:n], gf[:, c:c + 1], rs[:, 0:n], op0=ALU.mult, op1=ALU.mult,
                      r=[("fx", b), "frs", "gf"], w=[("fsq", c)])
            for sub in range(n // 128):
                ob = cnt % 2
                cnt += 1
                for c in range(KC):
                    p.tr(pst[ob][:, c, :], sq[:, c, sub * 128:(sub + 1) * 128], g.cst[:, C_ID:C_ID + 128],
                         r=[("fsq", c), "cst"], w=[("fpt", ob)])
                p.cp(ot[ob][:, :, :], pst[ob][:, :, :], r=[("fpt", ob)], w=[("fo", ob)], eng="act" if ob else "dve")
                t0 = s0 - NCTX + sub * 128
                p.dma("sp", out[t0:t0 + 128, :], ot[ob][:, :, :].rearrange("p c f -> p (c f)"), r=[("fo", ob)], w=["out"])
        p.barrier()


_NC_CACHE = {}


def kernel(**inputs):
    x = np.asarray(inputs["x"], np.float32)
    nb = x.shape[0]
    if "nc" not in _NC_CACHE:
        _NC_CACHE["nc"] = build()[0]
    nc = _NC_CACHE["nc"]
    consts = make_consts()
    rope = make_rope()
    shared = {k: np.ascontiguousarray(np.asarray(v, np.float32)) for k, v in inputs.items() if k not in ("x", "c", "ctx", "c_ctx")}
    c = np.asarray(inputs["c"], np.float32)
    cctx = np.asarray(inputs["c_ctx"], np.float32)
    in_maps = []
    for b in range(nb):
        m = dict(shared)
        m["x"] = np.ascontiguousarray(x[b])
        m["ctx"] = np.ascontiguousarray(np.asarray(inputs["ctx"], np.float32)[b])
        m["cc"] = np.ascontiguousarray(np.concatenate([c[b].reshape(8, 128), cctx.reshape(8, 128)], 0))
        m["consts"] = consts
        m["rope"] = rope
        in_maps.append(m)
    res = run_bass_kernel_spmd(nc, in_maps, core_ids=list(range(nb)))
    return np.stack([r["out"] for r in res.results], axis=0).astype(np.float32)
```
